# Optimizing a Trainium2 kernel written in Bass

```python
import jax, jax.numpy as jnp
from jax import lax
import numpy as np

D_MODEL = 2048
BATCH = 2
SEQ = 4096
DEPTH = 1

HEAD_DIM = 128
MIX_WIDTH = D_MODEL
N_HEADS_A = MIX_WIDTH // (2 * HEAD_DIM)
N_HEADS_B = MIX_WIDTH // (2 * HEAD_DIM)
GQA_REP = 4
N_KV_B = N_HEADS_B // GQA_REP
WIDTH_A = N_HEADS_A * HEAD_DIM
WIDTH_B = N_HEADS_B * HEAD_DIM
KV_WIDTH_B = N_KV_B * HEAD_DIM
DILATED_GROUPS = ((128, 1), (512, 4), (2048, 16))
HALF_WINDOW_B = 128
N_MEM = 256
N_HEADS_MEM = 4
WIDTH_MEM = N_HEADS_MEM * HEAD_DIM
N_EXPERTS = 16
EC_CAPACITY = 2
D_FF_EXPERT = D_MODEL
ROPE_THETA = 10000.0
EPS = 1e-6
NEG_INF = -1e30
IN_WIDTHS = (WIDTH_A, WIDTH_A, WIDTH_A, WIDTH_B, KV_WIDTH_B, KV_WIDTH_B)
IN_OFFSETS = tuple(int(o) for o in np.cumsum(IN_WIDTHS)[:-1])
D_IN = int(sum(IN_WIDTHS))

kernel_name = "hybrid_dilated_swa_sink_ec_moe_encoder"


def rmsnorm(x, g):
    xf = x.astype(jnp.float32)
    y = xf * lax.rsqrt(jnp.mean(xf * xf, axis=-1, keepdims=True) + EPS)
    return (y * g.astype(jnp.float32)).astype(x.dtype)


def rope_tables(positions):
    inv_freq = ROPE_THETA ** (-jnp.arange(0, HEAD_DIM, 2, dtype=jnp.float32) / HEAD_DIM)
    ang = positions.astype(jnp.float32)[..., None] * inv_freq
    return jnp.cos(ang)[:, None], jnp.sin(ang)[:, None]


def apply_rope(t, cos, sin):
    tf = t.astype(jnp.float32)
    t1, t2 = tf[..., : HEAD_DIM // 2], tf[..., HEAD_DIM // 2:]
    out = jnp.concatenate([t1 * cos - t2 * sin, t2 * cos + t1 * sin], axis=-1)
    return out.astype(t.dtype)


def to_heads(t, n):
    b, s, _ = t.shape
    return t.reshape(b, s, n, HEAD_DIM).transpose(0, 2, 1, 3)


def merge_heads(t):
    b, h, s, d = t.shape
    return t.transpose(0, 2, 1, 3).reshape(b, s, h * d)


def pad_axis(t, axis, before, after):
    widths = [(0, 0)] * t.ndim
    widths[axis % t.ndim] = (before, after)
    return jnp.pad(t, widths)


def three_blocks(t, nb, blk):
    t = t.reshape(t.shape[:-2] + (nb + 2, blk, t.shape[-1]))
    return jnp.concatenate([t[..., :nb, :, :], t[..., 1:nb + 1, :, :], t[..., 2:, :, :]], axis=-2)


def banded_attention(q, k, v, key_valid, half, sink=None):
    blk = half
    L = q.shape[-2]
    nb = -(-L // blk)
    extra = nb * blk - L
    qb = pad_axis(q, -2, 0, extra)
    qb = qb.reshape(qb.shape[:-2] + (nb, blk, qb.shape[-1]))
    kb = three_blocks(pad_axis(k, -2, blk, extra + blk), nb, blk)
    vb = three_blocks(pad_axis(v, -2, blk, extra + blk), nb, blk)
    valid = three_blocks(pad_axis(key_valid, -1, blk, extra + blk)[..., None], nb, blk)[..., 0]
    a = np.arange(blk)[:, None]
    c = np.arange(3 * blk)[None, :]
    band = jnp.asarray(np.abs(c - blk - a) <= half)
    mask = band & valid[..., None, :]
    scale = 1.0 / np.sqrt(HEAD_DIM)
    s = jnp.matmul(qb, jnp.swapaxes(kb, -1, -2), preferred_element_type=jnp.float32) * scale
    s = jnp.where(mask, s, NEG_INF)
    if sink is not None:
        s = jnp.concatenate([s, jnp.broadcast_to(sink.astype(jnp.float32), s.shape[:-1] + (1,))], axis=-1)
    lse = jax.nn.logsumexp(s, axis=-1, keepdims=True)
    p = jnp.exp(s - lse)
    if sink is not None:
        p = p[..., :-1]
    o = jnp.matmul(p.astype(v.dtype), vb, preferred_element_type=jnp.float32)
    o = o.reshape(o.shape[:-3] + (nb * blk, o.shape[-1]))[..., :L, :]
    lse = lse.reshape(lse.shape[:-3] + (nb * blk,))[..., :L]
    return o, lse


def dilated_mixture_attention(q, k, v):
    b, h, s, hd = q.shape
    outs, lses = [], []
    for (w, d) in DILATED_GROUPS:
        half = w // (2 * d)
        L = -(-s // d)
        sp = L * d

        def to_strided(t):
            t = pad_axis(t, 2, 0, sp - s)
            return jnp.swapaxes(t.reshape(b, h, L, d, hd), 2, 3)

        key_valid = jnp.asarray((np.arange(L)[None, :] * d + np.arange(d)[:, None]) < s)
        o, lse = banded_attention(to_strided(q), to_strided(k), to_strided(v), key_valid, half)
        outs.append(jnp.swapaxes(o, 2, 3).reshape(b, h, sp, hd)[:, :, :s])
        lses.append(jnp.swapaxes(lse, 2, 3).reshape(b, h, sp)[:, :, :s])
    weights = jax.nn.softmax(jnp.stack(lses), axis=0)
    return jnp.sum(weights[..., None] * jnp.stack(outs), axis=0)


def memory_cross_attention(h, m, w_q, w_kv, g_q, g_k, w_o):
    q = rmsnorm(to_heads(h @ w_q, N_HEADS_MEM), g_q)
    kv = m @ w_kv
    k = rmsnorm(to_heads(kv[..., :WIDTH_MEM], N_HEADS_MEM), g_k)
    v = to_heads(kv[..., WIDTH_MEM:], N_HEADS_MEM)
    s = jnp.einsum('bhqd,bhkd->bhqk', q, k, preferred_element_type=jnp.float32) / np.sqrt(HEAD_DIM)
    p = jax.nn.softmax(s, axis=-1)
    o = jnp.einsum('bhqk,bhkd->bhqd', p.astype(v.dtype), v)
    return merge_heads(o) @ w_o


def expert_choice_moe(h, w_router, w_gate, w_up, w_down):
    b, s, _ = h.shape
    cap = EC_CAPACITY * s // N_EXPERTS
    aff = jax.nn.softmax((h @ w_router).astype(jnp.float32), axis=-1)
    gate, idx = lax.top_k(jnp.swapaxes(aff, 1, 2), cap)
    b_idx = jnp.arange(b)[:, None, None]
    xe = h[b_idx, idx]
    g = jnp.einsum('becd,edf->becf', xe, w_gate)
    u = jnp.einsum('becd,edf->becf', xe, w_up)
    y = jnp.einsum('becf,efd->becd', jax.nn.silu(g) * u, w_down)
    y = y * gate[..., None].astype(y.dtype)
    return jnp.zeros_like(h).at[b_idx, idx].add(y)


def setup_inputs(seed: int = 0) -> dict:
    key = jax.random.key(seed)
    ks = jax.random.split(key, 26)
    f32 = jnp.float32

    def nrm(k, shape, scale):
        return jax.random.normal(k, shape, f32) * scale

    def gain(k, shape):
        return 1.0 + 0.02 * jax.random.normal(k, shape, f32)

    positions = jnp.broadcast_to(jnp.arange(SEQ, dtype=jnp.int32)[None], (BATCH, SEQ))
    return {
        "x": nrm(ks[0], (BATCH, SEQ, D_MODEL), 1.0),
        "mem": nrm(ks[1], (BATCH, N_MEM, D_MODEL), 1.0),
        "positions": positions,
        "g_mix": gain(ks[2], (DEPTH, D_MODEL)),
        "w_in": nrm(ks[3], (DEPTH, D_MODEL, D_IN), D_MODEL ** -0.5),
        "g_qa": gain(ks[4], (DEPTH, HEAD_DIM)),
        "g_ka": gain(ks[5], (DEPTH, HEAD_DIM)),
        "g_qb": gain(ks[6], (DEPTH, HEAD_DIM)),
        "g_kb": gain(ks[7], (DEPTH, HEAD_DIM)),
        "sink_b": nrm(ks[8], (DEPTH, N_HEADS_B), 0.5),
        "g_oa": gain(ks[9], (DEPTH, WIDTH_A)),
        "g_ob": gain(ks[10], (DEPTH, WIDTH_B)),
        "w_out": nrm(ks[11], (DEPTH, MIX_WIDTH, D_MODEL), MIX_WIDTH ** -0.5),
        "g_cross": gain(ks[12], (DEPTH, D_MODEL)),
        "g_mem": gain(ks[13], (DEPTH, D_MODEL)),
        "w_q_mem": nrm(ks[14], (DEPTH, D_MODEL, WIDTH_MEM), D_MODEL ** -0.5),
        "w_kv_mem": nrm(ks[15], (DEPTH, D_MODEL, 2 * WIDTH_MEM), D_MODEL ** -0.5),
        "g_qm": gain(ks[16], (DEPTH, HEAD_DIM)),
        "g_km": gain(ks[17], (DEPTH, HEAD_DIM)),
        "w_o_mem": nrm(ks[18], (DEPTH, WIDTH_MEM, D_MODEL), WIDTH_MEM ** -0.5),
        "g_moe": gain(ks[19], (DEPTH, D_MODEL)),
        "w_router": nrm(ks[20], (DEPTH, D_MODEL, N_EXPERTS), D_MODEL ** -0.5),
        "w_gate": nrm(ks[21], (DEPTH, N_EXPERTS, D_MODEL, D_FF_EXPERT), D_MODEL ** -0.5),
        "w_up": nrm(ks[22], (DEPTH, N_EXPERTS, D_MODEL, D_FF_EXPERT), D_MODEL ** -0.5),
        "w_down": nrm(ks[23], (DEPTH, N_EXPERTS, D_FF_EXPERT, D_MODEL), D_FF_EXPERT ** -0.5),
    }


def reference(x, mem, positions, g_mix, w_in, g_qa, g_ka, g_qb, g_kb, sink_b, g_oa, g_ob,
              w_out, g_cross, g_mem, w_q_mem, w_kv_mem, g_qm, g_km, w_o_mem, g_moe,
              w_router, w_gate, w_up, w_down):
    b, s, _ = x.shape
    cos, sin = rope_tables(positions)
    key_valid_b = jnp.ones((s,), dtype=bool)
    for l in range(DEPTH):
        h = rmsnorm(x, g_mix[l])
        proj = h @ w_in[l]
        qa, ka, va, qb, kb, vb = jnp.split(proj, IN_OFFSETS, axis=-1)
        qa = apply_rope(rmsnorm(to_heads(qa, N_HEADS_A), g_qa[l]), cos, sin)
        ka = apply_rope(rmsnorm(to_heads(ka, N_HEADS_A), g_ka[l]), cos, sin)
        va = to_heads(va, N_HEADS_A)
        oa = dilated_mixture_attention(qa, ka, va).astype(x.dtype)

        qb = apply_rope(rmsnorm(to_heads(qb, N_HEADS_B), g_qb[l]), cos, sin)
        kb = apply_rope(rmsnorm(to_heads(kb, N_KV_B), g_kb[l]), cos, sin)
        vb = to_heads(vb, N_KV_B)
        qb = qb.reshape(b, N_KV_B, GQA_REP, s, HEAD_DIM)
        sink = sink_b[l].reshape(N_KV_B, GQA_REP, 1, 1, 1)
        ob, _ = banded_attention(qb, kb[:, :, None], vb[:, :, None], key_valid_b,
                                 HALF_WINDOW_B, sink=sink)
        ob = ob.reshape(b, N_HEADS_B, s, HEAD_DIM).astype(x.dtype)

        mixed = jnp.concatenate([rmsnorm(merge_heads(oa), g_oa[l]),
                                 rmsnorm(merge_heads(ob), g_ob[l])], axis=-1)
        x = x + mixed @ w_out[l]

        x = x + memory_cross_attention(rmsnorm(x, g_cross[l]), rmsnorm(mem, g_mem[l]),
                                       w_q_mem[l], w_kv_mem[l], g_qm[l], g_km[l], w_o_mem[l])

        x = x + expert_choice_moe(rmsnorm(x, g_moe[l]), w_router[l], w_gate[l], w_up[l], w_down[l])
    return x
```

```python
import numpy as np
from contextlib import ExitStack
import concourse.bass as bass
import concourse.mybir as mybir
from concourse.bass_utils import run_bass_kernel_spmd

F32 = mybir.dt.float32
BF16 = mybir.dt.bfloat16
I32 = mybir.dt.int32
AF = mybir.ActivationFunctionType
ALU = mybir.AluOpType
AX = mybir.AxisListType

SAME_ENGINE_SYNC = True
MOE_COMPACT = True


class Reg:
    __slots__ = ("name", "last_w", "readers", "const")

    def __init__(self, name="", const=False):
        self.name = name
        self.last_w = None
        self.readers = []
        self.const = const


class Op:
    __slots__ = ("eng", "fn", "deps", "needed", "sig", "is_dma", "waits_only")

    def __init__(self, eng, fn, deps, is_dma=False):
        self.eng = eng
        self.fn = fn
        self.deps = deps
        self.needed = False
        self.sig = None
        self.is_dma = is_dma
        self.waits_only = False


class Sched:
    ENGS = ("pe", "act", "dve", "pool", "sp")
    NDMA = 12

    def __init__(self, nc, stack):
        self.nc = nc
        self.sem = {e: stack.enter_context(nc.semaphore("s_" + e)) for e in self.ENGS}
        self.cnt = {e: 0 for e in self.ENGS}
        self.dsem = {q: [stack.enter_context(nc.semaphore("d_%s%d" % (q, i))) for i in range(self.NDMA)]
                     for q in ("sp", "pool", "act")}
        self.dcnt = {q: 0 for q in ("sp", "pool", "act")}
        self.dlast = {q: [None] * self.NDMA for q in ("sp", "pool", "act")}
        self.ops = {e: [] for e in self.ENGS}
        self.known = {e: {} for e in self.ENGS}
        self.last_op = {e: None for e in self.ENGS}
        self.prev_phase_last = []

    def _mk(self, eng, fn, reads, writes, is_dma=False):
        deps = []
        for r in reads:
            if r.last_w is not None:
                deps.append(r.last_w)
        for w in writes:
            if w.last_w is not None:
                deps.append(w.last_w)
            deps.extend(w.readers)
        op = Op(eng, fn, deps, is_dma)
        for r in reads:
            if not r.const:
                r.readers.append(op)
        for w in writes:
            w.last_w = op
            w.readers = []
        self.ops[eng].append(op)
        return op

    def op(self, eng, fn, reads=(), writes=()):
        return self._mk(eng, fn, reads, writes)

    def dma(self, q, out, in_, reads=(), writes=(), **kw):
        return self._mk(q, lambda e: e.dma_start(out=out, in_=in_, **kw), reads, writes, is_dma=True)

    def dma_fn(self, q, fn, reads=(), writes=()):
        return self._mk(q, fn, reads, writes, is_dma=True)

    def finish(self, ops):
        o = Op("sp", None, list(ops))
        o.waits_only = True
        self.ops["sp"].append(o)

    def emit_phase(self):
        self.nphase = getattr(self, "nphase", 0) + 1
        with self.nc.named_scope("ph%02d" % self.nphase):
            self._emit_phase()

    def _emit_phase(self):
        nc = self.nc
        engs = {"pe": nc.tensor, "act": nc.scalar, "dve": nc.vector, "pool": nc.gpsimd, "sp": nc.sync}
        barrier = list(self.prev_phase_last)
        for e in self.ENGS:
            for op in self.ops[e]:
                for d in op.deps:
                    if d.eng == op.eng and not d.is_dma:
                        if e in ("pe", "sp") or not SAME_ENGINE_SYNC:
                            continue
                    d.needed = True
        lasts = []
        for e in self.ENGS:
            real = [o for o in self.ops[e] if not o.waits_only]
            if real:
                real[-1].needed = True
                lasts.append(real[-1])
        for e in self.ENGS:
            for op in self.ops[e]:
                if op.waits_only:
                    continue
                if op.is_dma:
                    q = e
                    j = self.dcnt[q]
                    self.dcnt[q] += 1
                    op.sig = (self.dsem[q][j % self.NDMA], 16 * (j // self.NDMA + 1), q, j)
                elif op.needed:
                    self.cnt[e] += 1
                    op.sig = (self.sem[e], self.cnt[e])
        dma_lasts = []
        with nc.Block() as block:
            def run(e):
                def body(eng):
                    known = self.known[e]

                    def wait(sig):
                        s, v = sig[0], sig[1]
                        k = id(s)
                        if known.get(k, 0) < v:
                            eng.wait_ge(s, v)
                            known[k] = v
                    for d in barrier:
                        if d.eng != e or d.is_dma:
                            wait(d.sig)
                    for op in self.ops[e]:
                        for d in op.deps:
                            if d.sig is None:
                                continue
                            if d.eng == e and not d.is_dma:
                                if e in ("pe", "sp") or not SAME_ENGINE_SYNC:
                                    continue
                            wait(d.sig)
                        if op.waits_only:
                            continue
                        if op.is_dma:
                            s, v, q, j = op.sig
                            if j >= self.NDMA:
                                wait((s, v - 16))
                            ins = op.fn(eng)
                            ins.then_inc(s, 16)
                        else:
                            ins = op.fn(eng)
                            if op.sig is not None:
                                ins.then_inc(op.sig[0], 1)
                return body
            block.tensor(run("pe"))
            block.scalar(run("act"))
            block.vector(run("dve"))
            block.gpsimd(run("pool"))
            block.sync(run("sp"))
        for q in ("sp", "pool", "act"):
            seen = {}
            for op in self.ops[q]:
                if op.is_dma:
                    seen[id(op.sig[0])] = op
            dma_lasts.extend(seen.values())
        self.prev_phase_last = lasts + dma_lasts + [d for d in self.prev_phase_last if d.is_dma and id(d.sig[0]) not in {id(x.sig[0]) for x in dma_lasts}]
        self.ops = {e: [] for e in self.ENGS}


D = 2048
SEQ = 4096
HD = 128
D_IN = 4608
NCH = D // 128
NT = SEQ // 128
EPS = 1e-6
N_EXP = 16
CAP = 512
OWN = 1024
TWO_PI = float(2.0 * np.pi)
SCALE = float(1.0 / np.sqrt(HD))
HC_COL = [128 * i for i in range(8)] + [1024 + 128 * i for i in range(8)] + \
         [3072 + 128 * i for i in range(8)] + [4096, 4224]
HC_FAM = [0] * 8 + [1] * 8 + [2] * 8 + [3] * 2
V_COLS = [(2048, 512), (2560, 512), (4352, 256)]


def mask_a_np():
    kp = np.arange(128)[:, None]
    qf = np.arange(512)[None, :]
    out = np.zeros((20, 128, 512), np.float32)
    for j in range(20):
        o = (-1024 + 128 * j) + kp - qf
        a = np.abs(o)
        out[j] = (a <= 64).astype(np.float32) + ((o % 4 == 0) & (a <= 256)) + ((o % 16 == 0) & (a <= 1024))
    return out


def mask_b_np():
    kp = np.arange(128)[:, None]
    qf = np.arange(512)[None, :]
    out = np.zeros((6, 128, 512), np.float32)
    for j in range(6):
        o = (-128 + 128 * j) + kp - qf
        out[j] = (np.abs(o) <= 128)
    return out


def host_consts():
    import ml_dtypes
    bf = ml_dtypes.bfloat16
    c = {}
    c["c_ident"] = np.eye(128, dtype=np.float32).astype(bf)
    c["c_identf"] = np.eye(128, dtype=np.float32)
    c["c_ones"] = np.ones((128, 128), np.float32).astype(bf)
    c["c_onesf"] = np.ones((128, 128), np.float32)
    p0 = np.zeros((128, 128), np.float32)
    for m in range(64):
        p0[m + 64, m] = -1.0
    for m in range(64, 128):
        p0[m - 64, m] = 1.0
    c["c_rot"] = p0
    i = np.arange(128) % 64
    c["c_invf"] = (10000.0 ** (-(2.0 * i) / 128.0)).astype(np.float32).reshape(128, 1)
    pp = np.arange(128)
    c["c_ut"] = (pp[:, None] <= pp[None, :]).astype(np.float32)
    c["c_iota"] = np.tile(np.arange(512, dtype=np.float32)[None, :], (128, 1))
    c["c_loc"] = (np.arange(8)[None, :] * 128 + pp[:, None]).astype(np.float32)
    c["c_dummy"] = (1024 + np.arange(4)[None, :] * 128 + pp[:, None]).astype(np.float32)
    c["c_maska"] = np.ascontiguousarray(mask_a_np().transpose(1, 0, 2)).astype(bf)
    c["c_maskb"] = np.ascontiguousarray(mask_b_np().transpose(1, 0, 2)).astype(bf)
    return c


class Ctx:
    pass


def build(stop_after=99, debug=False):
    nc = bass.Bass("TRN2", target_bir_lowering=False)
    K = Ctx()
    K.nc = nc

    def din(name, shape, dt):
        return nc.dram_tensor(name, list(shape), dt, kind="ExternalInput").ap()

    def dscr(name, shape, dt):
        kind = "ExternalOutput" if debug else "Internal"
        return nc.dram_tensor(name, list(shape), dt, kind=kind).ap()

    I = Ctx()
    I.x = din("x", [SEQ, D], F32)
    I.mem = din("mem", [256, D], F32)
    I.pos = din("positions", [SEQ], I32)
    I.own = din("own_idx", [128, OWN // 128], I32)
    for n in ("g_mix", "g_cross", "g_mem", "g_moe"):
        setattr(I, n, din(n, [D], F32))
    for n in ("g_qa", "g_ka", "g_qb", "g_kb", "g_qm", "g_km"):
        setattr(I, n, din(n, [HD], F32))
    I.sink = din("sink_b", [8], F32)
    I.g_oa = din("g_oa", [1024], F32)
    I.g_ob = din("g_ob", [1024], F32)
    I.w_in = din("w_in", [D, D_IN], F32)
    I.w_out = din("w_out", [D, D], F32)
    I.w_q_mem = din("w_q_mem", [D, 512], F32)
    I.w_kv_mem = din("w_kv_mem", [D, 1024], F32)
    I.w_o_mem = din("w_o_mem", [512, D], F32)
    I.w_router = din("w_router", [D, N_EXP], F32)
    I.w_gate = din("w_gate", [N_EXP, D, D], F32)
    I.w_up = din("w_up", [N_EXP, D, D], F32)
    I.w_down = din("w_down", [N_EXP, D, D], F32)
    I.c_ident = din("c_ident", [128, 128], BF16)
    I.c_identf = din("c_identf", [128, 128], F32)
    I.c_ones = din("c_ones", [128, 128], BF16)
    I.c_onesf = din("c_onesf", [128, 128], F32)
    I.c_rot = din("c_rot", [128, 128], F32)
    I.c_invf = din("c_invf", [128, 1], F32)
    I.c_ut = din("c_ut", [128, 128], F32)
    I.c_iota = din("c_iota", [128, 512], F32)
    I.c_loc = din("c_loc", [128, 8], F32)
    I.c_dummy = din("c_dummy", [128, 4], F32)
    I.c_maska = din("c_maska", [128, 20, 512], BF16)
    I.c_maskb = din("c_maskb", [128, 6, 512], BF16)
    K.I = I
    out = nc.dram_tensor("out", [OWN, D], F32, kind="ExternalOutput").ap()
    K.out = out

    Sx = Ctx()
    Sx.qkt = dscr("s_qkt", [26, 128, SEQ], BF16)
    Sx.v = dscr("s_v", [SEQ, 1280], BF16)
    Sx.ot = dscr("s_ot", [16, 128, SEQ], BF16)
    Sx.x2 = dscr("s_x2", [SEQ, D], F32)
    Sx.h3 = dscr("s_h3", [SEQ, D], BF16)
    Sx.gm = dscr("s_gm", [SEQ, N_EXP], F32)
    Sx.accd = dscr("s_accd", [OWN + CAP, D], F32)
    K.Sx = Sx
    R = Ctx()
    R.qkt = [[Reg("qkt%d_%d" % (h, f)) for f in range(2)] for h in range(26)]
    R.v = [Reg("v%d" % t) for t in range(NT)]
    R.ot = [Reg("ot%d" % h) for h in range(16)]
    R.x2 = [Reg("x2_%d" % t) for t in range(NT)]
    R.h3 = [Reg("h3_%d" % t) for t in range(NT)]
    R.gm = Reg("gm")
    R.accd = Reg("accd")
    K.R = R

    with ExitStack() as top:
        S = Sched(nc, top)
        K.S = S

        uid = [0]

        def sb(stack, name, shape, dt):
            uid[0] += 1
            return stack.enter_context(nc.sbuf_tensor("%s_%d" % (name, uid[0]), list(shape), dt))

        def ps(stack, name, shape, dt):
            uid[0] += 1
            return stack.enter_context(nc.psum_tensor("%s_%d" % (name, uid[0]), list(shape), dt))
        K.sb, K.ps = sb, ps

        def mm(out, lhsT, rhs, start, stop, reads, writes):
            return S.op("pe", lambda e: e.matmul(out, lhsT=lhsT, rhs=rhs, start=start, stop=stop), reads, writes)

        def tr(out, in_, reads, writes, ident=None):
            idn = C.ident[:] if ident is None else ident
            return S.op("pe", lambda e: e.transpose(out=out, in_=in_, identity=idn), list(reads) + [C.r], writes)

        def act(out, in_, func, reads, writes, **kw):
            return S.op("act", lambda e: e.activation(out=out, in_=in_, func=func, **kw), reads, writes)

        def ts(out, in0, s1, s2, op0, op1, reads, writes, eng="dve", **kw):
            if op1 is None:
                return S.op(eng, lambda e: e.tensor_scalar(out=out, in0=in0, scalar1=s1, scalar2=None, op0=op0, **kw), reads, writes)
            return S.op(eng, lambda e: e.tensor_scalar(out=out, in0=in0, scalar1=s1, scalar2=s2, op0=op0, op1=op1, **kw), reads, writes)

        def tt(out, in0, in1, op, reads, writes, eng="dve"):
            return S.op(eng, lambda e: e.tensor_tensor(out=out, in0=in0, in1=in1, op=op), reads, writes)

        def stt(out, in0, scalar, in1, op0, op1, reads, writes, eng="dve"):
            return S.op(eng, lambda e: e.scalar_tensor_tensor(out=out, in0=in0, scalar=scalar, in1=in1, op0=op0, op1=op1), reads, writes)

        def rsq(out, in_, scale, reads, writes):
            S.op("dve", lambda e: e.tensor_scalar(out=out, in0=in_, scalar1=scale, scalar2=EPS, op0=ALU.mult, op1=ALU.add), reads, writes)
            S.op("act", lambda e: e.activation(out=out, in_=out, func=AF.Ln), writes, writes)
            return S.op("act", lambda e: e.activation(out=out, in_=out, func=AF.Exp, scale=-0.5), writes, writes)
        K.rsq = rsq

        def recip(out, in_, reads, writes):
            return S.op("dve", lambda e: e.reciprocal(out=out, in_=in_), reads, writes)

        def cp(out, in_, reads, writes, eng="dve"):
            if eng == "act":
                return S.op("act", lambda e: e.copy(out=out, in_=in_), reads, writes)
            return S.op(eng, lambda e: e.tensor_copy(out=out, in_=in_), reads, writes)
        K.mm, K.tr, K.act, K.ts, K.tt, K.stt, K.recip, K.cp = mm, tr, act, ts, tt, stt, recip, cp

        C = Ctx()
        K.C = C
        C.ident = sb(top, "ident", [128, 128], BF16)
        C.identf = sb(top, "identf", [128, 128], F32)
        C.ones = sb(top, "ones", [128, 128], BF16)
        C.onesf = sb(top, "onesf", [128, 128], F32)
        C.rot0 = sb(top, "rot0", [128, 128], F32)
        C.rotg = sb(top, "rotg", [128, 4, 128], BF16)
        C.invf = sb(top, "invf", [128, 1], F32)
        C.gh = sb(top, "gh", [128, 6], F32)
        C.ss = sb(top, "ssab", [128, 2, NT], F32)
        C.esink = sb(top, "esink", [128, 8], F32)
        C.aff = sb(top, "aff", [128, NT, N_EXP], F32)
        C.r_aff = Reg("aff")
        C.r = Reg("consts", const=True)
        C.r_ss = Reg("ss")
        S.dma("sp", C.ident[:], I.c_ident, writes=[C.r])
        S.dma("sp", C.identf[:], I.c_identf, writes=[C.r])
        S.dma("sp", C.ones[:], I.c_ones, writes=[C.r])
        S.dma("sp", C.onesf[:], I.c_onesf, writes=[C.r])
        S.dma("sp", C.rot0[:], I.c_rot, writes=[C.r])
        S.dma("sp", C.invf[:], I.c_invf, writes=[C.r])
        for i, n in enumerate(("g_qa", "g_ka", "g_qb", "g_kb", "g_qm", "g_km")):
            S.dma("sp", C.gh[:, i:i + 1], getattr(I, n).rearrange("(p o) -> p o", o=1), writes=[C.r])
        S.dma("sp", C.esink[:], I.sink.partition_broadcast(128), writes=[C.r])
        S.op("act", lambda e: e.activation(out=C.esink[:], in_=C.esink[:], func=AF.Exp), reads=[C.r], writes=[C.r])
        for f in range(4):
            S.op("dve", lambda e, f=f: e.tensor_scalar(out=C.rotg[:, f, :], in0=C.rot0[:], scalar1=C.gh[:, f:f + 1],
                                                       scalar2=None, op0=ALU.mult), reads=[C.r], writes=[C.r])
        S.op("dve", lambda e: e.memset(C.ss[:], 0.0), writes=[C.r_ss])

        phase_a(K, top)
        if stop_after >= 2:
            phase_b(K, top)
        if stop_after >= 3:
            phase_c(K, top)
        if stop_after >= 4:
            phase_d(K, top)
        if stop_after >= 5:
            if MOE_COMPACT:
                phase_e2(K, top)
            else:
                phase_e(K, top)
        S.emit_phase()
    return nc


PI_LO = 3.1415925


def phase_a(K, top):
    nc, S, I, C, Sx, R, sb, ps = K.nc, K.S, K.I, K.C, K.Sx, K.R, K.sb, K.ps
    HALF = SEQ // 2
    w_in_v = I.w_in.rearrange("(c p) n -> p c n", p=128)
    for hf in range(2):
        with ExitStack() as st_x:
            xnT = sb(st_x, "xnT", [128, NCH, HALF], BF16)
            r_xnT = [Reg("xnT%d" % t) for t in range(16)]
            with ExitStack() as st:
                xs = [sb(st, "xs%d" % i, [128, D], F32) for i in range(2)]
                xn = [sb(st, "xn%d" % i, [128, D], BF16) for i in range(2)]
                junk = sb(st, "junk", [128, D], BF16)
                gbc = sb(st, "gbc", [128, D], F32)
                ssq = sb(st, "ssq", [128, 2], F32)
                rstd = sb(st, "rstd", [128, 2], F32)
                tp = [ps(st, "tp%d" % i, [128, 8, 128], BF16) for i in range(4)]
                r_xs = [Reg() for _ in range(2)]
                r_xn = [Reg() for _ in range(2)]
                r_junk, r_gbc = Reg(), Reg(const=True)
                r_ssq = [Reg() for _ in range(2)]
                r_rstd = [Reg() for _ in range(2)]
                r_tp = [Reg() for _ in range(4)]
                S.dma("sp", gbc[:], I.g_mix.partition_broadcast(128), writes=[r_gbc])
                for t in range(16):
                    b = t % 2
                    tok0 = hf * HALF + t * 128
                    S.dma("sp", xs[b][:], I.x[tok0:tok0 + 128, :], writes=[r_xs[b]])
                    S.op("act", lambda e, b=b: e.activation(out=junk[:], in_=xs[b][:], func=AF.Square,
                                                            accum_out=ssq[:, b:b + 1]),
                         reads=[r_xs[b]], writes=[r_junk, r_ssq[b]])
                    K.rsq(rstd[:, b:b + 1], ssq[:, b:b + 1], 1.0 / D, [r_ssq[b]], [r_rstd[b]])
                    S.op("dve", lambda e, b=b: e.scalar_tensor_tensor(out=xn[b][:], in0=xs[b][:], scalar=rstd[:, b:b + 1],
                                                                      in1=gbc[:], op0=ALU.mult, op1=ALU.mult),
                         reads=[r_xs[b], r_rstd[b], r_gbc], writes=[r_xn[b]])
                    for hh in range(2):
                        pt = tp[2 * b + hh]
                        rp = r_tp[2 * b + hh]
                        for c in range(8):
                            cc = hh * 8 + c
                            S.op("pe", lambda e, pt=pt, c=c, cc=cc, b=b: e.transpose(out=pt[:, c, :], in_=xn[b][:, cc * 128:(cc + 1) * 128],
                                                                                      identity=C.ident[:]),
                                 reads=[r_xn[b], C.r], writes=[rp])
                        eng = "act" if hh == 0 else "dve"
                        if eng == "act":
                            S.op("act", lambda e, pt=pt, hh=hh, t=t: e.copy(out=xnT[:, hh * 8:hh * 8 + 8, t * 128:(t + 1) * 128], in_=pt[:]),
                                 reads=[rp], writes=[r_xnT[t]])
                        else:
                            S.op("dve", lambda e, pt=pt, hh=hh, t=t: e.tensor_copy(out=xnT[:, hh * 8:hh * 8 + 8, t * 128:(t + 1) * 128], in_=pt[:]),
                                 reads=[rp], writes=[r_xnT[t]])
                S.emit_phase()
            with ExitStack() as st:
                cos = sb(st, "cos", [128, HALF], F32)
                sin = sb(st, "sin", [128, HALF], F32)
                posi = sb(st, "posi", [128, 1024], I32)
                ang = sb(st, "ang", [128, 1024], F32)
                kf = sb(st, "kf", [128, 1024], F32)
                ki = sb(st, "ki", [128, 1024], I32)
                r_cs = Reg(const=True)
                r_tmp = Reg()
                for qd in range(2):
                    t0 = hf * HALF + qd * 1024
                    sl = slice(qd * 1024, (qd + 1) * 1024)
                    S.dma("sp", posi[:], I.pos[t0:t0 + 1024].partition_broadcast(128), writes=[r_tmp])
                    S.op("dve", lambda e: e.tensor_copy(out=ang[:], in_=posi[:]), reads=[r_tmp], writes=[r_tmp])
                    S.op("dve", lambda e: e.tensor_scalar(out=ang[:], in0=ang[:], scalar1=C.invf[:, 0:1], scalar2=None, op0=ALU.mult),
                         reads=[r_tmp, C.r], writes=[r_tmp])
                    S.op("dve", lambda e: e.tensor_scalar(out=kf[:], in0=ang[:], scalar1=1.0 / TWO_PI, scalar2=None, op0=ALU.mult),
                         reads=[r_tmp], writes=[r_tmp])
                    S.op("dve", lambda e: e.tensor_copy(out=ki[:], in_=kf[:]), reads=[r_tmp], writes=[r_tmp])
                    S.op("dve", lambda e: e.tensor_copy(out=kf[:], in_=ki[:]), reads=[r_tmp], writes=[r_tmp])
                    S.op("dve", lambda e: e.scalar_tensor_tensor(out=ang[:], in0=kf[:], scalar=-TWO_PI, in1=ang[:], op0=ALU.mult, op1=ALU.add),
                         reads=[r_tmp], writes=[r_tmp])

                    def wrap_and_sin(dst, shift):
                        S.op("dve", lambda e: e.tensor_scalar(out=kf[:], in0=ang[:], scalar1=shift, scalar2=None, op0=ALU.add),
                             reads=[r_tmp], writes=[r_tmp])
                        S.op("dve", lambda e: e.tensor_scalar(out=posi[:].bitcast(F32), in0=kf[:], scalar1=float(np.pi), scalar2=-TWO_PI,
                                                              op0=ALU.is_gt, op1=ALU.mult), reads=[r_tmp], writes=[r_tmp])
                        S.op("dve", lambda e: e.tensor_tensor(out=kf[:], in0=kf[:], in1=posi[:].bitcast(F32), op=ALU.add),
                             reads=[r_tmp], writes=[r_tmp])
                        S.op("dve", lambda e: e.tensor_scalar(out=posi[:].bitcast(F32), in0=kf[:], scalar1=-float(np.pi), scalar2=TWO_PI,
                                                              op0=ALU.is_lt, op1=ALU.mult), reads=[r_tmp], writes=[r_tmp])
                        S.op("dve", lambda e: e.tensor_tensor(out=kf[:], in0=kf[:], in1=posi[:].bitcast(F32), op=ALU.add),
                             reads=[r_tmp], writes=[r_tmp])
                        S.op("dve", lambda e: e.tensor_scalar(out=kf[:], in0=kf[:], scalar1=PI_LO, scalar2=-PI_LO, op0=ALU.min, op1=ALU.max),
                             reads=[r_tmp], writes=[r_tmp])
                        S.op("act", lambda e: e.activation(out=dst, in_=kf[:], func=AF.Sin), reads=[r_tmp], writes=[r_cs, r_tmp])
                    wrap_and_sin(sin[:, sl], 0.0)
                    wrap_and_sin(cos[:, sl], float(np.pi / 2))

                wq = [sb(st, "wq%d" % i, [128, NCH, 128], BF16) for i in range(2)]
                r_wq = [Reg() for _ in range(2)]
                q2 = [sb(st, "q2_%d" % i, [128, 512], BF16) for i in range(2)]
                qb = [sb(st, "qb_%d" % i, [128, 512], BF16) for i in range(2)]
                rs = [sb(st, "rs_%d" % i, [128, 512], F32) for i in range(2)]
                ta = [sb(st, "ta_%d" % i, [128, 512], F32) for i in range(2)]
                tb = [sb(st, "tb_%d" % i, [128, 512], F32) for i in range(2)]
                stage = [sb(st, "stg%d" % i, [128, HALF], BF16) for i in range(2)]
                r_q2 = [Reg() for _ in range(2)]
                r_qb = [Reg() for _ in range(2)]
                r_rs = [Reg() for _ in range(2)]
                r_ta = [Reg() for _ in range(2)]
                r_tb = [Reg() for _ in range(2)]
                r_stage = [Reg() for _ in range(2)]
                qp = [ps(st, "qp%d" % i, [128, 512], F32) for i in range(2)]
                sp_ = [ps(st, "ssp%d" % i, [128, 512], F32) for i in range(2)]
                rp_ = [ps(st, "rtp%d" % i, [128, 512], F32) for i in range(2)]
                r_qp = [Reg() for _ in range(2)]
                r_sp = [Reg() for _ in range(2)]
                r_rp = [Reg() for _ in range(2)]
                it = 0
                for hc in range(26):
                    wb = hc % 2
                    fam = HC_FAM[hc]
                    c0 = HC_COL[hc]
                    S.dma("pool", wq[wb][:], w_in_v[:, :, c0:c0 + 128], writes=[r_wq[wb]])
                    for blk in range(4):
                        b = it % 2
                        it += 1
                        cs = slice(blk * 512, (blk + 1) * 512)
                        for c in range(NCH):
                            S.op("pe", lambda e, b=b, wb=wb, c=c, cs=cs: e.matmul(qp[b][:], lhsT=wq[wb][:, c, :], rhs=xnT[:, c, cs],
                                                                                    start=(c == 0), stop=(c == NCH - 1)),
                                 reads=[r_wq[wb]] + r_xnT[blk * 4:blk * 4 + 4], writes=[r_qp[b]])
                        S.op("act", lambda e, b=b: e.activation(out=q2[b][:], in_=qp[b][:], func=AF.Square),
                             reads=[r_qp[b]], writes=[r_q2[b]])
                        S.op("act", lambda e, b=b: e.copy(out=qb[b][:], in_=qp[b][:]), reads=[r_qp[b]], writes=[r_qb[b]])
                        S.op("pe", lambda e, b=b: e.matmul(sp_[b][:], lhsT=C.ones[:], rhs=q2[b][:], start=True, stop=True),
                             reads=[r_q2[b], C.r], writes=[r_sp[b]])
                        S.op("pe", lambda e, b=b, fam=fam: e.matmul(rp_[b][:], lhsT=C.rotg[:, fam, :], rhs=qb[b][:], start=True, stop=True),
                             reads=[r_qb[b], C.r], writes=[r_rp[b]])
                        K.rsq(rs[b][:], sp_[b][:], 1.0 / HD, [r_sp[b]], [r_rs[b]])
                        S.op("dve", lambda e, b=b, fam=fam, cs=cs: e.scalar_tensor_tensor(out=ta[b][:], in0=qp[b][:], scalar=C.gh[:, fam:fam + 1],
                                                                                         in1=cos[:, cs], op0=ALU.mult, op1=ALU.mult),
                             reads=[r_qp[b], r_cs, C.r], writes=[r_ta[b]])
                        S.op("dve", lambda e, b=b, cs=cs: e.tensor_tensor(out=tb[b][:], in0=rp_[b][:], in1=sin[:, cs], op=ALU.mult),
                             reads=[r_rp[b], r_cs], writes=[r_tb[b]])
                        S.op("dve", lambda e, b=b: e.tensor_tensor(out=ta[b][:], in0=ta[b][:], in1=tb[b][:], op=ALU.add),
                             reads=[r_ta[b], r_tb[b]], writes=[r_ta[b]])
                        S.op("dve", lambda e, b=b, wb=wb, cs=cs: e.tensor_tensor(out=stage[wb][:, cs], in0=ta[b][:], in1=rs[b][:], op=ALU.mult),
                             reads=[r_ta[b], r_rs[b]], writes=[r_stage[wb]])
                    S.dma("sp", Sx.qkt[hc, :, hf * HALF:(hf + 1) * HALF], stage[wb][:], reads=[r_stage[wb]], writes=[R.qkt[hc][hf]])
                S.emit_phase()
            with ExitStack() as st:
                wv = sb(st, "wv", [128, NCH, 1280], BF16)
                r_wv = [Reg() for _ in range(3)]
                vst = [sb(st, "vst%d" % i, [128, 1280], BF16) for i in range(2)]
                r_vst = [Reg() for _ in range(2)]
                vp = [ps(st, "vp%d" % i, [128, 512], F32) for i in range(4)]
                r_vp = [Reg() for _ in range(4)]
                off = 0
                pieces = []
                for i, (c0, n) in enumerate(V_COLS):
                    S.dma("pool", wv[:, :, off:off + n], w_in_v[:, :, c0:c0 + n], writes=[r_wv[i]])
                    pieces.append((off, n))
                    off += n
                it = 0
                for t in range(16):
                    b = t % 2
                    tok0 = hf * HALF + t * 128
                    for i, (o, n) in enumerate(pieces):
                        pb = it % 4
                        it += 1
                        for c in range(NCH):
                            S.op("pe", lambda e, pb=pb, c=c, t=t, o=o, n=n: e.matmul(vp[pb][:, 0:n], lhsT=xnT[:, c, t * 128:(t + 1) * 128],
                                                                                     rhs=wv[:, c, o:o + n], start=(c == 0), stop=(c == NCH - 1)),
                                 reads=[r_wv[i], r_xnT[t]], writes=[r_vp[pb]])
                        if i % 2 == 0:
                            S.op("act", lambda e, pb=pb, b=b, o=o, n=n: e.copy(out=vst[b][:, o:o + n], in_=vp[pb][:, 0:n]),
                                 reads=[r_vp[pb]], writes=[r_vst[b]])
                        else:
                            S.op("dve", lambda e, pb=pb, b=b, o=o, n=n: e.tensor_copy(out=vst[b][:, o:o + n], in_=vp[pb][:, 0:n]),
                                 reads=[r_vp[pb]], writes=[r_vst[b]])
                    S.dma("sp", Sx.v[tok0:tok0 + 128, :], vst[b][:], reads=[r_vst[b]], writes=[R.v[hf * 16 + t]])
                S.emit_phase()


def phase_b(K, top):
    nc, S, I, C, Sx, R, sb, ps = K.nc, K.S, K.I, K.C, K.Sx, K.R, K.sb, K.ps
    mm, tr, act, ts, tt, stt, recip, cp = K.mm, K.tr, K.act, K.ts, K.tt, K.stt, K.recip, K.cp
    LOOK = 3
    with ExitStack() as st:
        maskA = sb(st, "maskA", [128, 20, 512], BF16)
        maskB = sb(st, "maskB", [128, 6, 512], BF16)
        r_mask = Reg(const=True)
        S.dma("sp", maskA[:], I.c_maska, writes=[r_mask])
        S.dma("sp", maskB[:], I.c_maskb, writes=[r_mask])
        QT = [sb(st, "QT%d" % i, [128, SEQ], BF16) for i in range(2)]
        KT = [sb(st, "KT%d" % i, [128, SEQ], BF16) for i in range(2)]
        V1 = [sb(st, "V1%d" % i, [128, NT, 130], BF16) for i in range(2)]
        OTs = [sb(st, "OTs%d" % i, [128, SEQ], BF16) for i in range(2)]
        r_QT = [Reg() for _ in range(2)]
        r_KT = [Reg() for _ in range(2)]
        r_V1 = [Reg() for _ in range(2)]
        r_OTs = [Reg() for _ in range(2)]
        NE = 6
        NSP = 3
        E = [sb(st, "E%d" % i, [128, 512], BF16) for i in range(NE)]
        Pm = [sb(st, "Pm%d" % i, [128, 512], BF16) for i in range(NE)]
        r_E = [Reg() for _ in range(NE)]
        r_Pm = [Reg() for _ in range(NE)]
        NO = 8
        Osb = [sb(st, "Osb%d" % i, [128, 130], F32) for i in range(NO)]
        den = [sb(st, "den%d" % i, [128, 1], F32) for i in range(NO)]
        sst = [sb(st, "sst%d" % i, [128, 1], F32) for i in range(NO)]
        obf = [sb(st, "obf%d" % i, [128, 128], BF16) for i in range(NO)]
        junk = sb(st, "junkb", [128, 128], BF16)
        r_Osb = [Reg() for _ in range(NO)]
        r_den = [Reg() for _ in range(NO)]
        r_sst = [Reg() for _ in range(NO)]
        r_obf = [Reg() for _ in range(NO)]
        r_junk = Reg()
        Sp = [ps(st, "Sp%d" % i, [128, 512], F32) for i in range(NSP)]
        Op_ = [ps(st, "Op%d" % i, [128, 512], F32) for i in range(4)]
        Tp = ps(st, "Tp", [128, 512], F32)
        tpv = Tp[:].bitcast(BF16)
        r_Sp = [Reg() for _ in range(NSP)]
        r_Op = [Reg() for _ in range(4)]
        r_Tp = [Reg() for _ in range(8)]
        for i in range(2):
            S.op("dve", lambda e, i=i: e.memset(V1[i][:, :, 128:130], 1.0), writes=[r_V1[i]])

        iters = []
        for h in range(16):
            isA = h < 8
            nj, koff = (20, -1024) if isA else (6, -128)
            for qb in range(8):
                q0 = qb * 512
                js = [j for j in range(nj) if 0 <= q0 + koff + 128 * j < SEQ]
                for jn, j in enumerate(js):
                    iters.append(dict(h=h, qb=qb, j=j, k0=q0 + koff + 128 * j, first=(jn == 0), last=(jn == len(js) - 1),
                                      hstart=(qb == 0 and jn == 0), hend=(qb == 7 and jn == len(js) - 1)))
        state = {"osb": 0, "ts": 0}
        deferred = []

        def head_cfg(h):
            if h < 8:
                return h, 8 + h, 128 * h, maskA
            kvh = (h - 8) // 4
            return 16 + (h - 8), 24 + kvh, 1024 + 128 * kvh, maskB

        def load_head(h):
            hb = h % 2
            qhc, khc, vcol, _ = head_cfg(h)
            S.dma("sp", QT[hb][:], Sx.qkt[qhc], reads=R.qkt[qhc], writes=[r_QT[hb]])
            S.dma("sp", KT[hb][:], Sx.qkt[khc], reads=R.qkt[khc], writes=[r_KT[hb]])
            S.dma("sp", V1[hb][:, :, 0:128], Sx.v[:, vcol:vcol + 128].rearrange("(t p) d -> p t d", p=128),
                  reads=R.v, writes=[r_V1[hb]])

        def score(n):
            it = iters[n]
            hb = it["h"] % 2
            mask = head_cfg(it["h"])[3]
            sbi, ei = n % NSP, n % NE
            k0, q0, j = it["k0"], it["qb"] * 512, it["j"]
            mm(Sp[sbi][:], KT[hb][:, k0:k0 + 128], QT[hb][:, q0:q0 + 512], True, True, [r_KT[hb], r_QT[hb]], [r_Sp[sbi]])
            act(E[ei][:], Sp[sbi][:], AF.Exp, [r_Sp[sbi]], [r_E[ei]], scale=SCALE)
            tt(Pm[ei][:], E[ei][:], mask[:, j, :], ALU.mult, [r_E[ei], r_mask], [r_Pm[ei]])

        def post(h, qb, i, o):
            hb = h % 2
            grp = 0 if h < 8 else 1
            qt = 4 * qb + i
            if h < 8:
                recip(den[o][:], Osb[o][:, 128:129], [r_Osb[o]], [r_den[o]])
            else:
                tt(den[o][:], Osb[o][:, 128:129], C.esink[:, h - 8:h - 7], ALU.add, [r_Osb[o], C.r], [r_den[o]])
                recip(den[o][:], den[o][:], [r_den[o]], [r_den[o]])
            ts(obf[o][:], Osb[o][:, 0:128], den[o][:, 0:1], None, ALU.mult, None, [r_Osb[o], r_den[o]], [r_obf[o]])
            act(junk[:], obf[o][:], AF.Square, [r_obf[o]], [r_junk, r_sst[o]], accum_out=sst[o][:])
            tt(C.ss[:, grp, qt:qt + 1], C.ss[:, grp, qt:qt + 1], sst[o][:], ALU.add, [r_sst[o], C.r_ss], [C.r_ss])
            tsl = state["ts"] % 8
            state["ts"] += 1
            tr(tpv[:, tsl * 128:(tsl + 1) * 128], obf[o][:], [r_obf[o]], [r_Tp[tsl]])
            cp(OTs[hb][:, qt * 128:(qt + 1) * 128], tpv[:, tsl * 128:(tsl + 1) * 128], [r_Tp[tsl]], [r_OTs[hb]], eng="act")

        def pv(n):
            it = iters[n]
            h, qb = it["h"], it["qb"]
            hb = h % 2
            ei = n % NE
            k0 = it["k0"]
            for i in range(4):
                mm(Op_[i][:, 0:129], Pm[ei][:, 128 * i:128 * i + 128], V1[hb][:, k0 // 128, 0:129], it["first"], it["last"],
                   [r_Pm[ei], r_V1[hb]], [r_Op[i]])
            if it["last"]:
                for i in range(4):
                    o = state["osb"] % NO
                    state["osb"] += 1
                    cp(Osb[o][:, 0:129], Op_[i][:, 0:129], [r_Op[i]], [r_Osb[o]])
                    deferred.append([2, (lambda h=h, qb=qb, i=i, o=o: post(h, qb, i, o))])
            if it["hend"]:
                deferred.append([3, (lambda h=h, hb=hb: S.dma("sp", Sx.ot[h], OTs[hb][:], reads=[r_OTs[hb]], writes=[R.ot[h]]))])

        def tick():
            for d in deferred:
                d[0] -= 1
            while deferred and deferred[0][0] <= 0:
                deferred.pop(0)[1]()

        N = len(iters)
        load_head(0)
        for n in range(N + LOOK):
            if n < N:
                score(n)
            if n - LOOK >= 0:
                pv(n - LOOK)
                if iters[n - LOOK]["hstart"] and iters[n - LOOK]["h"] + 1 < 16:
                    load_head(iters[n - LOOK]["h"] + 1)
            tick()
        while deferred:
            tick()
        S.emit_phase()


def phase_c(K, top):
    nc, S, I, C, Sx, R, sb, ps = K.nc, K.S, K.I, K.C, K.Sx, K.R, K.sb, K.ps
    mm, tr, act, ts, tt, stt, recip, cp = K.mm, K.tr, K.act, K.ts, K.tt, K.stt, K.recip, K.cp
    with ExitStack() as st0:
        KmT = sb(st0, "KmT", [128, 4, 256], BF16)
        Vm = sb(st0, "Vm", [128, 2, 4, 130], BF16)
        r_km = Reg(const=True)
        with ExitStack() as st:
            memx = sb(st, "memx", [128, 2, D], F32)
            memn = sb(st, "memn", [128, 2, D], BF16)
            memT = sb(st, "memT", [128, NCH, 256], BF16)
            gbc = sb(st, "gbcm", [128, D], F32)
            junk = sb(st, "junkc0", [128, D], BF16)
            wkv = sb(st, "wkv", [128, NCH, 1024], BF16)
            ssq = sb(st, "ssqm", [128, 2], F32)
            k2 = sb(st, "k2", [128, 256], BF16)
            rsk = sb(st, "rsk", [128, 256], F32)
            bank = [ps(st, "c0b%d" % i, [128, 512], F32) for i in range(4)]
            rb = [Reg() for _ in range(4)]
            r1 = Reg()
            r_w = Reg()
            S.dma("sp", memx[:], I.mem.rearrange("(t p) d -> p t d", p=128), writes=[r1])
            S.dma("sp", gbc[:], I.g_mem.partition_broadcast(128), writes=[r1])
            S.dma("pool", wkv[:], I.w_kv_mem.rearrange("(c p) n -> p c n", p=128), writes=[r_w])
            S.op("dve", lambda e: e.memset(Vm[:], 1.0), writes=[r_km])
            for mt in range(2):
                act(junk[:], memx[:, mt, :], AF.Square, [r1], [r1], accum_out=ssq[:, mt:mt + 1])
                K.rsq(ssq[:, mt:mt + 1], ssq[:, mt:mt + 1], 1.0 / D, [r1], [r1])
                stt(memn[:, mt, :], memx[:, mt, :], ssq[:, mt:mt + 1], gbc[:], ALU.mult, ALU.mult, [r1], [r1])
                for hh in range(2):
                    tpv = bank[hh][:].bitcast(BF16)
                    for c in range(8):
                        cc = hh * 8 + c
                        tr(tpv[:, c * 128:(c + 1) * 128], memn[:, mt, cc * 128:(cc + 1) * 128], [r1], [rb[hh]])
                    cp(memT[:, hh * 8:hh * 8 + 8, mt * 128:(mt + 1) * 128], tpv[:, 0:1024].rearrange("p (c n) -> p c n", c=8), [rb[hh]], [r1])
            for hm in range(4):
                for c in range(NCH):
                    mm(bank[2][:, 0:256], wkv[:, c, hm * 128:(hm + 1) * 128], memT[:, c, :], c == 0, c == NCH - 1, [r_w, r1], [rb[2]])
                act(k2[:], bank[2][:, 0:256], AF.Square, [rb[2]], [r1])
                mm(bank[3][:, 0:256], C.ones[:], k2[:], True, True, [r1, C.r], [rb[3]])
                K.rsq(rsk[:], bank[3][:, 0:256], 1.0 / HD, [rb[3]], [r1])
                stt(KmT[:, hm, :], bank[2][:, 0:256], C.gh[:, 5:6], rsk[:], ALU.mult, ALU.mult, [rb[2], r1, C.r], [r_km])
            for mt in range(2):
                for c in range(NCH):
                    mm(bank[mt][:], memT[:, c, mt * 128:(mt + 1) * 128], wkv[:, c, 512:1024], c == 0, c == NCH - 1, [r_w, r1], [rb[mt]])
                cp(Vm[:, mt, :, 0:128], bank[mt][:].rearrange("p (h d) -> p h d", h=4), [rb[mt]], [r_km])
            S.emit_phase()
        with ExitStack() as st:
            wout = sb(st, "wout", [128, NCH, D], BF16)
            wq = sb(st, "wqm", [128, NCH, 512], BF16)
            wo = sb(st, "wom", [128, 4, D], BF16)
            wr = sb(st, "wr", [128, NCH, N_EXP], BF16)
            gcr = sb(st, "gcr", [128, D], F32)
            gmo = sb(st, "gmo", [128, D], F32)
            go = sb(st, "go", [128, 16], F32)
            rAB = sb(st, "rAB", [128, 2, NT], F32)
            r_w = Reg(const=True)
            S.dma("pool", wout[:], I.w_out.rearrange("(c p) n -> p c n", p=128), writes=[r_w])
            S.dma("pool", wq[:], I.w_q_mem.rearrange("(c p) n -> p c n", p=128), writes=[r_w])
            S.dma("pool", wo[:], I.w_o_mem.rearrange("(c p) n -> p c n", p=128), writes=[r_w])
            S.dma("pool", wr[:], I.w_router.rearrange("(c p) n -> p c n", p=128), writes=[r_w])
            S.dma("sp", gcr[:], I.g_cross.partition_broadcast(128), writes=[r_w])
            S.dma("sp", gmo[:], I.g_moe.partition_broadcast(128), writes=[r_w])
            S.dma("sp", go[:, 0:8], I.g_oa.rearrange("(c p) -> p c", p=128), writes=[r_w], allow_slow_non_contiguous=True)
            S.dma("sp", go[:, 8:16], I.g_ob.rearrange("(c p) -> p c", p=128), writes=[r_w], allow_slow_non_contiguous=True)
            for c in range(NCH):
                ts(wout[:, c, :], wout[:, c, :], go[:, c:c + 1], None, ALU.mult, None, [r_w], [r_w])
            K.rsq(rAB[:], C.ss[:], 1.0 / 1024, [C.r_ss], [r_w])

            xt = [sb(st, "xt%d" % i, [128, D], F32) for i in range(2)]
            otb = [sb(st, "otb%d" % i, [128, 16, 128], BF16) for i in range(2)]
            x12 = [sb(st, "x12_%d" % i, [128, D], F32) for i in range(2)]
            hn = [sb(st, "hn%d" % i, [128, D], BF16) for i in range(2)]
            hT = [sb(st, "hT%d" % i, [128, NCH, 128], BF16) for i in range(2)]
            junk = sb(st, "junkc1", [128, D], BF16)
            sq = sb(st, "sqc", [128, 4], F32)
            q2 = sb(st, "q2c", [128, 512], BF16)
            rsq = sb(st, "rsqc", [128, 512], F32)
            qn = sb(st, "qnc", [128, 4, 128], BF16)
            E2 = [sb(st, "E2_%d" % i, [128, 4, 128], BF16) for i in range(2)]
            rden = sb(st, "rdenc", [128, 4], F32)
            o2 = sb(st, "o2c", [128, 4, 128], BF16)
            o2T = sb(st, "o2T", [128, 4, 128], BF16)
            ex = sb(st, "exr", [128, N_EXP], F32)
            sume = sb(st, "sume", [128, 1], F32)
            r_xt = [Reg() for _ in range(2)]
            r_otb = [Reg() for _ in range(2)]
            r_x12 = [Reg() for _ in range(2)]
            r_hn = [Reg() for _ in range(2)]
            r_hT = [Reg() for _ in range(2)]
            r_junk, r_sq, r_q2, r_rsq, r_qn, r_rden, r_o2, r_o2T, r_ex, r_sume = [Reg() for _ in range(10)]
            r_E2 = [Reg() for _ in range(2)]
            B = [ps(st, "c1b%d" % i, [128, 512], F32) for i in range(8)]
            rB = [Reg() for _ in range(8)]
            tpv = B[4][:].bitcast(BF16)
            ot_v = Sx.ot.rearrange("h p n -> p h n")

            def rmsnorm_tile(src, r_src, gb, dst, r_dst, col):
                act(junk[:], src, AF.Square, [r_src], [r_junk, r_sq], accum_out=sq[:, col:col + 1])
                K.rsq(sq[:, col:col + 1], sq[:, col:col + 1], 1.0 / D, [r_sq], [r_sq])
                stt(dst, src, sq[:, col:col + 1], gb[:], ALU.mult, ALU.mult, [r_src, r_sq, r_w], [r_dst])

            def transpose16(src, r_src, dst, r_dst):
                for hh in range(2):
                    for c in range(8):
                        cc = hh * 8 + c
                        tr(tpv[:, c * 128:(c + 1) * 128], src[:, cc * 128:(cc + 1) * 128], [r_src], [rB[4]])
                    cp(dst[:, hh * 8:hh * 8 + 8, :], tpv.rearrange("p (c n) -> p c n", c=8), [rB[4]], [r_dst],
                       eng=("act" if hh == 0 else "dve"))

            def load(t):
                b = t % 2
                S.dma("sp", xt[b][:], I.x[t * 128:(t + 1) * 128, :], writes=[r_xt[b]])
                S.dma("sp", otb[b][:], ot_v[:, :, t * 128:(t + 1) * 128], reads=R.ot, writes=[r_otb[b]])
            def s1_cg(t, cg):
                b = t % 2
                X = x12[b]
                pa, pb = B[(cg % 2) * 2], B[(cg % 2) * 2 + 1]
                ra, rbb = rB[(cg % 2) * 2], rB[(cg % 2) * 2 + 1]
                cs = slice(cg * 512, (cg + 1) * 512)
                for c in range(8):
                    mm(pa[:], otb[b][:, c, :], wout[:, c, cs], c == 0, c == 7, [r_otb[b], r_w], [ra])
                for c in range(8, 16):
                    mm(pb[:], otb[b][:, c, :], wout[:, c, cs], c == 8, c == 15, [r_otb[b], r_w], [rbb])
                stt(X[:, cs], pa[:], rAB[:, 0, t:t + 1], xt[b][:, cs], ALU.mult, ALU.add, [ra, r_xt[b], r_w], [r_x12[b]])
                stt(X[:, cs], pb[:], rAB[:, 1, t:t + 1], X[:, cs], ALU.mult, ALU.add, [rbb, r_x12[b], r_w], [r_x12[b]])

            def nxt(t, cg):
                if t + 1 < NT:
                    s1_cg(t + 1, cg)

            load(0)
            load(1)
            for cg in range(4):
                s1_cg(0, cg)
            for t in range(NT):
                b = t % 2
                X = x12[b]
                rmsnorm_tile(X[:], r_x12[b], gcr, hn[0][:], r_hn[0], 0)
                nxt(t, 0)
                transpose16(hn[0], r_hn[0], hT[0], r_hT[0])
                for hm in range(4):
                    for c in range(NCH):
                        mm(B[5][:, hm * 128:(hm + 1) * 128], wq[:, c, hm * 128:(hm + 1) * 128], hT[0][:, c, :], c == 0, c == NCH - 1,
                           [r_w, r_hT[0]], [rB[5]])
                act(q2[:], B[5][:], AF.Square, [rB[5]], [r_q2])
                nxt(t, 1)
                mm(B[6][:], C.ones[:], q2[:], True, True, [r_q2, C.r], [rB[6]])
                K.rsq(rsq[:], B[6][:], 1.0 / HD, [rB[6]], [r_rsq])
                stt(qn[:].rearrange("p h n -> p (h n)"), B[5][:], C.gh[:, 4:5], rsq[:], ALU.mult, ALU.mult, [rB[5], r_rsq, C.r], [r_qn])
                nxt(t, 2)
                for mt in range(2):
                    bk = 7 if mt == 0 else 4
                    for hm in range(4):
                        mm(B[bk][:, hm * 128:(hm + 1) * 128], KmT[:, hm, mt * 128:(mt + 1) * 128], qn[:, hm, :], True, True,
                           [r_km, r_qn], [rB[bk]])
                    act(E2[mt][:].rearrange("p h n -> p (h n)"), B[bk][:], AF.Exp, [rB[bk]], [r_E2[mt]], scale=SCALE)
                nxt(t, 3)
                for hm in range(4):
                    bk = 5 if hm < 2 else 6
                    o0 = (hm % 2) * 130
                    for mt in range(2):
                        mm(B[bk][:, o0:o0 + 129], E2[mt][:, hm, :], Vm[:, mt, hm, 0:129], mt == 0, mt == 1, [r_E2[mt], r_km], [rB[bk]])
                for hm in range(4):
                    bk = 5 if hm < 2 else 6
                    o0 = (hm % 2) * 130
                    recip(rden[:, hm:hm + 1], B[bk][:, o0 + 128:o0 + 129], [rB[bk]], [r_rden])
                    ts(o2[:, hm, :], B[bk][:, o0:o0 + 128], rden[:, hm:hm + 1], None, ALU.mult, None, [rB[bk], r_rden], [r_o2])
                for hm in range(4):
                    tr(tpv[:, hm * 128:(hm + 1) * 128], o2[:, hm, :], [r_o2], [rB[4]])
                cp(o2T[:].rearrange("p h n -> p (h n)"), tpv[:, 0:512], [rB[4]], [r_o2T], eng="act")
                for cg in range(4):
                    pa = B[cg % 4]
                    ra = rB[cg % 4]
                    cs = slice(cg * 512, (cg + 1) * 512)
                    for hm in range(4):
                        mm(pa[:], o2T[:, hm, :], wo[:, hm, cs], hm == 0, hm == 3, [r_o2T, r_w], [ra])
                    tt(X[:, cs], pa[:], X[:, cs], ALU.add, [ra, r_x12[b]], [r_x12[b]])
                S.dma("sp", Sx.x2[t * 128:(t + 1) * 128, :], X[:], reads=[r_x12[b]], writes=[R.x2[t]])
                rmsnorm_tile(X[:], r_x12[b], gmo, hn[1][:], r_hn[1], 1)
                S.dma("sp", Sx.h3[t * 128:(t + 1) * 128, :], hn[1][:], reads=[r_hn[1]], writes=[R.h3[t]])
                if t + 2 < NT:
                    load(t + 2)
                transpose16(hn[1], r_hn[1], hT[1], r_hT[1])
                for c in range(NCH):
                    mm(B[5][:, 0:N_EXP], hT[1][:, c, :], wr[:, c, :], c == 0, c == NCH - 1, [r_hT[1], r_w], [rB[5]])
                act(ex[:], B[5][:, 0:N_EXP], AF.Exp, [rB[5]], [r_ex, r_sume], accum_out=sume[:])
                recip(sume[:], sume[:], [r_sume], [r_sume])
                ts(C.aff[:, t, :], ex[:], sume[:, 0:1], None, ALU.mult, None, [r_ex, r_sume], [C.r_aff])
            S.emit_phase()


N_BISECT = 34


def phase_d(K, top):
    nc, S, I, C, Sx, R, sb, ps = K.nc, K.S, K.I, K.C, K.Sx, K.R, K.sb, K.ps
    mm, tr, act, ts, tt, stt, recip, cp = K.mm, K.tr, K.act, K.ts, K.tt, K.stt, K.recip, K.cp
    with ExitStack() as st:
        lo = sb(st, "lo", [128, N_EXP], F32)
        hi = sb(st, "hi", [128, N_EXP], F32)
        mid = sb(st, "mid", [128, N_EXP], F32)
        ge = sb(st, "ge", [128, N_EXP], F32)
        dl = sb(st, "dl", [128, N_EXP], F32)
        cntp = sb(st, "cntp", [128, N_EXP], F32)
        cmp_ = sb(st, "cmp", [128, NT, N_EXP], F32)
        cnt = ps(st, "cnt", [128, 512], F32)
        r = Reg()
        r_cnt = Reg()
        S.op("dve", lambda e: e.memset(lo[:], 0.0), writes=[r])
        S.op("dve", lambda e: e.memset(hi[:], 1.0), writes=[r])

        def bc(t):
            return t[:, :].unsqueeze(1).to_broadcast([128, NT, N_EXP])
        for it in range(N_BISECT):
            tt(mid[:], lo[:], hi[:], ALU.add, [r], [r])
            ts(mid[:], mid[:], 0.5, None, ALU.mult, None, [r], [r])
            tt(cmp_[:], C.aff[:], bc(mid), ALU.is_gt, [r, C.r_aff], [r])
            S.op("dve", lambda e: e.tensor_reduce(out=cntp[:], in_=cmp_[:].rearrange("p t e -> p e t"), axis=AX.X, op=ALU.add),
                 reads=[r], writes=[r])
            mm(cnt[:, 0:N_EXP], C.onesf[:], cntp[:], True, True, [r, C.r], [r_cnt])
            ts(ge[:], cnt[:, 0:N_EXP], float(CAP), None, ALU.is_ge, None, [r_cnt], [r])
            tt(dl[:], mid[:], lo[:], ALU.subtract, [r], [r])
            tt(dl[:], dl[:], ge[:], ALU.mult, [r], [r])
            tt(lo[:], lo[:], dl[:], ALU.add, [r], [r])
            tt(dl[:], hi[:], mid[:], ALU.subtract, [r], [r])
            tt(dl[:], dl[:], ge[:], ALU.mult, [r], [r])
            tt(hi[:], mid[:], dl[:], ALU.add, [r], [r])
        tt(cmp_[:], C.aff[:], bc(lo), ALU.is_gt, [r, C.r_aff], [r])
        tt(cmp_[:], cmp_[:], C.aff[:], ALU.mult, [r, C.r_aff], [r])
        S.dma("sp", Sx.gm.rearrange("(t p) e -> p t e", p=128), cmp_[:], reads=[r], writes=[R.gm])
        S.emit_phase()


def phase_e(K, top):
    nc, S, I, C, Sx, R, sb, ps = K.nc, K.S, K.I, K.C, K.Sx, K.R, K.sb, K.ps
    mm, tr, act, ts, tt, stt, recip, cp = K.mm, K.tr, K.act, K.ts, K.tt, K.stt, K.recip, K.cp
    NOT = OWN // 128
    with ExitStack() as st0:
        own = sb(st0, "own", [128, NOT], I32)
        h3T = sb(st0, "h3T", [128, NCH, OWN], BF16)
        acc = sb(st0, "acc", [128, NOT, D], F32)
        gmo = sb(st0, "gmo_", [128, NOT, N_EXP], F32)
        r_own, r_h3T, r_gmo = Reg(const=True), Reg(const=True), Reg(const=True)
        r_acc = [Reg() for _ in range(NOT)]
        with ExitStack() as st:
            h3o = sb(st, "h3o", [128, NOT, D], BF16)
            r_h3o = [Reg() for _ in range(NOT)]
            tpb = [ps(st, "tpe%d" % i, [128, 512], F32) for i in range(2)]
            r_tpb = [Reg() for _ in range(2)]
            S.dma("sp", own[:], I.own, writes=[r_own])
            for j in range(NOT):
                off = bass.IndirectOffsetOnAxis(ap=own[:, j:j + 1], axis=0)
                S.dma_fn("pool", lambda e, j=j, off=off: e.indirect_dma_start(out=h3o[:, j, :], out_offset=None, in_=Sx.h3, in_offset=off),
                         reads=[r_own] + R.h3, writes=[r_h3o[j]])
                S.dma_fn("pool", lambda e, j=j, off=off: e.indirect_dma_start(out=acc[:, j, :], out_offset=None, in_=Sx.x2, in_offset=off),
                         reads=[r_own] + R.x2, writes=[r_acc[j]])
                S.dma_fn("pool", lambda e, j=j, off=off: e.indirect_dma_start(out=gmo[:, j, :], out_offset=None, in_=Sx.gm, in_offset=off),
                         reads=[r_own, R.gm], writes=[r_gmo])
            k = 0
            for j in range(NOT):
                for hh in range(2):
                    pb = k % 2
                    k += 1
                    tpv = tpb[pb][:].bitcast(BF16)
                    for c in range(8):
                        cc = hh * 8 + c
                        tr(tpv[:, c * 128:(c + 1) * 128], h3o[:, j, cc * 128:(cc + 1) * 128], [r_h3o[j]], [r_tpb[pb]])
                    cp(h3T[:, hh * 8:hh * 8 + 8, j * 128:(j + 1) * 128], tpv.rearrange("p (c n) -> p c n", c=8), [r_tpb[pb]], [r_h3T],
                       eng=("act" if hh == 0 else "dve"))
            S.emit_phase()
        with ExitStack() as st:
            FP = 256
            NFP = D // FP
            wg = [sb(st, "wg%d" % i, [128, NCH, FP], BF16) for i in range(2)]
            wu = [sb(st, "wu%d" % i, [128, NCH, FP], BF16) for i in range(2)]
            wd = [sb(st, "wd%d" % i, [128, NCH, FP], BF16) for i in range(2)]
            r_wg = [Reg() for _ in range(2)]
            r_wu = [Reg() for _ in range(2)]
            r_wd = [Reg() for _ in range(2)]
            actT = sb(st, "actT", [128, NCH, OWN], BF16)
            r_actT = [Reg() for _ in range(NCH)]
            sg = [sb(st, "sg%d" % i, [128, 512], F32) for i in range(2)]
            r_sg = [Reg() for _ in range(2)]
            Gp = [ps(st, "Gp%d" % i, [128, 512], F32) for i in range(2)]
            Up = [ps(st, "Up%d" % i, [128, 512], F32) for i in range(2)]
            Yp = [ps(st, "Yp%d" % i, [128, 512], F32) for i in range(3)]
            r_Gp = [Reg() for _ in range(2)]
            r_Up = [Reg() for _ in range(2)]
            r_Yp = [Reg() for _ in range(3)]
            wi = 0
            di = 0
            gi = 0
            yi = 0
            for ex in range(N_EXP):
                wgv = I.w_gate[ex].rearrange("(c p) f -> p c f", p=128)
                wuv = I.w_up[ex].rearrange("(c p) f -> p c f", p=128)
                wdv = I.w_down[ex].rearrange("(c p) f -> p c f", p=128)
                for fp in range(NFP):
                    wb = wi % 2
                    wi += 1
                    S.dma("pool", wg[wb][:], wgv[:, :, fp * FP:(fp + 1) * FP], writes=[r_wg[wb]])
                    S.dma("pool", wu[wb][:], wuv[:, :, fp * FP:(fp + 1) * FP], writes=[r_wu[wb]])
                    for f2 in range(FP // 128):
                        fc = fp * (FP // 128) + f2
                        for half in range(2):
                            gb = gi % 2
                            gi += 1
                            cs = slice(half * 512, (half + 1) * 512)
                            for c in range(NCH):
                                mm(Gp[gb][:], wg[wb][:, c, f2 * 128:(f2 + 1) * 128], h3T[:, c, cs], c == 0, c == NCH - 1,
                                   [r_wg[wb], r_h3T], [r_Gp[gb]])
                            for c in range(NCH):
                                mm(Up[gb][:], wu[wb][:, c, f2 * 128:(f2 + 1) * 128], h3T[:, c, cs], c == 0, c == NCH - 1,
                                   [r_wu[wb], r_h3T], [r_Up[gb]])
                            act(sg[gb][:], Gp[gb][:], AF.Silu, [r_Gp[gb]], [r_sg[gb]])
                            tt(actT[:, fc, cs], sg[gb][:], Up[gb][:], ALU.mult, [r_sg[gb], r_Up[gb]], [r_actT[fc]])
                for dp in range(NFP):
                    db = di % 2
                    di += 1
                    S.dma("pool", wd[db][:], wdv[:, :, dp * FP:(dp + 1) * FP], writes=[r_wd[db]])
                    for j in range(NOT):
                        yb = yi % 3
                        yi += 1
                        for fc in range(NCH):
                            mm(Yp[yb][:, 0:FP], actT[:, fc, j * 128:(j + 1) * 128], wd[db][:, fc, :], fc == 0, fc == NCH - 1,
                               [r_actT[fc], r_wd[db]], [r_Yp[yb]])
                        stt(acc[:, j, dp * FP:(dp + 1) * FP], Yp[yb][:, 0:FP], gmo[:, j, ex:ex + 1], acc[:, j, dp * FP:(dp + 1) * FP],
                            ALU.mult, ALU.add, [r_Yp[yb], r_gmo, r_acc[j]], [r_acc[j]])
            outs = []
            for j in range(NOT):
                outs.append(S.dma("sp", K.out[j * 128:(j + 1) * 128, :], acc[:, j, :], reads=[r_acc[j]], writes=[]))
            S.finish(outs)
            S.emit_phase()


_NC_CACHE = {}


def _core_inputs(inp, c, consts):
    b, q = c // 4, c % 4
    m = {}
    m["x"] = np.ascontiguousarray(inp["x"][b], dtype=np.float32)
    m["mem"] = np.ascontiguousarray(inp["mem"][b], dtype=np.float32)
    m["positions"] = np.ascontiguousarray(inp["positions"][b]).astype(np.int32)
    own = (q * OWN + np.arange(OWN)).reshape(OWN // 128, 128).T.astype(np.int32)
    m["own_idx"] = np.ascontiguousarray(own)
    for n in ("g_mix", "g_cross", "g_mem", "g_moe", "g_qa", "g_ka", "g_qb", "g_kb", "g_qm", "g_km", "g_oa", "g_ob",
              "sink_b", "w_in", "w_out", "w_q_mem", "w_kv_mem", "w_o_mem", "w_router", "w_gate", "w_up", "w_down"):
        m[n] = np.ascontiguousarray(np.asarray(inp[n])[0], dtype=np.float32)
    m.update(consts)
    return m


def kernel(**inputs):
    inp = {k: np.asarray(v) for k, v in inputs.items()}
    if "nc" not in _NC_CACHE:
        _NC_CACHE["nc"] = build()
    nc = _NC_CACHE["nc"]
    consts = host_consts()
    in_maps = [_core_inputs(inp, c, consts) for c in range(8)]
    res = run_bass_kernel_spmd(nc, in_maps, core_ids=list(range(8)))
    out = np.zeros((2, SEQ, D), np.float32)
    for c in range(8):
        b, q = c // 4, c % 4
        out[b, q * OWN:(q + 1) * OWN] = np.asarray(res.results[c]["out"], dtype=np.float32)
    return out


def phase_e2(K, top):
    nc, S, I, C, Sx, R, sb, ps = K.nc, K.S, K.I, K.C, K.Sx, K.R, K.sb, K.ps
    mm, tr, act, ts, tt, stt, recip, cp = K.mm, K.tr, K.act, K.ts, K.tt, K.stt, K.recip, K.cp
    NOT = OWN // 128
    NS = CAP // 128
    NTG = 3 + N_EXP
    with ExitStack() as st0:
        own = sb(st0, "own", [128, NOT], I32)
        gmo = sb(st0, "gmo_", [128, NOT, N_EXP], F32)
        mall = sb(st0, "mall", [128, NOT, N_EXP], F32)
        pos = sb(st0, "pos", [128, NOT, N_EXP], F32)
        TG = sb(st0, "TG", [128, NOT, NTG], F32)
        iota = sb(st0, "iota", [128, 512], F32)
        dummy = sb(st0, "dummy", [128, NS], F32)
        r_c = Reg(const=True)
        with ExitStack() as st:
            ut = sb(st, "ut", [128, 128], F32)
            loc = sb(st, "loc", [128, NOT], F32)
            offs = sb(st, "offs", [128, NOT, N_EXP], F32)
            x2b = [sb(st, "x2b%d" % i, [128, D], F32) for i in range(2)]
            r_x2b = [Reg() for _ in range(2)]
            pc = [ps(st, "pc%d" % i, [128, 512], F32) for i in range(2)]
            r_pc = [Reg() for _ in range(2)]
            r1 = Reg()
            S.dma("sp", own[:], I.own, writes=[r_c])
            S.dma("sp", ut[:], I.c_ut, writes=[r1])
            S.dma("sp", loc[:], I.c_loc, writes=[r1])
            S.dma("sp", iota[:], I.c_iota, writes=[r_c])
            S.dma("sp", dummy[:], I.c_dummy, writes=[r_c])
            for j in range(NOT):
                off = bass.IndirectOffsetOnAxis(ap=own[:, j:j + 1], axis=0)
                S.dma_fn("pool", lambda e, j=j, off=off: e.indirect_dma_start(out=gmo[:, j, :], out_offset=None, in_=Sx.gm, in_offset=off),
                         reads=[r_c, R.gm], writes=[r1])
                b = j % 2
                S.dma_fn("pool", lambda e, b=b, off=off: e.indirect_dma_start(out=x2b[b][:], out_offset=None, in_=Sx.x2, in_offset=off),
                         reads=[r_c] + R.x2, writes=[r_x2b[b]])
                S.dma("sp", Sx.accd[j * 128:(j + 1) * 128, :], x2b[b][:], reads=[r_x2b[b]], writes=[R.accd])
            ts(mall[:], gmo[:], 0.0, None, ALU.is_gt, None, [r1], [r1])
            flat = mall[:].rearrange("p j e -> p (j e)")
            mm(pc[0][:, 0:NOT * N_EXP], ut[:], flat, True, True, [r1], [r_pc[0]])
            mm(pc[1][:, 0:NOT * N_EXP], C.onesf[:], flat, True, True, [r1, C.r], [r_pc[1]])
            tot = pc[1][:, 0:NOT * N_EXP].rearrange("p (j e) -> p j e", e=N_EXP)
            cum = pc[0][:, 0:NOT * N_EXP].rearrange("p (j e) -> p j e", e=N_EXP)
            S.op("dve", lambda e: e.memset(offs[:, 0, :], 0.0), writes=[r1])
            for j in range(1, NOT):
                tt(offs[:, j, :], offs[:, j - 1, :], tot[:, j - 1, :], ALU.add, [r1, r_pc[1]], [r1])
            tt(pos[:], cum, offs[:], ALU.add, [r_pc[0], r1], [r1])
            ts(pos[:], pos[:], -1.0, None, ALU.add, None, [r1], [r1])
            cp(TG[:, :, 0], own[:], [r_c], [r1])
            cp(TG[:, :, 1], loc[:], [r1], [r1])
            S.op("dve", lambda e: e.memset(TG[:, :, 2], 1.0), writes=[r1])
            cp(TG[:, :, 3:NTG], gmo[:], [r1], [r1])
            S.emit_phase()
        with ExitStack() as st:
            FP = 512
            NFP = D // FP
            NB = 2
            wg = [sb(st, "wg%d" % i, [128, NCH, FP], BF16) for i in range(NB)]
            wu = [sb(st, "wu%d" % i, [128, NCH, FP], BF16) for i in range(NB)]
            wd = [sb(st, "wd%d" % i, [128, NCH, FP], BF16) for i in range(NB)]
            r_wg = [Reg() for _ in range(NB)]
            r_wu = [Reg() for _ in range(NB)]
            r_wd = [Reg() for _ in range(NB)]
            Sel = [sb(st, "Sel%d" % i, [128, NOT, 128], F32) for i in range(2)]
            r_Sel = [Reg() for _ in range(2)]
            sis = sb(st, "sis", [128, NS, 32], F32)
            sidf = sb(st, "sidf", [128, NS], F32)
            gi = [sb(st, "gi%d" % i, [128, NS], I32) for i in range(2)]
            si = [sb(st, "si%d" % i, [128, NS], I32) for i in range(2)]
            gs = [sb(st, "gs%d" % i, [128, NS], F32) for i in range(2)]
            r_sis = Reg()
            r_idx = [Reg() for _ in range(2)]
            xe = sb(st, "xe", [128, NS, D], BF16)
            r_xe = [Reg() for _ in range(NS)]
            xeT1 = sb(st, "xeT", [128, NCH, CAP], BF16)
            xeT = [xeT1, xeT1]
            r_xeT1 = Reg()
            r_xeT = [r_xeT1, r_xeT1]
            actT = sb(st, "actT", [128, NCH, CAP], BF16)
            r_actT = [Reg() for _ in range(NCH)]
            ygs = sb(st, "ygs", [128, NS, D], F32)
            r_ygs = [Reg() for _ in range(NS)]
            sg = [sb(st, "sg%d" % i, [128, 512], F32) for i in range(2)]
            r_sg = [Reg() for _ in range(2)]
            cnt_stg = {"n": 0}

            def wload(dst, r_dst, src):
                cnt_stg["n"] += 1
                S.dma("pool", dst, src, writes=[r_dst])
            Gp = [ps(st, "Gp%d" % i, [128, 512], F32) for i in range(2)]
            Up = [ps(st, "Up%d" % i, [128, 512], F32) for i in range(2)]
            Yp = [ps(st, "Yp%d" % i, [128, 512], F32) for i in range(2)]
            Tq = ps(st, "Tq", [128, 512], F32)
            SIp = ps(st, "SIp", [128, 512], F32)
            r_Gp = [Reg() for _ in range(2)]
            r_Up = [Reg() for _ in range(2)]
            r_Yp = [Reg() for _ in range(2)]
            r_Tq, r_SIp = Reg(), Reg()
            tqv = Tq[:].bitcast(BF16)
            cnt = {"w": 0, "d": 0, "g": 0, "y": 0}

            def prep_idx(ex):
                pb = ex % 2
                for s4 in range(NS):
                    sl = s4 % 2
                    for j in range(NOT):
                        ts(Sel[sl][:, j, :], iota[:, s4 * 128:(s4 + 1) * 128], pos[:, j, ex:ex + 1], mall[:, j, ex:ex + 1],
                           ALU.is_equal, ALU.mult, [r_c], [r_Sel[sl]])
                    for j in range(NOT):
                        mm(SIp[:, s4 * 32:s4 * 32 + NTG], Sel[sl][:, j, :], TG[:, j, :], j == 0, j == NOT - 1,
                           [r_Sel[sl], r_c], [r_SIp])
                cp(sis[:], SIp[:, 0:NS * 32].rearrange("p (s k) -> p s k", k=32), [r_SIp], [r_sis])
                cp(gi[pb][:], sis[:, :, 0], [r_sis], [r_idx[pb]])
                tt(sidf[:], sis[:, :, 2], dummy[:], ALU.mult, [r_sis, r_c], [r_sis])
                tt(sidf[:], dummy[:], sidf[:], ALU.subtract, [r_sis, r_c], [r_sis])
                tt(sidf[:], sidf[:], sis[:, :, 1], ALU.add, [r_sis], [r_sis])
                cp(si[pb][:], sidf[:], [r_sis], [r_idx[pb]])
                cp(gs[pb][:], sis[:, :, 3 + ex], [r_sis], [r_idx[pb]])
                for s4 in range(NS):
                    off = bass.IndirectOffsetOnAxis(ap=gi[pb][:, s4:s4 + 1], axis=0)
                    S.dma_fn("pool", lambda e, s4=s4, off=off: e.indirect_dma_start(out=xe[:, s4, :], out_offset=None, in_=Sx.h3, in_offset=off),
                             reads=[r_idx[pb]] + R.h3, writes=[r_xe[s4]])

            def prep_T(ex):
                pb = ex % 2
                k = 0
                for s4 in range(NS):
                    for hh in range(4):
                        for c in range(4):
                            cc = hh * 4 + c
                            tr(tqv[:, c * 128:(c + 1) * 128], xe[:, s4, cc * 128:(cc + 1) * 128], [r_xe[s4]], [r_Tq])
                        cp(xeT[pb][:, hh * 4:hh * 4 + 4, s4 * 128:(s4 + 1) * 128], tqv[:, 0:512].rearrange("p (c n) -> p c n", c=4),
                           [r_Tq], [r_xeT[pb]], eng=("act" if k % 2 == 0 else "dve"))
                        k += 1

            def load_gu(ex, fp):
                wgv = I.w_gate[ex].rearrange("(c p) f -> p c f", p=128)
                wuv = I.w_up[ex].rearrange("(c p) f -> p c f", p=128)
                wb = fp % NB
                wload(wg[wb][:], r_wg[wb], wgv[:, :, fp * FP:(fp + 1) * FP])
                wload(wu[wb][:], r_wu[wb], wuv[:, :, fp * FP:(fp + 1) * FP])

            def load_d(ex, dp):
                wdv = I.w_down[ex].rearrange("(c p) f -> p c f", p=128)
                db = dp % NB
                wload(wd[db][:], r_wd[db], wdv[:, :, dp * FP:(dp + 1) * FP])

            def compute_gu(ex, fp):
                pb = ex % 2
                wb = fp % NB
                for f2 in range(FP // 128):
                    fc = fp * (FP // 128) + f2
                    gb = cnt["g"] % 2
                    cnt["g"] += 1
                    for c in range(NCH):
                        mm(Gp[gb][:], wg[wb][:, c, f2 * 128:(f2 + 1) * 128], xeT[pb][:, c, :], c == 0, c == NCH - 1,
                           [r_wg[wb], r_xeT[pb]], [r_Gp[gb]])
                    for c in range(NCH):
                        mm(Up[gb][:], wu[wb][:, c, f2 * 128:(f2 + 1) * 128], xeT[pb][:, c, :], c == 0, c == NCH - 1,
                           [r_wu[wb], r_xeT[pb]], [r_Up[gb]])
                    act(sg[gb][:], Gp[gb][:], AF.Silu, [r_Gp[gb]], [r_sg[gb]])
                    tt(actT[:, fc, :], sg[gb][:], Up[gb][:], ALU.mult, [r_sg[gb], r_Up[gb]], [r_actT[fc]])

            def compute_d(ex, dp):
                pb = ex % 2
                db = dp % NB
                for s4 in range(NS):
                    yb = cnt["y"] % 2
                    cnt["y"] += 1
                    for fc in range(NCH):
                        mm(Yp[yb][:, 0:FP], actT[:, fc, s4 * 128:(s4 + 1) * 128], wd[db][:, fc, :], fc == 0, fc == NCH - 1,
                           [r_actT[fc], r_wd[db]], [r_Yp[yb]])
                    if s4 % 2 == 0:
                        ts(ygs[:, s4, dp * FP:(dp + 1) * FP], Yp[yb][:, 0:FP], gs[pb][:, s4:s4 + 1], None, ALU.mult, None,
                           [r_Yp[yb], r_idx[pb]], [r_ygs[s4]])
                    else:
                        act(ygs[:, s4, dp * FP:(dp + 1) * FP], Yp[yb][:, 0:FP], AF.Copy, [r_Yp[yb], r_idx[pb]], [r_ygs[s4]],
                            scale=gs[pb][:, s4:s4 + 1])

            def scatter(ex):
                pb = ex % 2
                for s4 in range(NS):
                    off = bass.IndirectOffsetOnAxis(ap=si[pb][:, s4:s4 + 1], axis=0)
                    S.dma_fn("pool", lambda e, s4=s4, off=off: e.indirect_dma_start(out=Sx.accd, out_offset=off, in_=ygs[:, s4, :], in_offset=None,
                                                                                  compute_op=ALU.add),
                             reads=[r_idx[pb], r_ygs[s4], R.accd], writes=[R.accd])

            assert NB == 2 and NFP == 4
            prep_idx(0)
            prep_T(0)
            load_gu(0, 0)
            load_gu(0, 1)
            for ex in range(N_EXP):
                load_d(ex, 0)
                load_d(ex, 1)
                for fp in range(NFP):
                    compute_gu(ex, fp)
                    if fp + 2 < NFP:
                        load_gu(ex, fp + 2)
                if ex + 1 < N_EXP:
                    prep_idx(ex + 1)
                    load_gu(ex + 1, 0)
                    load_gu(ex + 1, 1)
                for dp in range(NFP):
                    compute_d(ex, dp)
                    if dp + 2 < NFP:
                        load_d(ex, dp + 2)
                    if dp == 1 and ex + 1 < N_EXP:
                        prep_T(ex + 1)
                scatter(ex)
            fin = S.dma("sp", K.out, Sx.accd[0:OWN, :], reads=[R.accd], writes=[])
            S.finish([fin])
            S.emit_phase()
```

```python
import numpy as np
from contextlib import ExitStack
import concourse.bass as bass
import concourse.mybir as mybir
from concourse.bass_utils import run_bass_kernel_spmd

F32 = mybir.dt.float32
BF16 = mybir.dt.bfloat16
I32 = mybir.dt.int32
AF = mybir.ActivationFunctionType
ALU = mybir.AluOpType
AX = mybir.AxisListType

SAME_ENGINE_SYNC = True
MOE_COMPACT = True


class Reg:
    __slots__ = ("name", "last_w", "readers", "const")

    def __init__(self, name="", const=False):
        self.name = name
        self.last_w = None
        self.readers = []
        self.const = const


class Op:
    __slots__ = ("eng", "fn", "deps", "needed", "sig", "is_dma", "waits_only")

    def __init__(self, eng, fn, deps, is_dma=False):
        self.eng = eng
        self.fn = fn
        self.deps = deps
        self.needed = False
        self.sig = None
        self.is_dma = is_dma
        self.waits_only = False


class Sched:
    ENGS = ("pe", "act", "dve", "pool", "sp")
    NDMA = 12

    def __init__(self, nc, stack):
        self.nc = nc
        self.sem = {e: stack.enter_context(nc.semaphore("s_" + e)) for e in self.ENGS}
        self.cnt = {e: 0 for e in self.ENGS}
        self.dsem = {q: [stack.enter_context(nc.semaphore("d_%s%d" % (q, i))) for i in range(self.NDMA)]
                     for q in ("sp", "pool", "act")}
        self.dcnt = {q: 0 for q in ("sp", "pool", "act")}
        self.dlast = {q: [None] * self.NDMA for q in ("sp", "pool", "act")}
        self.ops = {e: [] for e in self.ENGS}
        self.known = {e: {} for e in self.ENGS}
        self.last_op = {e: None for e in self.ENGS}
        self.prev_phase_last = []

    def _mk(self, eng, fn, reads, writes, is_dma=False, extra=()):
        deps = list(extra)
        for r in reads:
            if r.last_w is not None:
                deps.append(r.last_w)
        for w in writes:
            if w.last_w is not None:
                deps.append(w.last_w)
            deps.extend(w.readers)
        op = Op(eng, fn, deps, is_dma)
        for r in reads:
            if not r.const:
                r.readers.append(op)
        for w in writes:
            w.last_w = op
            w.readers = []
        self.ops[eng].append(op)
        return op

    def op(self, eng, fn, reads=(), writes=()):
        return self._mk(eng, fn, reads, writes)

    def dma(self, q, out, in_, reads=(), writes=(), extra=(), **kw):
        return self._mk(q, lambda e: e.dma_start(out=out, in_=in_, **kw), reads, writes, is_dma=True, extra=extra)

    def dma_fn(self, q, fn, reads=(), writes=(), extra=()):
        return self._mk(q, fn, reads, writes, is_dma=True, extra=extra)

    def finish(self, ops):
        o = Op("sp", None, list(ops))
        o.waits_only = True
        self.ops["sp"].append(o)

    def emit_phase(self):
        self.nphase = getattr(self, "nphase", 0) + 1
        with self.nc.named_scope("ph%02d" % self.nphase):
            self._emit_phase()

    def _emit_phase(self):
        nc = self.nc
        engs = {"pe": nc.tensor, "act": nc.scalar, "dve": nc.vector, "pool": nc.gpsimd, "sp": nc.sync}
        barrier = list(self.prev_phase_last)
        for e in self.ENGS:
            for op in self.ops[e]:
                for d in op.deps:
                    if d.eng == op.eng and not d.is_dma:
                        if e in ("pe", "sp") or not SAME_ENGINE_SYNC:
                            continue
                    d.needed = True
        lasts = []
        for e in self.ENGS:
            real = [o for o in self.ops[e] if not o.waits_only]
            if real:
                real[-1].needed = True
                lasts.append(real[-1])
        for e in self.ENGS:
            for op in self.ops[e]:
                if op.waits_only:
                    continue
                if op.is_dma:
                    q = e
                    j = self.dcnt[q]
                    self.dcnt[q] += 1
                    op.sig = (self.dsem[q][j % self.NDMA], 16 * (j // self.NDMA + 1), q, j)
                elif op.needed:
                    self.cnt[e] += 1
                    op.sig = (self.sem[e], self.cnt[e])
        dma_lasts = []
        with nc.Block() as block:
            def run(e):
                def body(eng):
                    known = self.known[e]

                    def wait(sig):
                        s, v = sig[0], sig[1]
                        k = id(s)
                        if known.get(k, 0) < v:
                            eng.wait_ge(s, v)
                            known[k] = v
                    for d in barrier:
                        if d.eng != e or d.is_dma:
                            wait(d.sig)
                    for op in self.ops[e]:
                        for d in op.deps:
                            if d.sig is None:
                                continue
                            if d.eng == e and not d.is_dma:
                                if e in ("pe", "sp") or not SAME_ENGINE_SYNC:
                                    continue
                            wait(d.sig)
                        if op.waits_only:
                            continue
                        if op.is_dma:
                            s, v, q, j = op.sig
                            if j >= self.NDMA:
                                wait((s, v - 16))
                            ins = op.fn(eng)
                            ins.then_inc(s, 16)
                        else:
                            ins = op.fn(eng)
                            if op.sig is not None:
                                ins.then_inc(op.sig[0], 1)
                return body
            block.tensor(run("pe"))
            block.scalar(run("act"))
            block.vector(run("dve"))
            block.gpsimd(run("pool"))
            block.sync(run("sp"))
        for q in ("sp", "pool", "act"):
            seen = {}
            for op in self.ops[q]:
                if op.is_dma:
                    seen[id(op.sig[0])] = op
            dma_lasts.extend(seen.values())
        self.prev_phase_last = lasts + dma_lasts + [d for d in self.prev_phase_last if d.is_dma and id(d.sig[0]) not in {id(x.sig[0]) for x in dma_lasts}]
        self.ops = {e: [] for e in self.ENGS}


D = 2048
SEQ = 4096
HD = 128
D_IN = 4608
NCH = D // 128
NT = SEQ // 128
EPS = 1e-6
N_EXP = 16
CAP = 512
OWN = 1024
TWO_PI = float(2.0 * np.pi)
SCALE = float(1.0 / np.sqrt(HD))
HC_COL = [128 * i for i in range(8)] + [1024 + 128 * i for i in range(8)] + \
         [3072 + 128 * i for i in range(8)] + [4096, 4224]
HC_FAM = [0] * 8 + [1] * 8 + [2] * 8 + [3] * 2
V_COLS = [(2048, 512), (2560, 512), (4352, 256)]


def mask_a_np():
    kp = np.arange(128)[:, None]
    qf = np.arange(512)[None, :]
    out = np.zeros((20, 128, 512), np.float32)
    for j in range(20):
        o = (-1024 + 128 * j) + kp - qf
        a = np.abs(o)
        out[j] = (a <= 64).astype(np.float32) + ((o % 4 == 0) & (a <= 256)) + ((o % 16 == 0) & (a <= 1024))
    return out


def mask_b_np():
    kp = np.arange(128)[:, None]
    qf = np.arange(512)[None, :]
    out = np.zeros((6, 128, 512), np.float32)
    for j in range(6):
        o = (-128 + 128 * j) + kp - qf
        out[j] = (np.abs(o) <= 128)
    return out


def host_consts():
    import ml_dtypes
    bf = ml_dtypes.bfloat16
    c = {}
    c["c_ident"] = np.eye(128, dtype=np.float32).astype(bf)
    c["c_identf"] = np.eye(128, dtype=np.float32)
    c["c_ones"] = np.ones((128, 128), np.float32).astype(bf)
    c["c_onesf"] = np.ones((128, 128), np.float32)
    p0 = np.zeros((128, 128), np.float32)
    for m in range(64):
        p0[m + 64, m] = -1.0
    for m in range(64, 128):
        p0[m - 64, m] = 1.0
    c["c_rot"] = p0
    i = np.arange(128) % 64
    c["c_invf"] = (10000.0 ** (-(2.0 * i) / 128.0)).astype(np.float32).reshape(128, 1)
    pp = np.arange(128)
    c["c_ut"] = (pp[:, None] <= pp[None, :]).astype(np.float32)
    c["c_iota"] = np.tile(np.arange(512, dtype=np.float32)[None, :], (128, 1))
    c["c_loc"] = (np.arange(8)[None, :] * 128 + pp[:, None]).astype(np.float32)
    c["c_dummy"] = (1024 + np.arange(4)[None, :] * 128 + pp[:, None]).astype(np.float32)
    def lnm(m):
        out = np.full(m.shape, -3000.0, np.float32)
        nz = m > 0
        out[nz] = np.log(m[nz]) / SCALE
        return out
    c["c_maska"] = np.ascontiguousarray(lnm(mask_a_np()).transpose(1, 0, 2)).astype(bf)
    c["c_maskb"] = np.ascontiguousarray(lnm(mask_b_np()).transpose(1, 0, 2)).astype(bf)
    return c


class Ctx:
    pass


def build(stop_after=99, debug=False):
    nc = bass.Bass("TRN2", target_bir_lowering=False)
    K = Ctx()
    K.nc = nc

    def din(name, shape, dt):
        return nc.dram_tensor(name, list(shape), dt, kind="ExternalInput").ap()

    def dscr(name, shape, dt):
        kind = "ExternalOutput" if debug else "Internal"
        return nc.dram_tensor(name, list(shape), dt, kind=kind).ap()

    I = Ctx()
    I.x = din("x", [SEQ, D], F32)
    I.mem = din("mem", [256, D], F32)
    I.pos = din("positions", [SEQ], I32)
    I.own = din("own_idx", [128, OWN // 128], I32)
    for n in ("g_mix", "g_cross", "g_mem", "g_moe"):
        setattr(I, n, din(n, [D], F32))
    for n in ("g_qa", "g_ka", "g_qb", "g_kb", "g_qm", "g_km"):
        setattr(I, n, din(n, [HD], F32))
    I.sink = din("sink_b", [8], F32)
    I.g_oa = din("g_oa", [1024], F32)
    I.g_ob = din("g_ob", [1024], F32)
    I.w_in = din("w_in", [D, D_IN], F32)
    I.w_out = din("w_out", [D, D], F32)
    I.w_q_mem = din("w_q_mem", [D, 512], F32)
    I.w_kv_mem = din("w_kv_mem", [D, 1024], F32)
    I.w_o_mem = din("w_o_mem", [512, D], F32)
    I.w_router = din("w_router", [D, N_EXP], F32)
    I.w_gate = din("w_gate", [N_EXP, D, D], F32)
    I.w_up = din("w_up", [N_EXP, D, D], F32)
    I.w_down = din("w_down", [N_EXP, D, D], F32)
    I.c_ident = din("c_ident", [128, 128], BF16)
    I.c_identf = din("c_identf", [128, 128], F32)
    I.c_ones = din("c_ones", [128, 128], BF16)
    I.c_onesf = din("c_onesf", [128, 128], F32)
    I.c_rot = din("c_rot", [128, 128], F32)
    I.c_invf = din("c_invf", [128, 1], F32)
    I.c_ut = din("c_ut", [128, 128], F32)
    I.c_iota = din("c_iota", [128, 512], F32)
    I.c_loc = din("c_loc", [128, 8], F32)
    I.c_dummy = din("c_dummy", [128, 4], F32)
    I.c_maska = din("c_maska", [128, 20, 512], BF16)
    I.c_maskb = din("c_maskb", [128, 6, 512], BF16)
    K.I = I
    out = nc.dram_tensor("out", [OWN, D], F32, kind="ExternalOutput").ap()
    K.out = out

    Sx = Ctx()
    Sx.qkt = dscr("s_qkt", [26, 128, SEQ], BF16)
    Sx.v = dscr("s_v", [SEQ, 1280], BF16)
    Sx.ot = dscr("s_ot", [16, 128, SEQ], BF16)
    Sx.x2 = dscr("s_x2", [SEQ, D], F32)
    Sx.h3 = dscr("s_h3", [SEQ, D], BF16)
    Sx.gm = dscr("s_gm", [SEQ, N_EXP], F32)
    Sx.accd = dscr("s_accd", [OWN + CAP, D], F32)
    K.Sx = Sx
    R = Ctx()
    R.qkt = [[Reg("qkt%d_%d" % (h, f)) for f in range(2)] for h in range(26)]
    R.v = [Reg("v%d" % t) for t in range(NT)]
    R.ot = [Reg("ot%d" % h) for h in range(16)]
    R.x2 = [Reg("x2_%d" % t) for t in range(NT)]
    R.h3 = [Reg("h3_%d" % t) for t in range(NT)]
    R.gm = Reg("gm")
    R.accd = Reg("accd")
    K.R = R

    with ExitStack() as top:
        S = Sched(nc, top)
        K.S = S

        uid = [0]

        def sb(stack, name, shape, dt):
            uid[0] += 1
            return stack.enter_context(nc.sbuf_tensor("%s_%d" % (name, uid[0]), list(shape), dt))

        def ps(stack, name, shape, dt):
            uid[0] += 1
            return stack.enter_context(nc.psum_tensor("%s_%d" % (name, uid[0]), list(shape), dt))
        K.sb, K.ps = sb, ps

        def mm(out, lhsT, rhs, start, stop, reads, writes):
            return S.op("pe", lambda e: e.matmul(out, lhsT=lhsT, rhs=rhs, start=start, stop=stop), reads, writes)

        def tr(out, in_, reads, writes, ident=None):
            idn = C.ident[:] if ident is None else ident
            return S.op("pe", lambda e: e.transpose(out=out, in_=in_, identity=idn), list(reads) + [C.r], writes)

        def act(out, in_, func, reads, writes, **kw):
            return S.op("act", lambda e: e.activation(out=out, in_=in_, func=func, **kw), reads, writes)

        def ts(out, in0, s1, s2, op0, op1, reads, writes, eng="dve", **kw):
            if op1 is None:
                return S.op(eng, lambda e: e.tensor_scalar(out=out, in0=in0, scalar1=s1, scalar2=None, op0=op0, **kw), reads, writes)
            return S.op(eng, lambda e: e.tensor_scalar(out=out, in0=in0, scalar1=s1, scalar2=s2, op0=op0, op1=op1, **kw), reads, writes)

        def tt(out, in0, in1, op, reads, writes, eng="dve"):
            return S.op(eng, lambda e: e.tensor_tensor(out=out, in0=in0, in1=in1, op=op), reads, writes)

        def stt(out, in0, scalar, in1, op0, op1, reads, writes, eng="dve"):
            return S.op(eng, lambda e: e.scalar_tensor_tensor(out=out, in0=in0, scalar=scalar, in1=in1, op0=op0, op1=op1), reads, writes)

        def rsq(out, in_, scale, reads, writes):
            S.op("dve", lambda e: e.tensor_scalar(out=out, in0=in_, scalar1=scale, scalar2=EPS, op0=ALU.mult, op1=ALU.add), reads, writes)
            S.op("act", lambda e: e.activation(out=out, in_=out, func=AF.Ln), writes, writes)
            return S.op("act", lambda e: e.activation(out=out, in_=out, func=AF.Exp, scale=-0.5), writes, writes)
        K.rsq = rsq

        def recip(out, in_, reads, writes):
            return S.op("dve", lambda e: e.reciprocal(out=out, in_=in_), reads, writes)

        def cp(out, in_, reads, writes, eng="dve"):
            if eng == "act":
                return S.op("act", lambda e: e.copy(out=out, in_=in_), reads, writes)
            return S.op(eng, lambda e: e.tensor_copy(out=out, in_=in_), reads, writes)
        K.mm, K.tr, K.act, K.ts, K.tt, K.stt, K.recip, K.cp = mm, tr, act, ts, tt, stt, recip, cp

        C = Ctx()
        K.C = C
        C.ident = sb(top, "ident", [128, 128], BF16)
        C.identf = sb(top, "identf", [128, 128], F32)
        C.ones = sb(top, "ones", [128, 128], BF16)
        C.onesf = sb(top, "onesf", [128, 128], F32)
        C.rot0 = sb(top, "rot0", [128, 128], F32)
        C.rotg = sb(top, "rotg", [128, 4, 128], BF16)
        C.invf = sb(top, "invf", [128, 1], F32)
        C.gh = sb(top, "gh", [128, 6], F32)
        C.ss = sb(top, "ssab", [128, 2, NT], F32)
        C.esink = sb(top, "esink", [128, 8], F32)
        C.aff = sb(top, "aff", [128, NT, N_EXP], F32)
        C.r_aff = Reg("aff")
        C.r = Reg("consts", const=True)
        C.r_ss = Reg("ss")
        S.dma("sp", C.ident[:], I.c_ident, writes=[C.r])
        S.dma("sp", C.identf[:], I.c_identf, writes=[C.r])
        S.dma("sp", C.ones[:], I.c_ones, writes=[C.r])
        S.dma("sp", C.onesf[:], I.c_onesf, writes=[C.r])
        S.dma("sp", C.rot0[:], I.c_rot, writes=[C.r])
        S.dma("sp", C.invf[:], I.c_invf, writes=[C.r])
        for i, n in enumerate(("g_qa", "g_ka", "g_qb", "g_kb", "g_qm", "g_km")):
            S.dma("sp", C.gh[:, i:i + 1], getattr(I, n).rearrange("(p o) -> p o", o=1), writes=[C.r])
        S.dma("sp", C.esink[:], I.sink.partition_broadcast(128), writes=[C.r])
        S.op("act", lambda e: e.activation(out=C.esink[:], in_=C.esink[:], func=AF.Exp), reads=[C.r], writes=[C.r])
        for f in range(4):
            S.op("dve", lambda e, f=f: e.tensor_scalar(out=C.rotg[:, f, :], in0=C.rot0[:], scalar1=C.gh[:, f:f + 1],
                                                       scalar2=None, op0=ALU.mult), reads=[C.r], writes=[C.r])
        S.op("dve", lambda e: e.memset(C.ss[:], 0.0), writes=[C.r_ss])

        phase_a(K, top)
        if stop_after >= 2:
            phase_b(K, top)
        if stop_after >= 3:
            phase_c(K, top)
        if stop_after >= 4:
            phase_d(K, top)
        if stop_after >= 5:
            if MOE_COMPACT:
                phase_e2(K, top)
            else:
                phase_e(K, top)
        S.emit_phase()
    return nc


PI_LO = 3.1415925


def phase_a(K, top):
    nc, S, I, C, Sx, R, sb, ps = K.nc, K.S, K.I, K.C, K.Sx, K.R, K.sb, K.ps
    HALF = SEQ // 2
    w_in_v = I.w_in.rearrange("(c p) n -> p c n", p=128)
    for hf in range(2):
        with ExitStack() as st_x:
            xnT = sb(st_x, "xnT", [128, NCH, HALF], BF16)
            r_xnT = [Reg("xnT%d" % t) for t in range(16)]
            with ExitStack() as st:
                xs = [sb(st, "xs%d" % i, [128, D], F32) for i in range(2)]
                xn = [sb(st, "xn%d" % i, [128, D], BF16) for i in range(2)]
                junk = sb(st, "junk", [128, D], BF16)
                gbc = sb(st, "gbc", [128, D], F32)
                ssq = sb(st, "ssq", [128, 2], F32)
                rstd = sb(st, "rstd", [128, 2], F32)
                tp = [ps(st, "tp%d" % i, [128, 8, 128], BF16) for i in range(4)]
                r_xs = [Reg() for _ in range(2)]
                r_xn = [Reg() for _ in range(2)]
                r_junk, r_gbc = Reg(), Reg(const=True)
                r_ssq = [Reg() for _ in range(2)]
                r_rstd = [Reg() for _ in range(2)]
                r_tp = [Reg() for _ in range(4)]
                S.dma("sp", gbc[:], I.g_mix.partition_broadcast(128), writes=[r_gbc])
                for t in range(16):
                    b = t % 2
                    tok0 = hf * HALF + t * 128
                    S.dma("sp", xs[b][:], I.x[tok0:tok0 + 128, :], writes=[r_xs[b]])
                    S.op("act", lambda e, b=b: e.activation(out=junk[:], in_=xs[b][:], func=AF.Square,
                                                            accum_out=ssq[:, b:b + 1]),
                         reads=[r_xs[b]], writes=[r_junk, r_ssq[b]])
                    K.rsq(rstd[:, b:b + 1], ssq[:, b:b + 1], 1.0 / D, [r_ssq[b]], [r_rstd[b]])
                    S.op("dve", lambda e, b=b: e.scalar_tensor_tensor(out=xn[b][:], in0=xs[b][:], scalar=rstd[:, b:b + 1],
                                                                      in1=gbc[:], op0=ALU.mult, op1=ALU.mult),
                         reads=[r_xs[b], r_rstd[b], r_gbc], writes=[r_xn[b]])
                    for hh in range(2):
                        pt = tp[2 * b + hh]
                        rp = r_tp[2 * b + hh]
                        for c in range(8):
                            cc = hh * 8 + c
                            S.op("pe", lambda e, pt=pt, c=c, cc=cc, b=b: e.transpose(out=pt[:, c, :], in_=xn[b][:, cc * 128:(cc + 1) * 128],
                                                                                      identity=C.ident[:]),
                                 reads=[r_xn[b], C.r], writes=[rp])
                        eng = "act" if hh == 0 else "dve"
                        if eng == "act":
                            S.op("act", lambda e, pt=pt, hh=hh, t=t: e.copy(out=xnT[:, hh * 8:hh * 8 + 8, t * 128:(t + 1) * 128], in_=pt[:]),
                                 reads=[rp], writes=[r_xnT[t]])
                        else:
                            S.op("dve", lambda e, pt=pt, hh=hh, t=t: e.tensor_copy(out=xnT[:, hh * 8:hh * 8 + 8, t * 128:(t + 1) * 128], in_=pt[:]),
                                 reads=[rp], writes=[r_xnT[t]])
                S.emit_phase()
            with ExitStack() as st:
                cos = sb(st, "cos", [128, HALF], F32)
                sin = sb(st, "sin", [128, HALF], F32)
                posi = sb(st, "posi", [128, 1024], I32)
                ang = sb(st, "ang", [128, 1024], F32)
                kf = sb(st, "kf", [128, 1024], F32)
                ki = sb(st, "ki", [128, 1024], I32)
                r_cs = Reg(const=True)
                r_tmp = Reg()
                for qd in range(2):
                    t0 = hf * HALF + qd * 1024
                    sl = slice(qd * 1024, (qd + 1) * 1024)
                    S.dma("sp", posi[:], I.pos[t0:t0 + 1024].partition_broadcast(128), writes=[r_tmp])
                    S.op("dve", lambda e: e.tensor_copy(out=ang[:], in_=posi[:]), reads=[r_tmp], writes=[r_tmp])
                    S.op("dve", lambda e: e.tensor_scalar(out=ang[:], in0=ang[:], scalar1=C.invf[:, 0:1], scalar2=None, op0=ALU.mult),
                         reads=[r_tmp, C.r], writes=[r_tmp])
                    S.op("dve", lambda e: e.tensor_scalar(out=kf[:], in0=ang[:], scalar1=1.0 / TWO_PI, scalar2=None, op0=ALU.mult),
                         reads=[r_tmp], writes=[r_tmp])
                    S.op("dve", lambda e: e.tensor_copy(out=ki[:], in_=kf[:]), reads=[r_tmp], writes=[r_tmp])
                    S.op("dve", lambda e: e.tensor_copy(out=kf[:], in_=ki[:]), reads=[r_tmp], writes=[r_tmp])
                    S.op("dve", lambda e: e.scalar_tensor_tensor(out=ang[:], in0=kf[:], scalar=-TWO_PI, in1=ang[:], op0=ALU.mult, op1=ALU.add),
                         reads=[r_tmp], writes=[r_tmp])

                    def wrap_and_sin(dst, shift):
                        S.op("dve", lambda e: e.tensor_scalar(out=kf[:], in0=ang[:], scalar1=shift, scalar2=None, op0=ALU.add),
                             reads=[r_tmp], writes=[r_tmp])
                        S.op("dve", lambda e: e.tensor_scalar(out=posi[:].bitcast(F32), in0=kf[:], scalar1=float(np.pi), scalar2=-TWO_PI,
                                                              op0=ALU.is_gt, op1=ALU.mult), reads=[r_tmp], writes=[r_tmp])
                        S.op("dve", lambda e: e.tensor_tensor(out=kf[:], in0=kf[:], in1=posi[:].bitcast(F32), op=ALU.add),
                             reads=[r_tmp], writes=[r_tmp])
                        S.op("dve", lambda e: e.tensor_scalar(out=posi[:].bitcast(F32), in0=kf[:], scalar1=-float(np.pi), scalar2=TWO_PI,
                                                              op0=ALU.is_lt, op1=ALU.mult), reads=[r_tmp], writes=[r_tmp])
                        S.op("dve", lambda e: e.tensor_tensor(out=kf[:], in0=kf[:], in1=posi[:].bitcast(F32), op=ALU.add),
                             reads=[r_tmp], writes=[r_tmp])
                        S.op("dve", lambda e: e.tensor_scalar(out=kf[:], in0=kf[:], scalar1=PI_LO, scalar2=-PI_LO, op0=ALU.min, op1=ALU.max),
                             reads=[r_tmp], writes=[r_tmp])
                        S.op("act", lambda e: e.activation(out=dst, in_=kf[:], func=AF.Sin), reads=[r_tmp], writes=[r_cs, r_tmp])
                    wrap_and_sin(sin[:, sl], 0.0)
                    wrap_and_sin(cos[:, sl], float(np.pi / 2))

                wq = [sb(st, "wq%d" % i, [128, NCH, 128], BF16) for i in range(2)]
                r_wq = [Reg() for _ in range(2)]
                q2 = [sb(st, "q2_%d" % i, [128, 512], BF16) for i in range(2)]
                qb = [sb(st, "qb_%d" % i, [128, 512], BF16) for i in range(2)]
                rs = [sb(st, "rs_%d" % i, [128, 512], F32) for i in range(2)]
                ta = [sb(st, "ta_%d" % i, [128, 512], F32) for i in range(2)]
                tb = [sb(st, "tb_%d" % i, [128, 512], F32) for i in range(2)]
                stage = [sb(st, "stg%d" % i, [128, HALF], BF16) for i in range(2)]
                r_q2 = [Reg() for _ in range(2)]
                r_qb = [Reg() for _ in range(2)]
                r_rs = [Reg() for _ in range(2)]
                r_ta = [Reg() for _ in range(2)]
                r_tb = [Reg() for _ in range(2)]
                r_stage = [Reg() for _ in range(2)]
                qp = [ps(st, "qp%d" % i, [128, 512], F32) for i in range(2)]
                sp_ = [ps(st, "ssp%d" % i, [128, 512], F32) for i in range(2)]
                rp_ = [ps(st, "rtp%d" % i, [128, 512], F32) for i in range(2)]
                r_qp = [Reg() for _ in range(2)]
                r_sp = [Reg() for _ in range(2)]
                r_rp = [Reg() for _ in range(2)]
                it = 0
                for hc in range(26):
                    wb = hc % 2
                    fam = HC_FAM[hc]
                    c0 = HC_COL[hc]
                    S.dma("pool", wq[wb][:], w_in_v[:, :, c0:c0 + 128], writes=[r_wq[wb]])
                    for blk in range(4):
                        b = it % 2
                        it += 1
                        cs = slice(blk * 512, (blk + 1) * 512)
                        for c in range(NCH):
                            S.op("pe", lambda e, b=b, wb=wb, c=c, cs=cs: e.matmul(qp[b][:], lhsT=wq[wb][:, c, :], rhs=xnT[:, c, cs],
                                                                                    start=(c == 0), stop=(c == NCH - 1)),
                                 reads=[r_wq[wb]] + r_xnT[blk * 4:blk * 4 + 4], writes=[r_qp[b]])
                        S.op("act", lambda e, b=b: e.activation(out=q2[b][:], in_=qp[b][:], func=AF.Square),
                             reads=[r_qp[b]], writes=[r_q2[b]])
                        S.op("act", lambda e, b=b: e.copy(out=qb[b][:], in_=qp[b][:]), reads=[r_qp[b]], writes=[r_qb[b]])
                        S.op("pe", lambda e, b=b: e.matmul(sp_[b][:], lhsT=C.ones[:], rhs=q2[b][:], start=True, stop=True),
                             reads=[r_q2[b], C.r], writes=[r_sp[b]])
                        S.op("pe", lambda e, b=b, fam=fam: e.matmul(rp_[b][:], lhsT=C.rotg[:, fam, :], rhs=qb[b][:], start=True, stop=True),
                             reads=[r_qb[b], C.r], writes=[r_rp[b]])
                        K.rsq(rs[b][:], sp_[b][:], 1.0 / HD, [r_sp[b]], [r_rs[b]])
                        S.op("dve", lambda e, b=b, fam=fam, cs=cs: e.scalar_tensor_tensor(out=ta[b][:], in0=qp[b][:], scalar=C.gh[:, fam:fam + 1],
                                                                                         in1=cos[:, cs], op0=ALU.mult, op1=ALU.mult),
                             reads=[r_qp[b], r_cs, C.r], writes=[r_ta[b]])
                        S.op("dve", lambda e, b=b, cs=cs: e.tensor_tensor(out=tb[b][:], in0=rp_[b][:], in1=sin[:, cs], op=ALU.mult),
                             reads=[r_rp[b], r_cs], writes=[r_tb[b]])
                        S.op("dve", lambda e, b=b: e.tensor_tensor(out=ta[b][:], in0=ta[b][:], in1=tb[b][:], op=ALU.add),
                             reads=[r_ta[b], r_tb[b]], writes=[r_ta[b]])
                        S.op("dve", lambda e, b=b, wb=wb, cs=cs: e.tensor_tensor(out=stage[wb][:, cs], in0=ta[b][:], in1=rs[b][:], op=ALU.mult),
                             reads=[r_ta[b], r_rs[b]], writes=[r_stage[wb]])
                    S.dma("sp", Sx.qkt[hc, :, hf * HALF:(hf + 1) * HALF], stage[wb][:], reads=[r_stage[wb]], writes=[R.qkt[hc][hf]])
                S.emit_phase()
            with ExitStack() as st:
                wv = sb(st, "wv", [128, NCH, 1280], BF16)
                r_wv = [Reg() for _ in range(3)]
                vst = [sb(st, "vst%d" % i, [128, 1280], BF16) for i in range(2)]
                r_vst = [Reg() for _ in range(2)]
                vp = [ps(st, "vp%d" % i, [128, 512], F32) for i in range(4)]
                r_vp = [Reg() for _ in range(4)]
                off = 0
                pieces = []
                for i, (c0, n) in enumerate(V_COLS):
                    S.dma("pool", wv[:, :, off:off + n], w_in_v[:, :, c0:c0 + n], writes=[r_wv[i]])
                    pieces.append((off, n))
                    off += n
                it = 0
                for t in range(16):
                    b = t % 2
                    tok0 = hf * HALF + t * 128
                    for i, (o, n) in enumerate(pieces):
                        pb = it % 4
                        it += 1
                        for c in range(NCH):
                            S.op("pe", lambda e, pb=pb, c=c, t=t, o=o, n=n: e.matmul(vp[pb][:, 0:n], lhsT=xnT[:, c, t * 128:(t + 1) * 128],
                                                                                     rhs=wv[:, c, o:o + n], start=(c == 0), stop=(c == NCH - 1)),
                                 reads=[r_wv[i], r_xnT[t]], writes=[r_vp[pb]])
                        if i % 2 == 0:
                            S.op("act", lambda e, pb=pb, b=b, o=o, n=n: e.copy(out=vst[b][:, o:o + n], in_=vp[pb][:, 0:n]),
                                 reads=[r_vp[pb]], writes=[r_vst[b]])
                        else:
                            S.op("dve", lambda e, pb=pb, b=b, o=o, n=n: e.tensor_copy(out=vst[b][:, o:o + n], in_=vp[pb][:, 0:n]),
                                 reads=[r_vp[pb]], writes=[r_vst[b]])
                    S.dma("sp", Sx.v[tok0:tok0 + 128, :], vst[b][:], reads=[r_vst[b]], writes=[R.v[hf * 16 + t]])
                S.emit_phase()


def phase_b(K, top):
    nc, S, I, C, Sx, R, sb, ps = K.nc, K.S, K.I, K.C, K.Sx, K.R, K.sb, K.ps
    mm, tr, act, ts, tt, stt, recip, cp = K.mm, K.tr, K.act, K.ts, K.tt, K.stt, K.recip, K.cp
    LOOK = 3
    with ExitStack() as st:
        maskA = sb(st, "maskA", [128, 20, 512], BF16)
        maskB = sb(st, "maskB", [128, 6, 512], BF16)
        r_mask = Reg(const=True)
        S.dma("sp", maskA[:], I.c_maska, writes=[r_mask])
        S.dma("sp", maskB[:], I.c_maskb, writes=[r_mask])
        QT = [sb(st, "QT%d" % i, [128, SEQ], BF16) for i in range(2)]
        KT = [sb(st, "KT%d" % i, [128, SEQ], BF16) for i in range(2)]
        V1 = [sb(st, "V1%d" % i, [128, NT, 130], BF16) for i in range(2)]
        OTs = [sb(st, "OTs%d" % i, [128, SEQ], BF16) for i in range(2)]
        r_QT = [Reg() for _ in range(2)]
        r_KT = [Reg() for _ in range(2)]
        r_V1 = [Reg() for _ in range(2)]
        r_OTs = [Reg() for _ in range(2)]
        NE = 6
        NSP = 3
        E = [sb(st, "E%d" % i, [128, 512], BF16) for i in range(NE)]
        Pm = [sb(st, "Pm%d" % i, [128, 512], BF16) for i in range(NE)]
        r_E = [Reg() for _ in range(NE)]
        r_Pm = [Reg() for _ in range(NE)]
        NO = 8
        Osb = [sb(st, "Osb%d" % i, [128, 130], F32) for i in range(NO)]
        den = [sb(st, "den%d" % i, [128, 1], F32) for i in range(NO)]
        sst = [sb(st, "sst%d" % i, [128, 1], F32) for i in range(NO)]
        obf = [sb(st, "obf%d" % i, [128, 128], BF16) for i in range(NO)]
        junk = sb(st, "junkb", [128, 128], BF16)
        r_Osb = [Reg() for _ in range(NO)]
        r_den = [Reg() for _ in range(NO)]
        r_sst = [Reg() for _ in range(NO)]
        r_obf = [Reg() for _ in range(NO)]
        r_junk = Reg()
        Sp = [ps(st, "Sp%d" % i, [128, 512], F32) for i in range(NSP)]
        Op_ = [ps(st, "Op%d" % i, [128, 512], F32) for i in range(4)]
        Tp = ps(st, "Tp", [128, 512], F32)
        tpv = Tp[:].bitcast(BF16)
        r_Sp = [Reg() for _ in range(NSP)]
        r_Op = [Reg() for _ in range(4)]
        r_Tp = [Reg() for _ in range(8)]
        for i in range(2):
            S.op("dve", lambda e, i=i: e.memset(V1[i][:, :, 128:130], 1.0), writes=[r_V1[i]])

        iters = []
        for h in range(16):
            isA = h < 8
            nj, koff = (20, -1024) if isA else (6, -128)
            for qb in range(8):
                q0 = qb * 512
                js = [j for j in range(nj) if 0 <= q0 + koff + 128 * j < SEQ]
                for jn, j in enumerate(js):
                    iters.append(dict(h=h, qb=qb, j=j, k0=q0 + koff + 128 * j, first=(jn == 0), last=(jn == len(js) - 1),
                                      hstart=(qb == 0 and jn == 0), hend=(qb == 7 and jn == len(js) - 1)))
        state = {"osb": 0, "ts": 0}
        deferred = []

        def head_cfg(h):
            if h < 8:
                return h, 8 + h, 128 * h, maskA
            kvh = (h - 8) // 4
            return 16 + (h - 8), 24 + kvh, 1024 + 128 * kvh, maskB

        def load_head(h):
            hb = h % 2
            qhc, khc, vcol, _ = head_cfg(h)
            S.dma("sp", QT[hb][:], Sx.qkt[qhc], reads=R.qkt[qhc], writes=[r_QT[hb]])
            S.dma("sp", KT[hb][:], Sx.qkt[khc], reads=R.qkt[khc], writes=[r_KT[hb]])
            S.dma("sp", V1[hb][:, :, 0:128], Sx.v[:, vcol:vcol + 128].rearrange("(t p) d -> p t d", p=128),
                  reads=R.v, writes=[r_V1[hb]])

        def score(n):
            it = iters[n]
            hb = it["h"] % 2
            mask = head_cfg(it["h"])[3]
            sbi, ei = n % NSP, n % NE
            k0, q0, j = it["k0"], it["qb"] * 512, it["j"]
            mm(Sp[sbi][:], KT[hb][:, k0:k0 + 128], QT[hb][:, q0:q0 + 512], True, False, [r_KT[hb], r_QT[hb]], [r_Sp[sbi]])
            mm(Sp[sbi][:], C.ident[:], mask[:, j, :], False, True, [r_mask, C.r], [r_Sp[sbi]])
            act(Pm[ei][:], Sp[sbi][:], AF.Exp, [r_Sp[sbi]], [r_Pm[ei]], scale=SCALE)

        def post(h, qb, i, o):
            hb = h % 2
            grp = 0 if h < 8 else 1
            qt = 4 * qb + i
            if h < 8:
                recip(den[o][:], Osb[o][:, 128:129], [r_Osb[o]], [r_den[o]])
            else:
                tt(den[o][:], Osb[o][:, 128:129], C.esink[:, h - 8:h - 7], ALU.add, [r_Osb[o], C.r], [r_den[o]])
                recip(den[o][:], den[o][:], [r_den[o]], [r_den[o]])
            ts(obf[o][:], Osb[o][:, 0:128], den[o][:, 0:1], None, ALU.mult, None, [r_Osb[o], r_den[o]], [r_obf[o]])
            act(junk[:], obf[o][:], AF.Square, [r_obf[o]], [r_junk, r_sst[o]], accum_out=sst[o][:])
            tt(C.ss[:, grp, qt:qt + 1], C.ss[:, grp, qt:qt + 1], sst[o][:], ALU.add, [r_sst[o], C.r_ss], [C.r_ss])
            tsl = state["ts"] % 8
            state["ts"] += 1
            tr(tpv[:, tsl * 128:(tsl + 1) * 128], obf[o][:], [r_obf[o]], [r_Tp[tsl]])
            cp(OTs[hb][:, qt * 128:(qt + 1) * 128], tpv[:, tsl * 128:(tsl + 1) * 128], [r_Tp[tsl]], [r_OTs[hb]], eng="act")

        def pv(n):
            it = iters[n]
            h, qb = it["h"], it["qb"]
            hb = h % 2
            ei = n % NE
            k0 = it["k0"]
            for i in range(4):
                mm(Op_[i][:, 0:129], Pm[ei][:, 128 * i:128 * i + 128], V1[hb][:, k0 // 128, 0:129], it["first"], it["last"],
                   [r_Pm[ei], r_V1[hb]], [r_Op[i]])
            if it["last"]:
                for i in range(4):
                    o = state["osb"] % NO
                    state["osb"] += 1
                    cp(Osb[o][:, 0:129], Op_[i][:, 0:129], [r_Op[i]], [r_Osb[o]])
                    deferred.append([2, (lambda h=h, qb=qb, i=i, o=o: post(h, qb, i, o))])
            if it["hend"]:
                deferred.append([3, (lambda h=h, hb=hb: S.dma("sp", Sx.ot[h], OTs[hb][:], reads=[r_OTs[hb]], writes=[R.ot[h]]))])

        def tick():
            for d in deferred:
                d[0] -= 1
            while deferred and deferred[0][0] <= 0:
                deferred.pop(0)[1]()

        N = len(iters)
        load_head(0)
        for n in range(N + LOOK):
            if n < N:
                score(n)
            if n - LOOK >= 0:
                pv(n - LOOK)
                if iters[n - LOOK]["hstart"] and iters[n - LOOK]["h"] + 1 < 16:
                    load_head(iters[n - LOOK]["h"] + 1)
            tick()
        while deferred:
            tick()
        S.emit_phase()


def phase_c(K, top):
    nc, S, I, C, Sx, R, sb, ps = K.nc, K.S, K.I, K.C, K.Sx, K.R, K.sb, K.ps
    mm, tr, act, ts, tt, stt, recip, cp = K.mm, K.tr, K.act, K.ts, K.tt, K.stt, K.recip, K.cp
    with ExitStack() as st0:
        KmT = sb(st0, "KmT", [128, 4, 256], BF16)
        Vm = sb(st0, "Vm", [128, 2, 4, 130], BF16)
        r_km = Reg(const=True)
        with ExitStack() as st:
            memx = sb(st, "memx", [128, 2, D], F32)
            memn = sb(st, "memn", [128, 2, D], BF16)
            memT = sb(st, "memT", [128, NCH, 256], BF16)
            gbc = sb(st, "gbcm", [128, D], F32)
            junk = sb(st, "junkc0", [128, D], BF16)
            wkv = sb(st, "wkv", [128, NCH, 1024], BF16)
            ssq = sb(st, "ssqm", [128, 2], F32)
            k2 = sb(st, "k2", [128, 256], BF16)
            rsk = sb(st, "rsk", [128, 256], F32)
            bank = [ps(st, "c0b%d" % i, [128, 512], F32) for i in range(4)]
            rb = [Reg() for _ in range(4)]
            r1 = Reg()
            r_w = Reg()
            S.dma("sp", memx[:], I.mem.rearrange("(t p) d -> p t d", p=128), writes=[r1])
            S.dma("sp", gbc[:], I.g_mem.partition_broadcast(128), writes=[r1])
            S.dma("pool", wkv[:], I.w_kv_mem.rearrange("(c p) n -> p c n", p=128), writes=[r_w])
            S.op("dve", lambda e: e.memset(Vm[:], 1.0), writes=[r_km])
            for mt in range(2):
                act(junk[:], memx[:, mt, :], AF.Square, [r1], [r1], accum_out=ssq[:, mt:mt + 1])
                K.rsq(ssq[:, mt:mt + 1], ssq[:, mt:mt + 1], 1.0 / D, [r1], [r1])
                stt(memn[:, mt, :], memx[:, mt, :], ssq[:, mt:mt + 1], gbc[:], ALU.mult, ALU.mult, [r1], [r1])
                for hh in range(2):
                    tpv = bank[hh][:].bitcast(BF16)
                    for c in range(8):
                        cc = hh * 8 + c
                        tr(tpv[:, c * 128:(c + 1) * 128], memn[:, mt, cc * 128:(cc + 1) * 128], [r1], [rb[hh]])
                    cp(memT[:, hh * 8:hh * 8 + 8, mt * 128:(mt + 1) * 128], tpv[:, 0:1024].rearrange("p (c n) -> p c n", c=8), [rb[hh]], [r1])
            for hm in range(4):
                for c in range(NCH):
                    mm(bank[2][:, 0:256], wkv[:, c, hm * 128:(hm + 1) * 128], memT[:, c, :], c == 0, c == NCH - 1, [r_w, r1], [rb[2]])
                act(k2[:], bank[2][:, 0:256], AF.Square, [rb[2]], [r1])
                mm(bank[3][:, 0:256], C.ones[:], k2[:], True, True, [r1, C.r], [rb[3]])
                K.rsq(rsk[:], bank[3][:, 0:256], 1.0 / HD, [rb[3]], [r1])
                stt(KmT[:, hm, :], bank[2][:, 0:256], C.gh[:, 5:6], rsk[:], ALU.mult, ALU.mult, [rb[2], r1, C.r], [r_km])
            for mt in range(2):
                for c in range(NCH):
                    mm(bank[mt][:], memT[:, c, mt * 128:(mt + 1) * 128], wkv[:, c, 512:1024], c == 0, c == NCH - 1, [r_w, r1], [rb[mt]])
                cp(Vm[:, mt, :, 0:128], bank[mt][:].rearrange("p (h d) -> p h d", h=4), [rb[mt]], [r_km])
            S.emit_phase()
        with ExitStack() as st:
            wout = sb(st, "wout", [128, NCH, D], BF16)
            wq = sb(st, "wqm", [128, NCH, 512], BF16)
            wo = sb(st, "wom", [128, 4, D], BF16)
            wr = sb(st, "wr", [128, NCH, N_EXP], BF16)
            gcr = sb(st, "gcr", [128, D], F32)
            gmo = sb(st, "gmo", [128, D], F32)
            go = sb(st, "go", [128, 16], F32)
            rAB = sb(st, "rAB", [128, 2, NT], F32)
            r_w = Reg(const=True)
            S.dma("pool", wout[:], I.w_out.rearrange("(c p) n -> p c n", p=128), writes=[r_w])
            S.dma("pool", wq[:], I.w_q_mem.rearrange("(c p) n -> p c n", p=128), writes=[r_w])
            S.dma("pool", wo[:], I.w_o_mem.rearrange("(c p) n -> p c n", p=128), writes=[r_w])
            S.dma("pool", wr[:], I.w_router.rearrange("(c p) n -> p c n", p=128), writes=[r_w])
            S.dma("sp", gcr[:], I.g_cross.partition_broadcast(128), writes=[r_w])
            S.dma("sp", gmo[:], I.g_moe.partition_broadcast(128), writes=[r_w])
            S.dma("sp", go[:, 0:8], I.g_oa.rearrange("(c p) -> p c", p=128), writes=[r_w], allow_slow_non_contiguous=True)
            S.dma("sp", go[:, 8:16], I.g_ob.rearrange("(c p) -> p c", p=128), writes=[r_w], allow_slow_non_contiguous=True)
            for c in range(NCH):
                ts(wout[:, c, :], wout[:, c, :], go[:, c:c + 1], None, ALU.mult, None, [r_w], [r_w])
            K.rsq(rAB[:], C.ss[:], 1.0 / 1024, [C.r_ss], [r_w])

            xt = [sb(st, "xt%d" % i, [128, D], F32) for i in range(2)]
            otb = [sb(st, "otb%d" % i, [128, 16, 128], BF16) for i in range(2)]
            x12 = [sb(st, "x12_%d" % i, [128, D], F32) for i in range(2)]
            hn = [sb(st, "hn%d" % i, [128, D], BF16) for i in range(2)]
            hT = [sb(st, "hT%d" % i, [128, NCH, 128], BF16) for i in range(2)]
            junk = sb(st, "junkc1", [128, D], BF16)
            sq = sb(st, "sqc", [128, 4], F32)
            q2 = sb(st, "q2c", [128, 512], BF16)
            rsq = sb(st, "rsqc", [128, 512], F32)
            qn = sb(st, "qnc", [128, 4, 128], BF16)
            E2 = [sb(st, "E2_%d" % i, [128, 4, 128], BF16) for i in range(2)]
            rden = sb(st, "rdenc", [128, 4], F32)
            o2 = sb(st, "o2c", [128, 4, 128], BF16)
            o2T = sb(st, "o2T", [128, 4, 128], BF16)
            ex = sb(st, "exr", [128, N_EXP], F32)
            sume = sb(st, "sume", [128, 1], F32)
            r_xt = [Reg() for _ in range(2)]
            r_otb = [Reg() for _ in range(2)]
            r_x12 = [Reg() for _ in range(2)]
            r_hn = [Reg() for _ in range(2)]
            r_hT = [Reg() for _ in range(2)]
            r_junk, r_sq, r_q2, r_rsq, r_qn, r_rden, r_o2, r_o2T, r_ex, r_sume = [Reg() for _ in range(10)]
            r_E2 = [Reg() for _ in range(2)]
            B = [ps(st, "c1b%d" % i, [128, 512], F32) for i in range(8)]
            rB = [Reg() for _ in range(8)]
            tpv = B[4][:].bitcast(BF16)
            ot_v = Sx.ot.rearrange("h p n -> p h n")

            def rmsnorm_tile(src, r_src, gb, dst, r_dst, col):
                act(junk[:], src, AF.Square, [r_src], [r_junk, r_sq], accum_out=sq[:, col:col + 1])
                K.rsq(sq[:, col:col + 1], sq[:, col:col + 1], 1.0 / D, [r_sq], [r_sq])
                stt(dst, src, sq[:, col:col + 1], gb[:], ALU.mult, ALU.mult, [r_src, r_sq, r_w], [r_dst])

            def transpose16(src, r_src, dst, r_dst):
                for hh in range(2):
                    for c in range(8):
                        cc = hh * 8 + c
                        tr(tpv[:, c * 128:(c + 1) * 128], src[:, cc * 128:(cc + 1) * 128], [r_src], [rB[4]])
                    cp(dst[:, hh * 8:hh * 8 + 8, :], tpv.rearrange("p (c n) -> p c n", c=8), [rB[4]], [r_dst],
                       eng=("act" if hh == 0 else "dve"))

            def load(t):
                b = t % 2
                S.dma("sp", xt[b][:], I.x[t * 128:(t + 1) * 128, :], writes=[r_xt[b]])
                S.dma("sp", otb[b][:], ot_v[:, :, t * 128:(t + 1) * 128], reads=R.ot, writes=[r_otb[b]])
            def s1_cg(t, cg):
                b = t % 2
                X = x12[b]
                pa, pb = B[(cg % 2) * 2], B[(cg % 2) * 2 + 1]
                ra, rbb = rB[(cg % 2) * 2], rB[(cg % 2) * 2 + 1]
                cs = slice(cg * 512, (cg + 1) * 512)
                for c in range(8):
                    mm(pa[:], otb[b][:, c, :], wout[:, c, cs], c == 0, c == 7, [r_otb[b], r_w], [ra])
                for c in range(8, 16):
                    mm(pb[:], otb[b][:, c, :], wout[:, c, cs], c == 8, c == 15, [r_otb[b], r_w], [rbb])
                stt(X[:, cs], pa[:], rAB[:, 0, t:t + 1], xt[b][:, cs], ALU.mult, ALU.add, [ra, r_xt[b], r_w], [r_x12[b]])
                stt(X[:, cs], pb[:], rAB[:, 1, t:t + 1], X[:, cs], ALU.mult, ALU.add, [rbb, r_x12[b], r_w], [r_x12[b]])

            def nxt(t, cg):
                if t + 1 < NT:
                    s1_cg(t + 1, cg)

            load(0)
            load(1)
            for cg in range(4):
                s1_cg(0, cg)
            for t in range(NT):
                b = t % 2
                X = x12[b]
                rmsnorm_tile(X[:], r_x12[b], gcr, hn[0][:], r_hn[0], 0)
                nxt(t, 0)
                transpose16(hn[0], r_hn[0], hT[0], r_hT[0])
                for hm in range(4):
                    for c in range(NCH):
                        mm(B[5][:, hm * 128:(hm + 1) * 128], wq[:, c, hm * 128:(hm + 1) * 128], hT[0][:, c, :], c == 0, c == NCH - 1,
                           [r_w, r_hT[0]], [rB[5]])
                act(q2[:], B[5][:], AF.Square, [rB[5]], [r_q2])
                nxt(t, 1)
                mm(B[6][:], C.ones[:], q2[:], True, True, [r_q2, C.r], [rB[6]])
                K.rsq(rsq[:], B[6][:], 1.0 / HD, [rB[6]], [r_rsq])
                stt(qn[:].rearrange("p h n -> p (h n)"), B[5][:], C.gh[:, 4:5], rsq[:], ALU.mult, ALU.mult, [rB[5], r_rsq, C.r], [r_qn])
                nxt(t, 2)
                for mt in range(2):
                    bk = 7 if mt == 0 else 4
                    for hm in range(4):
                        mm(B[bk][:, hm * 128:(hm + 1) * 128], KmT[:, hm, mt * 128:(mt + 1) * 128], qn[:, hm, :], True, True,
                           [r_km, r_qn], [rB[bk]])
                    act(E2[mt][:].rearrange("p h n -> p (h n)"), B[bk][:], AF.Exp, [rB[bk]], [r_E2[mt]], scale=SCALE)
                nxt(t, 3)
                for hm in range(4):
                    bk = 5 if hm < 2 else 6
                    o0 = (hm % 2) * 130
                    for mt in range(2):
                        mm(B[bk][:, o0:o0 + 129], E2[mt][:, hm, :], Vm[:, mt, hm, 0:129], mt == 0, mt == 1, [r_E2[mt], r_km], [rB[bk]])
                for hm in range(4):
                    bk = 5 if hm < 2 else 6
                    o0 = (hm % 2) * 130
                    recip(rden[:, hm:hm + 1], B[bk][:, o0 + 128:o0 + 129], [rB[bk]], [r_rden])
                    ts(o2[:, hm, :], B[bk][:, o0:o0 + 128], rden[:, hm:hm + 1], None, ALU.mult, None, [rB[bk], r_rden], [r_o2])
                for hm in range(4):
                    tr(tpv[:, hm * 128:(hm + 1) * 128], o2[:, hm, :], [r_o2], [rB[4]])
                cp(o2T[:].rearrange("p h n -> p (h n)"), tpv[:, 0:512], [rB[4]], [r_o2T], eng="act")
                for cg in range(4):
                    pa = B[cg % 4]
                    ra = rB[cg % 4]
                    cs = slice(cg * 512, (cg + 1) * 512)
                    for hm in range(4):
                        mm(pa[:], o2T[:, hm, :], wo[:, hm, cs], hm == 0, hm == 3, [r_o2T, r_w], [ra])
                    tt(X[:, cs], pa[:], X[:, cs], ALU.add, [ra, r_x12[b]], [r_x12[b]])
                S.dma("sp", Sx.x2[t * 128:(t + 1) * 128, :], X[:], reads=[r_x12[b]], writes=[R.x2[t]])
                rmsnorm_tile(X[:], r_x12[b], gmo, hn[1][:], r_hn[1], 1)
                S.dma("sp", Sx.h3[t * 128:(t + 1) * 128, :], hn[1][:], reads=[r_hn[1]], writes=[R.h3[t]])
                if t + 2 < NT:
                    load(t + 2)
                transpose16(hn[1], r_hn[1], hT[1], r_hT[1])
                for c in range(NCH):
                    mm(B[5][:, 0:N_EXP], hT[1][:, c, :], wr[:, c, :], c == 0, c == NCH - 1, [r_hT[1], r_w], [rB[5]])
                act(ex[:], B[5][:, 0:N_EXP], AF.Exp, [rB[5]], [r_ex, r_sume], accum_out=sume[:])
                recip(sume[:], sume[:], [r_sume], [r_sume])
                ts(C.aff[:, t, :], ex[:], sume[:, 0:1], None, ALU.mult, None, [r_ex, r_sume], [C.r_aff])
            S.emit_phase()


N_BISECT = 34


def phase_d(K, top):
    nc, S, I, C, Sx, R, sb, ps = K.nc, K.S, K.I, K.C, K.Sx, K.R, K.sb, K.ps
    mm, tr, act, ts, tt, stt, recip, cp = K.mm, K.tr, K.act, K.ts, K.tt, K.stt, K.recip, K.cp
    with ExitStack() as st:
        lo = sb(st, "lo", [128, N_EXP], F32)
        hi = sb(st, "hi", [128, N_EXP], F32)
        mid = sb(st, "mid", [128, N_EXP], F32)
        ge = sb(st, "ge", [128, N_EXP], F32)
        dl = sb(st, "dl", [128, N_EXP], F32)
        cntp = sb(st, "cntp", [128, N_EXP], F32)
        cmp_ = sb(st, "cmp", [128, NT, N_EXP], F32)
        cnt = ps(st, "cnt", [128, 512], F32)
        r = Reg()
        r_cnt = Reg()
        S.op("dve", lambda e: e.memset(lo[:], 0.0), writes=[r])
        S.op("dve", lambda e: e.memset(hi[:], 1.0), writes=[r])

        def bc(t):
            return t[:, :].unsqueeze(1).to_broadcast([128, NT, N_EXP])
        for it in range(N_BISECT):
            tt(mid[:], lo[:], hi[:], ALU.add, [r], [r])
            ts(mid[:], mid[:], 0.5, None, ALU.mult, None, [r], [r])
            tt(cmp_[:], C.aff[:], bc(mid), ALU.is_gt, [r, C.r_aff], [r])
            S.op("dve", lambda e: e.tensor_reduce(out=cntp[:], in_=cmp_[:].rearrange("p t e -> p e t"), axis=AX.X, op=ALU.add),
                 reads=[r], writes=[r])
            mm(cnt[:, 0:N_EXP], C.onesf[:], cntp[:], True, True, [r, C.r], [r_cnt])
            ts(ge[:], cnt[:, 0:N_EXP], float(CAP), None, ALU.is_ge, None, [r_cnt], [r])
            tt(dl[:], mid[:], lo[:], ALU.subtract, [r], [r])
            tt(dl[:], dl[:], ge[:], ALU.mult, [r], [r])
            tt(lo[:], lo[:], dl[:], ALU.add, [r], [r])
            tt(dl[:], hi[:], mid[:], ALU.subtract, [r], [r])
            tt(dl[:], dl[:], ge[:], ALU.mult, [r], [r])
            tt(hi[:], mid[:], dl[:], ALU.add, [r], [r])
        tt(cmp_[:], C.aff[:], bc(lo), ALU.is_gt, [r, C.r_aff], [r])
        tt(cmp_[:], cmp_[:], C.aff[:], ALU.mult, [r, C.r_aff], [r])
        S.dma("sp", Sx.gm.rearrange("(t p) e -> p t e", p=128), cmp_[:], reads=[r], writes=[R.gm])
        S.emit_phase()


def phase_e(K, top):
    nc, S, I, C, Sx, R, sb, ps = K.nc, K.S, K.I, K.C, K.Sx, K.R, K.sb, K.ps
    mm, tr, act, ts, tt, stt, recip, cp = K.mm, K.tr, K.act, K.ts, K.tt, K.stt, K.recip, K.cp
    NOT = OWN // 128
    with ExitStack() as st0:
        own = sb(st0, "own", [128, NOT], I32)
        h3T = sb(st0, "h3T", [128, NCH, OWN], BF16)
        acc = sb(st0, "acc", [128, NOT, D], F32)
        gmo = sb(st0, "gmo_", [128, NOT, N_EXP], F32)
        r_own, r_h3T, r_gmo = Reg(const=True), Reg(const=True), Reg(const=True)
        r_acc = [Reg() for _ in range(NOT)]
        with ExitStack() as st:
            h3o = sb(st, "h3o", [128, NOT, D], BF16)
            r_h3o = [Reg() for _ in range(NOT)]
            tpb = [ps(st, "tpe%d" % i, [128, 512], F32) for i in range(2)]
            r_tpb = [Reg() for _ in range(2)]
            S.dma("sp", own[:], I.own, writes=[r_own])
            for j in range(NOT):
                off = bass.IndirectOffsetOnAxis(ap=own[:, j:j + 1], axis=0)
                S.dma_fn("pool", lambda e, j=j, off=off: e.indirect_dma_start(out=h3o[:, j, :], out_offset=None, in_=Sx.h3, in_offset=off),
                         reads=[r_own] + R.h3, writes=[r_h3o[j]])
                S.dma_fn("pool", lambda e, j=j, off=off: e.indirect_dma_start(out=acc[:, j, :], out_offset=None, in_=Sx.x2, in_offset=off),
                         reads=[r_own] + R.x2, writes=[r_acc[j]])
                S.dma_fn("pool", lambda e, j=j, off=off: e.indirect_dma_start(out=gmo[:, j, :], out_offset=None, in_=Sx.gm, in_offset=off),
                         reads=[r_own, R.gm], writes=[r_gmo])
            k = 0
            for j in range(NOT):
                for hh in range(2):
                    pb = k % 2
                    k += 1
                    tpv = tpb[pb][:].bitcast(BF16)
                    for c in range(8):
                        cc = hh * 8 + c
                        tr(tpv[:, c * 128:(c + 1) * 128], h3o[:, j, cc * 128:(cc + 1) * 128], [r_h3o[j]], [r_tpb[pb]])
                    cp(h3T[:, hh * 8:hh * 8 + 8, j * 128:(j + 1) * 128], tpv.rearrange("p (c n) -> p c n", c=8), [r_tpb[pb]], [r_h3T],
                       eng=("act" if hh == 0 else "dve"))
            S.emit_phase()
        with ExitStack() as st:
            FP = 256
            NFP = D // FP
            wg = [sb(st, "wg%d" % i, [128, NCH, FP], BF16) for i in range(2)]
            wu = [sb(st, "wu%d" % i, [128, NCH, FP], BF16) for i in range(2)]
            wd = [sb(st, "wd%d" % i, [128, NCH, FP], BF16) for i in range(2)]
            r_wg = [Reg() for _ in range(2)]
            r_wu = [Reg() for _ in range(2)]
            r_wd = [Reg() for _ in range(2)]
            actT = sb(st, "actT", [128, NCH, OWN], BF16)
            r_actT = [Reg() for _ in range(NCH)]
            sg = [sb(st, "sg%d" % i, [128, 512], F32) for i in range(2)]
            r_sg = [Reg() for _ in range(2)]
            Gp = [ps(st, "Gp%d" % i, [128, 512], F32) for i in range(2)]
            Up = [ps(st, "Up%d" % i, [128, 512], F32) for i in range(2)]
            Yp = [ps(st, "Yp%d" % i, [128, 512], F32) for i in range(3)]
            r_Gp = [Reg() for _ in range(2)]
            r_Up = [Reg() for _ in range(2)]
            r_Yp = [Reg() for _ in range(3)]
            wi = 0
            di = 0
            gi = 0
            yi = 0
            for ex in range(N_EXP):
                wgv = I.w_gate[ex].rearrange("(c p) f -> p c f", p=128)
                wuv = I.w_up[ex].rearrange("(c p) f -> p c f", p=128)
                wdv = I.w_down[ex].rearrange("(c p) f -> p c f", p=128)
                for fp in range(NFP):
                    wb = wi % 2
                    wi += 1
                    S.dma("pool", wg[wb][:], wgv[:, :, fp * FP:(fp + 1) * FP], writes=[r_wg[wb]])
                    S.dma("pool", wu[wb][:], wuv[:, :, fp * FP:(fp + 1) * FP], writes=[r_wu[wb]])
                    for f2 in range(FP // 128):
                        fc = fp * (FP // 128) + f2
                        for half in range(2):
                            gb = gi % 2
                            gi += 1
                            cs = slice(half * 512, (half + 1) * 512)
                            for c in range(NCH):
                                mm(Gp[gb][:], wg[wb][:, c, f2 * 128:(f2 + 1) * 128], h3T[:, c, cs], c == 0, c == NCH - 1,
                                   [r_wg[wb], r_h3T], [r_Gp[gb]])
                            for c in range(NCH):
                                mm(Up[gb][:], wu[wb][:, c, f2 * 128:(f2 + 1) * 128], h3T[:, c, cs], c == 0, c == NCH - 1,
                                   [r_wu[wb], r_h3T], [r_Up[gb]])
                            act(sg[gb][:], Gp[gb][:], AF.Silu, [r_Gp[gb]], [r_sg[gb]])
                            tt(actT[:, fc, cs], sg[gb][:], Up[gb][:], ALU.mult, [r_sg[gb], r_Up[gb]], [r_actT[fc]])
                for dp in range(NFP):
                    db = di % 2
                    di += 1
                    S.dma("pool", wd[db][:], wdv[:, :, dp * FP:(dp + 1) * FP], writes=[r_wd[db]])
                    for j in range(NOT):
                        yb = yi % 3
                        yi += 1
                        for fc in range(NCH):
                            mm(Yp[yb][:, 0:FP], actT[:, fc, j * 128:(j + 1) * 128], wd[db][:, fc, :], fc == 0, fc == NCH - 1,
                               [r_actT[fc], r_wd[db]], [r_Yp[yb]])
                        stt(acc[:, j, dp * FP:(dp + 1) * FP], Yp[yb][:, 0:FP], gmo[:, j, ex:ex + 1], acc[:, j, dp * FP:(dp + 1) * FP],
                            ALU.mult, ALU.add, [r_Yp[yb], r_gmo, r_acc[j]], [r_acc[j]])
            outs = []
            for j in range(NOT):
                outs.append(S.dma("sp", K.out[j * 128:(j + 1) * 128, :], acc[:, j, :], reads=[r_acc[j]], writes=[]))
            S.finish(outs)
            S.emit_phase()


_NC_CACHE = {}


def _core_inputs(inp, c, consts):
    b, q = c // 4, c % 4
    m = {}
    m["x"] = np.ascontiguousarray(inp["x"][b], dtype=np.float32)
    m["mem"] = np.ascontiguousarray(inp["mem"][b], dtype=np.float32)
    m["positions"] = np.ascontiguousarray(inp["positions"][b]).astype(np.int32)
    own = (q * OWN + np.arange(OWN)).reshape(OWN // 128, 128).T.astype(np.int32)
    m["own_idx"] = np.ascontiguousarray(own)
    for n in ("g_mix", "g_cross", "g_mem", "g_moe", "g_qa", "g_ka", "g_qb", "g_kb", "g_qm", "g_km", "g_oa", "g_ob",
              "sink_b", "w_in", "w_out", "w_q_mem", "w_kv_mem", "w_o_mem", "w_router", "w_gate", "w_up", "w_down"):
        m[n] = np.ascontiguousarray(np.asarray(inp[n])[0], dtype=np.float32)
    m.update(consts)
    return m


def kernel(**inputs):
    inp = {k: np.asarray(v) for k, v in inputs.items()}
    if "nc" not in _NC_CACHE:
        _NC_CACHE["nc"] = build()
    nc = _NC_CACHE["nc"]
    consts = host_consts()
    in_maps = [_core_inputs(inp, c, consts) for c in range(8)]
    res = run_bass_kernel_spmd(nc, in_maps, core_ids=list(range(8)))
    out = np.zeros((2, SEQ, D), np.float32)
    for c in range(8):
        b, q = c // 4, c % 4
        out[b, q * OWN:(q + 1) * OWN] = np.asarray(res.results[c]["out"], dtype=np.float32)
    return out


def phase_e2(K, top):
    nc, S, I, C, Sx, R, sb, ps = K.nc, K.S, K.I, K.C, K.Sx, K.R, K.sb, K.ps
    mm, tr, act, ts, tt, stt, recip, cp = K.mm, K.tr, K.act, K.ts, K.tt, K.stt, K.recip, K.cp
    NOT = OWN // 128
    NS = CAP // 128
    NTG = 3 + N_EXP
    with ExitStack() as st0:
        own = sb(st0, "own", [128, NOT], I32)
        gmo = sb(st0, "gmo_", [128, NOT, N_EXP], F32)
        mall = sb(st0, "mall", [128, NOT, N_EXP], F32)
        pos = sb(st0, "pos", [128, NOT, N_EXP], F32)
        TG = sb(st0, "TG", [128, NOT, NTG], F32)
        iota = sb(st0, "iota", [128, 512], F32)
        dummy = sb(st0, "dummy", [128, NS], F32)
        r_c = Reg(const=True)
        with ExitStack() as st:
            ut = sb(st, "ut", [128, 128], F32)
            loc = sb(st, "loc", [128, NOT], F32)
            offs = sb(st, "offs", [128, NOT, N_EXP], F32)
            x2b = [sb(st, "x2b%d" % i, [128, D], F32) for i in range(2)]
            r_x2b = [Reg() for _ in range(2)]
            pc = [ps(st, "pc%d" % i, [128, 512], F32) for i in range(2)]
            r_pc = [Reg() for _ in range(2)]
            r1 = Reg()
            S.dma("sp", own[:], I.own, writes=[r_c])
            S.dma("sp", ut[:], I.c_ut, writes=[r1])
            S.dma("sp", loc[:], I.c_loc, writes=[r1])
            S.dma("sp", iota[:], I.c_iota, writes=[r_c])
            S.dma("sp", dummy[:], I.c_dummy, writes=[r_c])
            for j in range(NOT):
                off = bass.IndirectOffsetOnAxis(ap=own[:, j:j + 1], axis=0)
                S.dma_fn("pool", lambda e, j=j, off=off: e.indirect_dma_start(out=gmo[:, j, :], out_offset=None, in_=Sx.gm, in_offset=off),
                         reads=[r_c, R.gm], writes=[r1])
                b = j % 2
                S.dma_fn("pool", lambda e, b=b, off=off: e.indirect_dma_start(out=x2b[b][:], out_offset=None, in_=Sx.x2, in_offset=off),
                         reads=[r_c] + R.x2, writes=[r_x2b[b]])
                S.dma("sp", Sx.accd[j * 128:(j + 1) * 128, :], x2b[b][:], reads=[r_x2b[b]], writes=[R.accd])
            ts(mall[:], gmo[:], 0.0, None, ALU.is_gt, None, [r1], [r1])
            flat = mall[:].rearrange("p j e -> p (j e)")
            mm(pc[0][:, 0:NOT * N_EXP], ut[:], flat, True, True, [r1], [r_pc[0]])
            mm(pc[1][:, 0:NOT * N_EXP], C.onesf[:], flat, True, True, [r1, C.r], [r_pc[1]])
            tot = pc[1][:, 0:NOT * N_EXP].rearrange("p (j e) -> p j e", e=N_EXP)
            cum = pc[0][:, 0:NOT * N_EXP].rearrange("p (j e) -> p j e", e=N_EXP)
            S.op("dve", lambda e: e.memset(offs[:, 0, :], 0.0), writes=[r1])
            for j in range(1, NOT):
                tt(offs[:, j, :], offs[:, j - 1, :], tot[:, j - 1, :], ALU.add, [r1, r_pc[1]], [r1])
            tt(pos[:], cum, offs[:], ALU.add, [r_pc[0], r1], [r1])
            ts(pos[:], pos[:], -1.0, None, ALU.add, None, [r1], [r1])
            cp(TG[:, :, 0], own[:], [r_c], [r1])
            cp(TG[:, :, 1], loc[:], [r1], [r1])
            S.op("dve", lambda e: e.memset(TG[:, :, 2], 1.0), writes=[r1])
            cp(TG[:, :, 3:NTG], gmo[:], [r1], [r1])
            S.emit_phase()
        with ExitStack() as st:
            FP = 512
            NFP = D // FP
            NW = 6
            Wb = [sb(st, "Wb%d" % i, [128, NCH, FP], BF16) for i in range(NW)]
            r_W = [Reg() for _ in range(NW)]
            PPE = 3 * NFP
            ring = {"next": 0}

            def piece_src(p):
                ex, k = divmod(p, PPE)
                if k < 2 * NFP:
                    fp, gu = divmod(k, 2)
                    w = I.w_gate if gu == 0 else I.w_up
                    return w[ex].rearrange("(c p) f -> p c f", p=128)[:, :, fp * FP:(fp + 1) * FP]
                dp = k - 2 * NFP
                return I.w_down[ex].rearrange("(c p) f -> p c f", p=128)[:, :, dp * FP:(dp + 1) * FP]

            def ensure(upto):
                last = N_EXP * PPE - 1
                while ring["next"] <= min(upto, last):
                    p = ring["next"]
                    ring["next"] += 1
                    S.dma("pool", Wb[p % NW][:], piece_src(p), writes=[r_W[p % NW]])
            Sel = [sb(st, "Sel%d" % i, [128, NOT, 128], F32) for i in range(2)]
            r_Sel = [Reg() for _ in range(2)]
            sis = sb(st, "sis", [128, NS, 32], F32)
            sidf = sb(st, "sidf", [128, NS], F32)
            gi = [sb(st, "gi%d" % i, [128, NS], I32) for i in range(2)]
            si = [sb(st, "si%d" % i, [128, NS], I32) for i in range(2)]
            gs = [sb(st, "gs%d" % i, [128, NS], F32) for i in range(2)]
            r_sis = Reg()
            r_idx = [Reg() for _ in range(2)]
            xe = sb(st, "xe", [128, NS, D], BF16)
            r_xe = [Reg() for _ in range(NS)]
            xeT1 = sb(st, "xeT", [128, NCH, CAP], BF16)
            xeT = [xeT1, xeT1]
            r_xeT1 = Reg()
            r_xeT = [r_xeT1, r_xeT1]
            actT = sb(st, "actT", [128, NCH, CAP], BF16)
            r_actT = [Reg() for _ in range(NCH)]
            ygs = sb(st, "ygs", [128, NS, D], F32)
            r_ygs = [Reg() for _ in range(NS)]
            sg = [sb(st, "sg%d" % i, [128, 512], F32) for i in range(2)]
            r_sg = [Reg() for _ in range(2)]
            cnt_stg = {"n": 0}

            def wload(dst, r_dst, src):
                cnt_stg["n"] += 1
                S.dma("pool", dst, src, writes=[r_dst])
            Gp = [ps(st, "Gp%d" % i, [128, 512], F32) for i in range(2)]
            Up = [ps(st, "Up%d" % i, [128, 512], F32) for i in range(2)]
            Yp = [ps(st, "Yp%d" % i, [128, 512], F32) for i in range(2)]
            Tq = ps(st, "Tq", [128, 512], F32)
            SIp = ps(st, "SIp", [128, 512], F32)
            r_Gp = [Reg() for _ in range(2)]
            r_Up = [Reg() for _ in range(2)]
            r_Yp = [Reg() for _ in range(2)]
            r_Tq, r_SIp = Reg(), Reg()
            tqv = Tq[:].bitcast(BF16)
            cnt = {"w": 0, "d": 0, "g": 0, "y": 0}

            def prep_idx(ex):
                pb = ex % 2
                for s4 in range(NS):
                    sl = s4 % 2
                    for j in range(NOT):
                        ts(Sel[sl][:, j, :], iota[:, s4 * 128:(s4 + 1) * 128], pos[:, j, ex:ex + 1], mall[:, j, ex:ex + 1],
                           ALU.is_equal, ALU.mult, [r_c], [r_Sel[sl]])
                    for j in range(NOT):
                        mm(SIp[:, s4 * 32:s4 * 32 + NTG], Sel[sl][:, j, :], TG[:, j, :], j == 0, j == NOT - 1,
                           [r_Sel[sl], r_c], [r_SIp])
                cp(sis[:], SIp[:, 0:NS * 32].rearrange("p (s k) -> p s k", k=32), [r_SIp], [r_sis])
                cp(gi[pb][:], sis[:, :, 0], [r_sis], [r_idx[pb]])
                tt(sidf[:], sis[:, :, 2], dummy[:], ALU.mult, [r_sis, r_c], [r_sis])
                tt(sidf[:], dummy[:], sidf[:], ALU.subtract, [r_sis, r_c], [r_sis])
                tt(sidf[:], sidf[:], sis[:, :, 1], ALU.add, [r_sis], [r_sis])
                cp(si[pb][:], sidf[:], [r_sis], [r_idx[pb]])
                cp(gs[pb][:], sis[:, :, 3 + ex], [r_sis], [r_idx[pb]])
                for s4 in range(NS):
                    off = bass.IndirectOffsetOnAxis(ap=gi[pb][:, s4:s4 + 1], axis=0)
                    S.dma_fn("pool", lambda e, s4=s4, off=off: e.indirect_dma_start(out=xe[:, s4, :], out_offset=None, in_=Sx.h3, in_offset=off),
                             reads=[r_idx[pb]] + R.h3, writes=[r_xe[s4]])

            def prep_T(ex):
                pb = ex % 2
                k = 0
                for s4 in range(NS):
                    for hh in range(4):
                        for c in range(4):
                            cc = hh * 4 + c
                            tr(tqv[:, c * 128:(c + 1) * 128], xe[:, s4, cc * 128:(cc + 1) * 128], [r_xe[s4]], [r_Tq])
                        cp(xeT[pb][:, hh * 4:hh * 4 + 4, s4 * 128:(s4 + 1) * 128], tqv[:, 0:512].rearrange("p (c n) -> p c n", c=4),
                           [r_Tq], [r_xeT[pb]], eng=("act" if k % 2 == 0 else "dve"))
                        k += 1

            def compute_gu(ex, fp):
                pb = ex % 2
                pg = ex * PPE + 2 * fp
                wgt, wut = Wb[pg % NW], Wb[(pg + 1) % NW]
                rg, ru = r_W[pg % NW], r_W[(pg + 1) % NW]
                for f2 in range(FP // 128):
                    fc = fp * (FP // 128) + f2
                    gb = cnt["g"] % 2
                    cnt["g"] += 1
                    for c in range(NCH):
                        mm(Gp[gb][:], wgt[:, c, f2 * 128:(f2 + 1) * 128], xeT[pb][:, c, :], c == 0, c == NCH - 1,
                           [rg, r_xeT[pb]], [r_Gp[gb]])
                    for c in range(NCH):
                        mm(Up[gb][:], wut[:, c, f2 * 128:(f2 + 1) * 128], xeT[pb][:, c, :], c == 0, c == NCH - 1,
                           [ru, r_xeT[pb]], [r_Up[gb]])
                    act(sg[gb][:], Gp[gb][:], AF.Silu, [r_Gp[gb]], [r_sg[gb]])
                    tt(actT[:, fc, :], sg[gb][:], Up[gb][:], ALU.mult, [r_sg[gb], r_Up[gb]], [r_actT[fc]])
                ensure(pg + 1 + NW)

            def compute_d(ex, dp):
                pb = ex % 2
                pd = ex * PPE + 2 * NFP + dp
                wdt, rd_ = Wb[pd % NW], r_W[pd % NW]
                for s4 in range(NS):
                    yb = cnt["y"] % 2
                    cnt["y"] += 1
                    for fc in range(NCH):
                        mm(Yp[yb][:, 0:FP], actT[:, fc, s4 * 128:(s4 + 1) * 128], wdt[:, fc, :], fc == 0, fc == NCH - 1,
                           [r_actT[fc], rd_], [r_Yp[yb]])
                    if s4 % 2 == 0:
                        ts(ygs[:, s4, dp * FP:(dp + 1) * FP], Yp[yb][:, 0:FP], gs[pb][:, s4:s4 + 1], None, ALU.mult, None,
                           [r_Yp[yb], r_idx[pb]], [r_ygs[s4]])
                    else:
                        act(ygs[:, s4, dp * FP:(dp + 1) * FP], Yp[yb][:, 0:FP], AF.Copy, [r_Yp[yb], r_idx[pb]], [r_ygs[s4]],
                            scale=gs[pb][:, s4:s4 + 1])
                ensure(pd + NW)

            sc_prev = {"ops": []}

            def scatter(ex):
                pb = ex % 2
                extra = list(sc_prev["ops"])
                if R.accd.last_w is not None:
                    extra.append(R.accd.last_w)
                ops = []
                for s4 in range(NS):
                    off = bass.IndirectOffsetOnAxis(ap=si[pb][:, s4:s4 + 1], axis=0)
                    ops.append(S.dma_fn("pool", lambda e, s4=s4, off=off: e.indirect_dma_start(out=Sx.accd, out_offset=off, in_=ygs[:, s4, :],
                                                                                             in_offset=None, compute_op=ALU.add),
                                        reads=[r_idx[pb], r_ygs[s4]], writes=[], extra=extra))
                sc_prev["ops"] = ops

            assert NFP == 4
            prep_idx(0)
            prep_T(0)
            ensure(NW - 1)
            for ex in range(N_EXP):
                for fp in range(NFP):
                    compute_gu(ex, fp)
                if ex + 1 < N_EXP:
                    prep_idx(ex + 1)
                for dp in range(NFP):
                    compute_d(ex, dp)
                    if dp == 1 and ex + 1 < N_EXP:
                        prep_T(ex + 1)
                scatter(ex)
            fin = S.dma("sp", K.out, Sx.accd[0:OWN, :], reads=[R.accd], writes=[], extra=sc_prev["ops"])
            S.finish([fin])
            S.emit_phase()
```

```python
import numpy as np
from contextlib import ExitStack
import concourse.bass as bass
import concourse.mybir as mybir
from concourse.bass_utils import run_bass_kernel_spmd

F32 = mybir.dt.float32
BF16 = mybir.dt.bfloat16
I32 = mybir.dt.int32
AF = mybir.ActivationFunctionType
ALU = mybir.AluOpType
AX = mybir.AxisListType

SAME_ENGINE_SYNC = True
MOE_COMPACT = True


class Reg:
    __slots__ = ("name", "last_w", "readers", "const")

    def __init__(self, name="", const=False):
        self.name = name
        self.last_w = None
        self.readers = []
        self.const = const


class Op:
    __slots__ = ("eng", "fn", "deps", "needed", "sig", "is_dma", "waits_only")

    def __init__(self, eng, fn, deps, is_dma=False):
        self.eng = eng
        self.fn = fn
        self.deps = deps
        self.needed = False
        self.sig = None
        self.is_dma = is_dma
        self.waits_only = False


class Sched:
    ENGS = ("pe", "act", "dve", "pool", "sp")
    NDMA = 12

    def __init__(self, nc, stack):
        self.nc = nc
        self.sem = {e: stack.enter_context(nc.semaphore("s_" + e)) for e in self.ENGS}
        self.cnt = {e: 0 for e in self.ENGS}
        self.dsem = {q: [stack.enter_context(nc.semaphore("d_%s%d" % (q, i))) for i in range(self.NDMA)]
                     for q in ("sp", "pool", "act")}
        self.dcnt = {q: 0 for q in ("sp", "pool", "act")}
        self.dlast = {q: [None] * self.NDMA for q in ("sp", "pool", "act")}
        self.ops = {e: [] for e in self.ENGS}
        self.known = {e: {} for e in self.ENGS}
        self.last_op = {e: None for e in self.ENGS}
        self.prev_phase_last = []

    def _mk(self, eng, fn, reads, writes, is_dma=False, extra=()):
        deps = list(extra)
        for r in reads:
            if r.last_w is not None:
                deps.append(r.last_w)
        for w in writes:
            if w.last_w is not None:
                deps.append(w.last_w)
            deps.extend(w.readers)
        op = Op(eng, fn, deps, is_dma)
        for r in reads:
            if not r.const:
                r.readers.append(op)
        for w in writes:
            w.last_w = op
            w.readers = []
        self.ops[eng].append(op)
        return op

    def op(self, eng, fn, reads=(), writes=()):
        return self._mk(eng, fn, reads, writes)

    def dma(self, q, out, in_, reads=(), writes=(), extra=(), **kw):
        return self._mk(q, lambda e: e.dma_start(out=out, in_=in_, **kw), reads, writes, is_dma=True, extra=extra)

    def dma_fn(self, q, fn, reads=(), writes=(), extra=()):
        return self._mk(q, fn, reads, writes, is_dma=True, extra=extra)

    def finish(self, ops):
        o = Op("sp", None, list(ops))
        o.waits_only = True
        self.ops["sp"].append(o)

    def emit_phase(self):
        self.nphase = getattr(self, "nphase", 0) + 1
        with self.nc.named_scope("ph%02d" % self.nphase):
            self._emit_phase()

    def _emit_phase(self):
        nc = self.nc
        engs = {"pe": nc.tensor, "act": nc.scalar, "dve": nc.vector, "pool": nc.gpsimd, "sp": nc.sync}
        barrier = list(self.prev_phase_last)
        for e in self.ENGS:
            for op in self.ops[e]:
                for d in op.deps:
                    if d.eng == op.eng and not d.is_dma:
                        if e in ("pe", "sp") or not SAME_ENGINE_SYNC:
                            continue
                    d.needed = True
        lasts = []
        for e in self.ENGS:
            real = [o for o in self.ops[e] if not o.waits_only]
            if real:
                real[-1].needed = True
                lasts.append(real[-1])
        for e in self.ENGS:
            for op in self.ops[e]:
                if op.waits_only:
                    continue
                if op.is_dma:
                    q = e
                    j = self.dcnt[q]
                    self.dcnt[q] += 1
                    op.sig = (self.dsem[q][j % self.NDMA], 16 * (j // self.NDMA + 1), q, j)
                elif op.needed:
                    self.cnt[e] += 1
                    op.sig = (self.sem[e], self.cnt[e])
        dma_lasts = []
        with nc.Block() as block:
            def run(e):
                def body(eng):
                    known = self.known[e]

                    def wait(sig):
                        s, v = sig[0], sig[1]
                        k = id(s)
                        if known.get(k, 0) < v:
                            eng.wait_ge(s, v)
                            known[k] = v
                    for d in barrier:
                        if d.eng != e or d.is_dma:
                            wait(d.sig)
                    for op in self.ops[e]:
                        for d in op.deps:
                            if d.sig is None:
                                continue
                            if d.eng == e and not d.is_dma:
                                if e in ("pe", "sp") or not SAME_ENGINE_SYNC:
                                    continue
                            wait(d.sig)
                        if op.waits_only:
                            continue
                        if op.is_dma:
                            s, v, q, j = op.sig
                            if j >= self.NDMA:
                                wait((s, v - 16))
                            ins = op.fn(eng)
                            ins.then_inc(s, 16)
                        else:
                            ins = op.fn(eng)
                            if op.sig is not None:
                                ins.then_inc(op.sig[0], 1)
                return body
            block.tensor(run("pe"))
            block.scalar(run("act"))
            block.vector(run("dve"))
            block.gpsimd(run("pool"))
            block.sync(run("sp"))
        for q in ("sp", "pool", "act"):
            seen = {}
            for op in self.ops[q]:
                if op.is_dma:
                    seen[id(op.sig[0])] = op
            dma_lasts.extend(seen.values())
        self.prev_phase_last = lasts + dma_lasts + [d for d in self.prev_phase_last if d.is_dma and id(d.sig[0]) not in {id(x.sig[0]) for x in dma_lasts}]
        self.ops = {e: [] for e in self.ENGS}


D = 2048
SEQ = 4096
HD = 128
D_IN = 4608
NCH = D // 128
NT = SEQ // 128
EPS = 1e-6
N_EXP = 16
CAP = 512
OWN = 1024
TWO_PI = float(2.0 * np.pi)
SCALE = float(1.0 / np.sqrt(HD))
HC_COL = [128 * i for i in range(8)] + [1024 + 128 * i for i in range(8)] + \
         [3072 + 128 * i for i in range(8)] + [4096, 4224]
HC_FAM = [0] * 8 + [1] * 8 + [2] * 8 + [3] * 2
V_COLS = [(2048, 512), (2560, 512), (4352, 256)]


def mask_a_np():
    kp = np.arange(128)[:, None]
    qf = np.arange(512)[None, :]
    out = np.zeros((20, 128, 512), np.float32)
    for j in range(20):
        o = (-1024 + 128 * j) + kp - qf
        a = np.abs(o)
        out[j] = (a <= 64).astype(np.float32) + ((o % 4 == 0) & (a <= 256)) + ((o % 16 == 0) & (a <= 1024))
    return out


def mask_b_np():
    kp = np.arange(128)[:, None]
    qf = np.arange(512)[None, :]
    out = np.zeros((6, 128, 512), np.float32)
    for j in range(6):
        o = (-128 + 128 * j) + kp - qf
        out[j] = (np.abs(o) <= 128)
    return out


def host_consts():
    import ml_dtypes
    bf = ml_dtypes.bfloat16
    c = {}
    c["c_ident"] = np.eye(128, dtype=np.float32).astype(bf)
    c["c_identf"] = np.eye(128, dtype=np.float32)
    c["c_ones"] = np.ones((128, 128), np.float32).astype(bf)
    c["c_onesf"] = np.ones((128, 128), np.float32)
    p0 = np.zeros((128, 128), np.float32)
    for m in range(64):
        p0[m + 64, m] = -1.0
    for m in range(64, 128):
        p0[m - 64, m] = 1.0
    c["c_rot"] = p0
    i = np.arange(128) % 64
    c["c_invf"] = (10000.0 ** (-(2.0 * i) / 128.0)).astype(np.float32).reshape(128, 1)
    pp = np.arange(128)
    c["c_ut"] = (pp[:, None] <= pp[None, :]).astype(np.float32)
    c["c_iota"] = np.tile(np.arange(512, dtype=np.float32)[None, :], (128, 1))
    c["c_loc"] = (np.arange(8)[None, :] * 128 + pp[:, None]).astype(np.float32)
    c["c_dummy"] = (1024 + np.arange(4)[None, :] * 128 + pp[:, None]).astype(np.float32)
    def lnm(m):
        out = np.full(m.shape, -3000.0, np.float32)
        nz = m > 0
        out[nz] = np.log(m[nz]) / SCALE
        return out
    c["c_maska"] = np.ascontiguousarray(lnm(mask_a_np()).transpose(1, 0, 2)).astype(bf)
    c["c_maskb"] = np.ascontiguousarray(lnm(mask_b_np()).transpose(1, 0, 2)).astype(bf)
    return c


class Ctx:
    pass


def build(stop_after=99, debug=False):
    nc = bass.Bass("TRN2", target_bir_lowering=False)
    K = Ctx()
    K.nc = nc

    def din(name, shape, dt):
        return nc.dram_tensor(name, list(shape), dt, kind="ExternalInput").ap()

    def dscr(name, shape, dt):
        kind = "ExternalOutput" if debug else "Internal"
        return nc.dram_tensor(name, list(shape), dt, kind=kind).ap()

    I = Ctx()
    I.x = din("x", [SEQ, D], F32)
    I.mem = din("mem", [256, D], F32)
    I.pos = din("positions", [SEQ], I32)
    I.own = din("own_idx", [128, OWN // 128], I32)
    for n in ("g_mix", "g_cross", "g_mem", "g_moe"):
        setattr(I, n, din(n, [D], F32))
    for n in ("g_qa", "g_ka", "g_qb", "g_kb", "g_qm", "g_km"):
        setattr(I, n, din(n, [HD], F32))
    I.sink = din("sink_b", [8], F32)
    I.g_oa = din("g_oa", [1024], F32)
    I.g_ob = din("g_ob", [1024], F32)
    I.w_in = din("w_in", [D, D_IN], F32)
    I.w_out = din("w_out", [D, D], F32)
    I.w_q_mem = din("w_q_mem", [D, 512], F32)
    I.w_kv_mem = din("w_kv_mem", [D, 1024], F32)
    I.w_o_mem = din("w_o_mem", [512, D], F32)
    I.w_router = din("w_router", [D, N_EXP], F32)
    I.w_gate = din("w_gate", [N_EXP, D, D], F32)
    I.w_up = din("w_up", [N_EXP, D, D], F32)
    I.w_down = din("w_down", [N_EXP, D, D], F32)
    I.c_ident = din("c_ident", [128, 128], BF16)
    I.c_identf = din("c_identf", [128, 128], F32)
    I.c_ones = din("c_ones", [128, 128], BF16)
    I.c_onesf = din("c_onesf", [128, 128], F32)
    I.c_rot = din("c_rot", [128, 128], F32)
    I.c_invf = din("c_invf", [128, 1], F32)
    I.c_ut = din("c_ut", [128, 128], F32)
    I.c_iota = din("c_iota", [128, 512], F32)
    I.c_loc = din("c_loc", [128, 8], F32)
    I.c_dummy = din("c_dummy", [128, 4], F32)
    I.c_maska = din("c_maska", [128, 20, 512], BF16)
    I.c_maskb = din("c_maskb", [128, 6, 512], BF16)
    K.I = I
    out = nc.dram_tensor("out", [OWN, D], F32, kind="ExternalOutput").ap()
    K.out = out

    Sx = Ctx()
    Sx.qkt = dscr("s_qkt", [26, 128, SEQ], BF16)
    Sx.v = dscr("s_v", [SEQ, 1280], BF16)
    Sx.ot = dscr("s_ot", [16, 128, SEQ], BF16)
    Sx.x2 = dscr("s_x2", [SEQ, D], F32)
    Sx.h3 = dscr("s_h3", [SEQ, D], BF16)
    Sx.gm = dscr("s_gm", [SEQ, N_EXP], F32)
    Sx.accd = dscr("s_accd", [OWN + CAP, D], F32)
    K.Sx = Sx
    R = Ctx()
    R.qkt = [[Reg("qkt%d_%d" % (h, f)) for f in range(2)] for h in range(26)]
    R.v = [Reg("v%d" % t) for t in range(NT)]
    R.ot = [Reg("ot%d" % h) for h in range(16)]
    R.x2 = [Reg("x2_%d" % t) for t in range(NT)]
    R.h3 = [Reg("h3_%d" % t) for t in range(NT)]
    R.gm = Reg("gm")
    R.accd = Reg("accd")
    K.R = R

    with ExitStack() as top:
        S = Sched(nc, top)
        K.S = S

        uid = [0]

        def sb(stack, name, shape, dt):
            uid[0] += 1
            return stack.enter_context(nc.sbuf_tensor("%s_%d" % (name, uid[0]), list(shape), dt))

        def ps(stack, name, shape, dt):
            uid[0] += 1
            return stack.enter_context(nc.psum_tensor("%s_%d" % (name, uid[0]), list(shape), dt))
        K.sb, K.ps = sb, ps

        def mm(out, lhsT, rhs, start, stop, reads, writes):
            return S.op("pe", lambda e: e.matmul(out, lhsT=lhsT, rhs=rhs, start=start, stop=stop), reads, writes)

        def tr(out, in_, reads, writes, ident=None):
            idn = C.ident[:] if ident is None else ident
            return S.op("pe", lambda e: e.transpose(out=out, in_=in_, identity=idn), list(reads) + [C.r], writes)

        def act(out, in_, func, reads, writes, **kw):
            return S.op("act", lambda e: e.activation(out=out, in_=in_, func=func, **kw), reads, writes)

        def ts(out, in0, s1, s2, op0, op1, reads, writes, eng="dve", **kw):
            if op1 is None:
                return S.op(eng, lambda e: e.tensor_scalar(out=out, in0=in0, scalar1=s1, scalar2=None, op0=op0, **kw), reads, writes)
            return S.op(eng, lambda e: e.tensor_scalar(out=out, in0=in0, scalar1=s1, scalar2=s2, op0=op0, op1=op1, **kw), reads, writes)

        def tt(out, in0, in1, op, reads, writes, eng="dve"):
            return S.op(eng, lambda e: e.tensor_tensor(out=out, in0=in0, in1=in1, op=op), reads, writes)

        def stt(out, in0, scalar, in1, op0, op1, reads, writes, eng="dve"):
            return S.op(eng, lambda e: e.scalar_tensor_tensor(out=out, in0=in0, scalar=scalar, in1=in1, op0=op0, op1=op1), reads, writes)

        def rsq(out, in_, scale, reads, writes):
            S.op("dve", lambda e: e.tensor_scalar(out=out, in0=in_, scalar1=scale, scalar2=EPS, op0=ALU.mult, op1=ALU.add), reads, writes)
            S.op("act", lambda e: e.activation(out=out, in_=out, func=AF.Ln), writes, writes)
            return S.op("act", lambda e: e.activation(out=out, in_=out, func=AF.Exp, scale=-0.5), writes, writes)
        K.rsq = rsq

        def recip(out, in_, reads, writes):
            return S.op("dve", lambda e: e.reciprocal(out=out, in_=in_), reads, writes)

        def cp(out, in_, reads, writes, eng="dve"):
            if eng == "act":
                return S.op("act", lambda e: e.copy(out=out, in_=in_), reads, writes)
            return S.op(eng, lambda e: e.tensor_copy(out=out, in_=in_), reads, writes)
        K.mm, K.tr, K.act, K.ts, K.tt, K.stt, K.recip, K.cp = mm, tr, act, ts, tt, stt, recip, cp

        C = Ctx()
        K.C = C
        C.ident = sb(top, "ident", [128, 128], BF16)
        C.identf = sb(top, "identf", [128, 128], F32)
        C.ones = sb(top, "ones", [128, 128], BF16)
        C.onesf = sb(top, "onesf", [128, 128], F32)
        C.rot0 = sb(top, "rot0", [128, 128], F32)
        C.rotg = sb(top, "rotg", [128, 4, 128], BF16)
        C.invf = sb(top, "invf", [128, 1], F32)
        C.gh = sb(top, "gh", [128, 6], F32)
        C.ss = sb(top, "ssab", [128, 2, NT], F32)
        C.esink = sb(top, "esink", [128, 8], F32)
        C.aff = sb(top, "aff", [128, NT, N_EXP], F32)
        C.r_aff = Reg("aff")
        C.r = Reg("consts", const=True)
        C.r_ss = Reg("ss")
        S.dma("sp", C.ident[:], I.c_ident, writes=[C.r])
        S.dma("sp", C.identf[:], I.c_identf, writes=[C.r])
        S.dma("sp", C.ones[:], I.c_ones, writes=[C.r])
        S.dma("sp", C.onesf[:], I.c_onesf, writes=[C.r])
        S.dma("sp", C.rot0[:], I.c_rot, writes=[C.r])
        S.dma("sp", C.invf[:], I.c_invf, writes=[C.r])
        for i, n in enumerate(("g_qa", "g_ka", "g_qb", "g_kb", "g_qm", "g_km")):
            S.dma("sp", C.gh[:, i:i + 1], getattr(I, n).rearrange("(p o) -> p o", o=1), writes=[C.r])
        S.dma("sp", C.esink[:], I.sink.partition_broadcast(128), writes=[C.r])
        S.op("act", lambda e: e.activation(out=C.esink[:], in_=C.esink[:], func=AF.Exp), reads=[C.r], writes=[C.r])
        for f in range(4):
            S.op("dve", lambda e, f=f: e.tensor_scalar(out=C.rotg[:, f, :], in0=C.rot0[:], scalar1=C.gh[:, f:f + 1],
                                                       scalar2=None, op0=ALU.mult), reads=[C.r], writes=[C.r])
        S.op("dve", lambda e: e.memset(C.ss[:], 0.0), writes=[C.r_ss])

        phase_a(K, top)
        if stop_after >= 2:
            with ExitStack() as st_bc:
                K.wout = sb(st_bc, "wout", [128, NCH, D], BF16)
                go = sb(st_bc, "go", [128, 16], F32)
                r_wo = Reg()
                S.dma("pool", K.wout[:], I.w_out.rearrange("(c p) n -> p c n", p=128), writes=[r_wo])
                S.dma("sp", go[:, 0:8], I.g_oa.rearrange("(c p) -> p c", p=128), writes=[r_wo], allow_slow_non_contiguous=True)
                S.dma("sp", go[:, 8:16], I.g_ob.rearrange("(c p) -> p c", p=128), writes=[r_wo], allow_slow_non_contiguous=True)
                for c in range(NCH):
                    ts(K.wout[:, c, :], K.wout[:, c, :], go[:, c:c + 1], None, ALU.mult, None, [r_wo], [r_wo])
                phase_b(K, top)
                if stop_after >= 3:
                    phase_c(K, top)
        if stop_after >= 4:
            with ExitStack() as st_moe:
                moe_ring_setup(K, st_moe)
                phase_d(K, top)
                if stop_after >= 5:
                    phase_e2(K, top)
        S.emit_phase()
    return nc


PI_LO = 3.1415925


def phase_a(K, top):
    nc, S, I, C, Sx, R, sb, ps = K.nc, K.S, K.I, K.C, K.Sx, K.R, K.sb, K.ps
    HALF = SEQ // 2
    w_in_v = I.w_in.rearrange("(c p) n -> p c n", p=128)
    for hf in range(2):
        with ExitStack() as st_x:
            xnT = sb(st_x, "xnT", [128, NCH, HALF], BF16)
            r_xnT = [Reg("xnT%d" % t) for t in range(16)]
            with ExitStack() as st:
                xs = [sb(st, "xs%d" % i, [128, D], F32) for i in range(2)]
                xn = [sb(st, "xn%d" % i, [128, D], BF16) for i in range(2)]
                junk = sb(st, "junk", [128, D], BF16)
                gbc = sb(st, "gbc", [128, D], F32)
                ssq = sb(st, "ssq", [128, 2], F32)
                rstd = sb(st, "rstd", [128, 2], F32)
                tp = [ps(st, "tp%d" % i, [128, 8, 128], BF16) for i in range(4)]
                r_xs = [Reg() for _ in range(2)]
                r_xn = [Reg() for _ in range(2)]
                r_junk, r_gbc = Reg(), Reg(const=True)
                r_ssq = [Reg() for _ in range(2)]
                r_rstd = [Reg() for _ in range(2)]
                r_tp = [Reg() for _ in range(4)]
                S.dma("sp", gbc[:], I.g_mix.partition_broadcast(128), writes=[r_gbc])
                for t in range(16):
                    b = t % 2
                    tok0 = hf * HALF + t * 128
                    S.dma("sp", xs[b][:], I.x[tok0:tok0 + 128, :], writes=[r_xs[b]])
                    S.op("act", lambda e, b=b: e.activation(out=junk[:], in_=xs[b][:], func=AF.Square,
                                                            accum_out=ssq[:, b:b + 1]),
                         reads=[r_xs[b]], writes=[r_junk, r_ssq[b]])
                    K.rsq(rstd[:, b:b + 1], ssq[:, b:b + 1], 1.0 / D, [r_ssq[b]], [r_rstd[b]])
                    S.op("dve", lambda e, b=b: e.scalar_tensor_tensor(out=xn[b][:], in0=xs[b][:], scalar=rstd[:, b:b + 1],
                                                                      in1=gbc[:], op0=ALU.mult, op1=ALU.mult),
                         reads=[r_xs[b], r_rstd[b], r_gbc], writes=[r_xn[b]])
                    for hh in range(2):
                        pt = tp[2 * b + hh]
                        rp = r_tp[2 * b + hh]
                        for c in range(8):
                            cc = hh * 8 + c
                            S.op("pe", lambda e, pt=pt, c=c, cc=cc, b=b: e.transpose(out=pt[:, c, :], in_=xn[b][:, cc * 128:(cc + 1) * 128],
                                                                                      identity=C.ident[:]),
                                 reads=[r_xn[b], C.r], writes=[rp])
                        eng = "act" if hh == 0 else "dve"
                        if eng == "act":
                            S.op("act", lambda e, pt=pt, hh=hh, t=t: e.copy(out=xnT[:, hh * 8:hh * 8 + 8, t * 128:(t + 1) * 128], in_=pt[:]),
                                 reads=[rp], writes=[r_xnT[t]])
                        else:
                            S.op("dve", lambda e, pt=pt, hh=hh, t=t: e.tensor_copy(out=xnT[:, hh * 8:hh * 8 + 8, t * 128:(t + 1) * 128], in_=pt[:]),
                                 reads=[rp], writes=[r_xnT[t]])
                S.emit_phase()
            with ExitStack() as st:
                cos = sb(st, "cos", [128, HALF], F32)
                sin = sb(st, "sin", [128, HALF], F32)
                posi = sb(st, "posi", [128, 1024], I32)
                ang = sb(st, "ang", [128, 1024], F32)
                kf = sb(st, "kf", [128, 1024], F32)
                ki = sb(st, "ki", [128, 1024], I32)
                r_cs = Reg(const=True)
                r_tmp = Reg()
                for qd in range(2):
                    t0 = hf * HALF + qd * 1024
                    sl = slice(qd * 1024, (qd + 1) * 1024)
                    S.dma("sp", posi[:], I.pos[t0:t0 + 1024].partition_broadcast(128), writes=[r_tmp])
                    S.op("dve", lambda e: e.tensor_copy(out=ang[:], in_=posi[:]), reads=[r_tmp], writes=[r_tmp])
                    S.op("dve", lambda e: e.tensor_scalar(out=ang[:], in0=ang[:], scalar1=C.invf[:, 0:1], scalar2=None, op0=ALU.mult),
                         reads=[r_tmp, C.r], writes=[r_tmp])
                    S.op("dve", lambda e: e.tensor_scalar(out=kf[:], in0=ang[:], scalar1=1.0 / TWO_PI, scalar2=None, op0=ALU.mult),
                         reads=[r_tmp], writes=[r_tmp])
                    S.op("dve", lambda e: e.tensor_copy(out=ki[:], in_=kf[:]), reads=[r_tmp], writes=[r_tmp])
                    S.op("dve", lambda e: e.tensor_copy(out=kf[:], in_=ki[:]), reads=[r_tmp], writes=[r_tmp])
                    S.op("dve", lambda e: e.scalar_tensor_tensor(out=ang[:], in0=kf[:], scalar=-TWO_PI, in1=ang[:], op0=ALU.mult, op1=ALU.add),
                         reads=[r_tmp], writes=[r_tmp])

                    def wrap_and_sin(dst, shift):
                        S.op("dve", lambda e: e.tensor_scalar(out=kf[:], in0=ang[:], scalar1=shift, scalar2=None, op0=ALU.add),
                             reads=[r_tmp], writes=[r_tmp])
                        S.op("dve", lambda e: e.tensor_scalar(out=posi[:].bitcast(F32), in0=kf[:], scalar1=float(np.pi), scalar2=-TWO_PI,
                                                              op0=ALU.is_gt, op1=ALU.mult), reads=[r_tmp], writes=[r_tmp])
                        S.op("dve", lambda e: e.tensor_tensor(out=kf[:], in0=kf[:], in1=posi[:].bitcast(F32), op=ALU.add),
                             reads=[r_tmp], writes=[r_tmp])
                        S.op("dve", lambda e: e.tensor_scalar(out=posi[:].bitcast(F32), in0=kf[:], scalar1=-float(np.pi), scalar2=TWO_PI,
                                                              op0=ALU.is_lt, op1=ALU.mult), reads=[r_tmp], writes=[r_tmp])
                        S.op("dve", lambda e: e.tensor_tensor(out=kf[:], in0=kf[:], in1=posi[:].bitcast(F32), op=ALU.add),
                             reads=[r_tmp], writes=[r_tmp])
                        S.op("dve", lambda e: e.tensor_scalar(out=kf[:], in0=kf[:], scalar1=PI_LO, scalar2=-PI_LO, op0=ALU.min, op1=ALU.max),
                             reads=[r_tmp], writes=[r_tmp])
                        S.op("act", lambda e: e.activation(out=dst, in_=kf[:], func=AF.Sin), reads=[r_tmp], writes=[r_cs, r_tmp])
                    wrap_and_sin(sin[:, sl], 0.0)
                    wrap_and_sin(cos[:, sl], float(np.pi / 2))

                wq = [sb(st, "wq%d" % i, [128, NCH, 128], BF16) for i in range(2)]
                r_wq = [Reg() for _ in range(2)]
                q2 = [sb(st, "q2_%d" % i, [128, 512], BF16) for i in range(2)]
                qb = [sb(st, "qb_%d" % i, [128, 512], BF16) for i in range(2)]
                rs = [sb(st, "rs_%d" % i, [128, 512], F32) for i in range(2)]
                ta = [sb(st, "ta_%d" % i, [128, 512], F32) for i in range(2)]
                tb = [sb(st, "tb_%d" % i, [128, 512], F32) for i in range(2)]
                stage = [sb(st, "stg%d" % i, [128, HALF], BF16) for i in range(2)]
                r_q2 = [Reg() for _ in range(2)]
                r_qb = [Reg() for _ in range(2)]
                r_rs = [Reg() for _ in range(2)]
                r_ta = [Reg() for _ in range(2)]
                r_tb = [Reg() for _ in range(2)]
                r_stage = [Reg() for _ in range(2)]
                qp = [ps(st, "qp%d" % i, [128, 512], F32) for i in range(2)]
                sp_ = [ps(st, "ssp%d" % i, [128, 512], F32) for i in range(2)]
                rp_ = [ps(st, "rtp%d" % i, [128, 512], F32) for i in range(2)]
                r_qp = [Reg() for _ in range(2)]
                r_sp = [Reg() for _ in range(2)]
                r_rp = [Reg() for _ in range(2)]
                it = 0
                for hc in range(26):
                    wb = hc % 2
                    fam = HC_FAM[hc]
                    c0 = HC_COL[hc]
                    S.dma("pool", wq[wb][:], w_in_v[:, :, c0:c0 + 128], writes=[r_wq[wb]])
                    for blk in range(4):
                        b = it % 2
                        it += 1
                        cs = slice(blk * 512, (blk + 1) * 512)
                        for c in range(NCH):
                            S.op("pe", lambda e, b=b, wb=wb, c=c, cs=cs: e.matmul(qp[b][:], lhsT=wq[wb][:, c, :], rhs=xnT[:, c, cs],
                                                                                    start=(c == 0), stop=(c == NCH - 1)),
                                 reads=[r_wq[wb]] + r_xnT[blk * 4:blk * 4 + 4], writes=[r_qp[b]])
                        S.op("act", lambda e, b=b: e.activation(out=q2[b][:], in_=qp[b][:], func=AF.Square),
                             reads=[r_qp[b]], writes=[r_q2[b]])
                        S.op("act", lambda e, b=b: e.copy(out=qb[b][:], in_=qp[b][:]), reads=[r_qp[b]], writes=[r_qb[b]])
                        S.op("pe", lambda e, b=b: e.matmul(sp_[b][:], lhsT=C.ones[:], rhs=q2[b][:], start=True, stop=True),
                             reads=[r_q2[b], C.r], writes=[r_sp[b]])
                        S.op("pe", lambda e, b=b, fam=fam: e.matmul(rp_[b][:], lhsT=C.rotg[:, fam, :], rhs=qb[b][:], start=True, stop=True),
                             reads=[r_qb[b], C.r], writes=[r_rp[b]])
                        K.rsq(rs[b][:], sp_[b][:], 1.0 / HD, [r_sp[b]], [r_rs[b]])
                        S.op("dve", lambda e, b=b, fam=fam, cs=cs: e.scalar_tensor_tensor(out=ta[b][:], in0=qp[b][:], scalar=C.gh[:, fam:fam + 1],
                                                                                         in1=cos[:, cs], op0=ALU.mult, op1=ALU.mult),
                             reads=[r_qp[b], r_cs, C.r], writes=[r_ta[b]])
                        S.op("dve", lambda e, b=b, cs=cs: e.tensor_tensor(out=tb[b][:], in0=rp_[b][:], in1=sin[:, cs], op=ALU.mult),
                             reads=[r_rp[b], r_cs], writes=[r_tb[b]])
                        S.op("dve", lambda e, b=b: e.tensor_tensor(out=ta[b][:], in0=ta[b][:], in1=tb[b][:], op=ALU.add),
                             reads=[r_ta[b], r_tb[b]], writes=[r_ta[b]])
                        S.op("dve", lambda e, b=b, wb=wb, cs=cs: e.tensor_tensor(out=stage[wb][:, cs], in0=ta[b][:], in1=rs[b][:], op=ALU.mult),
                             reads=[r_ta[b], r_rs[b]], writes=[r_stage[wb]])
                    S.dma("sp", Sx.qkt[hc, :, hf * HALF:(hf + 1) * HALF], stage[wb][:], reads=[r_stage[wb]], writes=[R.qkt[hc][hf]])
                S.emit_phase()
            with ExitStack() as st:
                wv = sb(st, "wv", [128, NCH, 1280], BF16)
                r_wv = [Reg() for _ in range(3)]
                vst = [sb(st, "vst%d" % i, [128, 1280], BF16) for i in range(2)]
                r_vst = [Reg() for _ in range(2)]
                vp = [ps(st, "vp%d" % i, [128, 512], F32) for i in range(4)]
                r_vp = [Reg() for _ in range(4)]
                off = 0
                pieces = []
                for i, (c0, n) in enumerate(V_COLS):
                    S.dma("pool", wv[:, :, off:off + n], w_in_v[:, :, c0:c0 + n], writes=[r_wv[i]])
                    pieces.append((off, n))
                    off += n
                it = 0
                for t in range(16):
                    b = t % 2
                    tok0 = hf * HALF + t * 128
                    for i, (o, n) in enumerate(pieces):
                        pb = it % 4
                        it += 1
                        for c in range(NCH):
                            S.op("pe", lambda e, pb=pb, c=c, t=t, o=o, n=n: e.matmul(vp[pb][:, 0:n], lhsT=xnT[:, c, t * 128:(t + 1) * 128],
                                                                                     rhs=wv[:, c, o:o + n], start=(c == 0), stop=(c == NCH - 1)),
                                 reads=[r_wv[i], r_xnT[t]], writes=[r_vp[pb]])
                        if i % 2 == 0:
                            S.op("act", lambda e, pb=pb, b=b, o=o, n=n: e.copy(out=vst[b][:, o:o + n], in_=vp[pb][:, 0:n]),
                                 reads=[r_vp[pb]], writes=[r_vst[b]])
                        else:
                            S.op("dve", lambda e, pb=pb, b=b, o=o, n=n: e.tensor_copy(out=vst[b][:, o:o + n], in_=vp[pb][:, 0:n]),
                                 reads=[r_vp[pb]], writes=[r_vst[b]])
                    S.dma("sp", Sx.v[tok0:tok0 + 128, :], vst[b][:], reads=[r_vst[b]], writes=[R.v[hf * 16 + t]])
                S.emit_phase()


def phase_b(K, top):
    nc, S, I, C, Sx, R, sb, ps = K.nc, K.S, K.I, K.C, K.Sx, K.R, K.sb, K.ps
    mm, tr, act, ts, tt, stt, recip, cp = K.mm, K.tr, K.act, K.ts, K.tt, K.stt, K.recip, K.cp
    LOOK = 3
    with ExitStack() as st:
        maskA = sb(st, "maskA", [128, 20, 512], BF16)
        maskB = sb(st, "maskB", [128, 6, 512], BF16)
        r_mask = Reg(const=True)
        S.dma("sp", maskA[:], I.c_maska, writes=[r_mask])
        S.dma("sp", maskB[:], I.c_maskb, writes=[r_mask])
        QT = [sb(st, "QT%d" % i, [128, SEQ], BF16) for i in range(2)]
        KT = [sb(st, "KT%d" % i, [128, SEQ], BF16) for i in range(2)]
        V1 = [sb(st, "V1%d" % i, [128, NT, 130], BF16) for i in range(2)]
        OTs = [sb(st, "OTs%d" % i, [128, SEQ], BF16) for i in range(2)]
        r_QT = [Reg() for _ in range(2)]
        r_KT = [Reg() for _ in range(2)]
        r_V1 = [Reg() for _ in range(2)]
        r_OTs = [Reg() for _ in range(2)]
        NE = 6
        NSP = 3
        E = [sb(st, "E%d" % i, [128, 512], BF16) for i in range(NE)]
        Pm = [sb(st, "Pm%d" % i, [128, 512], BF16) for i in range(NE)]
        r_E = [Reg() for _ in range(NE)]
        r_Pm = [Reg() for _ in range(NE)]
        NO = 8
        Osb = [sb(st, "Osb%d" % i, [128, 130], F32) for i in range(NO)]
        den = [sb(st, "den%d" % i, [128, 1], F32) for i in range(NO)]
        sst = [sb(st, "sst%d" % i, [128, 1], F32) for i in range(NO)]
        obf = [sb(st, "obf%d" % i, [128, 128], BF16) for i in range(NO)]
        junk = sb(st, "junkb", [128, 128], BF16)
        r_Osb = [Reg() for _ in range(NO)]
        r_den = [Reg() for _ in range(NO)]
        r_sst = [Reg() for _ in range(NO)]
        r_obf = [Reg() for _ in range(NO)]
        r_junk = Reg()
        Sp = [ps(st, "Sp%d" % i, [128, 512], F32) for i in range(NSP)]
        Op_ = [ps(st, "Op%d" % i, [128, 512], F32) for i in range(4)]
        Tp = ps(st, "Tp", [128, 512], F32)
        tpv = Tp[:].bitcast(BF16)
        r_Sp = [Reg() for _ in range(NSP)]
        r_Op = [Reg() for _ in range(4)]
        r_Tp = [Reg() for _ in range(8)]
        for i in range(2):
            S.op("dve", lambda e, i=i: e.memset(V1[i][:, :, 128:130], 1.0), writes=[r_V1[i]])

        iters = []
        for h in range(16):
            isA = h < 8
            nj, koff = (20, -1024) if isA else (6, -128)
            for qb in range(8):
                q0 = qb * 512
                js = [j for j in range(nj) if 0 <= q0 + koff + 128 * j < SEQ]
                for jn, j in enumerate(js):
                    iters.append(dict(h=h, qb=qb, j=j, k0=q0 + koff + 128 * j, first=(jn == 0), last=(jn == len(js) - 1),
                                      hstart=(qb == 0 and jn == 0), hend=(qb == 7 and jn == len(js) - 1)))
        state = {"osb": 0, "ts": 0}
        deferred = []

        def head_cfg(h):
            if h < 8:
                return h, 8 + h, 128 * h, maskA
            kvh = (h - 8) // 4
            return 16 + (h - 8), 24 + kvh, 1024 + 128 * kvh, maskB

        def load_head(h):
            hb = h % 2
            qhc, khc, vcol, _ = head_cfg(h)
            S.dma("sp", QT[hb][:], Sx.qkt[qhc], reads=R.qkt[qhc], writes=[r_QT[hb]])
            S.dma("sp", KT[hb][:], Sx.qkt[khc], reads=R.qkt[khc], writes=[r_KT[hb]])
            S.dma("sp", V1[hb][:, :, 0:128], Sx.v[:, vcol:vcol + 128].rearrange("(t p) d -> p t d", p=128),
                  reads=R.v, writes=[r_V1[hb]])

        def score(n):
            it = iters[n]
            hb = it["h"] % 2
            mask = head_cfg(it["h"])[3]
            sbi, ei = n % NSP, n % NE
            k0, q0, j = it["k0"], it["qb"] * 512, it["j"]
            mm(Sp[sbi][:], KT[hb][:, k0:k0 + 128], QT[hb][:, q0:q0 + 512], True, False, [r_KT[hb], r_QT[hb]], [r_Sp[sbi]])
            mm(Sp[sbi][:], C.ident[:], mask[:, j, :], False, True, [r_mask, C.r], [r_Sp[sbi]])
            act(Pm[ei][:], Sp[sbi][:], AF.Exp, [r_Sp[sbi]], [r_Pm[ei]], scale=SCALE)

        def post(h, qb, i, o):
            hb = h % 2
            grp = 0 if h < 8 else 1
            qt = 4 * qb + i
            if h < 8:
                recip(den[o][:], Osb[o][:, 128:129], [r_Osb[o]], [r_den[o]])
            else:
                tt(den[o][:], Osb[o][:, 128:129], C.esink[:, h - 8:h - 7], ALU.add, [r_Osb[o], C.r], [r_den[o]])
                recip(den[o][:], den[o][:], [r_den[o]], [r_den[o]])
            ts(obf[o][:], Osb[o][:, 0:128], den[o][:, 0:1], None, ALU.mult, None, [r_Osb[o], r_den[o]], [r_obf[o]])
            act(junk[:], obf[o][:], AF.Square, [r_obf[o]], [r_junk, r_sst[o]], accum_out=sst[o][:])
            tt(C.ss[:, grp, qt:qt + 1], C.ss[:, grp, qt:qt + 1], sst[o][:], ALU.add, [r_sst[o], C.r_ss], [C.r_ss])
            tsl = state["ts"] % 8
            state["ts"] += 1
            tr(tpv[:, tsl * 128:(tsl + 1) * 128], obf[o][:], [r_obf[o]], [r_Tp[tsl]])
            cp(OTs[hb][:, qt * 128:(qt + 1) * 128], tpv[:, tsl * 128:(tsl + 1) * 128], [r_Tp[tsl]], [r_OTs[hb]], eng="act")

        def pv(n):
            it = iters[n]
            h, qb = it["h"], it["qb"]
            hb = h % 2
            ei = n % NE
            k0 = it["k0"]
            for i in range(4):
                mm(Op_[i][:, 0:129], Pm[ei][:, 128 * i:128 * i + 128], V1[hb][:, k0 // 128, 0:129], it["first"], it["last"],
                   [r_Pm[ei], r_V1[hb]], [r_Op[i]])
            if it["last"]:
                for i in range(4):
                    o = state["osb"] % NO
                    state["osb"] += 1
                    cp(Osb[o][:, 0:129], Op_[i][:, 0:129], [r_Op[i]], [r_Osb[o]])
                    deferred.append([2, (lambda h=h, qb=qb, i=i, o=o: post(h, qb, i, o))])
            if it["hend"]:
                deferred.append([3, (lambda h=h, hb=hb: S.dma("sp", Sx.ot[h], OTs[hb][:], reads=[r_OTs[hb]], writes=[R.ot[h]]))])

        def tick():
            for d in deferred:
                d[0] -= 1
            while deferred and deferred[0][0] <= 0:
                deferred.pop(0)[1]()

        N = len(iters)
        load_head(0)
        for n in range(N + LOOK):
            if n < N:
                score(n)
            if n - LOOK >= 0:
                pv(n - LOOK)
                if iters[n - LOOK]["hstart"] and iters[n - LOOK]["h"] + 1 < 16:
                    load_head(iters[n - LOOK]["h"] + 1)
            tick()
        while deferred:
            tick()
        S.emit_phase()


def phase_c(K, top):
    nc, S, I, C, Sx, R, sb, ps = K.nc, K.S, K.I, K.C, K.Sx, K.R, K.sb, K.ps
    mm, tr, act, ts, tt, stt, recip, cp = K.mm, K.tr, K.act, K.ts, K.tt, K.stt, K.recip, K.cp
    with ExitStack() as st0:
        KmT = sb(st0, "KmT", [128, 4, 256], BF16)
        Vm = sb(st0, "Vm", [128, 2, 4, 130], BF16)
        r_km = Reg(const=True)
        with ExitStack() as st:
            memx = sb(st, "memx", [128, 2, D], F32)
            memn = sb(st, "memn", [128, 2, D], BF16)
            memT = sb(st, "memT", [128, NCH, 256], BF16)
            gbc = sb(st, "gbcm", [128, D], F32)
            junk = sb(st, "junkc0", [128, D], BF16)
            wkv = sb(st, "wkv", [128, NCH, 1024], BF16)
            ssq = sb(st, "ssqm", [128, 2], F32)
            k2 = sb(st, "k2", [128, 256], BF16)
            rsk = sb(st, "rsk", [128, 256], F32)
            bank = [ps(st, "c0b%d" % i, [128, 512], F32) for i in range(4)]
            rb = [Reg() for _ in range(4)]
            r1 = Reg()
            r_w = Reg()
            S.dma("sp", memx[:], I.mem.rearrange("(t p) d -> p t d", p=128), writes=[r1])
            S.dma("sp", gbc[:], I.g_mem.partition_broadcast(128), writes=[r1])
            S.dma("pool", wkv[:], I.w_kv_mem.rearrange("(c p) n -> p c n", p=128), writes=[r_w])
            S.op("dve", lambda e: e.memset(Vm[:], 1.0), writes=[r_km])
            for mt in range(2):
                act(junk[:], memx[:, mt, :], AF.Square, [r1], [r1], accum_out=ssq[:, mt:mt + 1])
                K.rsq(ssq[:, mt:mt + 1], ssq[:, mt:mt + 1], 1.0 / D, [r1], [r1])
                stt(memn[:, mt, :], memx[:, mt, :], ssq[:, mt:mt + 1], gbc[:], ALU.mult, ALU.mult, [r1], [r1])
                for hh in range(2):
                    tpv = bank[hh][:].bitcast(BF16)
                    for c in range(8):
                        cc = hh * 8 + c
                        tr(tpv[:, c * 128:(c + 1) * 128], memn[:, mt, cc * 128:(cc + 1) * 128], [r1], [rb[hh]])
                    cp(memT[:, hh * 8:hh * 8 + 8, mt * 128:(mt + 1) * 128], tpv[:, 0:1024].rearrange("p (c n) -> p c n", c=8), [rb[hh]], [r1])
            for hm in range(4):
                for c in range(NCH):
                    mm(bank[2][:, 0:256], wkv[:, c, hm * 128:(hm + 1) * 128], memT[:, c, :], c == 0, c == NCH - 1, [r_w, r1], [rb[2]])
                act(k2[:], bank[2][:, 0:256], AF.Square, [rb[2]], [r1])
                mm(bank[3][:, 0:256], C.ones[:], k2[:], True, True, [r1, C.r], [rb[3]])
                K.rsq(rsk[:], bank[3][:, 0:256], 1.0 / HD, [rb[3]], [r1])
                stt(KmT[:, hm, :], bank[2][:, 0:256], C.gh[:, 5:6], rsk[:], ALU.mult, ALU.mult, [rb[2], r1, C.r], [r_km])
            for mt in range(2):
                for c in range(NCH):
                    mm(bank[mt][:], memT[:, c, mt * 128:(mt + 1) * 128], wkv[:, c, 512:1024], c == 0, c == NCH - 1, [r_w, r1], [rb[mt]])
                cp(Vm[:, mt, :, 0:128], bank[mt][:].rearrange("p (h d) -> p h d", h=4), [rb[mt]], [r_km])
            S.emit_phase()
        with ExitStack() as st:
            wout = K.wout
            wq = sb(st, "wqm", [128, NCH, 512], BF16)
            wo = sb(st, "wom", [128, 4, D], BF16)
            wr = sb(st, "wr", [128, NCH, N_EXP], BF16)
            gcr = sb(st, "gcr", [128, D], F32)
            gmo = sb(st, "gmo", [128, D], F32)
            rAB = sb(st, "rAB", [128, 2, NT], F32)
            r_w = Reg(const=True)
            S.dma("pool", wq[:], I.w_q_mem.rearrange("(c p) n -> p c n", p=128), writes=[r_w])
            S.dma("pool", wo[:], I.w_o_mem.rearrange("(c p) n -> p c n", p=128), writes=[r_w])
            S.dma("pool", wr[:], I.w_router.rearrange("(c p) n -> p c n", p=128), writes=[r_w])
            S.dma("sp", gcr[:], I.g_cross.partition_broadcast(128), writes=[r_w])
            S.dma("sp", gmo[:], I.g_moe.partition_broadcast(128), writes=[r_w])
            K.rsq(rAB[:], C.ss[:], 1.0 / 1024, [C.r_ss], [r_w])

            xt = [sb(st, "xt%d" % i, [128, D], F32) for i in range(2)]
            otb = [sb(st, "otb%d" % i, [128, 16, 128], BF16) for i in range(2)]
            x12 = [sb(st, "x12_%d" % i, [128, D], F32) for i in range(2)]
            hn = [sb(st, "hn%d" % i, [128, D], BF16) for i in range(2)]
            hT = [sb(st, "hT%d" % i, [128, NCH, 128], BF16) for i in range(2)]
            junk = sb(st, "junkc1", [128, D], BF16)
            sq = sb(st, "sqc", [128, 4], F32)
            q2 = sb(st, "q2c", [128, 512], BF16)
            rsq = sb(st, "rsqc", [128, 512], F32)
            qn = sb(st, "qnc", [128, 4, 128], BF16)
            E2 = [sb(st, "E2_%d" % i, [128, 4, 128], BF16) for i in range(2)]
            rden = sb(st, "rdenc", [128, 4], F32)
            o2 = sb(st, "o2c", [128, 4, 128], BF16)
            o2T = sb(st, "o2T", [128, 4, 128], BF16)
            ex = sb(st, "exr", [128, N_EXP], F32)
            sume = sb(st, "sume", [128, 1], F32)
            r_xt = [Reg() for _ in range(2)]
            r_otb = [Reg() for _ in range(2)]
            r_x12 = [Reg() for _ in range(2)]
            r_hn = [Reg() for _ in range(2)]
            r_hT = [Reg() for _ in range(2)]
            r_junk, r_sq, r_q2, r_rsq, r_qn, r_rden, r_o2, r_o2T, r_ex, r_sume = [Reg() for _ in range(10)]
            r_E2 = [Reg() for _ in range(2)]
            B = [ps(st, "c1b%d" % i, [128, 512], F32) for i in range(8)]
            rB = [Reg() for _ in range(8)]
            tpv = B[4][:].bitcast(BF16)
            ot_v = Sx.ot.rearrange("h p n -> p h n")

            def rmsnorm_tile(src, r_src, gb, dst, r_dst, col):
                act(junk[:], src, AF.Square, [r_src], [r_junk, r_sq], accum_out=sq[:, col:col + 1])
                K.rsq(sq[:, col:col + 1], sq[:, col:col + 1], 1.0 / D, [r_sq], [r_sq])
                stt(dst, src, sq[:, col:col + 1], gb[:], ALU.mult, ALU.mult, [r_src, r_sq, r_w], [r_dst])

            def transpose16(src, r_src, dst, r_dst):
                for hh in range(2):
                    for c in range(8):
                        cc = hh * 8 + c
                        tr(tpv[:, c * 128:(c + 1) * 128], src[:, cc * 128:(cc + 1) * 128], [r_src], [rB[4]])
                    cp(dst[:, hh * 8:hh * 8 + 8, :], tpv.rearrange("p (c n) -> p c n", c=8), [rB[4]], [r_dst],
                       eng=("act" if hh == 0 else "dve"))

            def load(t):
                b = t % 2
                S.dma("sp", xt[b][:], I.x[t * 128:(t + 1) * 128, :], writes=[r_xt[b]])
                S.dma("sp", otb[b][:], ot_v[:, :, t * 128:(t + 1) * 128], reads=R.ot, writes=[r_otb[b]])
            def s1_cg(t, cg):
                b = t % 2
                X = x12[b]
                pa, pb = B[(cg % 2) * 2], B[(cg % 2) * 2 + 1]
                ra, rbb = rB[(cg % 2) * 2], rB[(cg % 2) * 2 + 1]
                cs = slice(cg * 512, (cg + 1) * 512)
                for c in range(8):
                    mm(pa[:], otb[b][:, c, :], wout[:, c, cs], c == 0, c == 7, [r_otb[b], r_w], [ra])
                for c in range(8, 16):
                    mm(pb[:], otb[b][:, c, :], wout[:, c, cs], c == 8, c == 15, [r_otb[b], r_w], [rbb])
                stt(X[:, cs], pa[:], rAB[:, 0, t:t + 1], xt[b][:, cs], ALU.mult, ALU.add, [ra, r_xt[b], r_w], [r_x12[b]])
                stt(X[:, cs], pb[:], rAB[:, 1, t:t + 1], X[:, cs], ALU.mult, ALU.add, [rbb, r_x12[b], r_w], [r_x12[b]])

            def nxt(t, cg):
                if t + 1 < NT:
                    s1_cg(t + 1, cg)

            load(0)
            load(1)
            for cg in range(4):
                s1_cg(0, cg)
            for t in range(NT):
                b = t % 2
                X = x12[b]
                rmsnorm_tile(X[:], r_x12[b], gcr, hn[0][:], r_hn[0], 0)
                nxt(t, 0)
                transpose16(hn[0], r_hn[0], hT[0], r_hT[0])
                for hm in range(4):
                    for c in range(NCH):
                        mm(B[5][:, hm * 128:(hm + 1) * 128], wq[:, c, hm * 128:(hm + 1) * 128], hT[0][:, c, :], c == 0, c == NCH - 1,
                           [r_w, r_hT[0]], [rB[5]])
                act(q2[:], B[5][:], AF.Square, [rB[5]], [r_q2])
                nxt(t, 1)
                mm(B[6][:], C.ones[:], q2[:], True, True, [r_q2, C.r], [rB[6]])
                K.rsq(rsq[:], B[6][:], 1.0 / HD, [rB[6]], [r_rsq])
                stt(qn[:].rearrange("p h n -> p (h n)"), B[5][:], C.gh[:, 4:5], rsq[:], ALU.mult, ALU.mult, [rB[5], r_rsq, C.r], [r_qn])
                nxt(t, 2)
                for mt in range(2):
                    bk = 7 if mt == 0 else 4
                    for hm in range(4):
                        mm(B[bk][:, hm * 128:(hm + 1) * 128], KmT[:, hm, mt * 128:(mt + 1) * 128], qn[:, hm, :], True, True,
                           [r_km, r_qn], [rB[bk]])
                    act(E2[mt][:].rearrange("p h n -> p (h n)"), B[bk][:], AF.Exp, [rB[bk]], [r_E2[mt]], scale=SCALE)
                nxt(t, 3)
                for hm in range(4):
                    bk = 5 if hm < 2 else 6
                    o0 = (hm % 2) * 130
                    for mt in range(2):
                        mm(B[bk][:, o0:o0 + 129], E2[mt][:, hm, :], Vm[:, mt, hm, 0:129], mt == 0, mt == 1, [r_E2[mt], r_km], [rB[bk]])
                for hm in range(4):
                    bk = 5 if hm < 2 else 6
                    o0 = (hm % 2) * 130
                    recip(rden[:, hm:hm + 1], B[bk][:, o0 + 128:o0 + 129], [rB[bk]], [r_rden])
                    ts(o2[:, hm, :], B[bk][:, o0:o0 + 128], rden[:, hm:hm + 1], None, ALU.mult, None, [rB[bk], r_rden], [r_o2])
                for hm in range(4):
                    tr(tpv[:, hm * 128:(hm + 1) * 128], o2[:, hm, :], [r_o2], [rB[4]])
                cp(o2T[:].rearrange("p h n -> p (h n)"), tpv[:, 0:512], [rB[4]], [r_o2T], eng="act")
                for cg in range(4):
                    pa = B[cg % 4]
                    ra = rB[cg % 4]
                    cs = slice(cg * 512, (cg + 1) * 512)
                    for hm in range(4):
                        mm(pa[:], o2T[:, hm, :], wo[:, hm, cs], hm == 0, hm == 3, [r_o2T, r_w], [ra])
                    tt(X[:, cs], pa[:], X[:, cs], ALU.add, [ra, r_x12[b]], [r_x12[b]])
                S.dma("sp", Sx.x2[t * 128:(t + 1) * 128, :], X[:], reads=[r_x12[b]], writes=[R.x2[t]])
                rmsnorm_tile(X[:], r_x12[b], gmo, hn[1][:], r_hn[1], 1)
                S.dma("sp", Sx.h3[t * 128:(t + 1) * 128, :], hn[1][:], reads=[r_hn[1]], writes=[R.h3[t]])
                if t + 2 < NT:
                    load(t + 2)
                transpose16(hn[1], r_hn[1], hT[1], r_hT[1])
                for c in range(NCH):
                    mm(B[5][:, 0:N_EXP], hT[1][:, c, :], wr[:, c, :], c == 0, c == NCH - 1, [r_hT[1], r_w], [rB[5]])
                act(ex[:], B[5][:, 0:N_EXP], AF.Exp, [rB[5]], [r_ex, r_sume], accum_out=sume[:])
                recip(sume[:], sume[:], [r_sume], [r_sume])
                ts(C.aff[:, t, :], ex[:], sume[:, 0:1], None, ALU.mult, None, [r_ex, r_sume], [C.r_aff])
            S.emit_phase()


N_BISECT = 34


def phase_d(K, top):
    nc, S, I, C, Sx, R, sb, ps = K.nc, K.S, K.I, K.C, K.Sx, K.R, K.sb, K.ps
    mm, tr, act, ts, tt, stt, recip, cp = K.mm, K.tr, K.act, K.ts, K.tt, K.stt, K.recip, K.cp
    with ExitStack() as st:
        lo = sb(st, "lo", [128, N_EXP], F32)
        hi = sb(st, "hi", [128, N_EXP], F32)
        mid = sb(st, "mid", [128, N_EXP], F32)
        ge = sb(st, "ge", [128, N_EXP], F32)
        dl = sb(st, "dl", [128, N_EXP], F32)
        cntp = sb(st, "cntp", [128, N_EXP], F32)
        cmp_ = sb(st, "cmp", [128, NT, N_EXP], F32)
        cnt = ps(st, "cnt", [128, 512], F32)
        r = Reg()
        r_cnt = Reg()
        S.op("dve", lambda e: e.memset(lo[:], 0.0), writes=[r])
        S.op("dve", lambda e: e.memset(hi[:], 1.0), writes=[r])

        def bc(t):
            return t[:, :].unsqueeze(1).to_broadcast([128, NT, N_EXP])
        for it in range(N_BISECT):
            tt(mid[:], lo[:], hi[:], ALU.add, [r], [r])
            ts(mid[:], mid[:], 0.5, None, ALU.mult, None, [r], [r])
            tt(cmp_[:], C.aff[:], bc(mid), ALU.is_gt, [r, C.r_aff], [r])
            S.op("dve", lambda e: e.tensor_reduce(out=cntp[:], in_=cmp_[:].rearrange("p t e -> p e t"), axis=AX.X, op=ALU.add),
                 reads=[r], writes=[r])
            mm(cnt[:, 0:N_EXP], C.onesf[:], cntp[:], True, True, [r, C.r], [r_cnt])
            ts(ge[:], cnt[:, 0:N_EXP], float(CAP), None, ALU.is_ge, None, [r_cnt], [r])
            tt(dl[:], mid[:], lo[:], ALU.subtract, [r], [r])
            tt(dl[:], dl[:], ge[:], ALU.mult, [r], [r])
            tt(lo[:], lo[:], dl[:], ALU.add, [r], [r])
            tt(dl[:], hi[:], mid[:], ALU.subtract, [r], [r])
            tt(dl[:], dl[:], ge[:], ALU.mult, [r], [r])
            tt(hi[:], mid[:], dl[:], ALU.add, [r], [r])
        tt(cmp_[:], C.aff[:], bc(lo), ALU.is_gt, [r, C.r_aff], [r])
        tt(cmp_[:], cmp_[:], C.aff[:], ALU.mult, [r, C.r_aff], [r])
        S.dma("sp", Sx.gm.rearrange("(t p) e -> p t e", p=128), cmp_[:], reads=[r], writes=[R.gm])
        S.emit_phase()


def phase_e(K, top):
    nc, S, I, C, Sx, R, sb, ps = K.nc, K.S, K.I, K.C, K.Sx, K.R, K.sb, K.ps
    mm, tr, act, ts, tt, stt, recip, cp = K.mm, K.tr, K.act, K.ts, K.tt, K.stt, K.recip, K.cp
    NOT = OWN // 128
    with ExitStack() as st0:
        own = sb(st0, "own", [128, NOT], I32)
        h3T = sb(st0, "h3T", [128, NCH, OWN], BF16)
        acc = sb(st0, "acc", [128, NOT, D], F32)
        gmo = sb(st0, "gmo_", [128, NOT, N_EXP], F32)
        r_own, r_h3T, r_gmo = Reg(const=True), Reg(const=True), Reg(const=True)
        r_acc = [Reg() for _ in range(NOT)]
        with ExitStack() as st:
            h3o = sb(st, "h3o", [128, NOT, D], BF16)
            r_h3o = [Reg() for _ in range(NOT)]
            tpb = [ps(st, "tpe%d" % i, [128, 512], F32) for i in range(2)]
            r_tpb = [Reg() for _ in range(2)]
            S.dma("sp", own[:], I.own, writes=[r_own])
            for j in range(NOT):
                off = bass.IndirectOffsetOnAxis(ap=own[:, j:j + 1], axis=0)
                S.dma_fn("pool", lambda e, j=j, off=off: e.indirect_dma_start(out=h3o[:, j, :], out_offset=None, in_=Sx.h3, in_offset=off),
                         reads=[r_own] + R.h3, writes=[r_h3o[j]])
                S.dma_fn("pool", lambda e, j=j, off=off: e.indirect_dma_start(out=acc[:, j, :], out_offset=None, in_=Sx.x2, in_offset=off),
                         reads=[r_own] + R.x2, writes=[r_acc[j]])
                S.dma_fn("pool", lambda e, j=j, off=off: e.indirect_dma_start(out=gmo[:, j, :], out_offset=None, in_=Sx.gm, in_offset=off),
                         reads=[r_own, R.gm], writes=[r_gmo])
            k = 0
            for j in range(NOT):
                for hh in range(2):
                    pb = k % 2
                    k += 1
                    tpv = tpb[pb][:].bitcast(BF16)
                    for c in range(8):
                        cc = hh * 8 + c
                        tr(tpv[:, c * 128:(c + 1) * 128], h3o[:, j, cc * 128:(cc + 1) * 128], [r_h3o[j]], [r_tpb[pb]])
                    cp(h3T[:, hh * 8:hh * 8 + 8, j * 128:(j + 1) * 128], tpv.rearrange("p (c n) -> p c n", c=8), [r_tpb[pb]], [r_h3T],
                       eng=("act" if hh == 0 else "dve"))
            S.emit_phase()
        with ExitStack() as st:
            FP = 256
            NFP = D // FP
            wg = [sb(st, "wg%d" % i, [128, NCH, FP], BF16) for i in range(2)]
            wu = [sb(st, "wu%d" % i, [128, NCH, FP], BF16) for i in range(2)]
            wd = [sb(st, "wd%d" % i, [128, NCH, FP], BF16) for i in range(2)]
            r_wg = [Reg() for _ in range(2)]
            r_wu = [Reg() for _ in range(2)]
            r_wd = [Reg() for _ in range(2)]
            actT = sb(st, "actT", [128, NCH, OWN], BF16)
            r_actT = [Reg() for _ in range(NCH)]
            sg = [sb(st, "sg%d" % i, [128, 512], F32) for i in range(2)]
            r_sg = [Reg() for _ in range(2)]
            Gp = [ps(st, "Gp%d" % i, [128, 512], F32) for i in range(2)]
            Up = [ps(st, "Up%d" % i, [128, 512], F32) for i in range(2)]
            Yp = [ps(st, "Yp%d" % i, [128, 512], F32) for i in range(3)]
            r_Gp = [Reg() for _ in range(2)]
            r_Up = [Reg() for _ in range(2)]
            r_Yp = [Reg() for _ in range(3)]
            wi = 0
            di = 0
            gi = 0
            yi = 0
            for ex in range(N_EXP):
                wgv = I.w_gate[ex].rearrange("(c p) f -> p c f", p=128)
                wuv = I.w_up[ex].rearrange("(c p) f -> p c f", p=128)
                wdv = I.w_down[ex].rearrange("(c p) f -> p c f", p=128)
                for fp in range(NFP):
                    wb = wi % 2
                    wi += 1
                    S.dma("pool", wg[wb][:], wgv[:, :, fp * FP:(fp + 1) * FP], writes=[r_wg[wb]])
                    S.dma("pool", wu[wb][:], wuv[:, :, fp * FP:(fp + 1) * FP], writes=[r_wu[wb]])
                    for f2 in range(FP // 128):
                        fc = fp * (FP // 128) + f2
                        for half in range(2):
                            gb = gi % 2
                            gi += 1
                            cs = slice(half * 512, (half + 1) * 512)
                            for c in range(NCH):
                                mm(Gp[gb][:], wg[wb][:, c, f2 * 128:(f2 + 1) * 128], h3T[:, c, cs], c == 0, c == NCH - 1,
                                   [r_wg[wb], r_h3T], [r_Gp[gb]])
                            for c in range(NCH):
                                mm(Up[gb][:], wu[wb][:, c, f2 * 128:(f2 + 1) * 128], h3T[:, c, cs], c == 0, c == NCH - 1,
                                   [r_wu[wb], r_h3T], [r_Up[gb]])
                            act(sg[gb][:], Gp[gb][:], AF.Silu, [r_Gp[gb]], [r_sg[gb]])
                            tt(actT[:, fc, cs], sg[gb][:], Up[gb][:], ALU.mult, [r_sg[gb], r_Up[gb]], [r_actT[fc]])
                for dp in range(NFP):
                    db = di % 2
                    di += 1
                    S.dma("pool", wd[db][:], wdv[:, :, dp * FP:(dp + 1) * FP], writes=[r_wd[db]])
                    for j in range(NOT):
                        yb = yi % 3
                        yi += 1
                        for fc in range(NCH):
                            mm(Yp[yb][:, 0:FP], actT[:, fc, j * 128:(j + 1) * 128], wd[db][:, fc, :], fc == 0, fc == NCH - 1,
                               [r_actT[fc], r_wd[db]], [r_Yp[yb]])
                        stt(acc[:, j, dp * FP:(dp + 1) * FP], Yp[yb][:, 0:FP], gmo[:, j, ex:ex + 1], acc[:, j, dp * FP:(dp + 1) * FP],
                            ALU.mult, ALU.add, [r_Yp[yb], r_gmo, r_acc[j]], [r_acc[j]])
            outs = []
            for j in range(NOT):
                outs.append(S.dma("sp", K.out[j * 128:(j + 1) * 128, :], acc[:, j, :], reads=[r_acc[j]], writes=[]))
            S.finish(outs)
            S.emit_phase()


_NC_CACHE = {}


def _core_inputs(inp, c, consts):
    b, q = c // 4, c % 4
    m = {}
    m["x"] = np.ascontiguousarray(inp["x"][b], dtype=np.float32)
    m["mem"] = np.ascontiguousarray(inp["mem"][b], dtype=np.float32)
    m["positions"] = np.ascontiguousarray(inp["positions"][b]).astype(np.int32)
    own = (q * OWN + np.arange(OWN)).reshape(OWN // 128, 128).T.astype(np.int32)
    m["own_idx"] = np.ascontiguousarray(own)
    for n in ("g_mix", "g_cross", "g_mem", "g_moe", "g_qa", "g_ka", "g_qb", "g_kb", "g_qm", "g_km", "g_oa", "g_ob",
              "sink_b", "w_in", "w_out", "w_q_mem", "w_kv_mem", "w_o_mem", "w_router", "w_gate", "w_up", "w_down"):
        m[n] = np.ascontiguousarray(np.asarray(inp[n])[0], dtype=np.float32)
    m.update(consts)
    return m


def kernel(**inputs):
    inp = {k: np.asarray(v) for k, v in inputs.items()}
    if "nc" not in _NC_CACHE:
        _NC_CACHE["nc"] = build()
    nc = _NC_CACHE["nc"]
    consts = host_consts()
    in_maps = [_core_inputs(inp, c, consts) for c in range(8)]
    res = run_bass_kernel_spmd(nc, in_maps, core_ids=list(range(8)))
    out = np.zeros((2, SEQ, D), np.float32)
    for c in range(8):
        b, q = c // 4, c % 4
        out[b, q * OWN:(q + 1) * OWN] = np.asarray(res.results[c]["out"], dtype=np.float32)
    return out


MOE_FP = 512
MOE_NW = 6


def moe_ring_setup(K, stack):
    S, I, sb = K.S, K.I, K.sb
    FP, NW = MOE_FP, MOE_NW
    NFP = D // FP
    PPE = 3 * NFP
    K.Wb = [sb(stack, "Wb%d" % i, [128, NCH, FP], BF16) for i in range(NW)]
    K.r_W = [Reg() for _ in range(NW)]
    ring = {"next": 0}

    def piece_src(p):
        ex, k = divmod(p, PPE)
        if k < 2 * NFP:
            fp, gu = divmod(k, 2)
            w = I.w_gate if gu == 0 else I.w_up
            return w[ex].rearrange("(c p) f -> p c f", p=128)[:, :, fp * FP:(fp + 1) * FP]
        dp = k - 2 * NFP
        return I.w_down[ex].rearrange("(c p) f -> p c f", p=128)[:, :, dp * FP:(dp + 1) * FP]

    def ensure(upto):
        last = N_EXP * PPE - 1
        while ring["next"] <= min(upto, last):
            p = ring["next"]
            ring["next"] += 1
            S.dma("pool", K.Wb[p % NW][:], piece_src(p), writes=[K.r_W[p % NW]])
    K.ring_ensure = ensure
    ensure(NW - 1)


def phase_e2(K, top):
    nc, S, I, C, Sx, R, sb, ps = K.nc, K.S, K.I, K.C, K.Sx, K.R, K.sb, K.ps
    mm, tr, act, ts, tt, stt, recip, cp = K.mm, K.tr, K.act, K.ts, K.tt, K.stt, K.recip, K.cp
    NOT = OWN // 128
    NS = CAP // 128
    NTG = 3 + N_EXP
    with ExitStack() as st0:
        own = sb(st0, "own", [128, NOT], I32)
        gmo = sb(st0, "gmo_", [128, NOT, N_EXP], F32)
        mall = sb(st0, "mall", [128, NOT, N_EXP], F32)
        pos = sb(st0, "pos", [128, NOT, N_EXP], F32)
        TG = sb(st0, "TG", [128, NOT, NTG], F32)
        iota = sb(st0, "iota", [128, 512], F32)
        dummy = sb(st0, "dummy", [128, NS], F32)
        r_c = Reg(const=True)
        with ExitStack() as st:
            ut = sb(st, "ut", [128, 128], F32)
            loc = sb(st, "loc", [128, NOT], F32)
            offs = sb(st, "offs", [128, NOT, N_EXP], F32)
            x2b = [sb(st, "x2b%d" % i, [128, D], F32) for i in range(2)]
            r_x2b = [Reg() for _ in range(2)]
            pc = [ps(st, "pc%d" % i, [128, 512], F32) for i in range(2)]
            r_pc = [Reg() for _ in range(2)]
            r1 = Reg()
            S.dma("sp", own[:], I.own, writes=[r_c])
            S.dma("sp", ut[:], I.c_ut, writes=[r1])
            S.dma("sp", loc[:], I.c_loc, writes=[r1])
            S.dma("sp", iota[:], I.c_iota, writes=[r_c])
            S.dma("sp", dummy[:], I.c_dummy, writes=[r_c])
            for j in range(NOT):
                off = bass.IndirectOffsetOnAxis(ap=own[:, j:j + 1], axis=0)
                S.dma_fn("pool", lambda e, j=j, off=off: e.indirect_dma_start(out=gmo[:, j, :], out_offset=None, in_=Sx.gm, in_offset=off),
                         reads=[r_c, R.gm], writes=[r1])
                b = j % 2
                S.dma_fn("pool", lambda e, b=b, off=off: e.indirect_dma_start(out=x2b[b][:], out_offset=None, in_=Sx.x2, in_offset=off),
                         reads=[r_c] + R.x2, writes=[r_x2b[b]])
                S.dma("sp", Sx.accd[j * 128:(j + 1) * 128, :], x2b[b][:], reads=[r_x2b[b]], writes=[R.accd])
            ts(mall[:], gmo[:], 0.0, None, ALU.is_gt, None, [r1], [r1])
            flat = mall[:].rearrange("p j e -> p (j e)")
            mm(pc[0][:, 0:NOT * N_EXP], ut[:], flat, True, True, [r1], [r_pc[0]])
            mm(pc[1][:, 0:NOT * N_EXP], C.onesf[:], flat, True, True, [r1, C.r], [r_pc[1]])
            tot = pc[1][:, 0:NOT * N_EXP].rearrange("p (j e) -> p j e", e=N_EXP)
            cum = pc[0][:, 0:NOT * N_EXP].rearrange("p (j e) -> p j e", e=N_EXP)
            S.op("dve", lambda e: e.memset(offs[:, 0, :], 0.0), writes=[r1])
            for j in range(1, NOT):
                tt(offs[:, j, :], offs[:, j - 1, :], tot[:, j - 1, :], ALU.add, [r1, r_pc[1]], [r1])
            tt(pos[:], cum, offs[:], ALU.add, [r_pc[0], r1], [r1])
            ts(pos[:], pos[:], -1.0, None, ALU.add, None, [r1], [r1])
            cp(TG[:, :, 0], own[:], [r_c], [r1])
            cp(TG[:, :, 1], loc[:], [r1], [r1])
            S.op("dve", lambda e: e.memset(TG[:, :, 2], 1.0), writes=[r1])
            cp(TG[:, :, 3:NTG], gmo[:], [r1], [r1])
            S.emit_phase()
        with ExitStack() as st:
            FP, NFP, NW, PPE = MOE_FP, D // MOE_FP, MOE_NW, 3 * (D // MOE_FP)
            Wb, r_W, ensure = K.Wb, K.r_W, K.ring_ensure
            Sel = [sb(st, "Sel%d" % i, [128, NOT, 128], F32) for i in range(2)]
            r_Sel = [Reg() for _ in range(2)]
            sis = sb(st, "sis", [128, NS, 32], F32)
            sidf = sb(st, "sidf", [128, NS], F32)
            gi = [sb(st, "gi%d" % i, [128, NS], I32) for i in range(2)]
            si = [sb(st, "si%d" % i, [128, NS], I32) for i in range(2)]
            gs = [sb(st, "gs%d" % i, [128, NS], F32) for i in range(2)]
            r_sis = Reg()
            r_idx = [Reg() for _ in range(2)]
            xe = sb(st, "xe", [128, NS, D], BF16)
            r_xe = [Reg() for _ in range(NS)]
            xeT1 = sb(st, "xeT", [128, NCH, CAP], BF16)
            xeT = [xeT1, xeT1]
            r_xeT1 = Reg()
            r_xeT = [r_xeT1, r_xeT1]
            actT = sb(st, "actT", [128, NCH, CAP], BF16)
            r_actT = [Reg() for _ in range(NCH)]
            ygs = sb(st, "ygs", [128, NS, D], F32)
            r_ygs = [Reg() for _ in range(NS)]
            sg = [sb(st, "sg%d" % i, [128, 512], F32) for i in range(2)]
            r_sg = [Reg() for _ in range(2)]
            cnt_stg = {"n": 0}

            def wload(dst, r_dst, src):
                cnt_stg["n"] += 1
                S.dma("pool", dst, src, writes=[r_dst])
            Gp = [ps(st, "Gp%d" % i, [128, 512], F32) for i in range(2)]
            Up = [ps(st, "Up%d" % i, [128, 512], F32) for i in range(2)]
            Yp = [ps(st, "Yp%d" % i, [128, 512], F32) for i in range(2)]
            Tq = ps(st, "Tq", [128, 512], F32)
            SIp = ps(st, "SIp", [128, 512], F32)
            r_Gp = [Reg() for _ in range(2)]
            r_Up = [Reg() for _ in range(2)]
            r_Yp = [Reg() for _ in range(2)]
            r_Tq, r_SIp = Reg(), Reg()
            tqv = Tq[:].bitcast(BF16)
            cnt = {"w": 0, "d": 0, "g": 0, "y": 0}

            def prep_idx(ex):
                pb = ex % 2
                for s4 in range(NS):
                    sl = s4 % 2
                    for j in range(NOT):
                        ts(Sel[sl][:, j, :], iota[:, s4 * 128:(s4 + 1) * 128], pos[:, j, ex:ex + 1], mall[:, j, ex:ex + 1],
                           ALU.is_equal, ALU.mult, [r_c], [r_Sel[sl]])
                    for j in range(NOT):
                        mm(SIp[:, s4 * 32:s4 * 32 + NTG], Sel[sl][:, j, :], TG[:, j, :], j == 0, j == NOT - 1,
                           [r_Sel[sl], r_c], [r_SIp])
                cp(sis[:], SIp[:, 0:NS * 32].rearrange("p (s k) -> p s k", k=32), [r_SIp], [r_sis])
                cp(gi[pb][:], sis[:, :, 0], [r_sis], [r_idx[pb]])
                tt(sidf[:], sis[:, :, 2], dummy[:], ALU.mult, [r_sis, r_c], [r_sis])
                tt(sidf[:], dummy[:], sidf[:], ALU.subtract, [r_sis, r_c], [r_sis])
                tt(sidf[:], sidf[:], sis[:, :, 1], ALU.add, [r_sis], [r_sis])
                cp(si[pb][:], sidf[:], [r_sis], [r_idx[pb]])
                cp(gs[pb][:], sis[:, :, 3 + ex], [r_sis], [r_idx[pb]])
                for s4 in range(NS):
                    off = bass.IndirectOffsetOnAxis(ap=gi[pb][:, s4:s4 + 1], axis=0)
                    S.dma_fn("pool", lambda e, s4=s4, off=off: e.indirect_dma_start(out=xe[:, s4, :], out_offset=None, in_=Sx.h3, in_offset=off),
                             reads=[r_idx[pb]] + R.h3, writes=[r_xe[s4]])

            def prep_T(ex):
                pb = ex % 2
                k = 0
                for s4 in range(NS):
                    for hh in range(4):
                        for c in range(4):
                            cc = hh * 4 + c
                            tr(tqv[:, c * 128:(c + 1) * 128], xe[:, s4, cc * 128:(cc + 1) * 128], [r_xe[s4]], [r_Tq])
                        cp(xeT[pb][:, hh * 4:hh * 4 + 4, s4 * 128:(s4 + 1) * 128], tqv[:, 0:512].rearrange("p (c n) -> p c n", c=4),
                           [r_Tq], [r_xeT[pb]], eng=("act" if k % 2 == 0 else "dve"))
                        k += 1

            def compute_gu(ex, fp):
                pb = ex % 2
                pg = ex * PPE + 2 * fp
                wgt, wut = Wb[pg % NW], Wb[(pg + 1) % NW]
                rg, ru = r_W[pg % NW], r_W[(pg + 1) % NW]
                for f2 in range(FP // 128):
                    fc = fp * (FP // 128) + f2
                    gb = cnt["g"] % 2
                    cnt["g"] += 1
                    for c in range(NCH):
                        mm(Gp[gb][:], wgt[:, c, f2 * 128:(f2 + 1) * 128], xeT[pb][:, c, :], c == 0, c == NCH - 1,
                           [rg, r_xeT[pb]], [r_Gp[gb]])
                    for c in range(NCH):
                        mm(Up[gb][:], wut[:, c, f2 * 128:(f2 + 1) * 128], xeT[pb][:, c, :], c == 0, c == NCH - 1,
                           [ru, r_xeT[pb]], [r_Up[gb]])
                    act(sg[gb][:], Gp[gb][:], AF.Silu, [r_Gp[gb]], [r_sg[gb]])
                    tt(actT[:, fc, :], sg[gb][:], Up[gb][:], ALU.mult, [r_sg[gb], r_Up[gb]], [r_actT[fc]])
                ensure(pg + 1 + NW)

            def compute_d(ex, dp):
                pb = ex % 2
                pd = ex * PPE + 2 * NFP + dp
                wdt, rd_ = Wb[pd % NW], r_W[pd % NW]
                for s4 in range(NS):
                    yb = cnt["y"] % 2
                    cnt["y"] += 1
                    for fc in range(NCH):
                        mm(Yp[yb][:, 0:FP], actT[:, fc, s4 * 128:(s4 + 1) * 128], wdt[:, fc, :], fc == 0, fc == NCH - 1,
                           [r_actT[fc], rd_], [r_Yp[yb]])
                    if s4 % 2 == 0:
                        ts(ygs[:, s4, dp * FP:(dp + 1) * FP], Yp[yb][:, 0:FP], gs[pb][:, s4:s4 + 1], None, ALU.mult, None,
                           [r_Yp[yb], r_idx[pb]], [r_ygs[s4]])
                    else:
                        act(ygs[:, s4, dp * FP:(dp + 1) * FP], Yp[yb][:, 0:FP], AF.Copy, [r_Yp[yb], r_idx[pb]], [r_ygs[s4]],
                            scale=gs[pb][:, s4:s4 + 1])
                ensure(pd + NW)

            sc_prev = {"ops": []}

            def scatter(ex):
                pb = ex % 2
                extra = list(sc_prev["ops"])
                if R.accd.last_w is not None:
                    extra.append(R.accd.last_w)
                ops = []
                for s4 in range(NS):
                    off = bass.IndirectOffsetOnAxis(ap=si[pb][:, s4:s4 + 1], axis=0)
                    ops.append(S.dma_fn("pool", lambda e, s4=s4, off=off: e.indirect_dma_start(out=Sx.accd, out_offset=off, in_=ygs[:, s4, :],
                                                                                             in_offset=None, compute_op=ALU.add),
                                        reads=[r_idx[pb], r_ygs[s4]], writes=[], extra=extra))
                sc_prev["ops"] = ops

            assert NFP == 4
            prep_idx(0)
            prep_T(0)
            ensure(NW - 1)
            for ex in range(N_EXP):
                for fp in range(NFP):
                    compute_gu(ex, fp)
                if ex + 1 < N_EXP:
                    prep_idx(ex + 1)
                for dp in range(NFP):
                    compute_d(ex, dp)
                    if dp == 1 and ex + 1 < N_EXP:
                        prep_T(ex + 1)
                scatter(ex)
            fin = S.dma("sp", K.out, Sx.accd[0:OWN, :], reads=[R.accd], writes=[], extra=sc_prev["ops"])
            S.finish([fin])
            S.emit_phase()
```

```python
import numpy as np
from contextlib import ExitStack
import concourse.bass as bass
import concourse.mybir as mybir
from concourse.bass_utils import run_bass_kernel_spmd

F32 = mybir.dt.float32
BF16 = mybir.dt.bfloat16
I32 = mybir.dt.int32
AF = mybir.ActivationFunctionType
ALU = mybir.AluOpType
AX = mybir.AxisListType

SAME_ENGINE_SYNC = True
MOE_COMPACT = True


class Reg:
    __slots__ = ("name", "last_w", "readers", "const")

    def __init__(self, name="", const=False):
        self.name = name
        self.last_w = None
        self.readers = []
        self.const = const


class Op:
    __slots__ = ("eng", "fn", "deps", "needed", "sig", "is_dma", "waits_only")

    def __init__(self, eng, fn, deps, is_dma=False):
        self.eng = eng
        self.fn = fn
        self.deps = deps
        self.needed = False
        self.sig = None
        self.is_dma = is_dma
        self.waits_only = False


class Sched:
    ENGS = ("pe", "act", "dve", "pool", "sp")
    NDMA = 12

    def __init__(self, nc, stack):
        self.nc = nc
        self.sem = {e: stack.enter_context(nc.semaphore("s_" + e)) for e in self.ENGS}
        self.cnt = {e: 0 for e in self.ENGS}
        self.dsem = {q: [stack.enter_context(nc.semaphore("d_%s%d" % (q, i))) for i in range(self.NDMA)]
                     for q in ("sp", "pool", "act")}
        self.dcnt = {q: 0 for q in ("sp", "pool", "act")}
        self.dlast = {q: [None] * self.NDMA for q in ("sp", "pool", "act")}
        self.ops = {e: [] for e in self.ENGS}
        self.known = {e: {} for e in self.ENGS}
        self.last_op = {e: None for e in self.ENGS}
        self.prev_phase_last = []

    def _mk(self, eng, fn, reads, writes, is_dma=False, extra=()):
        deps = list(extra)
        for r in reads:
            if r.last_w is not None:
                deps.append(r.last_w)
        for w in writes:
            if w.last_w is not None:
                deps.append(w.last_w)
            deps.extend(w.readers)
        op = Op(eng, fn, deps, is_dma)
        for r in reads:
            if not r.const:
                r.readers.append(op)
        for w in writes:
            w.last_w = op
            w.readers = []
        self.ops[eng].append(op)
        return op

    def op(self, eng, fn, reads=(), writes=()):
        return self._mk(eng, fn, reads, writes)

    def dma(self, q, out, in_, reads=(), writes=(), extra=(), **kw):
        return self._mk(q, lambda e: e.dma_start(out=out, in_=in_, **kw), reads, writes, is_dma=True, extra=extra)

    def dma_fn(self, q, fn, reads=(), writes=(), extra=()):
        return self._mk(q, fn, reads, writes, is_dma=True, extra=extra)

    def finish(self, ops):
        o = Op("sp", None, list(ops))
        o.waits_only = True
        self.ops["sp"].append(o)

    def emit_phase(self):
        self.nphase = getattr(self, "nphase", 0) + 1
        with self.nc.named_scope("ph%02d" % self.nphase):
            self._emit_phase()

    def _emit_phase(self):
        nc = self.nc
        engs = {"pe": nc.tensor, "act": nc.scalar, "dve": nc.vector, "pool": nc.gpsimd, "sp": nc.sync}
        barrier = list(self.prev_phase_last)
        for e in self.ENGS:
            for op in self.ops[e]:
                for d in op.deps:
                    if d.eng == op.eng and not d.is_dma:
                        if e in ("pe", "sp") or not SAME_ENGINE_SYNC:
                            continue
                    d.needed = True
        lasts = []
        for e in self.ENGS:
            real = [o for o in self.ops[e] if not o.waits_only]
            if real:
                real[-1].needed = True
                lasts.append(real[-1])
        for e in self.ENGS:
            for op in self.ops[e]:
                if op.waits_only:
                    continue
                if op.is_dma:
                    q = e
                    j = self.dcnt[q]
                    self.dcnt[q] += 1
                    op.sig = (self.dsem[q][j % self.NDMA], 16 * (j // self.NDMA + 1), q, j)
                elif op.needed:
                    self.cnt[e] += 1
                    op.sig = (self.sem[e], self.cnt[e])
        dma_lasts = []
        with nc.Block() as block:
            def run(e):
                def body(eng):
                    known = self.known[e]

                    def wait(sig):
                        s, v = sig[0], sig[1]
                        k = id(s)
                        if known.get(k, 0) < v:
                            eng.wait_ge(s, v)
                            known[k] = v
                    for d in barrier:
                        if d.eng != e or d.is_dma:
                            wait(d.sig)
                    for op in self.ops[e]:
                        for d in op.deps:
                            if d.sig is None:
                                continue
                            if d.eng == e and not d.is_dma:
                                if e in ("pe", "sp") or not SAME_ENGINE_SYNC:
                                    continue
                            wait(d.sig)
                        if op.waits_only:
                            continue
                        if op.is_dma:
                            s, v, q, j = op.sig
                            if j >= self.NDMA:
                                wait((s, v - 16))
                            ins = op.fn(eng)
                            ins.then_inc(s, 16)
                        else:
                            ins = op.fn(eng)
                            if op.sig is not None:
                                ins.then_inc(op.sig[0], 1)
                return body
            block.tensor(run("pe"))
            block.scalar(run("act"))
            block.vector(run("dve"))
            block.gpsimd(run("pool"))
            block.sync(run("sp"))
        for q in ("sp", "pool", "act"):
            seen = {}
            for op in self.ops[q]:
                if op.is_dma:
                    seen[id(op.sig[0])] = op
            dma_lasts.extend(seen.values())
        self.prev_phase_last = lasts + dma_lasts + [d for d in self.prev_phase_last if d.is_dma and id(d.sig[0]) not in {id(x.sig[0]) for x in dma_lasts}]
        self.ops = {e: [] for e in self.ENGS}


D = 2048
SEQ = 4096
HD = 128
D_IN = 4608
NCH = D // 128
NT = SEQ // 128
EPS = 1e-6
N_EXP = 16
CAP = 512
OWN = 1024
TWO_PI = float(2.0 * np.pi)
SCALE = float(1.0 / np.sqrt(HD))
HC_COL = [128 * i for i in range(8)] + [1024 + 128 * i for i in range(8)] + \
         [3072 + 128 * i for i in range(8)] + [4096, 4224]
HC_FAM = [0] * 8 + [1] * 8 + [2] * 8 + [3] * 2
V_COLS = [(2048, 512), (2560, 512), (4352, 256)]


def mask_a_np():
    kp = np.arange(128)[:, None]
    qf = np.arange(512)[None, :]
    out = np.zeros((20, 128, 512), np.float32)
    for j in range(20):
        o = (-1024 + 128 * j) + kp - qf
        a = np.abs(o)
        out[j] = (a <= 64).astype(np.float32) + ((o % 4 == 0) & (a <= 256)) + ((o % 16 == 0) & (a <= 1024))
    return out


def mask_b_np():
    kp = np.arange(128)[:, None]
    qf = np.arange(512)[None, :]
    out = np.zeros((6, 128, 512), np.float32)
    for j in range(6):
        o = (-128 + 128 * j) + kp - qf
        out[j] = (np.abs(o) <= 128)
    return out


def host_consts():
    import ml_dtypes
    bf = ml_dtypes.bfloat16
    c = {}
    c["c_ident"] = np.eye(128, dtype=np.float32).astype(bf)
    c["c_identf"] = np.eye(128, dtype=np.float32)
    c["c_ones"] = np.ones((128, 128), np.float32).astype(bf)
    c["c_onesf"] = np.ones((128, 128), np.float32)
    p0 = np.zeros((128, 128), np.float32)
    for m in range(64):
        p0[m + 64, m] = -1.0
    for m in range(64, 128):
        p0[m - 64, m] = 1.0
    c["c_rot"] = p0
    i = np.arange(128) % 64
    c["c_invf"] = (10000.0 ** (-(2.0 * i) / 128.0)).astype(np.float32).reshape(128, 1)
    pp = np.arange(128)
    c["c_ut"] = (pp[:, None] <= pp[None, :]).astype(np.float32)
    c["c_iota"] = np.tile(np.arange(512, dtype=np.float32)[None, :], (128, 1))
    c["c_loc"] = (np.arange(8)[None, :] * 128 + pp[:, None]).astype(np.float32)
    c["c_dummy"] = (1024 + np.arange(4)[None, :] * 128 + pp[:, None]).astype(np.float32)
    def lnm(m):
        out = np.full(m.shape, -3000.0, np.float32)
        nz = m > 0
        out[nz] = np.log(m[nz]) / SCALE
        return out
    c["c_maska"] = np.ascontiguousarray(lnm(mask_a_np()).transpose(1, 0, 2)).astype(bf)
    c["c_maskb"] = np.ascontiguousarray(lnm(mask_b_np()).transpose(1, 0, 2)).astype(bf)
    return c


class Ctx:
    pass


def build(stop_after=99, debug=False):
    nc = bass.Bass("TRN2", target_bir_lowering=False)
    K = Ctx()
    K.nc = nc

    def din(name, shape, dt):
        return nc.dram_tensor(name, list(shape), dt, kind="ExternalInput").ap()

    def dscr(name, shape, dt):
        kind = "ExternalOutput" if debug else "Internal"
        return nc.dram_tensor(name, list(shape), dt, kind=kind).ap()

    I = Ctx()
    I.x = din("x", [SEQ, D], F32)
    I.mem = din("mem", [256, D], F32)
    I.pos = din("positions", [SEQ], I32)
    I.own = din("own_idx", [128, OWN // 128], I32)
    for n in ("g_mix", "g_cross", "g_mem", "g_moe"):
        setattr(I, n, din(n, [D], F32))
    for n in ("g_qa", "g_ka", "g_qb", "g_kb", "g_qm", "g_km"):
        setattr(I, n, din(n, [HD], F32))
    I.sink = din("sink_b", [8], F32)
    I.g_oa = din("g_oa", [1024], F32)
    I.g_ob = din("g_ob", [1024], F32)
    I.w_in = din("w_in", [D, D_IN], F32)
    I.w_out = din("w_out", [D, D], F32)
    I.w_q_mem = din("w_q_mem", [D, 512], F32)
    I.w_kv_mem = din("w_kv_mem", [D, 1024], F32)
    I.w_o_mem = din("w_o_mem", [512, D], F32)
    I.w_router = din("w_router", [D, N_EXP], F32)
    I.w_gate = din("w_gate", [N_EXP, D, D], F32)
    I.w_up = din("w_up", [N_EXP, D, D], F32)
    I.w_down = din("w_down", [N_EXP, D, D], F32)
    I.c_ident = din("c_ident", [128, 128], BF16)
    I.c_identf = din("c_identf", [128, 128], F32)
    I.c_ones = din("c_ones", [128, 128], BF16)
    I.c_onesf = din("c_onesf", [128, 128], F32)
    I.c_rot = din("c_rot", [128, 128], F32)
    I.c_invf = din("c_invf", [128, 1], F32)
    I.c_ut = din("c_ut", [128, 128], F32)
    I.c_iota = din("c_iota", [128, 512], F32)
    I.c_loc = din("c_loc", [128, 8], F32)
    I.c_dummy = din("c_dummy", [128, 4], F32)
    I.c_maska = din("c_maska", [128, 20, 512], BF16)
    I.c_maskb = din("c_maskb", [128, 6, 512], BF16)
    K.I = I
    out = nc.dram_tensor("out", [OWN, D], F32, kind="ExternalOutput").ap()
    K.out = out

    Sx = Ctx()
    Sx.qkt = dscr("s_qkt", [26, 128, SEQ], BF16)
    Sx.v = dscr("s_v", [SEQ, 1280], BF16)
    Sx.ot = dscr("s_ot", [16, 128, SEQ], BF16)
    Sx.x2 = dscr("s_x2", [SEQ, D], F32)
    Sx.h3 = dscr("s_h3", [SEQ, D], BF16)
    Sx.gm = dscr("s_gm", [SEQ, N_EXP], F32)
    Sx.accd = dscr("s_accd", [OWN + CAP, D], F32)
    K.Sx = Sx
    R = Ctx()
    R.qkt = [[Reg("qkt%d_%d" % (h, f)) for f in range(2)] for h in range(26)]
    R.v = [Reg("v%d" % t) for t in range(NT)]
    R.ot = [Reg("ot%d" % h) for h in range(16)]
    R.x2 = [Reg("x2_%d" % t) for t in range(NT)]
    R.h3 = [Reg("h3_%d" % t) for t in range(NT)]
    R.gm = Reg("gm")
    R.accd = Reg("accd")
    K.R = R

    with ExitStack() as top:
        S = Sched(nc, top)
        K.S = S

        uid = [0]

        def sb(stack, name, shape, dt):
            uid[0] += 1
            return stack.enter_context(nc.sbuf_tensor("%s_%d" % (name, uid[0]), list(shape), dt))

        def ps(stack, name, shape, dt):
            uid[0] += 1
            return stack.enter_context(nc.psum_tensor("%s_%d" % (name, uid[0]), list(shape), dt))
        K.sb, K.ps = sb, ps

        def mm(out, lhsT, rhs, start, stop, reads, writes):
            return S.op("pe", lambda e: e.matmul(out, lhsT=lhsT, rhs=rhs, start=start, stop=stop), reads, writes)

        def tr(out, in_, reads, writes, ident=None):
            idn = C.ident[:] if ident is None else ident
            return S.op("pe", lambda e: e.transpose(out=out, in_=in_, identity=idn), list(reads) + [C.r], writes)

        def act(out, in_, func, reads, writes, **kw):
            return S.op("act", lambda e: e.activation(out=out, in_=in_, func=func, **kw), reads, writes)

        def ts(out, in0, s1, s2, op0, op1, reads, writes, eng="dve", **kw):
            if op1 is None:
                return S.op(eng, lambda e: e.tensor_scalar(out=out, in0=in0, scalar1=s1, scalar2=None, op0=op0, **kw), reads, writes)
            return S.op(eng, lambda e: e.tensor_scalar(out=out, in0=in0, scalar1=s1, scalar2=s2, op0=op0, op1=op1, **kw), reads, writes)

        def tt(out, in0, in1, op, reads, writes, eng="dve"):
            return S.op(eng, lambda e: e.tensor_tensor(out=out, in0=in0, in1=in1, op=op), reads, writes)

        def stt(out, in0, scalar, in1, op0, op1, reads, writes, eng="dve"):
            return S.op(eng, lambda e: e.scalar_tensor_tensor(out=out, in0=in0, scalar=scalar, in1=in1, op0=op0, op1=op1), reads, writes)

        def rsq(out, in_, scale, reads, writes):
            S.op("dve", lambda e: e.tensor_scalar(out=out, in0=in_, scalar1=scale, scalar2=EPS, op0=ALU.mult, op1=ALU.add), reads, writes)
            S.op("act", lambda e: e.activation(out=out, in_=out, func=AF.Ln), writes, writes)
            return S.op("act", lambda e: e.activation(out=out, in_=out, func=AF.Exp, scale=-0.5), writes, writes)
        K.rsq = rsq

        def recip(out, in_, reads, writes):
            return S.op("dve", lambda e: e.reciprocal(out=out, in_=in_), reads, writes)

        def cp(out, in_, reads, writes, eng="dve"):
            if eng == "act":
                return S.op("act", lambda e: e.copy(out=out, in_=in_), reads, writes)
            return S.op(eng, lambda e: e.tensor_copy(out=out, in_=in_), reads, writes)
        K.mm, K.tr, K.act, K.ts, K.tt, K.stt, K.recip, K.cp = mm, tr, act, ts, tt, stt, recip, cp

        C = Ctx()
        K.C = C
        C.ident = sb(top, "ident", [128, 128], BF16)
        C.identf = sb(top, "identf", [128, 128], F32)
        C.ones = sb(top, "ones", [128, 128], BF16)
        C.onesf = sb(top, "onesf", [128, 128], F32)
        C.rot0 = sb(top, "rot0", [128, 128], F32)
        C.rotg = sb(top, "rotg", [128, 4, 128], BF16)
        C.invf = sb(top, "invf", [128, 1], F32)
        C.gh = sb(top, "gh", [128, 6], F32)
        C.ss = sb(top, "ssab", [128, 2, NT], F32)
        C.esink = sb(top, "esink", [128, 8], F32)
        C.aff = sb(top, "aff", [128, NT, N_EXP], F32)
        C.r_aff = Reg("aff")
        C.r = Reg("consts", const=True)
        C.r_ss = Reg("ss")
        S.dma("sp", C.ident[:], I.c_ident, writes=[C.r])
        S.dma("sp", C.identf[:], I.c_identf, writes=[C.r])
        S.dma("sp", C.ones[:], I.c_ones, writes=[C.r])
        S.dma("sp", C.onesf[:], I.c_onesf, writes=[C.r])
        S.dma("sp", C.rot0[:], I.c_rot, writes=[C.r])
        S.dma("sp", C.invf[:], I.c_invf, writes=[C.r])
        for i, n in enumerate(("g_qa", "g_ka", "g_qb", "g_kb", "g_qm", "g_km")):
            S.dma("sp", C.gh[:, i:i + 1], getattr(I, n).rearrange("(p o) -> p o", o=1), writes=[C.r])
        S.dma("sp", C.esink[:], I.sink.partition_broadcast(128), writes=[C.r])
        S.op("act", lambda e: e.activation(out=C.esink[:], in_=C.esink[:], func=AF.Exp), reads=[C.r], writes=[C.r])
        for f in range(4):
            S.op("dve", lambda e, f=f: e.tensor_scalar(out=C.rotg[:, f, :], in0=C.rot0[:], scalar1=C.gh[:, f:f + 1],
                                                       scalar2=None, op0=ALU.mult), reads=[C.r], writes=[C.r])
        S.op("dve", lambda e: e.memset(C.ss[:], 0.0), writes=[C.r_ss])

        phase_a(K, top)
        if stop_after >= 2:
            with ExitStack() as st_bc:
                K.wout = sb(st_bc, "wout", [128, NCH, D], BF16)
                go = sb(st_bc, "go", [128, 16], F32)
                r_wo = Reg()
                S.dma("pool", K.wout[:], I.w_out.rearrange("(c p) n -> p c n", p=128), writes=[r_wo])
                S.dma("sp", go[:, 0:8], I.g_oa.rearrange("(c p) -> p c", p=128), writes=[r_wo], allow_slow_non_contiguous=True)
                S.dma("sp", go[:, 8:16], I.g_ob.rearrange("(c p) -> p c", p=128), writes=[r_wo], allow_slow_non_contiguous=True)
                for c in range(NCH):
                    ts(K.wout[:, c, :], K.wout[:, c, :], go[:, c:c + 1], None, ALU.mult, None, [r_wo], [r_wo])
                phase_b(K, top)
                if stop_after >= 3:
                    phase_c(K, top)
        if stop_after >= 4:
            with ExitStack() as st_moe:
                moe_ring_setup(K, st_moe)
                phase_d(K, top)
                if stop_after >= 5:
                    phase_e2(K, top)
        S.emit_phase()
    return nc


PI_LO = 3.1415925


def phase_a(K, top):
    nc, S, I, C, Sx, R, sb, ps = K.nc, K.S, K.I, K.C, K.Sx, K.R, K.sb, K.ps
    HALF = SEQ // 2
    w_in_v = I.w_in.rearrange("(c p) n -> p c n", p=128)
    for hf in range(2):
        with ExitStack() as st_x:
            xnT = sb(st_x, "xnT", [128, NCH, HALF], BF16)
            r_xnT = [Reg("xnT%d" % t) for t in range(16)]
            with ExitStack() as st:
                xs = [sb(st, "xs%d" % i, [128, D], F32) for i in range(2)]
                xn = [sb(st, "xn%d" % i, [128, D], BF16) for i in range(2)]
                junk = sb(st, "junk", [128, D], BF16)
                gbc = sb(st, "gbc", [128, D], F32)
                ssq = sb(st, "ssq", [128, 2], F32)
                rstd = sb(st, "rstd", [128, 2], F32)
                tp = [ps(st, "tp%d" % i, [128, 8, 128], BF16) for i in range(4)]
                r_xs = [Reg() for _ in range(2)]
                r_xn = [Reg() for _ in range(2)]
                r_junk, r_gbc = Reg(), Reg(const=True)
                r_ssq = [Reg() for _ in range(2)]
                r_rstd = [Reg() for _ in range(2)]
                r_tp = [Reg() for _ in range(4)]
                S.dma("sp", gbc[:], I.g_mix.partition_broadcast(128), writes=[r_gbc])
                for t in range(16):
                    b = t % 2
                    tok0 = hf * HALF + t * 128
                    S.dma("sp", xs[b][:], I.x[tok0:tok0 + 128, :], writes=[r_xs[b]])
                    S.op("act", lambda e, b=b: e.activation(out=junk[:], in_=xs[b][:], func=AF.Square,
                                                            accum_out=ssq[:, b:b + 1]),
                         reads=[r_xs[b]], writes=[r_junk, r_ssq[b]])
                    K.rsq(rstd[:, b:b + 1], ssq[:, b:b + 1], 1.0 / D, [r_ssq[b]], [r_rstd[b]])
                    S.op("dve", lambda e, b=b: e.scalar_tensor_tensor(out=xn[b][:], in0=xs[b][:], scalar=rstd[:, b:b + 1],
                                                                      in1=gbc[:], op0=ALU.mult, op1=ALU.mult),
                         reads=[r_xs[b], r_rstd[b], r_gbc], writes=[r_xn[b]])
                    for hh in range(2):
                        pt = tp[2 * b + hh]
                        rp = r_tp[2 * b + hh]
                        for c in range(8):
                            cc = hh * 8 + c
                            S.op("pe", lambda e, pt=pt, c=c, cc=cc, b=b: e.transpose(out=pt[:, c, :], in_=xn[b][:, cc * 128:(cc + 1) * 128],
                                                                                      identity=C.ident[:]),
                                 reads=[r_xn[b], C.r], writes=[rp])
                        eng = "act" if hh == 0 else "dve"
                        if eng == "act":
                            S.op("act", lambda e, pt=pt, hh=hh, t=t: e.copy(out=xnT[:, hh * 8:hh * 8 + 8, t * 128:(t + 1) * 128], in_=pt[:]),
                                 reads=[rp], writes=[r_xnT[t]])
                        else:
                            S.op("dve", lambda e, pt=pt, hh=hh, t=t: e.tensor_copy(out=xnT[:, hh * 8:hh * 8 + 8, t * 128:(t + 1) * 128], in_=pt[:]),
                                 reads=[rp], writes=[r_xnT[t]])
                S.emit_phase()
            with ExitStack() as st:
                cos = sb(st, "cos", [128, HALF], F32)
                sin = sb(st, "sin", [128, HALF], F32)
                posi = sb(st, "posi", [128, 1024], I32)
                ang = sb(st, "ang", [128, 1024], F32)
                kf = sb(st, "kf", [128, 1024], F32)
                ki = sb(st, "ki", [128, 1024], I32)
                r_cs = Reg(const=True)
                r_tmp = Reg()
                for qd in range(2):
                    t0 = hf * HALF + qd * 1024
                    sl = slice(qd * 1024, (qd + 1) * 1024)
                    S.dma("sp", posi[:], I.pos[t0:t0 + 1024].partition_broadcast(128), writes=[r_tmp])
                    S.op("dve", lambda e: e.tensor_copy(out=ang[:], in_=posi[:]), reads=[r_tmp], writes=[r_tmp])
                    S.op("dve", lambda e: e.tensor_scalar(out=ang[:], in0=ang[:], scalar1=C.invf[:, 0:1], scalar2=None, op0=ALU.mult),
                         reads=[r_tmp, C.r], writes=[r_tmp])
                    S.op("dve", lambda e: e.tensor_scalar(out=kf[:], in0=ang[:], scalar1=1.0 / TWO_PI, scalar2=None, op0=ALU.mult),
                         reads=[r_tmp], writes=[r_tmp])
                    S.op("dve", lambda e: e.tensor_copy(out=ki[:], in_=kf[:]), reads=[r_tmp], writes=[r_tmp])
                    S.op("dve", lambda e: e.tensor_copy(out=kf[:], in_=ki[:]), reads=[r_tmp], writes=[r_tmp])
                    S.op("dve", lambda e: e.scalar_tensor_tensor(out=ang[:], in0=kf[:], scalar=-TWO_PI, in1=ang[:], op0=ALU.mult, op1=ALU.add),
                         reads=[r_tmp], writes=[r_tmp])

                    def wrap_and_sin(dst, shift):
                        S.op("dve", lambda e: e.tensor_scalar(out=kf[:], in0=ang[:], scalar1=shift, scalar2=None, op0=ALU.add),
                             reads=[r_tmp], writes=[r_tmp])
                        S.op("dve", lambda e: e.tensor_scalar(out=posi[:].bitcast(F32), in0=kf[:], scalar1=float(np.pi), scalar2=-TWO_PI,
                                                              op0=ALU.is_gt, op1=ALU.mult), reads=[r_tmp], writes=[r_tmp])
                        S.op("dve", lambda e: e.tensor_tensor(out=kf[:], in0=kf[:], in1=posi[:].bitcast(F32), op=ALU.add),
                             reads=[r_tmp], writes=[r_tmp])
                        S.op("dve", lambda e: e.tensor_scalar(out=posi[:].bitcast(F32), in0=kf[:], scalar1=-float(np.pi), scalar2=TWO_PI,
                                                              op0=ALU.is_lt, op1=ALU.mult), reads=[r_tmp], writes=[r_tmp])
                        S.op("dve", lambda e: e.tensor_tensor(out=kf[:], in0=kf[:], in1=posi[:].bitcast(F32), op=ALU.add),
                             reads=[r_tmp], writes=[r_tmp])
                        S.op("dve", lambda e: e.tensor_scalar(out=kf[:], in0=kf[:], scalar1=PI_LO, scalar2=-PI_LO, op0=ALU.min, op1=ALU.max),
                             reads=[r_tmp], writes=[r_tmp])
                        S.op("act", lambda e: e.activation(out=dst, in_=kf[:], func=AF.Sin), reads=[r_tmp], writes=[r_cs, r_tmp])
                    wrap_and_sin(sin[:, sl], 0.0)
                    wrap_and_sin(cos[:, sl], float(np.pi / 2))

                wq = [sb(st, "wq%d" % i, [128, NCH, 128], BF16) for i in range(2)]
                r_wq = [Reg() for _ in range(2)]
                q2 = [sb(st, "q2_%d" % i, [128, 512], BF16) for i in range(2)]
                qb = [sb(st, "qb_%d" % i, [128, 512], BF16) for i in range(2)]
                rs = [sb(st, "rs_%d" % i, [128, 512], F32) for i in range(2)]
                ta = [sb(st, "ta_%d" % i, [128, 512], F32) for i in range(2)]
                tb = [sb(st, "tb_%d" % i, [128, 512], F32) for i in range(2)]
                stage = [sb(st, "stg%d" % i, [128, HALF], BF16) for i in range(2)]
                r_q2 = [Reg() for _ in range(2)]
                r_qb = [Reg() for _ in range(2)]
                r_rs = [Reg() for _ in range(2)]
                r_ta = [Reg() for _ in range(2)]
                r_tb = [Reg() for _ in range(2)]
                r_stage = [Reg() for _ in range(2)]
                qp = [ps(st, "qp%d" % i, [128, 512], F32) for i in range(2)]
                sp_ = [ps(st, "ssp%d" % i, [128, 512], F32) for i in range(2)]
                rp_ = [ps(st, "rtp%d" % i, [128, 512], F32) for i in range(2)]
                r_qp = [Reg() for _ in range(2)]
                r_sp = [Reg() for _ in range(2)]
                r_rp = [Reg() for _ in range(2)]
                it = 0
                for hc in range(26):
                    wb = hc % 2
                    fam = HC_FAM[hc]
                    c0 = HC_COL[hc]
                    S.dma("pool", wq[wb][:], w_in_v[:, :, c0:c0 + 128], writes=[r_wq[wb]])
                    for blk in range(4):
                        b = it % 2
                        it += 1
                        cs = slice(blk * 512, (blk + 1) * 512)
                        for c in range(NCH):
                            S.op("pe", lambda e, b=b, wb=wb, c=c, cs=cs: e.matmul(qp[b][:], lhsT=wq[wb][:, c, :], rhs=xnT[:, c, cs],
                                                                                    start=(c == 0), stop=(c == NCH - 1)),
                                 reads=[r_wq[wb]] + r_xnT[blk * 4:blk * 4 + 4], writes=[r_qp[b]])
                        S.op("act", lambda e, b=b: e.activation(out=q2[b][:], in_=qp[b][:], func=AF.Square),
                             reads=[r_qp[b]], writes=[r_q2[b]])
                        S.op("act", lambda e, b=b: e.copy(out=qb[b][:], in_=qp[b][:]), reads=[r_qp[b]], writes=[r_qb[b]])
                        S.op("pe", lambda e, b=b: e.matmul(sp_[b][:], lhsT=C.ones[:], rhs=q2[b][:], start=True, stop=True),
                             reads=[r_q2[b], C.r], writes=[r_sp[b]])
                        S.op("pe", lambda e, b=b, fam=fam: e.matmul(rp_[b][:], lhsT=C.rotg[:, fam, :], rhs=qb[b][:], start=True, stop=True),
                             reads=[r_qb[b], C.r], writes=[r_rp[b]])
                        K.rsq(rs[b][:], sp_[b][:], 1.0 / HD, [r_sp[b]], [r_rs[b]])
                        S.op("dve", lambda e, b=b, fam=fam, cs=cs: e.scalar_tensor_tensor(out=ta[b][:], in0=qp[b][:], scalar=C.gh[:, fam:fam + 1],
                                                                                         in1=cos[:, cs], op0=ALU.mult, op1=ALU.mult),
                             reads=[r_qp[b], r_cs, C.r], writes=[r_ta[b]])
                        S.op("dve", lambda e, b=b, cs=cs: e.tensor_tensor(out=tb[b][:], in0=rp_[b][:], in1=sin[:, cs], op=ALU.mult),
                             reads=[r_rp[b], r_cs], writes=[r_tb[b]])
                        S.op("dve", lambda e, b=b: e.tensor_tensor(out=ta[b][:], in0=ta[b][:], in1=tb[b][:], op=ALU.add),
                             reads=[r_ta[b], r_tb[b]], writes=[r_ta[b]])
                        S.op("dve", lambda e, b=b, wb=wb, cs=cs: e.tensor_tensor(out=stage[wb][:, cs], in0=ta[b][:], in1=rs[b][:], op=ALU.mult),
                             reads=[r_ta[b], r_rs[b]], writes=[r_stage[wb]])
                    S.dma("sp", Sx.qkt[hc, :, hf * HALF:(hf + 1) * HALF], stage[wb][:], reads=[r_stage[wb]], writes=[R.qkt[hc][hf]])
                S.emit_phase()
            with ExitStack() as st:
                wv = sb(st, "wv", [128, NCH, 1280], BF16)
                r_wv = [Reg() for _ in range(3)]
                vst = [sb(st, "vst%d" % i, [128, 1280], BF16) for i in range(2)]
                r_vst = [Reg() for _ in range(2)]
                vp = [ps(st, "vp%d" % i, [128, 512], F32) for i in range(4)]
                r_vp = [Reg() for _ in range(4)]
                off = 0
                pieces = []
                for i, (c0, n) in enumerate(V_COLS):
                    S.dma("pool", wv[:, :, off:off + n], w_in_v[:, :, c0:c0 + n], writes=[r_wv[i]])
                    pieces.append((off, n))
                    off += n
                it = 0
                for t in range(16):
                    b = t % 2
                    tok0 = hf * HALF + t * 128
                    for i, (o, n) in enumerate(pieces):
                        pb = it % 4
                        it += 1
                        for c in range(NCH):
                            S.op("pe", lambda e, pb=pb, c=c, t=t, o=o, n=n: e.matmul(vp[pb][:, 0:n], lhsT=xnT[:, c, t * 128:(t + 1) * 128],
                                                                                     rhs=wv[:, c, o:o + n], start=(c == 0), stop=(c == NCH - 1)),
                                 reads=[r_wv[i], r_xnT[t]], writes=[r_vp[pb]])
                        if i % 2 == 0:
                            S.op("act", lambda e, pb=pb, b=b, o=o, n=n: e.copy(out=vst[b][:, o:o + n], in_=vp[pb][:, 0:n]),
                                 reads=[r_vp[pb]], writes=[r_vst[b]])
                        else:
                            S.op("dve", lambda e, pb=pb, b=b, o=o, n=n: e.tensor_copy(out=vst[b][:, o:o + n], in_=vp[pb][:, 0:n]),
                                 reads=[r_vp[pb]], writes=[r_vst[b]])
                    S.dma("sp", Sx.v[tok0:tok0 + 128, :], vst[b][:], reads=[r_vst[b]], writes=[R.v[hf * 16 + t]])
                S.emit_phase()


def phase_b(K, top):
    nc, S, I, C, Sx, R, sb, ps = K.nc, K.S, K.I, K.C, K.Sx, K.R, K.sb, K.ps
    mm, tr, act, ts, tt, stt, recip, cp = K.mm, K.tr, K.act, K.ts, K.tt, K.stt, K.recip, K.cp
    LOOK = 3
    with ExitStack() as st:
        maskA = sb(st, "maskA", [128, 20, 512], BF16)
        maskB = sb(st, "maskB", [128, 6, 512], BF16)
        r_mask = Reg(const=True)
        S.dma("sp", maskA[:], I.c_maska, writes=[r_mask])
        S.dma("sp", maskB[:], I.c_maskb, writes=[r_mask])
        QT = [sb(st, "QT%d" % i, [128, SEQ], BF16) for i in range(2)]
        KT = [sb(st, "KT%d" % i, [128, SEQ], BF16) for i in range(2)]
        V1 = [sb(st, "V1%d" % i, [128, NT, 130], BF16) for i in range(2)]
        OTs = [sb(st, "OTs%d" % i, [128, SEQ], BF16) for i in range(2)]
        r_QT = [Reg() for _ in range(2)]
        r_KT = [Reg() for _ in range(2)]
        r_V1 = [Reg() for _ in range(2)]
        r_OTs = [Reg() for _ in range(2)]
        NE = 6
        NSP = 3
        E = [sb(st, "E%d" % i, [128, 512], BF16) for i in range(NE)]
        Pm = [sb(st, "Pm%d" % i, [128, 512], BF16) for i in range(NE)]
        r_E = [Reg() for _ in range(NE)]
        r_Pm = [Reg() for _ in range(NE)]
        NO = 8
        Osb = [sb(st, "Osb%d" % i, [128, 130], F32) for i in range(NO)]
        den = [sb(st, "den%d" % i, [128, 1], F32) for i in range(NO)]
        sst = [sb(st, "sst%d" % i, [128, 1], F32) for i in range(NO)]
        obf = [sb(st, "obf%d" % i, [128, 128], BF16) for i in range(NO)]
        junk = sb(st, "junkb", [128, 128], BF16)
        r_Osb = [Reg() for _ in range(NO)]
        r_den = [Reg() for _ in range(NO)]
        r_sst = [Reg() for _ in range(NO)]
        r_obf = [Reg() for _ in range(NO)]
        r_junk = Reg()
        Sp = [ps(st, "Sp%d" % i, [128, 512], F32) for i in range(NSP)]
        Op_ = [ps(st, "Op%d" % i, [128, 512], F32) for i in range(4)]
        Tp = ps(st, "Tp", [128, 512], F32)
        tpv = Tp[:].bitcast(BF16)
        r_Sp = [Reg() for _ in range(NSP)]
        r_Op = [Reg() for _ in range(4)]
        r_Tp = [Reg() for _ in range(8)]
        for i in range(2):
            S.op("dve", lambda e, i=i: e.memset(V1[i][:, :, 128:130], 1.0), writes=[r_V1[i]])

        iters = []
        for h in range(16):
            isA = h < 8
            nj, koff = (20, -1024) if isA else (6, -128)
            for qb in range(8):
                q0 = qb * 512
                js = [j for j in range(nj) if 0 <= q0 + koff + 128 * j < SEQ]
                for jn, j in enumerate(js):
                    iters.append(dict(h=h, qb=qb, j=j, k0=q0 + koff + 128 * j, first=(jn == 0), last=(jn == len(js) - 1),
                                      hstart=(qb == 0 and jn == 0), hend=(qb == 7 and jn == len(js) - 1)))
        state = {"osb": 0, "ts": 0}
        deferred = []

        def head_cfg(h):
            if h < 8:
                return h, 8 + h, 128 * h, maskA
            kvh = (h - 8) // 4
            return 16 + (h - 8), 24 + kvh, 1024 + 128 * kvh, maskB

        def load_head(h):
            hb = h % 2
            qhc, khc, vcol, _ = head_cfg(h)
            S.dma("sp", QT[hb][:], Sx.qkt[qhc], reads=R.qkt[qhc], writes=[r_QT[hb]])
            S.dma("sp", KT[hb][:], Sx.qkt[khc], reads=R.qkt[khc], writes=[r_KT[hb]])
            S.dma("sp", V1[hb][:, :, 0:128], Sx.v[:, vcol:vcol + 128].rearrange("(t p) d -> p t d", p=128),
                  reads=R.v, writes=[r_V1[hb]])

        def score(n):
            it = iters[n]
            hb = it["h"] % 2
            mask = head_cfg(it["h"])[3]
            sbi, ei = n % NSP, n % NE
            k0, q0, j = it["k0"], it["qb"] * 512, it["j"]
            mm(Sp[sbi][:], KT[hb][:, k0:k0 + 128], QT[hb][:, q0:q0 + 512], True, False, [r_KT[hb], r_QT[hb]], [r_Sp[sbi]])
            mm(Sp[sbi][:], C.ident[:], mask[:, j, :], False, True, [r_mask, C.r], [r_Sp[sbi]])
            act(Pm[ei][:], Sp[sbi][:], AF.Exp, [r_Sp[sbi]], [r_Pm[ei]], scale=SCALE)

        def post(h, qb, i, o):
            hb = h % 2
            grp = 0 if h < 8 else 1
            qt = 4 * qb + i
            if h < 8:
                recip(den[o][:], Osb[o][:, 128:129], [r_Osb[o]], [r_den[o]])
            else:
                tt(den[o][:], Osb[o][:, 128:129], C.esink[:, h - 8:h - 7], ALU.add, [r_Osb[o], C.r], [r_den[o]])
                recip(den[o][:], den[o][:], [r_den[o]], [r_den[o]])
            ts(obf[o][:], Osb[o][:, 0:128], den[o][:, 0:1], None, ALU.mult, None, [r_Osb[o], r_den[o]], [r_obf[o]])
            act(junk[:], obf[o][:], AF.Square, [r_obf[o]], [r_junk, r_sst[o]], accum_out=sst[o][:])
            tt(C.ss[:, grp, qt:qt + 1], C.ss[:, grp, qt:qt + 1], sst[o][:], ALU.add, [r_sst[o], C.r_ss], [C.r_ss])
            tsl = state["ts"] % 8
            state["ts"] += 1
            tr(tpv[:, tsl * 128:(tsl + 1) * 128], obf[o][:], [r_obf[o]], [r_Tp[tsl]])
            cp(OTs[hb][:, qt * 128:(qt + 1) * 128], tpv[:, tsl * 128:(tsl + 1) * 128], [r_Tp[tsl]], [r_OTs[hb]], eng="act")

        def pv(n):
            it = iters[n]
            h, qb = it["h"], it["qb"]
            hb = h % 2
            ei = n % NE
            k0 = it["k0"]
            for i in range(4):
                mm(Op_[i][:, 0:129], Pm[ei][:, 128 * i:128 * i + 128], V1[hb][:, k0 // 128, 0:129], it["first"], it["last"],
                   [r_Pm[ei], r_V1[hb]], [r_Op[i]])
            if it["last"]:
                for i in range(4):
                    o = state["osb"] % NO
                    state["osb"] += 1
                    cp(Osb[o][:, 0:129], Op_[i][:, 0:129], [r_Op[i]], [r_Osb[o]])
                    deferred.append([2, (lambda h=h, qb=qb, i=i, o=o: post(h, qb, i, o))])
            if it["hend"]:
                deferred.append([3, (lambda h=h, hb=hb: S.dma("sp", Sx.ot[h], OTs[hb][:], reads=[r_OTs[hb]], writes=[R.ot[h]]))])

        def tick():
            for d in deferred:
                d[0] -= 1
            while deferred and deferred[0][0] <= 0:
                deferred.pop(0)[1]()

        N = len(iters)
        load_head(0)
        for n in range(N + LOOK):
            if n < N:
                score(n)
            if n - LOOK >= 0:
                pv(n - LOOK)
                if iters[n - LOOK]["hstart"] and iters[n - LOOK]["h"] + 1 < 16:
                    load_head(iters[n - LOOK]["h"] + 1)
            tick()
        while deferred:
            tick()
        S.emit_phase()


def phase_c(K, top):
    nc, S, I, C, Sx, R, sb, ps = K.nc, K.S, K.I, K.C, K.Sx, K.R, K.sb, K.ps
    mm, tr, act, ts, tt, stt, recip, cp = K.mm, K.tr, K.act, K.ts, K.tt, K.stt, K.recip, K.cp
    with ExitStack() as st0:
        KmT = sb(st0, "KmT", [128, 4, 256], BF16)
        Vm = sb(st0, "Vm", [128, 2, 4, 130], BF16)
        r_km = Reg(const=True)
        with ExitStack() as st:
            memx = sb(st, "memx", [128, 2, D], F32)
            memn = sb(st, "memn", [128, 2, D], BF16)
            memT = sb(st, "memT", [128, NCH, 256], BF16)
            gbc = sb(st, "gbcm", [128, D], F32)
            junk = sb(st, "junkc0", [128, D], BF16)
            wkv = sb(st, "wkv", [128, NCH, 1024], BF16)
            ssq = sb(st, "ssqm", [128, 2], F32)
            k2 = sb(st, "k2", [128, 256], BF16)
            rsk = sb(st, "rsk", [128, 256], F32)
            bank = [ps(st, "c0b%d" % i, [128, 512], F32) for i in range(4)]
            rb = [Reg() for _ in range(4)]
            r1 = Reg()
            r_w = Reg()
            S.dma("sp", memx[:], I.mem.rearrange("(t p) d -> p t d", p=128), writes=[r1])
            S.dma("sp", gbc[:], I.g_mem.partition_broadcast(128), writes=[r1])
            S.dma("pool", wkv[:], I.w_kv_mem.rearrange("(c p) n -> p c n", p=128), writes=[r_w])
            S.op("dve", lambda e: e.memset(Vm[:], 1.0), writes=[r_km])
            for mt in range(2):
                act(junk[:], memx[:, mt, :], AF.Square, [r1], [r1], accum_out=ssq[:, mt:mt + 1])
                K.rsq(ssq[:, mt:mt + 1], ssq[:, mt:mt + 1], 1.0 / D, [r1], [r1])
                stt(memn[:, mt, :], memx[:, mt, :], ssq[:, mt:mt + 1], gbc[:], ALU.mult, ALU.mult, [r1], [r1])
                for hh in range(2):
                    tpv = bank[hh][:].bitcast(BF16)
                    for c in range(8):
                        cc = hh * 8 + c
                        tr(tpv[:, c * 128:(c + 1) * 128], memn[:, mt, cc * 128:(cc + 1) * 128], [r1], [rb[hh]])
                    cp(memT[:, hh * 8:hh * 8 + 8, mt * 128:(mt + 1) * 128], tpv[:, 0:1024].rearrange("p (c n) -> p c n", c=8), [rb[hh]], [r1])
            for hm in range(4):
                for c in range(NCH):
                    mm(bank[2][:, 0:256], wkv[:, c, hm * 128:(hm + 1) * 128], memT[:, c, :], c == 0, c == NCH - 1, [r_w, r1], [rb[2]])
                act(k2[:], bank[2][:, 0:256], AF.Square, [rb[2]], [r1])
                mm(bank[3][:, 0:256], C.ones[:], k2[:], True, True, [r1, C.r], [rb[3]])
                K.rsq(rsk[:], bank[3][:, 0:256], 1.0 / HD, [rb[3]], [r1])
                stt(KmT[:, hm, :], bank[2][:, 0:256], C.gh[:, 5:6], rsk[:], ALU.mult, ALU.mult, [rb[2], r1, C.r], [r_km])
            for mt in range(2):
                for c in range(NCH):
                    mm(bank[mt][:], memT[:, c, mt * 128:(mt + 1) * 128], wkv[:, c, 512:1024], c == 0, c == NCH - 1, [r_w, r1], [rb[mt]])
                cp(Vm[:, mt, :, 0:128], bank[mt][:].rearrange("p (h d) -> p h d", h=4), [rb[mt]], [r_km])
            S.emit_phase()
        with ExitStack() as st:
            wout = K.wout
            wq = sb(st, "wqm", [128, NCH, 512], BF16)
            wo = sb(st, "wom", [128, 4, D], BF16)
            wr = sb(st, "wr", [128, NCH, N_EXP], BF16)
            gcr = sb(st, "gcr", [128, D], F32)
            gmo = sb(st, "gmo", [128, D], F32)
            rAB = sb(st, "rAB", [128, 2, NT], F32)
            r_w = Reg(const=True)
            S.dma("pool", wq[:], I.w_q_mem.rearrange("(c p) n -> p c n", p=128), writes=[r_w])
            S.dma("pool", wo[:], I.w_o_mem.rearrange("(c p) n -> p c n", p=128), writes=[r_w])
            S.dma("pool", wr[:], I.w_router.rearrange("(c p) n -> p c n", p=128), writes=[r_w])
            S.dma("sp", gcr[:], I.g_cross.partition_broadcast(128), writes=[r_w])
            S.dma("sp", gmo[:], I.g_moe.partition_broadcast(128), writes=[r_w])
            K.rsq(rAB[:], C.ss[:], 1.0 / 1024, [C.r_ss], [r_w])

            xt = [sb(st, "xt%d" % i, [128, D], F32) for i in range(2)]
            otb = [sb(st, "otb%d" % i, [128, 16, 128], BF16) for i in range(2)]
            x12 = [sb(st, "x12_%d" % i, [128, D], F32) for i in range(2)]
            hn = [sb(st, "hn%d" % i, [128, D], BF16) for i in range(2)]
            hT = [sb(st, "hT%d" % i, [128, NCH, 128], BF16) for i in range(2)]
            junk = sb(st, "junkc1", [128, D], BF16)
            sq = sb(st, "sqc", [128, 4], F32)
            q2 = sb(st, "q2c", [128, 512], BF16)
            rsq = sb(st, "rsqc", [128, 512], F32)
            qn = sb(st, "qnc", [128, 4, 128], BF16)
            E2 = [sb(st, "E2_%d" % i, [128, 4, 128], BF16) for i in range(2)]
            rden = sb(st, "rdenc", [128, 4], F32)
            o2 = sb(st, "o2c", [128, 4, 128], BF16)
            o2T = sb(st, "o2T", [128, 4, 128], BF16)
            ex = sb(st, "exr", [128, N_EXP], F32)
            sume = sb(st, "sume", [128, 1], F32)
            r_xt = [Reg() for _ in range(2)]
            r_otb = [Reg() for _ in range(2)]
            r_x12 = [Reg() for _ in range(2)]
            r_hn = [Reg() for _ in range(2)]
            r_hT = [Reg() for _ in range(2)]
            r_junk, r_sq, r_q2, r_rsq, r_qn, r_rden, r_o2, r_o2T, r_ex, r_sume = [Reg() for _ in range(10)]
            r_E2 = [Reg() for _ in range(2)]
            B = [ps(st, "c1b%d" % i, [128, 512], F32) for i in range(8)]
            rB = [Reg() for _ in range(8)]
            tpv = B[4][:].bitcast(BF16)
            ot_v = Sx.ot.rearrange("h p n -> p h n")

            def rmsnorm_tile(src, r_src, gb, dst, r_dst, col):
                act(junk[:], src, AF.Square, [r_src], [r_junk, r_sq], accum_out=sq[:, col:col + 1])
                K.rsq(sq[:, col:col + 1], sq[:, col:col + 1], 1.0 / D, [r_sq], [r_sq])
                stt(dst, src, sq[:, col:col + 1], gb[:], ALU.mult, ALU.mult, [r_src, r_sq, r_w], [r_dst])

            def transpose16(src, r_src, dst, r_dst):
                for hh in range(2):
                    for c in range(8):
                        cc = hh * 8 + c
                        tr(tpv[:, c * 128:(c + 1) * 128], src[:, cc * 128:(cc + 1) * 128], [r_src], [rB[4]])
                    cp(dst[:, hh * 8:hh * 8 + 8, :], tpv.rearrange("p (c n) -> p c n", c=8), [rB[4]], [r_dst],
                       eng=("act" if hh == 0 else "dve"))

            def load(t):
                b = t % 2
                S.dma("sp", xt[b][:], I.x[t * 128:(t + 1) * 128, :], writes=[r_xt[b]])
                S.dma("sp", otb[b][:], ot_v[:, :, t * 128:(t + 1) * 128], reads=R.ot, writes=[r_otb[b]])
            def s1_cg(t, cg):
                b = t % 2
                X = x12[b]
                pa, pb = B[(cg % 2) * 2], B[(cg % 2) * 2 + 1]
                ra, rbb = rB[(cg % 2) * 2], rB[(cg % 2) * 2 + 1]
                cs = slice(cg * 512, (cg + 1) * 512)
                for c in range(8):
                    mm(pa[:], otb[b][:, c, :], wout[:, c, cs], c == 0, c == 7, [r_otb[b], r_w], [ra])
                for c in range(8, 16):
                    mm(pb[:], otb[b][:, c, :], wout[:, c, cs], c == 8, c == 15, [r_otb[b], r_w], [rbb])
                stt(X[:, cs], pa[:], rAB[:, 0, t:t + 1], xt[b][:, cs], ALU.mult, ALU.add, [ra, r_xt[b], r_w], [r_x12[b]])
                stt(X[:, cs], pb[:], rAB[:, 1, t:t + 1], X[:, cs], ALU.mult, ALU.add, [rbb, r_x12[b], r_w], [r_x12[b]])

            def nxt(t, cg):
                if t + 1 < NT:
                    s1_cg(t + 1, cg)

            load(0)
            load(1)
            for cg in range(4):
                s1_cg(0, cg)
            for t in range(NT):
                b = t % 2
                X = x12[b]
                rmsnorm_tile(X[:], r_x12[b], gcr, hn[0][:], r_hn[0], 0)
                nxt(t, 0)
                transpose16(hn[0], r_hn[0], hT[0], r_hT[0])
                for hm in range(4):
                    for c in range(NCH):
                        mm(B[5][:, hm * 128:(hm + 1) * 128], wq[:, c, hm * 128:(hm + 1) * 128], hT[0][:, c, :], c == 0, c == NCH - 1,
                           [r_w, r_hT[0]], [rB[5]])
                act(q2[:], B[5][:], AF.Square, [rB[5]], [r_q2])
                nxt(t, 1)
                mm(B[6][:], C.ones[:], q2[:], True, True, [r_q2, C.r], [rB[6]])
                K.rsq(rsq[:], B[6][:], 1.0 / HD, [rB[6]], [r_rsq])
                stt(qn[:].rearrange("p h n -> p (h n)"), B[5][:], C.gh[:, 4:5], rsq[:], ALU.mult, ALU.mult, [rB[5], r_rsq, C.r], [r_qn])
                nxt(t, 2)
                for mt in range(2):
                    bk = 7 if mt == 0 else 4
                    for hm in range(4):
                        mm(B[bk][:, hm * 128:(hm + 1) * 128], KmT[:, hm, mt * 128:(mt + 1) * 128], qn[:, hm, :], True, True,
                           [r_km, r_qn], [rB[bk]])
                    act(E2[mt][:].rearrange("p h n -> p (h n)"), B[bk][:], AF.Exp, [rB[bk]], [r_E2[mt]], scale=SCALE)
                nxt(t, 3)
                for hm in range(4):
                    bk = 5 if hm < 2 else 6
                    o0 = (hm % 2) * 130
                    for mt in range(2):
                        mm(B[bk][:, o0:o0 + 129], E2[mt][:, hm, :], Vm[:, mt, hm, 0:129], mt == 0, mt == 1, [r_E2[mt], r_km], [rB[bk]])
                for hm in range(4):
                    bk = 5 if hm < 2 else 6
                    o0 = (hm % 2) * 130
                    recip(rden[:, hm:hm + 1], B[bk][:, o0 + 128:o0 + 129], [rB[bk]], [r_rden])
                    ts(o2[:, hm, :], B[bk][:, o0:o0 + 128], rden[:, hm:hm + 1], None, ALU.mult, None, [rB[bk], r_rden], [r_o2])
                for hm in range(4):
                    tr(tpv[:, hm * 128:(hm + 1) * 128], o2[:, hm, :], [r_o2], [rB[4]])
                cp(o2T[:].rearrange("p h n -> p (h n)"), tpv[:, 0:512], [rB[4]], [r_o2T], eng="act")
                for cg in range(4):
                    pa = B[cg % 4]
                    ra = rB[cg % 4]
                    cs = slice(cg * 512, (cg + 1) * 512)
                    for hm in range(4):
                        mm(pa[:], o2T[:, hm, :], wo[:, hm, cs], hm == 0, hm == 3, [r_o2T, r_w], [ra])
                    tt(X[:, cs], pa[:], X[:, cs], ALU.add, [ra, r_x12[b]], [r_x12[b]])
                S.dma("sp", Sx.x2[t * 128:(t + 1) * 128, :], X[:], reads=[r_x12[b]], writes=[R.x2[t]])
                rmsnorm_tile(X[:], r_x12[b], gmo, hn[1][:], r_hn[1], 1)
                S.dma("sp", Sx.h3[t * 128:(t + 1) * 128, :], hn[1][:], reads=[r_hn[1]], writes=[R.h3[t]])
                if t + 2 < NT:
                    load(t + 2)
                transpose16(hn[1], r_hn[1], hT[1], r_hT[1])
                for c in range(NCH):
                    mm(B[5][:, 0:N_EXP], hT[1][:, c, :], wr[:, c, :], c == 0, c == NCH - 1, [r_hT[1], r_w], [rB[5]])
                act(ex[:], B[5][:, 0:N_EXP], AF.Exp, [rB[5]], [r_ex, r_sume], accum_out=sume[:])
                recip(sume[:], sume[:], [r_sume], [r_sume])
                ts(C.aff[:, t, :], ex[:], sume[:, 0:1], None, ALU.mult, None, [r_ex, r_sume], [C.r_aff])
            S.emit_phase()


N_BISECT = 34


def phase_d(K, top):
    nc, S, I, C, Sx, R, sb, ps = K.nc, K.S, K.I, K.C, K.Sx, K.R, K.sb, K.ps
    mm, tr, act, ts, tt, stt, recip, cp = K.mm, K.tr, K.act, K.ts, K.tt, K.stt, K.recip, K.cp
    with ExitStack() as st:
        lo = sb(st, "lo", [128, N_EXP], F32)
        hi = sb(st, "hi", [128, N_EXP], F32)
        mid = sb(st, "mid", [128, N_EXP], F32)
        ge = sb(st, "ge", [128, N_EXP], F32)
        dl = sb(st, "dl", [128, N_EXP], F32)
        cntp = sb(st, "cntp", [128, N_EXP], F32)
        cmp_ = sb(st, "cmp", [128, NT, N_EXP], F32)
        cnt = ps(st, "cnt", [128, 512], F32)
        r = Reg()
        r_cnt = Reg()
        S.op("dve", lambda e: e.memset(lo[:], 0.0), writes=[r])
        S.op("dve", lambda e: e.memset(hi[:], 1.0), writes=[r])

        def bc(t):
            return t[:, :].unsqueeze(1).to_broadcast([128, NT, N_EXP])
        for it in range(N_BISECT):
            tt(mid[:], lo[:], hi[:], ALU.add, [r], [r])
            ts(mid[:], mid[:], 0.5, None, ALU.mult, None, [r], [r])
            tt(cmp_[:], C.aff[:], bc(mid), ALU.is_gt, [r, C.r_aff], [r])
            S.op("dve", lambda e: e.tensor_reduce(out=cntp[:], in_=cmp_[:].rearrange("p t e -> p e t"), axis=AX.X, op=ALU.add),
                 reads=[r], writes=[r])
            mm(cnt[:, 0:N_EXP], C.onesf[:], cntp[:], True, True, [r, C.r], [r_cnt])
            ts(ge[:], cnt[:, 0:N_EXP], float(CAP), None, ALU.is_ge, None, [r_cnt], [r])
            tt(dl[:], mid[:], lo[:], ALU.subtract, [r], [r])
            tt(dl[:], dl[:], ge[:], ALU.mult, [r], [r])
            tt(lo[:], lo[:], dl[:], ALU.add, [r], [r])
            tt(dl[:], hi[:], mid[:], ALU.subtract, [r], [r])
            tt(dl[:], dl[:], ge[:], ALU.mult, [r], [r])
            tt(hi[:], mid[:], dl[:], ALU.add, [r], [r])
        tt(cmp_[:], C.aff[:], bc(lo), ALU.is_gt, [r, C.r_aff], [r])
        tt(cmp_[:], cmp_[:], C.aff[:], ALU.mult, [r, C.r_aff], [r])
        S.dma("sp", Sx.gm.rearrange("(t p) e -> p t e", p=128), cmp_[:], reads=[r], writes=[R.gm])
        S.emit_phase()


def phase_e(K, top):
    nc, S, I, C, Sx, R, sb, ps = K.nc, K.S, K.I, K.C, K.Sx, K.R, K.sb, K.ps
    mm, tr, act, ts, tt, stt, recip, cp = K.mm, K.tr, K.act, K.ts, K.tt, K.stt, K.recip, K.cp
    NOT = OWN // 128
    with ExitStack() as st0:
        own = sb(st0, "own", [128, NOT], I32)
        h3T = sb(st0, "h3T", [128, NCH, OWN], BF16)
        acc = sb(st0, "acc", [128, NOT, D], F32)
        gmo = sb(st0, "gmo_", [128, NOT, N_EXP], F32)
        r_own, r_h3T, r_gmo = Reg(const=True), Reg(const=True), Reg(const=True)
        r_acc = [Reg() for _ in range(NOT)]
        with ExitStack() as st:
            h3o = sb(st, "h3o", [128, NOT, D], BF16)
            r_h3o = [Reg() for _ in range(NOT)]
            tpb = [ps(st, "tpe%d" % i, [128, 512], F32) for i in range(2)]
            r_tpb = [Reg() for _ in range(2)]
            S.dma("sp", own[:], I.own, writes=[r_own])
            for j in range(NOT):
                off = bass.IndirectOffsetOnAxis(ap=own[:, j:j + 1], axis=0)
                S.dma_fn("pool", lambda e, j=j, off=off: e.indirect_dma_start(out=h3o[:, j, :], out_offset=None, in_=Sx.h3, in_offset=off),
                         reads=[r_own] + R.h3, writes=[r_h3o[j]])
                S.dma_fn("pool", lambda e, j=j, off=off: e.indirect_dma_start(out=acc[:, j, :], out_offset=None, in_=Sx.x2, in_offset=off),
                         reads=[r_own] + R.x2, writes=[r_acc[j]])
                S.dma_fn("pool", lambda e, j=j, off=off: e.indirect_dma_start(out=gmo[:, j, :], out_offset=None, in_=Sx.gm, in_offset=off),
                         reads=[r_own, R.gm], writes=[r_gmo])
            k = 0
            for j in range(NOT):
                for hh in range(2):
                    pb = k % 2
                    k += 1
                    tpv = tpb[pb][:].bitcast(BF16)
                    for c in range(8):
                        cc = hh * 8 + c
                        tr(tpv[:, c * 128:(c + 1) * 128], h3o[:, j, cc * 128:(cc + 1) * 128], [r_h3o[j]], [r_tpb[pb]])
                    cp(h3T[:, hh * 8:hh * 8 + 8, j * 128:(j + 1) * 128], tpv.rearrange("p (c n) -> p c n", c=8), [r_tpb[pb]], [r_h3T],
                       eng=("act" if hh == 0 else "dve"))
            S.emit_phase()
        with ExitStack() as st:
            FP = 256
            NFP = D // FP
            wg = [sb(st, "wg%d" % i, [128, NCH, FP], BF16) for i in range(2)]
            wu = [sb(st, "wu%d" % i, [128, NCH, FP], BF16) for i in range(2)]
            wd = [sb(st, "wd%d" % i, [128, NCH, FP], BF16) for i in range(2)]
            r_wg = [Reg() for _ in range(2)]
            r_wu = [Reg() for _ in range(2)]
            r_wd = [Reg() for _ in range(2)]
            actT = sb(st, "actT", [128, NCH, OWN], BF16)
            r_actT = [Reg() for _ in range(NCH)]
            sg = [sb(st, "sg%d" % i, [128, 512], F32) for i in range(2)]
            r_sg = [Reg() for _ in range(2)]
            Gp = [ps(st, "Gp%d" % i, [128, 512], F32) for i in range(2)]
            Up = [ps(st, "Up%d" % i, [128, 512], F32) for i in range(2)]
            Yp = [ps(st, "Yp%d" % i, [128, 512], F32) for i in range(3)]
            r_Gp = [Reg() for _ in range(2)]
            r_Up = [Reg() for _ in range(2)]
            r_Yp = [Reg() for _ in range(3)]
            wi = 0
            di = 0
            gi = 0
            yi = 0
            for ex in range(N_EXP):
                wgv = I.w_gate[ex].rearrange("(c p) f -> p c f", p=128)
                wuv = I.w_up[ex].rearrange("(c p) f -> p c f", p=128)
                wdv = I.w_down[ex].rearrange("(c p) f -> p c f", p=128)
                for fp in range(NFP):
                    wb = wi % 2
                    wi += 1
                    S.dma("pool", wg[wb][:], wgv[:, :, fp * FP:(fp + 1) * FP], writes=[r_wg[wb]])
                    S.dma("pool", wu[wb][:], wuv[:, :, fp * FP:(fp + 1) * FP], writes=[r_wu[wb]])
                    for f2 in range(FP // 128):
                        fc = fp * (FP // 128) + f2
                        for half in range(2):
                            gb = gi % 2
                            gi += 1
                            cs = slice(half * 512, (half + 1) * 512)
                            for c in range(NCH):
                                mm(Gp[gb][:], wg[wb][:, c, f2 * 128:(f2 + 1) * 128], h3T[:, c, cs], c == 0, c == NCH - 1,
                                   [r_wg[wb], r_h3T], [r_Gp[gb]])
                            for c in range(NCH):
                                mm(Up[gb][:], wu[wb][:, c, f2 * 128:(f2 + 1) * 128], h3T[:, c, cs], c == 0, c == NCH - 1,
                                   [r_wu[wb], r_h3T], [r_Up[gb]])
                            act(sg[gb][:], Gp[gb][:], AF.Silu, [r_Gp[gb]], [r_sg[gb]])
                            tt(actT[:, fc, cs], sg[gb][:], Up[gb][:], ALU.mult, [r_sg[gb], r_Up[gb]], [r_actT[fc]])
                for dp in range(NFP):
                    db = di % 2
                    di += 1
                    S.dma("pool", wd[db][:], wdv[:, :, dp * FP:(dp + 1) * FP], writes=[r_wd[db]])
                    for j in range(NOT):
                        yb = yi % 3
                        yi += 1
                        for fc in range(NCH):
                            mm(Yp[yb][:, 0:FP], actT[:, fc, j * 128:(j + 1) * 128], wd[db][:, fc, :], fc == 0, fc == NCH - 1,
                               [r_actT[fc], r_wd[db]], [r_Yp[yb]])
                        stt(acc[:, j, dp * FP:(dp + 1) * FP], Yp[yb][:, 0:FP], gmo[:, j, ex:ex + 1], acc[:, j, dp * FP:(dp + 1) * FP],
                            ALU.mult, ALU.add, [r_Yp[yb], r_gmo, r_acc[j]], [r_acc[j]])
            outs = []
            for j in range(NOT):
                outs.append(S.dma("sp", K.out[j * 128:(j + 1) * 128, :], acc[:, j, :], reads=[r_acc[j]], writes=[]))
            S.finish(outs)
            S.emit_phase()


_NC_CACHE = {}


def _core_inputs(inp, c, consts):
    b, q = c // 4, c % 4
    m = {}
    m["x"] = np.ascontiguousarray(inp["x"][b], dtype=np.float32)
    m["mem"] = np.ascontiguousarray(inp["mem"][b], dtype=np.float32)
    m["positions"] = np.ascontiguousarray(inp["positions"][b]).astype(np.int32)
    own = (q * OWN + np.arange(OWN)).reshape(OWN // 128, 128).T.astype(np.int32)
    m["own_idx"] = np.ascontiguousarray(own)
    for n in ("g_mix", "g_cross", "g_mem", "g_moe", "g_qa", "g_ka", "g_qb", "g_kb", "g_qm", "g_km", "g_oa", "g_ob",
              "sink_b", "w_in", "w_out", "w_q_mem", "w_kv_mem", "w_o_mem", "w_router", "w_gate", "w_up", "w_down"):
        m[n] = np.ascontiguousarray(np.asarray(inp[n])[0], dtype=np.float32)
    m.update(consts)
    return m


def kernel(**inputs):
    inp = {k: np.asarray(v) for k, v in inputs.items()}
    if "nc" not in _NC_CACHE:
        _NC_CACHE["nc"] = build()
    nc = _NC_CACHE["nc"]
    consts = host_consts()
    in_maps = [_core_inputs(inp, c, consts) for c in range(8)]
    res = run_bass_kernel_spmd(nc, in_maps, core_ids=list(range(8)))
    out = np.zeros((2, SEQ, D), np.float32)
    for c in range(8):
        b, q = c // 4, c % 4
        out[b, q * OWN:(q + 1) * OWN] = np.asarray(res.results[c]["out"], dtype=np.float32)
    return out


MOE_FP = 512
MOE_NW = 6


def moe_ring_setup(K, stack):
    S, I, sb = K.S, K.I, K.sb
    FP, NW = MOE_FP, MOE_NW
    NFP = D // FP
    PPE = 3 * NFP
    K.Wb = [sb(stack, "Wb%d" % i, [128, NCH, FP], BF16) for i in range(NW)]
    K.r_W = [Reg() for _ in range(NW)]
    ring = {"next": 0}

    def piece_src(p):
        ex, k = divmod(p, PPE)
        if k < 2 * NFP:
            fp, gu = divmod(k, 2)
            w = I.w_gate if gu == 0 else I.w_up
            return w[ex].rearrange("(c p) f -> p c f", p=128)[:, :, fp * FP:(fp + 1) * FP]
        dp = k - 2 * NFP
        return I.w_down[ex].rearrange("(c p) f -> p c f", p=128)[:, :, dp * FP:(dp + 1) * FP]

    def ensure(upto):
        last = N_EXP * PPE - 1
        while ring["next"] <= min(upto, last):
            p = ring["next"]
            ring["next"] += 1
            S.dma("pool", K.Wb[p % NW][:], piece_src(p), writes=[K.r_W[p % NW]])
    K.ring_ensure = ensure
    ensure(NW - 1)


def phase_e2(K, top):
    nc, S, I, C, Sx, R, sb, ps = K.nc, K.S, K.I, K.C, K.Sx, K.R, K.sb, K.ps
    mm, tr, act, ts, tt, stt, recip, cp = K.mm, K.tr, K.act, K.ts, K.tt, K.stt, K.recip, K.cp
    NOT = OWN // 128
    NS = CAP // 128
    NTG = 3 + N_EXP
    with ExitStack() as st0:
        own = sb(st0, "own", [128, NOT], I32)
        gmo = sb(st0, "gmo_", [128, NOT, N_EXP], F32)
        mall = sb(st0, "mall", [128, NOT, N_EXP], F32)
        pos = sb(st0, "pos", [128, NOT, N_EXP], F32)
        TG = sb(st0, "TG", [128, NOT, NTG], F32)
        iota = sb(st0, "iota", [128, 512], F32)
        dummy = sb(st0, "dummy", [128, NS], F32)
        r_c = Reg(const=True)
        with ExitStack() as st:
            ut = sb(st, "ut", [128, 128], F32)
            loc = sb(st, "loc", [128, NOT], F32)
            offs = sb(st, "offs", [128, NOT, N_EXP], F32)
            x2b = [sb(st, "x2b%d" % i, [128, D], F32) for i in range(2)]
            r_x2b = [Reg() for _ in range(2)]
            pc = [ps(st, "pc%d" % i, [128, 512], F32) for i in range(2)]
            r_pc = [Reg() for _ in range(2)]
            r1 = Reg()
            S.dma("sp", own[:], I.own, writes=[r_c])
            S.dma("sp", ut[:], I.c_ut, writes=[r1])
            S.dma("sp", loc[:], I.c_loc, writes=[r1])
            S.dma("sp", iota[:], I.c_iota, writes=[r_c])
            S.dma("sp", dummy[:], I.c_dummy, writes=[r_c])
            for j in range(NOT):
                off = bass.IndirectOffsetOnAxis(ap=own[:, j:j + 1], axis=0)
                S.dma_fn("pool", lambda e, j=j, off=off: e.indirect_dma_start(out=gmo[:, j, :], out_offset=None, in_=Sx.gm, in_offset=off),
                         reads=[r_c, R.gm], writes=[r1])
                b = j % 2
                S.dma_fn("pool", lambda e, b=b, off=off: e.indirect_dma_start(out=x2b[b][:], out_offset=None, in_=Sx.x2, in_offset=off),
                         reads=[r_c] + R.x2, writes=[r_x2b[b]])
                S.dma("sp", Sx.accd[j * 128:(j + 1) * 128, :], x2b[b][:], reads=[r_x2b[b]], writes=[R.accd])
            ts(mall[:], gmo[:], 0.0, None, ALU.is_gt, None, [r1], [r1])
            flat = mall[:].rearrange("p j e -> p (j e)")
            mm(pc[0][:, 0:NOT * N_EXP], ut[:], flat, True, True, [r1], [r_pc[0]])
            mm(pc[1][:, 0:NOT * N_EXP], C.onesf[:], flat, True, True, [r1, C.r], [r_pc[1]])
            tot = pc[1][:, 0:NOT * N_EXP].rearrange("p (j e) -> p j e", e=N_EXP)
            cum = pc[0][:, 0:NOT * N_EXP].rearrange("p (j e) -> p j e", e=N_EXP)
            S.op("dve", lambda e: e.memset(offs[:, 0, :], 0.0), writes=[r1])
            for j in range(1, NOT):
                tt(offs[:, j, :], offs[:, j - 1, :], tot[:, j - 1, :], ALU.add, [r1, r_pc[1]], [r1])
            tt(pos[:], cum, offs[:], ALU.add, [r_pc[0], r1], [r1])
            ts(pos[:], pos[:], -1.0, None, ALU.add, None, [r1], [r1])
            cp(TG[:, :, 0], own[:], [r_c], [r1])
            cp(TG[:, :, 1], loc[:], [r1], [r1])
            S.op("dve", lambda e: e.memset(TG[:, :, 2], 1.0), writes=[r1])
            cp(TG[:, :, 3:NTG], gmo[:], [r1], [r1])
            S.emit_phase()
        with ExitStack() as st:
            FP, NFP, NW, PPE = MOE_FP, D // MOE_FP, MOE_NW, 3 * (D // MOE_FP)
            Wb, r_W, ensure = K.Wb, K.r_W, K.ring_ensure
            Sel = [sb(st, "Sel%d" % i, [128, NOT, 128], F32) for i in range(NS)]
            r_Sel = [Reg() for _ in range(NS)]
            sis = sb(st, "sis", [128, NS, 32], F32)
            sidf = sb(st, "sidf", [128, NS], F32)
            gi = [sb(st, "gi%d" % i, [128, NS], I32) for i in range(2)]
            si = [sb(st, "si%d" % i, [128, NS], I32) for i in range(2)]
            gs = [sb(st, "gs%d" % i, [128, NS], F32) for i in range(2)]
            r_sis = Reg()
            r_idx = [Reg() for _ in range(2)]
            xe = sb(st, "xe", [128, NS, D], BF16)
            r_xe = [Reg() for _ in range(NS)]
            xeT1 = sb(st, "xeT", [128, NCH, CAP], BF16)
            xeT = [xeT1, xeT1]
            r_xeT1 = Reg()
            r_xeT = [r_xeT1, r_xeT1]
            actT = sb(st, "actT", [128, NCH, CAP], BF16)
            r_actT = [Reg() for _ in range(NCH)]
            ygs = sb(st, "ygs", [128, NS, D], F32)
            r_ygs = [Reg() for _ in range(NS)]
            sg = [sb(st, "sg%d" % i, [128, 512], F32) for i in range(2)]
            r_sg = [Reg() for _ in range(2)]
            cnt_stg = {"n": 0}

            def wload(dst, r_dst, src):
                cnt_stg["n"] += 1
                S.dma("pool", dst, src, writes=[r_dst])
            Gp = [ps(st, "Gp%d" % i, [128, 512], F32) for i in range(2)]
            Up = [ps(st, "Up%d" % i, [128, 512], F32) for i in range(2)]
            Yp = [ps(st, "Yp%d" % i, [128, 512], F32) for i in range(2)]
            Tq = ps(st, "Tq", [128, 512], F32)
            SIp = ps(st, "SIp", [128, 512], F32)
            r_Gp = [Reg() for _ in range(2)]
            r_Up = [Reg() for _ in range(2)]
            r_Yp = [Reg() for _ in range(2)]
            r_Tq, r_SIp = Reg(), Reg()
            tqv = Tq[:].bitcast(BF16)
            cnt = {"w": 0, "d": 0, "g": 0, "y": 0}

            def prep_sel(ex):
                for s4 in range(NS):
                    for j in range(NOT):
                        ts(Sel[s4][:, j, :], iota[:, s4 * 128:(s4 + 1) * 128], pos[:, j, ex:ex + 1], mall[:, j, ex:ex + 1],
                           ALU.is_equal, ALU.mult, [r_c], [r_Sel[s4]])

            def prep_idx(ex):
                pb = ex % 2
                for s4 in range(NS):
                    sl = s4
                    for j in range(NOT):
                        mm(SIp[:, s4 * 32:s4 * 32 + NTG], Sel[sl][:, j, :], TG[:, j, :], j == 0, j == NOT - 1,
                           [r_Sel[sl], r_c], [r_SIp])
                cp(sis[:], SIp[:, 0:NS * 32].rearrange("p (s k) -> p s k", k=32), [r_SIp], [r_sis])
                cp(gi[pb][:], sis[:, :, 0], [r_sis], [r_idx[pb]])
                tt(sidf[:], sis[:, :, 2], dummy[:], ALU.mult, [r_sis, r_c], [r_sis])
                tt(sidf[:], dummy[:], sidf[:], ALU.subtract, [r_sis, r_c], [r_sis])
                tt(sidf[:], sidf[:], sis[:, :, 1], ALU.add, [r_sis], [r_sis])
                cp(si[pb][:], sidf[:], [r_sis], [r_idx[pb]])
                cp(gs[pb][:], sis[:, :, 3 + ex], [r_sis], [r_idx[pb]])
                for s4 in range(NS):
                    off = bass.IndirectOffsetOnAxis(ap=gi[pb][:, s4:s4 + 1], axis=0)
                    S.dma_fn("pool", lambda e, s4=s4, off=off: e.indirect_dma_start(out=xe[:, s4, :], out_offset=None, in_=Sx.h3, in_offset=off),
                             reads=[r_idx[pb]] + R.h3, writes=[r_xe[s4]])

            def prep_T(ex):
                pb = ex % 2
                k = 0
                for s4 in range(NS):
                    for hh in range(4):
                        for c in range(4):
                            cc = hh * 4 + c
                            tr(tqv[:, c * 128:(c + 1) * 128], xe[:, s4, cc * 128:(cc + 1) * 128], [r_xe[s4]], [r_Tq])
                        cp(xeT[pb][:, hh * 4:hh * 4 + 4, s4 * 128:(s4 + 1) * 128], tqv[:, 0:512].rearrange("p (c n) -> p c n", c=4),
                           [r_Tq], [r_xeT[pb]], eng=("act" if k % 2 == 0 else "dve"))
                        k += 1

            def compute_gu(ex, fp):
                pb = ex % 2
                pg = ex * PPE + 2 * fp
                wgt, wut = Wb[pg % NW], Wb[(pg + 1) % NW]
                rg, ru = r_W[pg % NW], r_W[(pg + 1) % NW]
                for f2 in range(FP // 128):
                    fc = fp * (FP // 128) + f2
                    gb = cnt["g"] % 2
                    cnt["g"] += 1
                    for c in range(NCH):
                        mm(Gp[gb][:], wgt[:, c, f2 * 128:(f2 + 1) * 128], xeT[pb][:, c, :], c == 0, c == NCH - 1,
                           [rg, r_xeT[pb]], [r_Gp[gb]])
                    for c in range(NCH):
                        mm(Up[gb][:], wut[:, c, f2 * 128:(f2 + 1) * 128], xeT[pb][:, c, :], c == 0, c == NCH - 1,
                           [ru, r_xeT[pb]], [r_Up[gb]])
                    act(sg[gb][:], Gp[gb][:], AF.Silu, [r_Gp[gb]], [r_sg[gb]])
                    tt(actT[:, fc, :], sg[gb][:], Up[gb][:], ALU.mult, [r_sg[gb], r_Up[gb]], [r_actT[fc]])
                ensure(pg + 1 + NW)

            def compute_d(ex, dp):
                pb = ex % 2
                pd = ex * PPE + 2 * NFP + dp
                wdt, rd_ = Wb[pd % NW], r_W[pd % NW]
                for s4 in range(NS):
                    yb = cnt["y"] % 2
                    cnt["y"] += 1
                    for fc in range(NCH):
                        mm(Yp[yb][:, 0:FP], actT[:, fc, s4 * 128:(s4 + 1) * 128], wdt[:, fc, :], fc == 0, fc == NCH - 1,
                           [r_actT[fc], rd_], [r_Yp[yb]])
                    if s4 % 2 == 0:
                        ts(ygs[:, s4, dp * FP:(dp + 1) * FP], Yp[yb][:, 0:FP], gs[pb][:, s4:s4 + 1], None, ALU.mult, None,
                           [r_Yp[yb], r_idx[pb]], [r_ygs[s4]])
                    else:
                        act(ygs[:, s4, dp * FP:(dp + 1) * FP], Yp[yb][:, 0:FP], AF.Copy, [r_Yp[yb], r_idx[pb]], [r_ygs[s4]],
                            scale=gs[pb][:, s4:s4 + 1])
                ensure(pd + NW)

            sc_prev = {"ops": []}

            def scatter(ex):
                pb = ex % 2
                extra = list(sc_prev["ops"])
                if R.accd.last_w is not None:
                    extra.append(R.accd.last_w)
                ops = []
                for s4 in range(NS):
                    off = bass.IndirectOffsetOnAxis(ap=si[pb][:, s4:s4 + 1], axis=0)
                    ops.append(S.dma_fn("pool", lambda e, s4=s4, off=off: e.indirect_dma_start(out=Sx.accd, out_offset=off, in_=ygs[:, s4, :],
                                                                                             in_offset=None, compute_op=ALU.add),
                                        reads=[r_idx[pb], r_ygs[s4]], writes=[], extra=extra))
                sc_prev["ops"] = ops

            assert NFP == 4
            prep_sel(0)
            prep_idx(0)
            prep_T(0)
            ensure(NW - 1)
            for ex in range(N_EXP):
                for fp in range(NFP):
                    compute_gu(ex, fp)
                    if fp == NFP - 2 and ex + 1 < N_EXP:
                        prep_sel(ex + 1)
                if ex + 1 < N_EXP:
                    prep_idx(ex + 1)
                for dp in range(NFP):
                    compute_d(ex, dp)
                    if dp == 1 and ex + 1 < N_EXP:
                        prep_T(ex + 1)
                scatter(ex)
            fin = S.dma("sp", K.out, Sx.accd[0:OWN, :], reads=[R.accd], writes=[], extra=sc_prev["ops"])
            S.finish([fin])
            S.emit_phase()
```

```python
import numpy as np
from contextlib import ExitStack
import concourse.bass as bass
import concourse.mybir as mybir
from concourse.bass_utils import run_bass_kernel_spmd

F32 = mybir.dt.float32
BF16 = mybir.dt.bfloat16
I32 = mybir.dt.int32
AF = mybir.ActivationFunctionType
ALU = mybir.AluOpType
AX = mybir.AxisListType

SAME_ENGINE_SYNC = True
MOE_COMPACT = True


class Reg:
    __slots__ = ("name", "last_w", "readers", "const")

    def __init__(self, name="", const=False):
        self.name = name
        self.last_w = None
        self.readers = []
        self.const = const


class Op:
    __slots__ = ("eng", "fn", "deps", "needed", "sig", "is_dma", "waits_only")

    def __init__(self, eng, fn, deps, is_dma=False):
        self.eng = eng
        self.fn = fn
        self.deps = deps
        self.needed = False
        self.sig = None
        self.is_dma = is_dma
        self.waits_only = False


class Sched:
    ENGS = ("pe", "act", "dve", "pool", "sp")
    NDMA = 12

    def __init__(self, nc, stack):
        self.nc = nc
        self.sem = {e: stack.enter_context(nc.semaphore("s_" + e)) for e in self.ENGS}
        self.cnt = {e: 0 for e in self.ENGS}
        self.dsem = {q: [stack.enter_context(nc.semaphore("d_%s%d" % (q, i))) for i in range(self.NDMA)]
                     for q in ("sp", "pool", "act")}
        self.dcnt = {q: 0 for q in ("sp", "pool", "act")}
        self.dlast = {q: [None] * self.NDMA for q in ("sp", "pool", "act")}
        self.ops = {e: [] for e in self.ENGS}
        self.known = {e: {} for e in self.ENGS}
        self.last_op = {e: None for e in self.ENGS}
        self.prev_phase_last = []

    def _mk(self, eng, fn, reads, writes, is_dma=False, extra=()):
        deps = list(extra)
        for r in reads:
            if r.last_w is not None:
                deps.append(r.last_w)
        for w in writes:
            if w.last_w is not None:
                deps.append(w.last_w)
            deps.extend(w.readers)
        op = Op(eng, fn, deps, is_dma)
        for r in reads:
            if not r.const:
                r.readers.append(op)
        for w in writes:
            w.last_w = op
            w.readers = []
        self.ops[eng].append(op)
        return op

    def op(self, eng, fn, reads=(), writes=()):
        return self._mk(eng, fn, reads, writes)

    def dma(self, q, out, in_, reads=(), writes=(), extra=(), **kw):
        return self._mk(q, lambda e: e.dma_start(out=out, in_=in_, **kw), reads, writes, is_dma=True, extra=extra)

    def dma_fn(self, q, fn, reads=(), writes=(), extra=()):
        return self._mk(q, fn, reads, writes, is_dma=True, extra=extra)

    def finish(self, ops):
        o = Op("sp", None, list(ops))
        o.waits_only = True
        self.ops["sp"].append(o)

    def emit_phase(self):
        self.nphase = getattr(self, "nphase", 0) + 1
        with self.nc.named_scope("ph%02d" % self.nphase):
            self._emit_phase()

    def _emit_phase(self):
        nc = self.nc
        engs = {"pe": nc.tensor, "act": nc.scalar, "dve": nc.vector, "pool": nc.gpsimd, "sp": nc.sync}
        barrier = list(self.prev_phase_last)
        for e in self.ENGS:
            for op in self.ops[e]:
                for d in op.deps:
                    if d.eng == op.eng and not d.is_dma:
                        if e in ("pe", "sp") or not SAME_ENGINE_SYNC:
                            continue
                    d.needed = True
        lasts = []
        for e in self.ENGS:
            real = [o for o in self.ops[e] if not o.waits_only]
            if real:
                real[-1].needed = True
                lasts.append(real[-1])
        for e in self.ENGS:
            for op in self.ops[e]:
                if op.waits_only:
                    continue
                if op.is_dma:
                    q = e
                    j = self.dcnt[q]
                    self.dcnt[q] += 1
                    op.sig = (self.dsem[q][j % self.NDMA], 16 * (j // self.NDMA + 1), q, j)
                elif op.needed:
                    self.cnt[e] += 1
                    op.sig = (self.sem[e], self.cnt[e])
        dma_lasts = []
        with nc.Block() as block:
            def run(e):
                def body(eng):
                    known = self.known[e]

                    def wait(sig):
                        s, v = sig[0], sig[1]
                        k = id(s)
                        if known.get(k, 0) < v:
                            eng.wait_ge(s, v)
                            known[k] = v
                    for d in barrier:
                        if d.eng != e or d.is_dma:
                            wait(d.sig)
                    for op in self.ops[e]:
                        for d in op.deps:
                            if d.sig is None:
                                continue
                            if d.eng == e and not d.is_dma:
                                if e in ("pe", "sp") or not SAME_ENGINE_SYNC:
                                    continue
                            wait(d.sig)
                        if op.waits_only:
                            continue
                        if op.is_dma:
                            s, v, q, j = op.sig
                            if j >= self.NDMA:
                                wait((s, v - 16))
                            ins = op.fn(eng)
                            ins.then_inc(s, 16)
                        else:
                            ins = op.fn(eng)
                            if op.sig is not None:
                                ins.then_inc(op.sig[0], 1)
                return body
            block.tensor(run("pe"))
            block.scalar(run("act"))
            block.vector(run("dve"))
            block.gpsimd(run("pool"))
            block.sync(run("sp"))
        for q in ("sp", "pool", "act"):
            seen = {}
            for op in self.ops[q]:
                if op.is_dma:
                    seen[id(op.sig[0])] = op
            dma_lasts.extend(seen.values())
        self.prev_phase_last = lasts + dma_lasts + [d for d in self.prev_phase_last if d.is_dma and id(d.sig[0]) not in {id(x.sig[0]) for x in dma_lasts}]
        self.ops = {e: [] for e in self.ENGS}


D = 2048
SEQ = 4096
HD = 128
D_IN = 4608
NCH = D // 128
NT = SEQ // 128
EPS = 1e-6
N_EXP = 16
CAP = 512
OWN = 1024
TWO_PI = float(2.0 * np.pi)
SCALE = float(1.0 / np.sqrt(HD))
HC_COL = [128 * i for i in range(8)] + [1024 + 128 * i for i in range(8)] + \
         [3072 + 128 * i for i in range(8)] + [4096, 4224]
HC_FAM = [0] * 8 + [1] * 8 + [2] * 8 + [3] * 2
V_COLS = [(2048, 512), (2560, 512), (4352, 256)]


def mask_a_np():
    kp = np.arange(128)[:, None]
    qf = np.arange(512)[None, :]
    out = np.zeros((20, 128, 512), np.float32)
    for j in range(20):
        o = (-1024 + 128 * j) + kp - qf
        a = np.abs(o)
        out[j] = (a <= 64).astype(np.float32) + ((o % 4 == 0) & (a <= 256)) + ((o % 16 == 0) & (a <= 1024))
    return out


def mask_b_np():
    kp = np.arange(128)[:, None]
    qf = np.arange(512)[None, :]
    out = np.zeros((6, 128, 512), np.float32)
    for j in range(6):
        o = (-128 + 128 * j) + kp - qf
        out[j] = (np.abs(o) <= 128)
    return out


def host_consts():
    import ml_dtypes
    bf = ml_dtypes.bfloat16
    c = {}
    c["c_ident"] = np.eye(128, dtype=np.float32).astype(bf)
    c["c_identf"] = np.eye(128, dtype=np.float32)
    c["c_ones"] = np.ones((128, 128), np.float32).astype(bf)
    c["c_onesf"] = np.ones((128, 128), np.float32)
    p0 = np.zeros((128, 128), np.float32)
    for m in range(64):
        p0[m + 64, m] = -1.0
    for m in range(64, 128):
        p0[m - 64, m] = 1.0
    c["c_rot"] = p0
    i = np.arange(128) % 64
    c["c_invf"] = (10000.0 ** (-(2.0 * i) / 128.0)).astype(np.float32).reshape(128, 1)
    pp = np.arange(128)
    c["c_ut"] = (pp[:, None] <= pp[None, :]).astype(np.float32)
    c["c_iota"] = np.tile(np.arange(512, dtype=np.float32)[None, :], (128, 1))
    c["c_loc"] = (np.arange(8)[None, :] * 128 + pp[:, None]).astype(np.float32)
    c["c_dummy"] = (1024 + np.arange(4)[None, :] * 128 + pp[:, None]).astype(np.float32)
    def lnm(m):
        out = np.full(m.shape, -3000.0, np.float32)
        nz = m > 0
        out[nz] = np.log(m[nz]) / SCALE
        return out
    c["c_maska"] = np.ascontiguousarray(lnm(mask_a_np()).transpose(1, 0, 2)).astype(bf)
    c["c_maskb"] = np.ascontiguousarray(lnm(mask_b_np()).transpose(1, 0, 2)).astype(bf)
    return c


class Ctx:
    pass


def build(stop_after=99, debug=False):
    nc = bass.Bass("TRN2", target_bir_lowering=False)
    K = Ctx()
    K.nc = nc

    def din(name, shape, dt):
        return nc.dram_tensor(name, list(shape), dt, kind="ExternalInput").ap()

    def dscr(name, shape, dt):
        kind = "ExternalOutput" if debug else "Internal"
        return nc.dram_tensor(name, list(shape), dt, kind=kind).ap()

    I = Ctx()
    I.x = din("x", [SEQ, D], F32)
    I.mem = din("mem", [256, D], F32)
    I.pos = din("positions", [SEQ], I32)
    I.own = din("own_idx", [128, OWN // 128], I32)
    for n in ("g_mix", "g_cross", "g_mem", "g_moe"):
        setattr(I, n, din(n, [D], F32))
    for n in ("g_qa", "g_ka", "g_qb", "g_kb", "g_qm", "g_km"):
        setattr(I, n, din(n, [HD], F32))
    I.sink = din("sink_b", [8], F32)
    I.g_oa = din("g_oa", [1024], F32)
    I.g_ob = din("g_ob", [1024], F32)
    I.w_in = din("w_in", [D, D_IN], F32)
    I.w_out = din("w_out", [D, D], F32)
    I.w_q_mem = din("w_q_mem", [D, 512], F32)
    I.w_kv_mem = din("w_kv_mem", [D, 1024], F32)
    I.w_o_mem = din("w_o_mem", [512, D], F32)
    I.w_router = din("w_router", [D, N_EXP], F32)
    I.w_gate = din("w_gate", [N_EXP, D, D], F32)
    I.w_up = din("w_up", [N_EXP, D, D], F32)
    I.w_down = din("w_down", [N_EXP, D, D], F32)
    I.c_ident = din("c_ident", [128, 128], BF16)
    I.c_identf = din("c_identf", [128, 128], F32)
    I.c_ones = din("c_ones", [128, 128], BF16)
    I.c_onesf = din("c_onesf", [128, 128], F32)
    I.c_rot = din("c_rot", [128, 128], F32)
    I.c_invf = din("c_invf", [128, 1], F32)
    I.c_ut = din("c_ut", [128, 128], F32)
    I.c_iota = din("c_iota", [128, 512], F32)
    I.c_loc = din("c_loc", [128, 8], F32)
    I.c_dummy = din("c_dummy", [128, 4], F32)
    I.c_maska = din("c_maska", [128, 20, 512], BF16)
    I.c_maskb = din("c_maskb", [128, 6, 512], BF16)
    K.I = I
    out = nc.dram_tensor("out", [OWN, D], F32, kind="ExternalOutput").ap()
    K.out = out

    Sx = Ctx()
    Sx.qkt = dscr("s_qkt", [26, 128, SEQ], BF16)
    Sx.v = dscr("s_v", [SEQ, 1280], BF16)
    Sx.ot = dscr("s_ot", [16, 128, SEQ], BF16)
    Sx.x2 = dscr("s_x2", [SEQ, D], F32)
    Sx.h3 = dscr("s_h3", [SEQ, D], BF16)
    Sx.gm = dscr("s_gm", [SEQ, N_EXP], F32)
    Sx.accd = dscr("s_accd", [OWN + CAP, D], F32)
    K.Sx = Sx
    R = Ctx()
    R.qkt = [[Reg("qkt%d_%d" % (h, f)) for f in range(2)] for h in range(26)]
    R.v = [Reg("v%d" % t) for t in range(NT)]
    R.ot = [Reg("ot%d" % h) for h in range(16)]
    R.x2 = [Reg("x2_%d" % t) for t in range(NT)]
    R.h3 = [Reg("h3_%d" % t) for t in range(NT)]
    R.gm = Reg("gm")
    R.accd = Reg("accd")
    K.R = R

    with ExitStack() as top:
        S = Sched(nc, top)
        K.S = S

        uid = [0]

        def sb(stack, name, shape, dt):
            uid[0] += 1
            return stack.enter_context(nc.sbuf_tensor("%s_%d" % (name, uid[0]), list(shape), dt))

        def ps(stack, name, shape, dt):
            uid[0] += 1
            return stack.enter_context(nc.psum_tensor("%s_%d" % (name, uid[0]), list(shape), dt))
        K.sb, K.ps = sb, ps

        def mm(out, lhsT, rhs, start, stop, reads, writes):
            return S.op("pe", lambda e: e.matmul(out, lhsT=lhsT, rhs=rhs, start=start, stop=stop), reads, writes)

        def tr(out, in_, reads, writes, ident=None):
            idn = C.ident[:] if ident is None else ident
            return S.op("pe", lambda e: e.transpose(out=out, in_=in_, identity=idn), list(reads) + [C.r], writes)

        def act(out, in_, func, reads, writes, **kw):
            return S.op("act", lambda e: e.activation(out=out, in_=in_, func=func, **kw), reads, writes)

        def ts(out, in0, s1, s2, op0, op1, reads, writes, eng="dve", **kw):
            if op1 is None:
                return S.op(eng, lambda e: e.tensor_scalar(out=out, in0=in0, scalar1=s1, scalar2=None, op0=op0, **kw), reads, writes)
            return S.op(eng, lambda e: e.tensor_scalar(out=out, in0=in0, scalar1=s1, scalar2=s2, op0=op0, op1=op1, **kw), reads, writes)

        def tt(out, in0, in1, op, reads, writes, eng="dve"):
            return S.op(eng, lambda e: e.tensor_tensor(out=out, in0=in0, in1=in1, op=op), reads, writes)

        def stt(out, in0, scalar, in1, op0, op1, reads, writes, eng="dve"):
            return S.op(eng, lambda e: e.scalar_tensor_tensor(out=out, in0=in0, scalar=scalar, in1=in1, op0=op0, op1=op1), reads, writes)

        def rsq(out, in_, scale, reads, writes):
            S.op("dve", lambda e: e.tensor_scalar(out=out, in0=in_, scalar1=scale, scalar2=EPS, op0=ALU.mult, op1=ALU.add), reads, writes)
            S.op("act", lambda e: e.activation(out=out, in_=out, func=AF.Ln), writes, writes)
            return S.op("act", lambda e: e.activation(out=out, in_=out, func=AF.Exp, scale=-0.5), writes, writes)
        K.rsq = rsq

        def recip(out, in_, reads, writes):
            return S.op("dve", lambda e: e.reciprocal(out=out, in_=in_), reads, writes)

        def cp(out, in_, reads, writes, eng="dve"):
            if eng == "act":
                return S.op("act", lambda e: e.copy(out=out, in_=in_), reads, writes)
            return S.op(eng, lambda e: e.tensor_copy(out=out, in_=in_), reads, writes)
        K.mm, K.tr, K.act, K.ts, K.tt, K.stt, K.recip, K.cp = mm, tr, act, ts, tt, stt, recip, cp

        C = Ctx()
        K.C = C
        C.ident = sb(top, "ident", [128, 128], BF16)
        C.identf = sb(top, "identf", [128, 128], F32)
        C.ones = sb(top, "ones", [128, 128], BF16)
        C.onesf = sb(top, "onesf", [128, 128], F32)
        C.rot0 = sb(top, "rot0", [128, 128], F32)
        C.rotg = sb(top, "rotg", [128, 4, 128], BF16)
        C.invf = sb(top, "invf", [128, 1], F32)
        C.gh = sb(top, "gh", [128, 6], F32)
        C.ss = sb(top, "ssab", [128, 2, NT], F32)
        C.esink = sb(top, "esink", [128, 8], F32)
        C.aff = sb(top, "aff", [128, NT, N_EXP], F32)
        C.r_aff = Reg("aff")
        C.r = Reg("consts", const=True)
        C.r_ss = Reg("ss")
        S.dma("sp", C.ident[:], I.c_ident, writes=[C.r])
        S.dma("sp", C.identf[:], I.c_identf, writes=[C.r])
        S.dma("sp", C.ones[:], I.c_ones, writes=[C.r])
        S.dma("sp", C.onesf[:], I.c_onesf, writes=[C.r])
        S.dma("sp", C.rot0[:], I.c_rot, writes=[C.r])
        S.dma("sp", C.invf[:], I.c_invf, writes=[C.r])
        for i, n in enumerate(("g_qa", "g_ka", "g_qb", "g_kb", "g_qm", "g_km")):
            S.dma("sp", C.gh[:, i:i + 1], getattr(I, n).rearrange("(p o) -> p o", o=1), writes=[C.r])
        S.dma("sp", C.esink[:], I.sink.partition_broadcast(128), writes=[C.r])
        S.op("act", lambda e: e.activation(out=C.esink[:], in_=C.esink[:], func=AF.Exp), reads=[C.r], writes=[C.r])
        for f in range(4):
            S.op("dve", lambda e, f=f: e.tensor_scalar(out=C.rotg[:, f, :], in0=C.rot0[:], scalar1=C.gh[:, f:f + 1],
                                                       scalar2=None, op0=ALU.mult), reads=[C.r], writes=[C.r])
        S.op("dve", lambda e: e.memset(C.ss[:], 0.0), writes=[C.r_ss])

        phase_a(K, top)
        if stop_after >= 2:
            with ExitStack() as st_bc:
                K.wout = sb(st_bc, "wout", [128, NCH, D], BF16)
                go = sb(st_bc, "go", [128, 16], F32)
                r_wo = Reg()
                S.dma("pool", K.wout[:], I.w_out.rearrange("(c p) n -> p c n", p=128), writes=[r_wo])
                S.dma("sp", go[:, 0:8], I.g_oa.rearrange("(c p) -> p c", p=128), writes=[r_wo], allow_slow_non_contiguous=True)
                S.dma("sp", go[:, 8:16], I.g_ob.rearrange("(c p) -> p c", p=128), writes=[r_wo], allow_slow_non_contiguous=True)
                for c in range(NCH):
                    ts(K.wout[:, c, :], K.wout[:, c, :], go[:, c:c + 1], None, ALU.mult, None, [r_wo], [r_wo])
                phase_b(K, top)
                if stop_after >= 3:
                    phase_c(K, top)
        if stop_after >= 4:
            with ExitStack() as st_moe:
                moe_ring_setup(K, st_moe)
                phase_d(K, top)
                if stop_after >= 5:
                    phase_e2(K, top)
        S.emit_phase()
    return nc


PI_LO = 3.1415925


def phase_a(K, top):
    nc, S, I, C, Sx, R, sb, ps = K.nc, K.S, K.I, K.C, K.Sx, K.R, K.sb, K.ps
    HALF = SEQ // 2
    w_in_v = I.w_in.rearrange("(c p) n -> p c n", p=128)
    for hf in range(2):
        with ExitStack() as st_x:
            xnT = sb(st_x, "xnT", [128, NCH, HALF], BF16)
            r_xnT = [Reg("xnT%d" % t) for t in range(16)]
            with ExitStack() as st:
                xs = [sb(st, "xs%d" % i, [128, D], F32) for i in range(2)]
                xn = [sb(st, "xn%d" % i, [128, D], BF16) for i in range(2)]
                junk = sb(st, "junk", [128, D], BF16)
                gbc = sb(st, "gbc", [128, D], F32)
                ssq = sb(st, "ssq", [128, 2], F32)
                rstd = sb(st, "rstd", [128, 2], F32)
                tp = [ps(st, "tp%d" % i, [128, 8, 128], BF16) for i in range(4)]
                r_xs = [Reg() for _ in range(2)]
                r_xn = [Reg() for _ in range(2)]
                r_junk, r_gbc = Reg(), Reg(const=True)
                r_ssq = [Reg() for _ in range(2)]
                r_rstd = [Reg() for _ in range(2)]
                r_tp = [Reg() for _ in range(4)]
                S.dma("sp", gbc[:], I.g_mix.partition_broadcast(128), writes=[r_gbc])
                for t in range(16):
                    b = t % 2
                    tok0 = hf * HALF + t * 128
                    S.dma("sp", xs[b][:], I.x[tok0:tok0 + 128, :], writes=[r_xs[b]])
                    S.op("act", lambda e, b=b: e.activation(out=junk[:], in_=xs[b][:], func=AF.Square,
                                                            accum_out=ssq[:, b:b + 1]),
                         reads=[r_xs[b]], writes=[r_junk, r_ssq[b]])
                    K.rsq(rstd[:, b:b + 1], ssq[:, b:b + 1], 1.0 / D, [r_ssq[b]], [r_rstd[b]])
                    S.op("dve", lambda e, b=b: e.scalar_tensor_tensor(out=xn[b][:], in0=xs[b][:], scalar=rstd[:, b:b + 1],
                                                                      in1=gbc[:], op0=ALU.mult, op1=ALU.mult),
                         reads=[r_xs[b], r_rstd[b], r_gbc], writes=[r_xn[b]])
                    for hh in range(2):
                        pt = tp[2 * b + hh]
                        rp = r_tp[2 * b + hh]
                        for c in range(8):
                            cc = hh * 8 + c
                            S.op("pe", lambda e, pt=pt, c=c, cc=cc, b=b: e.transpose(out=pt[:, c, :], in_=xn[b][:, cc * 128:(cc + 1) * 128],
                                                                                      identity=C.ident[:]),
                                 reads=[r_xn[b], C.r], writes=[rp])
                        eng = "act" if hh == 0 else "dve"
                        if eng == "act":
                            S.op("act", lambda e, pt=pt, hh=hh, t=t: e.copy(out=xnT[:, hh * 8:hh * 8 + 8, t * 128:(t + 1) * 128], in_=pt[:]),
                                 reads=[rp], writes=[r_xnT[t]])
                        else:
                            S.op("dve", lambda e, pt=pt, hh=hh, t=t: e.tensor_copy(out=xnT[:, hh * 8:hh * 8 + 8, t * 128:(t + 1) * 128], in_=pt[:]),
                                 reads=[rp], writes=[r_xnT[t]])
                S.emit_phase()
            with ExitStack() as st:
                cos = sb(st, "cos", [128, HALF], F32)
                sin = sb(st, "sin", [128, HALF], F32)
                posi = sb(st, "posi", [128, 1024], I32)
                ang = sb(st, "ang", [128, 1024], F32)
                kf = sb(st, "kf", [128, 1024], F32)
                ki = sb(st, "ki", [128, 1024], I32)
                r_cs = Reg(const=True)
                r_tmp = Reg()
                for qd in range(2):
                    t0 = hf * HALF + qd * 1024
                    sl = slice(qd * 1024, (qd + 1) * 1024)
                    S.dma("sp", posi[:], I.pos[t0:t0 + 1024].partition_broadcast(128), writes=[r_tmp])
                    S.op("dve", lambda e: e.tensor_copy(out=ang[:], in_=posi[:]), reads=[r_tmp], writes=[r_tmp])
                    S.op("dve", lambda e: e.tensor_scalar(out=ang[:], in0=ang[:], scalar1=C.invf[:, 0:1], scalar2=None, op0=ALU.mult),
                         reads=[r_tmp, C.r], writes=[r_tmp])
                    S.op("dve", lambda e: e.tensor_scalar(out=kf[:], in0=ang[:], scalar1=1.0 / TWO_PI, scalar2=None, op0=ALU.mult),
                         reads=[r_tmp], writes=[r_tmp])
                    S.op("dve", lambda e: e.tensor_copy(out=ki[:], in_=kf[:]), reads=[r_tmp], writes=[r_tmp])
                    S.op("dve", lambda e: e.tensor_copy(out=kf[:], in_=ki[:]), reads=[r_tmp], writes=[r_tmp])
                    S.op("dve", lambda e: e.scalar_tensor_tensor(out=ang[:], in0=kf[:], scalar=-TWO_PI, in1=ang[:], op0=ALU.mult, op1=ALU.add),
                         reads=[r_tmp], writes=[r_tmp])

                    def wrap_and_sin(dst, shift):
                        S.op("dve", lambda e: e.tensor_scalar(out=kf[:], in0=ang[:], scalar1=shift, scalar2=None, op0=ALU.add),
                             reads=[r_tmp], writes=[r_tmp])
                        S.op("dve", lambda e: e.tensor_scalar(out=posi[:].bitcast(F32), in0=kf[:], scalar1=float(np.pi), scalar2=-TWO_PI,
                                                              op0=ALU.is_gt, op1=ALU.mult), reads=[r_tmp], writes=[r_tmp])
                        S.op("dve", lambda e: e.tensor_tensor(out=kf[:], in0=kf[:], in1=posi[:].bitcast(F32), op=ALU.add),
                             reads=[r_tmp], writes=[r_tmp])
                        S.op("dve", lambda e: e.tensor_scalar(out=posi[:].bitcast(F32), in0=kf[:], scalar1=-float(np.pi), scalar2=TWO_PI,
                                                              op0=ALU.is_lt, op1=ALU.mult), reads=[r_tmp], writes=[r_tmp])
                        S.op("dve", lambda e: e.tensor_tensor(out=kf[:], in0=kf[:], in1=posi[:].bitcast(F32), op=ALU.add),
                             reads=[r_tmp], writes=[r_tmp])
                        S.op("dve", lambda e: e.tensor_scalar(out=kf[:], in0=kf[:], scalar1=PI_LO, scalar2=-PI_LO, op0=ALU.min, op1=ALU.max),
                             reads=[r_tmp], writes=[r_tmp])
                        S.op("act", lambda e: e.activation(out=dst, in_=kf[:], func=AF.Sin), reads=[r_tmp], writes=[r_cs, r_tmp])
                    wrap_and_sin(sin[:, sl], 0.0)
                    wrap_and_sin(cos[:, sl], float(np.pi / 2))

                wq = [sb(st, "wq%d" % i, [128, NCH, 128], BF16) for i in range(2)]
                r_wq = [Reg() for _ in range(2)]
                q2 = [sb(st, "q2_%d" % i, [128, 512], BF16) for i in range(2)]
                qb = [sb(st, "qb_%d" % i, [128, 512], BF16) for i in range(2)]
                rs = [sb(st, "rs_%d" % i, [128, 512], F32) for i in range(2)]
                ta = [sb(st, "ta_%d" % i, [128, 512], F32) for i in range(2)]
                tb = [sb(st, "tb_%d" % i, [128, 512], F32) for i in range(2)]
                stage = [sb(st, "stg%d" % i, [128, HALF], BF16) for i in range(2)]
                r_q2 = [Reg() for _ in range(2)]
                r_qb = [Reg() for _ in range(2)]
                r_rs = [Reg() for _ in range(2)]
                r_ta = [Reg() for _ in range(2)]
                r_tb = [Reg() for _ in range(2)]
                r_stage = [Reg() for _ in range(2)]
                qp = [ps(st, "qp%d" % i, [128, 512], F32) for i in range(2)]
                sp_ = [ps(st, "ssp%d" % i, [128, 512], F32) for i in range(2)]
                rp_ = [ps(st, "rtp%d" % i, [128, 512], F32) for i in range(2)]
                r_qp = [Reg() for _ in range(2)]
                r_sp = [Reg() for _ in range(2)]
                r_rp = [Reg() for _ in range(2)]
                it = 0
                for hc in range(26):
                    wb = hc % 2
                    fam = HC_FAM[hc]
                    c0 = HC_COL[hc]
                    S.dma("pool", wq[wb][:], w_in_v[:, :, c0:c0 + 128], writes=[r_wq[wb]])
                    for blk in range(4):
                        b = it % 2
                        it += 1
                        cs = slice(blk * 512, (blk + 1) * 512)
                        for c in range(NCH):
                            S.op("pe", lambda e, b=b, wb=wb, c=c, cs=cs: e.matmul(qp[b][:], lhsT=wq[wb][:, c, :], rhs=xnT[:, c, cs],
                                                                                    start=(c == 0), stop=(c == NCH - 1)),
                                 reads=[r_wq[wb]] + r_xnT[blk * 4:blk * 4 + 4], writes=[r_qp[b]])
                        S.op("act", lambda e, b=b: e.activation(out=q2[b][:], in_=qp[b][:], func=AF.Square),
                             reads=[r_qp[b]], writes=[r_q2[b]])
                        S.op("act", lambda e, b=b: e.copy(out=qb[b][:], in_=qp[b][:]), reads=[r_qp[b]], writes=[r_qb[b]])
                        S.op("pe", lambda e, b=b: e.matmul(sp_[b][:], lhsT=C.ones[:], rhs=q2[b][:], start=True, stop=True),
                             reads=[r_q2[b], C.r], writes=[r_sp[b]])
                        S.op("pe", lambda e, b=b, fam=fam: e.matmul(rp_[b][:], lhsT=C.rotg[:, fam, :], rhs=qb[b][:], start=True, stop=True),
                             reads=[r_qb[b], C.r], writes=[r_rp[b]])
                        K.rsq(rs[b][:], sp_[b][:], 1.0 / HD, [r_sp[b]], [r_rs[b]])
                        S.op("dve", lambda e, b=b, fam=fam, cs=cs: e.scalar_tensor_tensor(out=ta[b][:], in0=qp[b][:], scalar=C.gh[:, fam:fam + 1],
                                                                                         in1=cos[:, cs], op0=ALU.mult, op1=ALU.mult),
                             reads=[r_qp[b], r_cs, C.r], writes=[r_ta[b]])
                        S.op("dve", lambda e, b=b, cs=cs: e.tensor_tensor(out=tb[b][:], in0=rp_[b][:], in1=sin[:, cs], op=ALU.mult),
                             reads=[r_rp[b], r_cs], writes=[r_tb[b]])
                        S.op("dve", lambda e, b=b: e.tensor_tensor(out=ta[b][:], in0=ta[b][:], in1=tb[b][:], op=ALU.add),
                             reads=[r_ta[b], r_tb[b]], writes=[r_ta[b]])
                        S.op("dve", lambda e, b=b, wb=wb, cs=cs: e.tensor_tensor(out=stage[wb][:, cs], in0=ta[b][:], in1=rs[b][:], op=ALU.mult),
                             reads=[r_ta[b], r_rs[b]], writes=[r_stage[wb]])
                    S.dma("sp", Sx.qkt[hc, :, hf * HALF:(hf + 1) * HALF], stage[wb][:], reads=[r_stage[wb]], writes=[R.qkt[hc][hf]])
                S.emit_phase()
            with ExitStack() as st:
                wv = sb(st, "wv", [128, NCH, 1280], BF16)
                r_wv = [Reg() for _ in range(3)]
                vst = [sb(st, "vst%d" % i, [128, 1280], BF16) for i in range(2)]
                r_vst = [Reg() for _ in range(2)]
                vp = [ps(st, "vp%d" % i, [128, 512], F32) for i in range(4)]
                r_vp = [Reg() for _ in range(4)]
                off = 0
                pieces = []
                for i, (c0, n) in enumerate(V_COLS):
                    S.dma("pool", wv[:, :, off:off + n], w_in_v[:, :, c0:c0 + n], writes=[r_wv[i]])
                    pieces.append((off, n))
                    off += n
                it = 0
                for t in range(16):
                    b = t % 2
                    tok0 = hf * HALF + t * 128
                    for i, (o, n) in enumerate(pieces):
                        pb = it % 4
                        it += 1
                        for c in range(NCH):
                            S.op("pe", lambda e, pb=pb, c=c, t=t, o=o, n=n: e.matmul(vp[pb][:, 0:n], lhsT=xnT[:, c, t * 128:(t + 1) * 128],
                                                                                     rhs=wv[:, c, o:o + n], start=(c == 0), stop=(c == NCH - 1)),
                                 reads=[r_wv[i], r_xnT[t]], writes=[r_vp[pb]])
                        if i % 2 == 0:
                            S.op("act", lambda e, pb=pb, b=b, o=o, n=n: e.copy(out=vst[b][:, o:o + n], in_=vp[pb][:, 0:n]),
                                 reads=[r_vp[pb]], writes=[r_vst[b]])
                        else:
                            S.op("dve", lambda e, pb=pb, b=b, o=o, n=n: e.tensor_copy(out=vst[b][:, o:o + n], in_=vp[pb][:, 0:n]),
                                 reads=[r_vp[pb]], writes=[r_vst[b]])
                    S.dma("sp", Sx.v[tok0:tok0 + 128, :], vst[b][:], reads=[r_vst[b]], writes=[R.v[hf * 16 + t]])
                S.emit_phase()


def phase_b(K, top):
    nc, S, I, C, Sx, R, sb, ps = K.nc, K.S, K.I, K.C, K.Sx, K.R, K.sb, K.ps
    mm, tr, act, ts, tt, stt, recip, cp = K.mm, K.tr, K.act, K.ts, K.tt, K.stt, K.recip, K.cp
    LOOK = 3
    with ExitStack() as st:
        maskA = sb(st, "maskA", [128, 20, 512], BF16)
        maskB = sb(st, "maskB", [128, 6, 512], BF16)
        r_mask = Reg(const=True)
        S.dma("sp", maskA[:], I.c_maska, writes=[r_mask])
        S.dma("sp", maskB[:], I.c_maskb, writes=[r_mask])
        QT = [sb(st, "QT%d" % i, [128, SEQ], BF16) for i in range(2)]
        KT = [sb(st, "KT%d" % i, [128, SEQ], BF16) for i in range(2)]
        V1 = [sb(st, "V1%d" % i, [128, NT, 130], BF16) for i in range(2)]
        OTs = [sb(st, "OTs%d" % i, [128, SEQ], BF16) for i in range(2)]
        r_QT = [Reg() for _ in range(2)]
        r_KT = [Reg() for _ in range(2)]
        r_V1 = [Reg() for _ in range(2)]
        r_OTs = [Reg() for _ in range(2)]
        NE = 6
        NSP = 3
        E = [sb(st, "E%d" % i, [128, 512], BF16) for i in range(NE)]
        Pm = [sb(st, "Pm%d" % i, [128, 512], BF16) for i in range(NE)]
        r_E = [Reg() for _ in range(NE)]
        r_Pm = [Reg() for _ in range(NE)]
        NO = 8
        Osb = [sb(st, "Osb%d" % i, [128, 130], F32) for i in range(NO)]
        den = [sb(st, "den%d" % i, [128, 1], F32) for i in range(NO)]
        sst = [sb(st, "sst%d" % i, [128, 1], F32) for i in range(NO)]
        obf = [sb(st, "obf%d" % i, [128, 128], BF16) for i in range(NO)]
        junk = sb(st, "junkb", [128, 128], BF16)
        r_Osb = [Reg() for _ in range(NO)]
        r_den = [Reg() for _ in range(NO)]
        r_sst = [Reg() for _ in range(NO)]
        r_obf = [Reg() for _ in range(NO)]
        r_junk = Reg()
        Sp = [ps(st, "Sp%d" % i, [128, 512], F32) for i in range(NSP)]
        Op_ = [ps(st, "Op%d" % i, [128, 512], F32) for i in range(4)]
        Tp = ps(st, "Tp", [128, 512], F32)
        tpv = Tp[:].bitcast(BF16)
        r_Sp = [Reg() for _ in range(NSP)]
        r_Op = [Reg() for _ in range(4)]
        r_Tp = [Reg() for _ in range(8)]
        for i in range(2):
            S.op("dve", lambda e, i=i: e.memset(V1[i][:, :, 128:130], 1.0), writes=[r_V1[i]])

        iters = []
        for h in range(16):
            isA = h < 8
            nj, koff = (20, -1024) if isA else (6, -128)
            for qb in range(8):
                q0 = qb * 512
                js = [j for j in range(nj) if 0 <= q0 + koff + 128 * j < SEQ]
                half = 1024 if isA else 128
                rng = {}
                for j in js:
                    rel = koff + 128 * j
                    c0 = max(0, rel - half) // 128
                    c1 = min(512, rel + 127 + half + 1 + 127) // 128
                    rng[j] = (c0, min(4, c1))
                for jn, j in enumerate(js):
                    c0, c1 = rng[j]
                    fl = {}
                    for i in range(c0, c1):
                        cov = [jj for jj in js if rng[jj][0] <= i < rng[jj][1]]
                        fl[i] = (j == cov[0], j == cov[-1])
                    iters.append(dict(h=h, qb=qb, j=j, k0=q0 + koff + 128 * j, first=(jn == 0), last=(jn == len(js) - 1),
                                      c0=c0, c1=c1, fl=fl,
                                      hstart=(qb == 0 and jn == 0), hend=(qb == 7 and jn == len(js) - 1)))
        state = {"osb": 0, "ts": 0}
        deferred = []

        def head_cfg(h):
            if h < 8:
                return h, 8 + h, 128 * h, maskA
            kvh = (h - 8) // 4
            return 16 + (h - 8), 24 + kvh, 1024 + 128 * kvh, maskB

        def load_head(h):
            hb = h % 2
            qhc, khc, vcol, _ = head_cfg(h)
            S.dma("sp", QT[hb][:], Sx.qkt[qhc], reads=R.qkt[qhc], writes=[r_QT[hb]])
            S.dma("sp", KT[hb][:], Sx.qkt[khc], reads=R.qkt[khc], writes=[r_KT[hb]])
            S.dma("sp", V1[hb][:, :, 0:128], Sx.v[:, vcol:vcol + 128].rearrange("(t p) d -> p t d", p=128),
                  reads=R.v, writes=[r_V1[hb]])

        def score(n):
            it = iters[n]
            hb = it["h"] % 2
            mask = head_cfg(it["h"])[3]
            sbi, ei = n % NSP, n % NE
            k0, q0, j = it["k0"], it["qb"] * 512, it["j"]
            a0, a1 = it["c0"] * 128, it["c1"] * 128
            mm(Sp[sbi][:, a0:a1], KT[hb][:, k0:k0 + 128], QT[hb][:, q0 + a0:q0 + a1], True, False, [r_KT[hb], r_QT[hb]], [r_Sp[sbi]])
            mm(Sp[sbi][:, a0:a1], C.ident[:], mask[:, j, a0:a1], False, True, [r_mask, C.r], [r_Sp[sbi]])
            act(Pm[ei][:, a0:a1], Sp[sbi][:, a0:a1], AF.Exp, [r_Sp[sbi]], [r_Pm[ei]], scale=SCALE)

        def post(h, qb, i, o):
            hb = h % 2
            grp = 0 if h < 8 else 1
            qt = 4 * qb + i
            if h < 8:
                recip(den[o][:], Osb[o][:, 128:129], [r_Osb[o]], [r_den[o]])
            else:
                tt(den[o][:], Osb[o][:, 128:129], C.esink[:, h - 8:h - 7], ALU.add, [r_Osb[o], C.r], [r_den[o]])
                recip(den[o][:], den[o][:], [r_den[o]], [r_den[o]])
            ts(obf[o][:], Osb[o][:, 0:128], den[o][:, 0:1], None, ALU.mult, None, [r_Osb[o], r_den[o]], [r_obf[o]])
            act(junk[:], obf[o][:], AF.Square, [r_obf[o]], [r_junk, r_sst[o]], accum_out=sst[o][:])
            tt(C.ss[:, grp, qt:qt + 1], C.ss[:, grp, qt:qt + 1], sst[o][:], ALU.add, [r_sst[o], C.r_ss], [C.r_ss])
            tsl = state["ts"] % 8
            state["ts"] += 1
            tr(tpv[:, tsl * 128:(tsl + 1) * 128], obf[o][:], [r_obf[o]], [r_Tp[tsl]])
            cp(OTs[hb][:, qt * 128:(qt + 1) * 128], tpv[:, tsl * 128:(tsl + 1) * 128], [r_Tp[tsl]], [r_OTs[hb]], eng="act")

        def pv(n):
            it = iters[n]
            h, qb = it["h"], it["qb"]
            hb = h % 2
            ei = n % NE
            k0 = it["k0"]
            for i in range(it["c0"], it["c1"]):
                mm(Op_[i][:, 0:129], Pm[ei][:, 128 * i:128 * i + 128], V1[hb][:, k0 // 128, 0:129], it["fl"][i][0], it["fl"][i][1],
                   [r_Pm[ei], r_V1[hb]], [r_Op[i]])
            if it["last"]:
                for i in range(4):
                    o = state["osb"] % NO
                    state["osb"] += 1
                    cp(Osb[o][:, 0:129], Op_[i][:, 0:129], [r_Op[i]], [r_Osb[o]])
                    deferred.append([2, (lambda h=h, qb=qb, i=i, o=o: post(h, qb, i, o))])
            if it["hend"]:
                deferred.append([3, (lambda h=h, hb=hb: S.dma("sp", Sx.ot[h], OTs[hb][:], reads=[r_OTs[hb]], writes=[R.ot[h]]))])

        def tick():
            for d in deferred:
                d[0] -= 1
            while deferred and deferred[0][0] <= 0:
                deferred.pop(0)[1]()

        N = len(iters)
        load_head(0)
        for n in range(N + LOOK):
            if n < N:
                score(n)
            if n - LOOK >= 0:
                pv(n - LOOK)
                if iters[n - LOOK]["hstart"] and iters[n - LOOK]["h"] + 1 < 16:
                    load_head(iters[n - LOOK]["h"] + 1)
            tick()
        while deferred:
            tick()
        S.emit_phase()


def phase_c(K, top):
    nc, S, I, C, Sx, R, sb, ps = K.nc, K.S, K.I, K.C, K.Sx, K.R, K.sb, K.ps
    mm, tr, act, ts, tt, stt, recip, cp = K.mm, K.tr, K.act, K.ts, K.tt, K.stt, K.recip, K.cp
    with ExitStack() as st0:
        KmT = sb(st0, "KmT", [128, 4, 256], BF16)
        Vm = sb(st0, "Vm", [128, 2, 4, 130], BF16)
        r_km = Reg(const=True)
        with ExitStack() as st:
            memx = sb(st, "memx", [128, 2, D], F32)
            memn = sb(st, "memn", [128, 2, D], BF16)
            memT = sb(st, "memT", [128, NCH, 256], BF16)
            gbc = sb(st, "gbcm", [128, D], F32)
            junk = sb(st, "junkc0", [128, D], BF16)
            wkv = sb(st, "wkv", [128, NCH, 1024], BF16)
            ssq = sb(st, "ssqm", [128, 2], F32)
            k2 = sb(st, "k2", [128, 256], BF16)
            rsk = sb(st, "rsk", [128, 256], F32)
            bank = [ps(st, "c0b%d" % i, [128, 512], F32) for i in range(4)]
            rb = [Reg() for _ in range(4)]
            r1 = Reg()
            r_w = Reg()
            S.dma("sp", memx[:], I.mem.rearrange("(t p) d -> p t d", p=128), writes=[r1])
            S.dma("sp", gbc[:], I.g_mem.partition_broadcast(128), writes=[r1])
            S.dma("pool", wkv[:], I.w_kv_mem.rearrange("(c p) n -> p c n", p=128), writes=[r_w])
            S.op("dve", lambda e: e.memset(Vm[:], 1.0), writes=[r_km])
            for mt in range(2):
                act(junk[:], memx[:, mt, :], AF.Square, [r1], [r1], accum_out=ssq[:, mt:mt + 1])
                K.rsq(ssq[:, mt:mt + 1], ssq[:, mt:mt + 1], 1.0 / D, [r1], [r1])
                stt(memn[:, mt, :], memx[:, mt, :], ssq[:, mt:mt + 1], gbc[:], ALU.mult, ALU.mult, [r1], [r1])
                for hh in range(2):
                    tpv = bank[hh][:].bitcast(BF16)
                    for c in range(8):
                        cc = hh * 8 + c
                        tr(tpv[:, c * 128:(c + 1) * 128], memn[:, mt, cc * 128:(cc + 1) * 128], [r1], [rb[hh]])
                    cp(memT[:, hh * 8:hh * 8 + 8, mt * 128:(mt + 1) * 128], tpv[:, 0:1024].rearrange("p (c n) -> p c n", c=8), [rb[hh]], [r1])
            for hm in range(4):
                for c in range(NCH):
                    mm(bank[2][:, 0:256], wkv[:, c, hm * 128:(hm + 1) * 128], memT[:, c, :], c == 0, c == NCH - 1, [r_w, r1], [rb[2]])
                act(k2[:], bank[2][:, 0:256], AF.Square, [rb[2]], [r1])
                mm(bank[3][:, 0:256], C.ones[:], k2[:], True, True, [r1, C.r], [rb[3]])
                K.rsq(rsk[:], bank[3][:, 0:256], 1.0 / HD, [rb[3]], [r1])
                stt(KmT[:, hm, :], bank[2][:, 0:256], C.gh[:, 5:6], rsk[:], ALU.mult, ALU.mult, [rb[2], r1, C.r], [r_km])
            for mt in range(2):
                for c in range(NCH):
                    mm(bank[mt][:], memT[:, c, mt * 128:(mt + 1) * 128], wkv[:, c, 512:1024], c == 0, c == NCH - 1, [r_w, r1], [rb[mt]])
                cp(Vm[:, mt, :, 0:128], bank[mt][:].rearrange("p (h d) -> p h d", h=4), [rb[mt]], [r_km])
            S.emit_phase()
        with ExitStack() as st:
            wout = K.wout
            wq = sb(st, "wqm", [128, NCH, 512], BF16)
            wo = sb(st, "wom", [128, 4, D], BF16)
            wr = sb(st, "wr", [128, NCH, N_EXP], BF16)
            gcr = sb(st, "gcr", [128, D], F32)
            gmo = sb(st, "gmo", [128, D], F32)
            rAB = sb(st, "rAB", [128, 2, NT], F32)
            r_w = Reg(const=True)
            S.dma("pool", wq[:], I.w_q_mem.rearrange("(c p) n -> p c n", p=128), writes=[r_w])
            S.dma("pool", wo[:], I.w_o_mem.rearrange("(c p) n -> p c n", p=128), writes=[r_w])
            S.dma("pool", wr[:], I.w_router.rearrange("(c p) n -> p c n", p=128), writes=[r_w])
            S.dma("sp", gcr[:], I.g_cross.partition_broadcast(128), writes=[r_w])
            S.dma("sp", gmo[:], I.g_moe.partition_broadcast(128), writes=[r_w])
            K.rsq(rAB[:], C.ss[:], 1.0 / 1024, [C.r_ss], [r_w])

            xt = [sb(st, "xt%d" % i, [128, D], F32) for i in range(2)]
            otb = [sb(st, "otb%d" % i, [128, 16, 128], BF16) for i in range(2)]
            x12 = [sb(st, "x12_%d" % i, [128, D], F32) for i in range(2)]
            hn = [sb(st, "hn%d" % i, [128, D], BF16) for i in range(2)]
            hT = [sb(st, "hT%d" % i, [128, NCH, 128], BF16) for i in range(2)]
            junk = sb(st, "junkc1", [128, D], BF16)
            sq = sb(st, "sqc", [128, 4], F32)
            q2 = sb(st, "q2c", [128, 512], BF16)
            rsq = sb(st, "rsqc", [128, 512], F32)
            qn = sb(st, "qnc", [128, 4, 128], BF16)
            E2 = [sb(st, "E2_%d" % i, [128, 4, 128], BF16) for i in range(2)]
            rden = sb(st, "rdenc", [128, 4], F32)
            o2 = sb(st, "o2c", [128, 4, 128], BF16)
            o2T = sb(st, "o2T", [128, 4, 128], BF16)
            ex = sb(st, "exr", [128, N_EXP], F32)
            sume = sb(st, "sume", [128, 1], F32)
            r_xt = [Reg() for _ in range(2)]
            r_otb = [Reg() for _ in range(2)]
            r_x12 = [Reg() for _ in range(2)]
            r_hn = [Reg() for _ in range(2)]
            r_hT = [Reg() for _ in range(2)]
            r_junk, r_sq, r_q2, r_rsq, r_qn, r_rden, r_o2, r_o2T, r_ex, r_sume = [Reg() for _ in range(10)]
            r_E2 = [Reg() for _ in range(2)]
            B = [ps(st, "c1b%d" % i, [128, 512], F32) for i in range(8)]
            rB = [Reg() for _ in range(8)]
            tpv = B[4][:].bitcast(BF16)
            ot_v = Sx.ot.rearrange("h p n -> p h n")

            def rmsnorm_tile(src, r_src, gb, dst, r_dst, col):
                act(junk[:], src, AF.Square, [r_src], [r_junk, r_sq], accum_out=sq[:, col:col + 1])
                K.rsq(sq[:, col:col + 1], sq[:, col:col + 1], 1.0 / D, [r_sq], [r_sq])
                stt(dst, src, sq[:, col:col + 1], gb[:], ALU.mult, ALU.mult, [r_src, r_sq, r_w], [r_dst])

            def transpose16(src, r_src, dst, r_dst):
                for hh in range(2):
                    for c in range(8):
                        cc = hh * 8 + c
                        tr(tpv[:, c * 128:(c + 1) * 128], src[:, cc * 128:(cc + 1) * 128], [r_src], [rB[4]])
                    cp(dst[:, hh * 8:hh * 8 + 8, :], tpv.rearrange("p (c n) -> p c n", c=8), [rB[4]], [r_dst],
                       eng=("act" if hh == 0 else "dve"))

            def load(t):
                b = t % 2
                S.dma("sp", xt[b][:], I.x[t * 128:(t + 1) * 128, :], writes=[r_xt[b]])
                S.dma("sp", otb[b][:], ot_v[:, :, t * 128:(t + 1) * 128], reads=R.ot, writes=[r_otb[b]])
            def s1_cg(t, cg):
                b = t % 2
                X = x12[b]
                pa, pb = B[(cg % 2) * 2], B[(cg % 2) * 2 + 1]
                ra, rbb = rB[(cg % 2) * 2], rB[(cg % 2) * 2 + 1]
                cs = slice(cg * 512, (cg + 1) * 512)
                for c in range(8):
                    mm(pa[:], otb[b][:, c, :], wout[:, c, cs], c == 0, c == 7, [r_otb[b], r_w], [ra])
                for c in range(8, 16):
                    mm(pb[:], otb[b][:, c, :], wout[:, c, cs], c == 8, c == 15, [r_otb[b], r_w], [rbb])
                stt(X[:, cs], pa[:], rAB[:, 0, t:t + 1], xt[b][:, cs], ALU.mult, ALU.add, [ra, r_xt[b], r_w], [r_x12[b]])
                stt(X[:, cs], pb[:], rAB[:, 1, t:t + 1], X[:, cs], ALU.mult, ALU.add, [rbb, r_x12[b], r_w], [r_x12[b]])

            def nxt(t, cg):
                if t + 1 < NT:
                    s1_cg(t + 1, cg)

            load(0)
            load(1)
            for cg in range(4):
                s1_cg(0, cg)
            for t in range(NT):
                b = t % 2
                X = x12[b]
                rmsnorm_tile(X[:], r_x12[b], gcr, hn[0][:], r_hn[0], 0)
                nxt(t, 0)
                transpose16(hn[0], r_hn[0], hT[0], r_hT[0])
                for hm in range(4):
                    for c in range(NCH):
                        mm(B[5][:, hm * 128:(hm + 1) * 128], wq[:, c, hm * 128:(hm + 1) * 128], hT[0][:, c, :], c == 0, c == NCH - 1,
                           [r_w, r_hT[0]], [rB[5]])
                act(q2[:], B[5][:], AF.Square, [rB[5]], [r_q2])
                nxt(t, 1)
                mm(B[6][:], C.ones[:], q2[:], True, True, [r_q2, C.r], [rB[6]])
                K.rsq(rsq[:], B[6][:], 1.0 / HD, [rB[6]], [r_rsq])
                stt(qn[:].rearrange("p h n -> p (h n)"), B[5][:], C.gh[:, 4:5], rsq[:], ALU.mult, ALU.mult, [rB[5], r_rsq, C.r], [r_qn])
                nxt(t, 2)
                for mt in range(2):
                    bk = 7 if mt == 0 else 4
                    for hm in range(4):
                        mm(B[bk][:, hm * 128:(hm + 1) * 128], KmT[:, hm, mt * 128:(mt + 1) * 128], qn[:, hm, :], True, True,
                           [r_km, r_qn], [rB[bk]])
                    act(E2[mt][:].rearrange("p h n -> p (h n)"), B[bk][:], AF.Exp, [rB[bk]], [r_E2[mt]], scale=SCALE)
                nxt(t, 3)
                for hm in range(4):
                    bk = 5 if hm < 2 else 6
                    o0 = (hm % 2) * 130
                    for mt in range(2):
                        mm(B[bk][:, o0:o0 + 129], E2[mt][:, hm, :], Vm[:, mt, hm, 0:129], mt == 0, mt == 1, [r_E2[mt], r_km], [rB[bk]])
                for hm in range(4):
                    bk = 5 if hm < 2 else 6
                    o0 = (hm % 2) * 130
                    recip(rden[:, hm:hm + 1], B[bk][:, o0 + 128:o0 + 129], [rB[bk]], [r_rden])
                    ts(o2[:, hm, :], B[bk][:, o0:o0 + 128], rden[:, hm:hm + 1], None, ALU.mult, None, [rB[bk], r_rden], [r_o2])
                for hm in range(4):
                    tr(tpv[:, hm * 128:(hm + 1) * 128], o2[:, hm, :], [r_o2], [rB[4]])
                cp(o2T[:].rearrange("p h n -> p (h n)"), tpv[:, 0:512], [rB[4]], [r_o2T], eng="act")
                for cg in range(4):
                    pa = B[cg % 4]
                    ra = rB[cg % 4]
                    cs = slice(cg * 512, (cg + 1) * 512)
                    for hm in range(4):
                        mm(pa[:], o2T[:, hm, :], wo[:, hm, cs], hm == 0, hm == 3, [r_o2T, r_w], [ra])
                    tt(X[:, cs], pa[:], X[:, cs], ALU.add, [ra, r_x12[b]], [r_x12[b]])
                S.dma("sp", Sx.x2[t * 128:(t + 1) * 128, :], X[:], reads=[r_x12[b]], writes=[R.x2[t]])
                rmsnorm_tile(X[:], r_x12[b], gmo, hn[1][:], r_hn[1], 1)
                S.dma("sp", Sx.h3[t * 128:(t + 1) * 128, :], hn[1][:], reads=[r_hn[1]], writes=[R.h3[t]])
                if t + 2 < NT:
                    load(t + 2)
                transpose16(hn[1], r_hn[1], hT[1], r_hT[1])
                for c in range(NCH):
                    mm(B[5][:, 0:N_EXP], hT[1][:, c, :], wr[:, c, :], c == 0, c == NCH - 1, [r_hT[1], r_w], [rB[5]])
                act(ex[:], B[5][:, 0:N_EXP], AF.Exp, [rB[5]], [r_ex, r_sume], accum_out=sume[:])
                recip(sume[:], sume[:], [r_sume], [r_sume])
                ts(C.aff[:, t, :], ex[:], sume[:, 0:1], None, ALU.mult, None, [r_ex, r_sume], [C.r_aff])
            S.emit_phase()


N_BISECT = 34


def phase_d(K, top):
    nc, S, I, C, Sx, R, sb, ps = K.nc, K.S, K.I, K.C, K.Sx, K.R, K.sb, K.ps
    mm, tr, act, ts, tt, stt, recip, cp = K.mm, K.tr, K.act, K.ts, K.tt, K.stt, K.recip, K.cp
    with ExitStack() as st:
        lo = sb(st, "lo", [128, N_EXP], F32)
        hi = sb(st, "hi", [128, N_EXP], F32)
        mid = sb(st, "mid", [128, N_EXP], F32)
        ge = sb(st, "ge", [128, N_EXP], F32)
        dl = sb(st, "dl", [128, N_EXP], F32)
        cntp = sb(st, "cntp", [128, N_EXP], F32)
        cmp_ = sb(st, "cmp", [128, NT, N_EXP], F32)
        cnt = ps(st, "cnt", [128, 512], F32)
        r = Reg()
        r_cnt = Reg()
        S.op("dve", lambda e: e.memset(lo[:], 0.0), writes=[r])
        S.op("dve", lambda e: e.memset(hi[:], 1.0), writes=[r])

        def bc(t):
            return t[:, :].unsqueeze(1).to_broadcast([128, NT, N_EXP])
        for it in range(N_BISECT):
            tt(mid[:], lo[:], hi[:], ALU.add, [r], [r])
            ts(mid[:], mid[:], 0.5, None, ALU.mult, None, [r], [r])
            tt(cmp_[:], C.aff[:], bc(mid), ALU.is_gt, [r, C.r_aff], [r])
            S.op("dve", lambda e: e.tensor_reduce(out=cntp[:], in_=cmp_[:].rearrange("p t e -> p e t"), axis=AX.X, op=ALU.add),
                 reads=[r], writes=[r])
            mm(cnt[:, 0:N_EXP], C.onesf[:], cntp[:], True, True, [r, C.r], [r_cnt])
            ts(ge[:], cnt[:, 0:N_EXP], float(CAP), None, ALU.is_ge, None, [r_cnt], [r])
            tt(dl[:], mid[:], lo[:], ALU.subtract, [r], [r])
            tt(dl[:], dl[:], ge[:], ALU.mult, [r], [r])
            tt(lo[:], lo[:], dl[:], ALU.add, [r], [r])
            tt(dl[:], hi[:], mid[:], ALU.subtract, [r], [r])
            tt(dl[:], dl[:], ge[:], ALU.mult, [r], [r])
            tt(hi[:], mid[:], dl[:], ALU.add, [r], [r])
        tt(cmp_[:], C.aff[:], bc(lo), ALU.is_gt, [r, C.r_aff], [r])
        tt(cmp_[:], cmp_[:], C.aff[:], ALU.mult, [r, C.r_aff], [r])
        S.dma("sp", Sx.gm.rearrange("(t p) e -> p t e", p=128), cmp_[:], reads=[r], writes=[R.gm])
        S.emit_phase()


def phase_e(K, top):
    nc, S, I, C, Sx, R, sb, ps = K.nc, K.S, K.I, K.C, K.Sx, K.R, K.sb, K.ps
    mm, tr, act, ts, tt, stt, recip, cp = K.mm, K.tr, K.act, K.ts, K.tt, K.stt, K.recip, K.cp
    NOT = OWN // 128
    with ExitStack() as st0:
        own = sb(st0, "own", [128, NOT], I32)
        h3T = sb(st0, "h3T", [128, NCH, OWN], BF16)
        acc = sb(st0, "acc", [128, NOT, D], F32)
        gmo = sb(st0, "gmo_", [128, NOT, N_EXP], F32)
        r_own, r_h3T, r_gmo = Reg(const=True), Reg(const=True), Reg(const=True)
        r_acc = [Reg() for _ in range(NOT)]
        with ExitStack() as st:
            h3o = sb(st, "h3o", [128, NOT, D], BF16)
            r_h3o = [Reg() for _ in range(NOT)]
            tpb = [ps(st, "tpe%d" % i, [128, 512], F32) for i in range(2)]
            r_tpb = [Reg() for _ in range(2)]
            S.dma("sp", own[:], I.own, writes=[r_own])
            for j in range(NOT):
                off = bass.IndirectOffsetOnAxis(ap=own[:, j:j + 1], axis=0)
                S.dma_fn("pool", lambda e, j=j, off=off: e.indirect_dma_start(out=h3o[:, j, :], out_offset=None, in_=Sx.h3, in_offset=off),
                         reads=[r_own] + R.h3, writes=[r_h3o[j]])
                S.dma_fn("pool", lambda e, j=j, off=off: e.indirect_dma_start(out=acc[:, j, :], out_offset=None, in_=Sx.x2, in_offset=off),
                         reads=[r_own] + R.x2, writes=[r_acc[j]])
                S.dma_fn("pool", lambda e, j=j, off=off: e.indirect_dma_start(out=gmo[:, j, :], out_offset=None, in_=Sx.gm, in_offset=off),
                         reads=[r_own, R.gm], writes=[r_gmo])
            k = 0
            for j in range(NOT):
                for hh in range(2):
                    pb = k % 2
                    k += 1
                    tpv = tpb[pb][:].bitcast(BF16)
                    for c in range(8):
                        cc = hh * 8 + c
                        tr(tpv[:, c * 128:(c + 1) * 128], h3o[:, j, cc * 128:(cc + 1) * 128], [r_h3o[j]], [r_tpb[pb]])
                    cp(h3T[:, hh * 8:hh * 8 + 8, j * 128:(j + 1) * 128], tpv.rearrange("p (c n) -> p c n", c=8), [r_tpb[pb]], [r_h3T],
                       eng=("act" if hh == 0 else "dve"))
            S.emit_phase()
        with ExitStack() as st:
            FP = 256
            NFP = D // FP
            wg = [sb(st, "wg%d" % i, [128, NCH, FP], BF16) for i in range(2)]
            wu = [sb(st, "wu%d" % i, [128, NCH, FP], BF16) for i in range(2)]
            wd = [sb(st, "wd%d" % i, [128, NCH, FP], BF16) for i in range(2)]
            r_wg = [Reg() for _ in range(2)]
            r_wu = [Reg() for _ in range(2)]
            r_wd = [Reg() for _ in range(2)]
            actT = sb(st, "actT", [128, NCH, OWN], BF16)
            r_actT = [Reg() for _ in range(NCH)]
            sg = [sb(st, "sg%d" % i, [128, 512], F32) for i in range(2)]
            r_sg = [Reg() for _ in range(2)]
            Gp = [ps(st, "Gp%d" % i, [128, 512], F32) for i in range(2)]
            Up = [ps(st, "Up%d" % i, [128, 512], F32) for i in range(2)]
            Yp = [ps(st, "Yp%d" % i, [128, 512], F32) for i in range(3)]
            r_Gp = [Reg() for _ in range(2)]
            r_Up = [Reg() for _ in range(2)]
            r_Yp = [Reg() for _ in range(3)]
            wi = 0
            di = 0
            gi = 0
            yi = 0
            for ex in range(N_EXP):
                wgv = I.w_gate[ex].rearrange("(c p) f -> p c f", p=128)
                wuv = I.w_up[ex].rearrange("(c p) f -> p c f", p=128)
                wdv = I.w_down[ex].rearrange("(c p) f -> p c f", p=128)
                for fp in range(NFP):
                    wb = wi % 2
                    wi += 1
                    S.dma("pool", wg[wb][:], wgv[:, :, fp * FP:(fp + 1) * FP], writes=[r_wg[wb]])
                    S.dma("pool", wu[wb][:], wuv[:, :, fp * FP:(fp + 1) * FP], writes=[r_wu[wb]])
                    for f2 in range(FP // 128):
                        fc = fp * (FP // 128) + f2
                        for half in range(2):
                            gb = gi % 2
                            gi += 1
                            cs = slice(half * 512, (half + 1) * 512)
                            for c in range(NCH):
                                mm(Gp[gb][:], wg[wb][:, c, f2 * 128:(f2 + 1) * 128], h3T[:, c, cs], c == 0, c == NCH - 1,
                                   [r_wg[wb], r_h3T], [r_Gp[gb]])
                            for c in range(NCH):
                                mm(Up[gb][:], wu[wb][:, c, f2 * 128:(f2 + 1) * 128], h3T[:, c, cs], c == 0, c == NCH - 1,
                                   [r_wu[wb], r_h3T], [r_Up[gb]])
                            act(sg[gb][:], Gp[gb][:], AF.Silu, [r_Gp[gb]], [r_sg[gb]])
                            tt(actT[:, fc, cs], sg[gb][:], Up[gb][:], ALU.mult, [r_sg[gb], r_Up[gb]], [r_actT[fc]])
                for dp in range(NFP):
                    db = di % 2
                    di += 1
                    S.dma("pool", wd[db][:], wdv[:, :, dp * FP:(dp + 1) * FP], writes=[r_wd[db]])
                    for j in range(NOT):
                        yb = yi % 3
                        yi += 1
                        for fc in range(NCH):
                            mm(Yp[yb][:, 0:FP], actT[:, fc, j * 128:(j + 1) * 128], wd[db][:, fc, :], fc == 0, fc == NCH - 1,
                               [r_actT[fc], r_wd[db]], [r_Yp[yb]])
                        stt(acc[:, j, dp * FP:(dp + 1) * FP], Yp[yb][:, 0:FP], gmo[:, j, ex:ex + 1], acc[:, j, dp * FP:(dp + 1) * FP],
                            ALU.mult, ALU.add, [r_Yp[yb], r_gmo, r_acc[j]], [r_acc[j]])
            outs = []
            for j in range(NOT):
                outs.append(S.dma("sp", K.out[j * 128:(j + 1) * 128, :], acc[:, j, :], reads=[r_acc[j]], writes=[]))
            S.finish(outs)
            S.emit_phase()


_NC_CACHE = {}


def _core_inputs(inp, c, consts):
    b, q = c // 4, c % 4
    m = {}
    m["x"] = np.ascontiguousarray(inp["x"][b], dtype=np.float32)
    m["mem"] = np.ascontiguousarray(inp["mem"][b], dtype=np.float32)
    m["positions"] = np.ascontiguousarray(inp["positions"][b]).astype(np.int32)
    own = (q * OWN + np.arange(OWN)).reshape(OWN // 128, 128).T.astype(np.int32)
    m["own_idx"] = np.ascontiguousarray(own)
    for n in ("g_mix", "g_cross", "g_mem", "g_moe", "g_qa", "g_ka", "g_qb", "g_kb", "g_qm", "g_km", "g_oa", "g_ob",
              "sink_b", "w_in", "w_out", "w_q_mem", "w_kv_mem", "w_o_mem", "w_router", "w_gate", "w_up", "w_down"):
        m[n] = np.ascontiguousarray(np.asarray(inp[n])[0], dtype=np.float32)
    m.update(consts)
    return m


def kernel(**inputs):
    inp = {k: np.asarray(v) for k, v in inputs.items()}
    if "nc" not in _NC_CACHE:
        _NC_CACHE["nc"] = build()
    nc = _NC_CACHE["nc"]
    consts = host_consts()
    in_maps = [_core_inputs(inp, c, consts) for c in range(8)]
    res = run_bass_kernel_spmd(nc, in_maps, core_ids=list(range(8)))
    out = np.zeros((2, SEQ, D), np.float32)
    for c in range(8):
        b, q = c // 4, c % 4
        out[b, q * OWN:(q + 1) * OWN] = np.asarray(res.results[c]["out"], dtype=np.float32)
    return out


MOE_FP = 512
MOE_NW = 6


def moe_ring_setup(K, stack):
    S, I, sb = K.S, K.I, K.sb
    FP, NW = MOE_FP, MOE_NW
    NFP = D // FP
    PPE = 3 * NFP
    K.Wb = [sb(stack, "Wb%d" % i, [128, NCH, FP], BF16) for i in range(NW)]
    K.r_W = [Reg() for _ in range(NW)]
    ring = {"next": 0}

    def piece_src(p):
        ex, k = divmod(p, PPE)
        if k < 2 * NFP:
            fp, gu = divmod(k, 2)
            w = I.w_gate if gu == 0 else I.w_up
            return w[ex].rearrange("(c p) f -> p c f", p=128)[:, :, fp * FP:(fp + 1) * FP]
        dp = k - 2 * NFP
        return I.w_down[ex].rearrange("(c p) f -> p c f", p=128)[:, :, dp * FP:(dp + 1) * FP]

    def ensure(upto):
        last = N_EXP * PPE - 1
        while ring["next"] <= min(upto, last):
            p = ring["next"]
            ring["next"] += 1
            S.dma("pool", K.Wb[p % NW][:], piece_src(p), writes=[K.r_W[p % NW]])
    K.ring_ensure = ensure
    ensure(NW - 1)


def phase_e2(K, top):
    nc, S, I, C, Sx, R, sb, ps = K.nc, K.S, K.I, K.C, K.Sx, K.R, K.sb, K.ps
    mm, tr, act, ts, tt, stt, recip, cp = K.mm, K.tr, K.act, K.ts, K.tt, K.stt, K.recip, K.cp
    NOT = OWN // 128
    NS = CAP // 128
    NTG = 3 + N_EXP
    with ExitStack() as st0:
        own = sb(st0, "own", [128, NOT], I32)
        gmo = sb(st0, "gmo_", [128, NOT, N_EXP], F32)
        mall = sb(st0, "mall", [128, NOT, N_EXP], F32)
        pos = sb(st0, "pos", [128, NOT, N_EXP], F32)
        TG = sb(st0, "TG", [128, NOT, NTG], F32)
        iota = sb(st0, "iota", [128, 512], F32)
        dummy = sb(st0, "dummy", [128, NS], F32)
        r_c = Reg(const=True)
        with ExitStack() as st:
            ut = sb(st, "ut", [128, 128], F32)
            loc = sb(st, "loc", [128, NOT], F32)
            offs = sb(st, "offs", [128, NOT, N_EXP], F32)
            x2b = [sb(st, "x2b%d" % i, [128, D], F32) for i in range(2)]
            r_x2b = [Reg() for _ in range(2)]
            pc = [ps(st, "pc%d" % i, [128, 512], F32) for i in range(2)]
            r_pc = [Reg() for _ in range(2)]
            r1 = Reg()
            S.dma("sp", own[:], I.own, writes=[r_c])
            S.dma("sp", ut[:], I.c_ut, writes=[r1])
            S.dma("sp", loc[:], I.c_loc, writes=[r1])
            S.dma("sp", iota[:], I.c_iota, writes=[r_c])
            S.dma("sp", dummy[:], I.c_dummy, writes=[r_c])
            for j in range(NOT):
                off = bass.IndirectOffsetOnAxis(ap=own[:, j:j + 1], axis=0)
                S.dma_fn("pool", lambda e, j=j, off=off: e.indirect_dma_start(out=gmo[:, j, :], out_offset=None, in_=Sx.gm, in_offset=off),
                         reads=[r_c, R.gm], writes=[r1])
                b = j % 2
                S.dma_fn("pool", lambda e, b=b, off=off: e.indirect_dma_start(out=x2b[b][:], out_offset=None, in_=Sx.x2, in_offset=off),
                         reads=[r_c] + R.x2, writes=[r_x2b[b]])
                S.dma("sp", Sx.accd[j * 128:(j + 1) * 128, :], x2b[b][:], reads=[r_x2b[b]], writes=[R.accd])
            ts(mall[:], gmo[:], 0.0, None, ALU.is_gt, None, [r1], [r1])
            flat = mall[:].rearrange("p j e -> p (j e)")
            mm(pc[0][:, 0:NOT * N_EXP], ut[:], flat, True, True, [r1], [r_pc[0]])
            mm(pc[1][:, 0:NOT * N_EXP], C.onesf[:], flat, True, True, [r1, C.r], [r_pc[1]])
            tot = pc[1][:, 0:NOT * N_EXP].rearrange("p (j e) -> p j e", e=N_EXP)
            cum = pc[0][:, 0:NOT * N_EXP].rearrange("p (j e) -> p j e", e=N_EXP)
            S.op("dve", lambda e: e.memset(offs[:, 0, :], 0.0), writes=[r1])
            for j in range(1, NOT):
                tt(offs[:, j, :], offs[:, j - 1, :], tot[:, j - 1, :], ALU.add, [r1, r_pc[1]], [r1])
            tt(pos[:], cum, offs[:], ALU.add, [r_pc[0], r1], [r1])
            ts(pos[:], pos[:], -1.0, None, ALU.add, None, [r1], [r1])
            cp(TG[:, :, 0], own[:], [r_c], [r1])
            cp(TG[:, :, 1], loc[:], [r1], [r1])
            S.op("dve", lambda e: e.memset(TG[:, :, 2], 1.0), writes=[r1])
            cp(TG[:, :, 3:NTG], gmo[:], [r1], [r1])
            S.emit_phase()
        with ExitStack() as st:
            FP, NFP, NW, PPE = MOE_FP, D // MOE_FP, MOE_NW, 3 * (D // MOE_FP)
            Wb, r_W, ensure = K.Wb, K.r_W, K.ring_ensure
            Sel = [sb(st, "Sel%d" % i, [128, NOT, 128], F32) for i in range(2)]
            r_Sel = [Reg() for _ in range(2)]
            sis = sb(st, "sis", [128, NS, 32], F32)
            sidf = sb(st, "sidf", [128, NS], F32)
            gi = [sb(st, "gi%d" % i, [128, NS], I32) for i in range(2)]
            si = [sb(st, "si%d" % i, [128, NS], I32) for i in range(2)]
            gs = [sb(st, "gs%d" % i, [128, NS], F32) for i in range(2)]
            r_sis = Reg()
            r_idx = [Reg() for _ in range(2)]
            xe = sb(st, "xe", [128, NS, D], BF16)
            r_xe = [Reg() for _ in range(NS)]
            xeT1 = sb(st, "xeT", [128, NCH, CAP], BF16)
            xeT = [xeT1, xeT1]
            r_xeT1 = Reg()
            r_xeT = [r_xeT1, r_xeT1]
            actT = sb(st, "actT", [128, NCH, CAP], BF16)
            r_actT = [Reg() for _ in range(NCH)]
            ygs = sb(st, "ygs", [128, NS, D], F32)
            r_ygs = [Reg() for _ in range(NS)]
            sg = [sb(st, "sg%d" % i, [128, 512], F32) for i in range(2)]
            r_sg = [Reg() for _ in range(2)]
            cnt_stg = {"n": 0}

            def wload(dst, r_dst, src):
                cnt_stg["n"] += 1
                S.dma("pool", dst, src, writes=[r_dst])
            Gp = [ps(st, "Gp%d" % i, [128, 512], F32) for i in range(2)]
            Up = [ps(st, "Up%d" % i, [128, 512], F32) for i in range(2)]
            Yp = [ps(st, "Yp%d" % i, [128, 512], F32) for i in range(2)]
            Tq = ps(st, "Tq", [128, 512], F32)
            SIp = ps(st, "SIp", [128, 512], F32)
            r_Gp = [Reg() for _ in range(2)]
            r_Up = [Reg() for _ in range(2)]
            r_Yp = [Reg() for _ in range(2)]
            r_Tq, r_SIp = Reg(), Reg()
            tqv = Tq[:].bitcast(BF16)
            cnt = {"w": 0, "d": 0, "g": 0, "y": 0}

            def prep_idx(ex):
                pb = ex % 2
                for s4 in range(NS):
                    sl = s4 % 2
                    for j in range(NOT):
                        ts(Sel[sl][:, j, :], iota[:, s4 * 128:(s4 + 1) * 128], pos[:, j, ex:ex + 1], mall[:, j, ex:ex + 1],
                           ALU.is_equal, ALU.mult, [r_c], [r_Sel[sl]])
                    for j in range(NOT):
                        mm(SIp[:, s4 * 32:s4 * 32 + NTG], Sel[sl][:, j, :], TG[:, j, :], j == 0, j == NOT - 1,
                           [r_Sel[sl], r_c], [r_SIp])
                cp(sis[:], SIp[:, 0:NS * 32].rearrange("p (s k) -> p s k", k=32), [r_SIp], [r_sis])
                cp(gi[pb][:], sis[:, :, 0], [r_sis], [r_idx[pb]])
                tt(sidf[:], sis[:, :, 2], dummy[:], ALU.mult, [r_sis, r_c], [r_sis])
                tt(sidf[:], dummy[:], sidf[:], ALU.subtract, [r_sis, r_c], [r_sis])
                tt(sidf[:], sidf[:], sis[:, :, 1], ALU.add, [r_sis], [r_sis])
                cp(si[pb][:], sidf[:], [r_sis], [r_idx[pb]])
                cp(gs[pb][:], sis[:, :, 3 + ex], [r_sis], [r_idx[pb]])
                for s4 in range(NS):
                    off = bass.IndirectOffsetOnAxis(ap=gi[pb][:, s4:s4 + 1], axis=0)
                    S.dma_fn("pool", lambda e, s4=s4, off=off: e.indirect_dma_start(out=xe[:, s4, :], out_offset=None, in_=Sx.h3, in_offset=off),
                             reads=[r_idx[pb]] + R.h3, writes=[r_xe[s4]])

            def prep_T(ex):
                pb = ex % 2
                k = 0
                for s4 in range(NS):
                    for hh in range(4):
                        for c in range(4):
                            cc = hh * 4 + c
                            tr(tqv[:, c * 128:(c + 1) * 128], xe[:, s4, cc * 128:(cc + 1) * 128], [r_xe[s4]], [r_Tq])
                        cp(xeT[pb][:, hh * 4:hh * 4 + 4, s4 * 128:(s4 + 1) * 128], tqv[:, 0:512].rearrange("p (c n) -> p c n", c=4),
                           [r_Tq], [r_xeT[pb]], eng=("act" if k % 2 == 0 else "dve"))
                        k += 1

            def compute_gu(ex, fp):
                pb = ex % 2
                pg = ex * PPE + 2 * fp
                wgt, wut = Wb[pg % NW], Wb[(pg + 1) % NW]
                rg, ru = r_W[pg % NW], r_W[(pg + 1) % NW]
                for f2 in range(FP // 128):
                    fc = fp * (FP // 128) + f2
                    gb = cnt["g"] % 2
                    cnt["g"] += 1
                    for c in range(NCH):
                        mm(Gp[gb][:], wgt[:, c, f2 * 128:(f2 + 1) * 128], xeT[pb][:, c, :], c == 0, c == NCH - 1,
                           [rg, r_xeT[pb]], [r_Gp[gb]])
                    for c in range(NCH):
                        mm(Up[gb][:], wut[:, c, f2 * 128:(f2 + 1) * 128], xeT[pb][:, c, :], c == 0, c == NCH - 1,
                           [ru, r_xeT[pb]], [r_Up[gb]])
                    act(sg[gb][:], Gp[gb][:], AF.Silu, [r_Gp[gb]], [r_sg[gb]])
                    tt(actT[:, fc, :], sg[gb][:], Up[gb][:], ALU.mult, [r_sg[gb], r_Up[gb]], [r_actT[fc]])
                ensure(pg + 1 + NW)

            def compute_d(ex, dp):
                pb = ex % 2
                pd = ex * PPE + 2 * NFP + dp
                wdt, rd_ = Wb[pd % NW], r_W[pd % NW]
                for s4 in range(NS):
                    yb = cnt["y"] % 2
                    cnt["y"] += 1
                    for fc in range(NCH):
                        mm(Yp[yb][:, 0:FP], actT[:, fc, s4 * 128:(s4 + 1) * 128], wdt[:, fc, :], fc == 0, fc == NCH - 1,
                           [r_actT[fc], rd_], [r_Yp[yb]])
                    if s4 % 2 == 0:
                        ts(ygs[:, s4, dp * FP:(dp + 1) * FP], Yp[yb][:, 0:FP], gs[pb][:, s4:s4 + 1], None, ALU.mult, None,
                           [r_Yp[yb], r_idx[pb]], [r_ygs[s4]])
                    else:
                        act(ygs[:, s4, dp * FP:(dp + 1) * FP], Yp[yb][:, 0:FP], AF.Copy, [r_Yp[yb], r_idx[pb]], [r_ygs[s4]],
                            scale=gs[pb][:, s4:s4 + 1])
                ensure(pd + NW)

            sc_prev = {"ops": []}

            def scatter(ex):
                pb = ex % 2
                extra = list(sc_prev["ops"])
                if R.accd.last_w is not None:
                    extra.append(R.accd.last_w)
                ops = []
                for s4 in range(NS):
                    off = bass.IndirectOffsetOnAxis(ap=si[pb][:, s4:s4 + 1], axis=0)
                    ops.append(S.dma_fn("pool", lambda e, s4=s4, off=off: e.indirect_dma_start(out=Sx.accd, out_offset=off, in_=ygs[:, s4, :],
                                                                                             in_offset=None, compute_op=ALU.add),
                                        reads=[r_idx[pb], r_ygs[s4]], writes=[], extra=extra))
                sc_prev["ops"] = ops

            assert NFP == 4
            prep_idx(0)
            prep_T(0)
            ensure(NW - 1)
            for ex in range(N_EXP):
                for fp in range(NFP):
                    compute_gu(ex, fp)
                if ex + 1 < N_EXP:
                    prep_idx(ex + 1)
                for dp in range(NFP):
                    compute_d(ex, dp)
                    if dp == 1 and ex + 1 < N_EXP:
                        prep_T(ex + 1)
                scatter(ex)
            fin = S.dma("sp", K.out, Sx.accd[0:OWN, :], reads=[R.accd], writes=[], extra=sc_prev["ops"])
            S.finish([fin])
            S.emit_phase()
```

```python
import numpy as np
from contextlib import ExitStack
import concourse.bass as bass
import concourse.mybir as mybir
from concourse.bass_utils import run_bass_kernel_spmd

F32 = mybir.dt.float32
BF16 = mybir.dt.bfloat16
I32 = mybir.dt.int32
AF = mybir.ActivationFunctionType
ALU = mybir.AluOpType
AX = mybir.AxisListType

SAME_ENGINE_SYNC = True
MOE_COMPACT = True


class Reg:
    __slots__ = ("name", "last_w", "readers", "const")

    def __init__(self, name="", const=False):
        self.name = name
        self.last_w = None
        self.readers = []
        self.const = const


class Op:
    __slots__ = ("eng", "fn", "deps", "needed", "sig", "is_dma", "waits_only")

    def __init__(self, eng, fn, deps, is_dma=False):
        self.eng = eng
        self.fn = fn
        self.deps = deps
        self.needed = False
        self.sig = None
        self.is_dma = is_dma
        self.waits_only = False


class Sched:
    ENGS = ("pe", "act", "dve", "pool", "sp")
    NDMA = 12

    def __init__(self, nc, stack):
        self.nc = nc
        self.sem = {e: stack.enter_context(nc.semaphore("s_" + e)) for e in self.ENGS}
        self.cnt = {e: 0 for e in self.ENGS}
        self.dsem = {q: [stack.enter_context(nc.semaphore("d_%s%d" % (q, i))) for i in range(self.NDMA)]
                     for q in ("sp", "pool", "act")}
        self.dcnt = {q: 0 for q in ("sp", "pool", "act")}
        self.dlast = {q: [None] * self.NDMA for q in ("sp", "pool", "act")}
        self.ops = {e: [] for e in self.ENGS}
        self.known = {e: {} for e in self.ENGS}
        self.last_op = {e: None for e in self.ENGS}
        self.prev_phase_last = []

    def _mk(self, eng, fn, reads, writes, is_dma=False, extra=()):
        deps = list(extra)
        for r in reads:
            if r.last_w is not None:
                deps.append(r.last_w)
        for w in writes:
            if w.last_w is not None:
                deps.append(w.last_w)
            deps.extend(w.readers)
        op = Op(eng, fn, deps, is_dma)
        for r in reads:
            if not r.const:
                r.readers.append(op)
        for w in writes:
            w.last_w = op
            w.readers = []
        self.ops[eng].append(op)
        return op

    def op(self, eng, fn, reads=(), writes=()):
        return self._mk(eng, fn, reads, writes)

    def dma(self, q, out, in_, reads=(), writes=(), extra=(), **kw):
        return self._mk(q, lambda e: e.dma_start(out=out, in_=in_, **kw), reads, writes, is_dma=True, extra=extra)

    def dma_fn(self, q, fn, reads=(), writes=(), extra=()):
        return self._mk(q, fn, reads, writes, is_dma=True, extra=extra)

    def finish(self, ops):
        o = Op("sp", None, list(ops))
        o.waits_only = True
        self.ops["sp"].append(o)

    def emit_phase(self):
        self.nphase = getattr(self, "nphase", 0) + 1
        with self.nc.named_scope("ph%02d" % self.nphase):
            self._emit_phase()

    def _emit_phase(self):
        nc = self.nc
        engs = {"pe": nc.tensor, "act": nc.scalar, "dve": nc.vector, "pool": nc.gpsimd, "sp": nc.sync}
        barrier = list(self.prev_phase_last)
        for e in self.ENGS:
            for op in self.ops[e]:
                for d in op.deps:
                    if d.eng == op.eng and not d.is_dma:
                        if e in ("pe", "sp") or not SAME_ENGINE_SYNC:
                            continue
                    d.needed = True
        lasts = []
        for e in self.ENGS:
            real = [o for o in self.ops[e] if not o.waits_only]
            if real:
                real[-1].needed = True
                lasts.append(real[-1])
        for e in self.ENGS:
            for op in self.ops[e]:
                if op.waits_only:
                    continue
                if op.is_dma:
                    q = e
                    j = self.dcnt[q]
                    self.dcnt[q] += 1
                    op.sig = (self.dsem[q][j % self.NDMA], 16 * (j // self.NDMA + 1), q, j)
                elif op.needed:
                    self.cnt[e] += 1
                    op.sig = (self.sem[e], self.cnt[e])
        dma_lasts = []
        with nc.Block() as block:
            def run(e):
                def body(eng):
                    known = self.known[e]

                    def wait(sig):
                        s, v = sig[0], sig[1]
                        k = id(s)
                        if known.get(k, 0) < v:
                            eng.wait_ge(s, v)
                            known[k] = v
                    for d in barrier:
                        if d.eng != e or d.is_dma:
                            wait(d.sig)
                    for op in self.ops[e]:
                        for d in op.deps:
                            if d.sig is None:
                                continue
                            if d.eng == e and not d.is_dma:
                                if e in ("pe", "sp") or not SAME_ENGINE_SYNC:
                                    continue
                            wait(d.sig)
                        if op.waits_only:
                            continue
                        if op.is_dma:
                            s, v, q, j = op.sig
                            if j >= self.NDMA:
                                wait((s, v - 16))
                            ins = op.fn(eng)
                            ins.then_inc(s, 16)
                        else:
                            ins = op.fn(eng)
                            if op.sig is not None:
                                ins.then_inc(op.sig[0], 1)
                return body
            block.tensor(run("pe"))
            block.scalar(run("act"))
            block.vector(run("dve"))
            block.gpsimd(run("pool"))
            block.sync(run("sp"))
        for q in ("sp", "pool", "act"):
            seen = {}
            for op in self.ops[q]:
                if op.is_dma:
                    seen[id(op.sig[0])] = op
            dma_lasts.extend(seen.values())
        self.prev_phase_last = lasts + dma_lasts + [d for d in self.prev_phase_last if d.is_dma and id(d.sig[0]) not in {id(x.sig[0]) for x in dma_lasts}]
        self.ops = {e: [] for e in self.ENGS}


D = 2048
SEQ = 4096
HD = 128
D_IN = 4608
NCH = D // 128
NT = SEQ // 128
EPS = 1e-6
N_EXP = 16
CAP = 512
OWN = 1024
TWO_PI = float(2.0 * np.pi)
SCALE = float(1.0 / np.sqrt(HD))
HC_COL = [128 * i for i in range(8)] + [1024 + 128 * i for i in range(8)] + \
         [3072 + 128 * i for i in range(8)] + [4096, 4224]
HC_FAM = [0] * 8 + [1] * 8 + [2] * 8 + [3] * 2
V_COLS = [(2048, 512), (2560, 512), (4352, 256)]


def mask_a_np():
    kp = np.arange(128)[:, None]
    qf = np.arange(512)[None, :]
    out = np.zeros((20, 128, 512), np.float32)
    for j in range(20):
        o = (-1024 + 128 * j) + kp - qf
        a = np.abs(o)
        out[j] = (a <= 64).astype(np.float32) + ((o % 4 == 0) & (a <= 256)) + ((o % 16 == 0) & (a <= 1024))
    return out


def mask_b_np():
    kp = np.arange(128)[:, None]
    qf = np.arange(512)[None, :]
    out = np.zeros((6, 128, 512), np.float32)
    for j in range(6):
        o = (-128 + 128 * j) + kp - qf
        out[j] = (np.abs(o) <= 128)
    return out


def host_consts():
    import ml_dtypes
    bf = ml_dtypes.bfloat16
    c = {}
    c["c_ident"] = np.eye(128, dtype=np.float32).astype(bf)
    c["c_identf"] = np.eye(128, dtype=np.float32)
    c["c_ones"] = np.ones((128, 128), np.float32).astype(bf)
    c["c_onesf"] = np.ones((128, 128), np.float32)
    p0 = np.zeros((128, 128), np.float32)
    for m in range(64):
        p0[m + 64, m] = -1.0
    for m in range(64, 128):
        p0[m - 64, m] = 1.0
    c["c_rot"] = p0
    i = np.arange(128) % 64
    c["c_invf"] = (10000.0 ** (-(2.0 * i) / 128.0)).astype(np.float32).reshape(128, 1)
    pp = np.arange(128)
    c["c_ut"] = (pp[:, None] <= pp[None, :]).astype(np.float32)
    c["c_iota"] = np.tile(np.arange(512, dtype=np.float32)[None, :], (128, 1))
    c["c_loc"] = (np.arange(8)[None, :] * 128 + pp[:, None]).astype(np.float32)
    c["c_dummy"] = (1024 + np.arange(4)[None, :] * 128 + pp[:, None]).astype(np.float32)
    def lnm(m):
        out = np.full(m.shape, -3000.0, np.float32)
        nz = m > 0
        out[nz] = np.log(m[nz]) / SCALE
        return out
    c["c_maska"] = np.ascontiguousarray(lnm(mask_a_np()).transpose(1, 0, 2)).astype(bf)
    c["c_maskb"] = np.ascontiguousarray(lnm(mask_b_np()).transpose(1, 0, 2)).astype(bf)
    return c


class Ctx:
    pass


def build(stop_after=99, debug=False):
    nc = bass.Bass("TRN2", target_bir_lowering=False)
    K = Ctx()
    K.nc = nc

    def din(name, shape, dt):
        return nc.dram_tensor(name, list(shape), dt, kind="ExternalInput").ap()

    def dscr(name, shape, dt):
        kind = "ExternalOutput" if debug else "Internal"
        return nc.dram_tensor(name, list(shape), dt, kind=kind).ap()

    I = Ctx()
    I.x = din("x", [SEQ, D], F32)
    I.mem = din("mem", [256, D], F32)
    I.pos = din("positions", [SEQ], I32)
    I.own = din("own_idx", [128, OWN // 128], I32)
    for n in ("g_mix", "g_cross", "g_mem", "g_moe"):
        setattr(I, n, din(n, [D], F32))
    for n in ("g_qa", "g_ka", "g_qb", "g_kb", "g_qm", "g_km"):
        setattr(I, n, din(n, [HD], F32))
    I.sink = din("sink_b", [8], F32)
    I.g_oa = din("g_oa", [1024], F32)
    I.g_ob = din("g_ob", [1024], F32)
    I.w_in = din("w_in", [D, D_IN], F32)
    I.w_out = din("w_out", [D, D], F32)
    I.w_q_mem = din("w_q_mem", [D, 512], F32)
    I.w_kv_mem = din("w_kv_mem", [D, 1024], F32)
    I.w_o_mem = din("w_o_mem", [512, D], F32)
    I.w_router = din("w_router", [D, N_EXP], F32)
    I.w_gate = din("w_gate", [N_EXP, D, D], F32)
    I.w_up = din("w_up", [N_EXP, D, D], F32)
    I.w_down = din("w_down", [N_EXP, D, D], F32)
    I.c_ident = din("c_ident", [128, 128], BF16)
    I.c_identf = din("c_identf", [128, 128], F32)
    I.c_ones = din("c_ones", [128, 128], BF16)
    I.c_onesf = din("c_onesf", [128, 128], F32)
    I.c_rot = din("c_rot", [128, 128], F32)
    I.c_invf = din("c_invf", [128, 1], F32)
    I.c_ut = din("c_ut", [128, 128], F32)
    I.c_iota = din("c_iota", [128, 512], F32)
    I.c_loc = din("c_loc", [128, 8], F32)
    I.c_dummy = din("c_dummy", [128, 4], F32)
    I.c_maska = din("c_maska", [128, 20, 512], BF16)
    I.c_maskb = din("c_maskb", [128, 6, 512], BF16)
    K.I = I
    out = nc.dram_tensor("out", [OWN, D], F32, kind="ExternalOutput").ap()
    K.out = out

    Sx = Ctx()
    Sx.qkt = dscr("s_qkt", [26, 128, SEQ], BF16)
    Sx.v = dscr("s_v", [SEQ, 1280], BF16)
    Sx.ot = dscr("s_ot", [16, 128, SEQ], BF16)
    Sx.x2 = dscr("s_x2", [SEQ, D], F32)
    Sx.h3 = dscr("s_h3", [SEQ, D], BF16)
    Sx.gm = dscr("s_gm", [SEQ, N_EXP], F32)
    Sx.accd = dscr("s_accd", [OWN + CAP, D], F32)
    K.Sx = Sx
    R = Ctx()
    R.qkt = [[Reg("qkt%d_%d" % (h, f)) for f in range(2)] for h in range(26)]
    R.v = [Reg("v%d" % t) for t in range(NT)]
    R.ot = [Reg("ot%d" % h) for h in range(16)]
    R.x2 = [Reg("x2_%d" % t) for t in range(NT)]
    R.h3 = [Reg("h3_%d" % t) for t in range(NT)]
    R.gm = Reg("gm")
    R.accd = Reg("accd")
    K.R = R

    with ExitStack() as top:
        S = Sched(nc, top)
        K.S = S

        uid = [0]

        def sb(stack, name, shape, dt):
            uid[0] += 1
            return stack.enter_context(nc.sbuf_tensor("%s_%d" % (name, uid[0]), list(shape), dt))

        def ps(stack, name, shape, dt):
            uid[0] += 1
            return stack.enter_context(nc.psum_tensor("%s_%d" % (name, uid[0]), list(shape), dt))
        K.sb, K.ps = sb, ps

        def mm(out, lhsT, rhs, start, stop, reads, writes):
            return S.op("pe", lambda e: e.matmul(out, lhsT=lhsT, rhs=rhs, start=start, stop=stop), reads, writes)

        def tr(out, in_, reads, writes, ident=None):
            idn = C.ident[:] if ident is None else ident
            return S.op("pe", lambda e: e.transpose(out=out, in_=in_, identity=idn), list(reads) + [C.r], writes)

        def act(out, in_, func, reads, writes, **kw):
            return S.op("act", lambda e: e.activation(out=out, in_=in_, func=func, **kw), reads, writes)

        def ts(out, in0, s1, s2, op0, op1, reads, writes, eng="dve", **kw):
            if op1 is None:
                return S.op(eng, lambda e: e.tensor_scalar(out=out, in0=in0, scalar1=s1, scalar2=None, op0=op0, **kw), reads, writes)
            return S.op(eng, lambda e: e.tensor_scalar(out=out, in0=in0, scalar1=s1, scalar2=s2, op0=op0, op1=op1, **kw), reads, writes)

        def tt(out, in0, in1, op, reads, writes, eng="dve"):
            return S.op(eng, lambda e: e.tensor_tensor(out=out, in0=in0, in1=in1, op=op), reads, writes)

        def stt(out, in0, scalar, in1, op0, op1, reads, writes, eng="dve"):
            return S.op(eng, lambda e: e.scalar_tensor_tensor(out=out, in0=in0, scalar=scalar, in1=in1, op0=op0, op1=op1), reads, writes)

        def rsq(out, in_, scale, reads, writes):
            S.op("dve", lambda e: e.tensor_scalar(out=out, in0=in_, scalar1=scale, scalar2=EPS, op0=ALU.mult, op1=ALU.add), reads, writes)
            S.op("act", lambda e: e.activation(out=out, in_=out, func=AF.Ln), writes, writes)
            return S.op("act", lambda e: e.activation(out=out, in_=out, func=AF.Exp, scale=-0.5), writes, writes)
        K.rsq = rsq

        def recip(out, in_, reads, writes):
            return S.op("dve", lambda e: e.reciprocal(out=out, in_=in_), reads, writes)

        def cp(out, in_, reads, writes, eng="dve"):
            if eng == "act":
                return S.op("act", lambda e: e.copy(out=out, in_=in_), reads, writes)
            return S.op(eng, lambda e: e.tensor_copy(out=out, in_=in_), reads, writes)
        K.mm, K.tr, K.act, K.ts, K.tt, K.stt, K.recip, K.cp = mm, tr, act, ts, tt, stt, recip, cp

        C = Ctx()
        K.C = C
        C.ident = sb(top, "ident", [128, 128], BF16)
        C.identf = sb(top, "identf", [128, 128], F32)
        C.ones = sb(top, "ones", [128, 128], BF16)
        C.onesf = sb(top, "onesf", [128, 128], F32)
        C.rot0 = sb(top, "rot0", [128, 128], F32)
        C.rotg = sb(top, "rotg", [128, 4, 128], BF16)
        C.invf = sb(top, "invf", [128, 1], F32)
        C.gh = sb(top, "gh", [128, 6], F32)
        C.ss = sb(top, "ssab", [128, 2, NT], F32)
        C.esink = sb(top, "esink", [128, 8], F32)
        C.aff = sb(top, "aff", [128, NT, N_EXP], F32)
        C.r_aff = Reg("aff")
        C.r = Reg("consts", const=True)
        C.r_ss = Reg("ss")
        S.dma("sp", C.ident[:], I.c_ident, writes=[C.r])
        S.dma("sp", C.identf[:], I.c_identf, writes=[C.r])
        S.dma("sp", C.ones[:], I.c_ones, writes=[C.r])
        S.dma("sp", C.onesf[:], I.c_onesf, writes=[C.r])
        S.dma("sp", C.rot0[:], I.c_rot, writes=[C.r])
        S.dma("sp", C.invf[:], I.c_invf, writes=[C.r])
        for i, n in enumerate(("g_qa", "g_ka", "g_qb", "g_kb", "g_qm", "g_km")):
            S.dma("sp", C.gh[:, i:i + 1], getattr(I, n).rearrange("(p o) -> p o", o=1), writes=[C.r])
        S.dma("sp", C.esink[:], I.sink.partition_broadcast(128), writes=[C.r])
        S.op("act", lambda e: e.activation(out=C.esink[:], in_=C.esink[:], func=AF.Exp), reads=[C.r], writes=[C.r])
        for f in range(4):
            S.op("dve", lambda e, f=f: e.tensor_scalar(out=C.rotg[:, f, :], in0=C.rot0[:], scalar1=C.gh[:, f:f + 1],
                                                       scalar2=None, op0=ALU.mult), reads=[C.r], writes=[C.r])
        S.op("dve", lambda e: e.memset(C.ss[:], 0.0), writes=[C.r_ss])

        phase_a(K, top)
        if stop_after >= 2:
            with ExitStack() as st_bc:
                K.wout = sb(st_bc, "wout", [128, NCH, D], BF16)
                go = sb(st_bc, "go", [128, 16], F32)
                r_wo = Reg()
                S.dma("pool", K.wout[:], I.w_out.rearrange("(c p) n -> p c n", p=128), writes=[r_wo])
                S.dma("sp", go[:, 0:8], I.g_oa.rearrange("(c p) -> p c", p=128), writes=[r_wo], allow_slow_non_contiguous=True)
                S.dma("sp", go[:, 8:16], I.g_ob.rearrange("(c p) -> p c", p=128), writes=[r_wo], allow_slow_non_contiguous=True)
                for c in range(NCH):
                    ts(K.wout[:, c, :], K.wout[:, c, :], go[:, c:c + 1], None, ALU.mult, None, [r_wo], [r_wo])
                phase_b(K, top)
                if stop_after >= 3:
                    phase_c(K, top)
        if stop_after >= 4:
            with ExitStack() as st_moe:
                moe_ring_setup(K, st_moe)
                phase_d(K, top)
                if stop_after >= 5:
                    phase_e2(K, top)
        S.emit_phase()
    return nc


PI_LO = 3.1415925


def phase_a(K, top):
    nc, S, I, C, Sx, R, sb, ps = K.nc, K.S, K.I, K.C, K.Sx, K.R, K.sb, K.ps
    HALF = SEQ // 2
    w_in_v = I.w_in.rearrange("(c p) n -> p c n", p=128)
    for hf in range(2):
        with ExitStack() as st_x:
            xnT = sb(st_x, "xnT", [128, NCH, HALF], BF16)
            r_xnT = [Reg("xnT%d" % t) for t in range(16)]
            with ExitStack() as st:
                xs = [sb(st, "xs%d" % i, [128, D], F32) for i in range(2)]
                xn = [sb(st, "xn%d" % i, [128, D], BF16) for i in range(2)]
                junk = sb(st, "junk", [128, D], BF16)
                gbc = sb(st, "gbc", [128, D], F32)
                ssq = sb(st, "ssq", [128, 2], F32)
                rstd = sb(st, "rstd", [128, 2], F32)
                tp = [ps(st, "tp%d" % i, [128, 8, 128], BF16) for i in range(4)]
                r_xs = [Reg() for _ in range(2)]
                r_xn = [Reg() for _ in range(2)]
                r_junk, r_gbc = Reg(), Reg(const=True)
                r_ssq = [Reg() for _ in range(2)]
                r_rstd = [Reg() for _ in range(2)]
                r_tp = [Reg() for _ in range(4)]
                S.dma("sp", gbc[:], I.g_mix.partition_broadcast(128), writes=[r_gbc])
                for t in range(16):
                    b = t % 2
                    tok0 = hf * HALF + t * 128
                    S.dma("sp", xs[b][:], I.x[tok0:tok0 + 128, :], writes=[r_xs[b]])
                    S.op("act", lambda e, b=b: e.activation(out=junk[:], in_=xs[b][:], func=AF.Square,
                                                            accum_out=ssq[:, b:b + 1]),
                         reads=[r_xs[b]], writes=[r_junk, r_ssq[b]])
                    K.rsq(rstd[:, b:b + 1], ssq[:, b:b + 1], 1.0 / D, [r_ssq[b]], [r_rstd[b]])
                    S.op("dve", lambda e, b=b: e.scalar_tensor_tensor(out=xn[b][:], in0=xs[b][:], scalar=rstd[:, b:b + 1],
                                                                      in1=gbc[:], op0=ALU.mult, op1=ALU.mult),
                         reads=[r_xs[b], r_rstd[b], r_gbc], writes=[r_xn[b]])
                    for hh in range(2):
                        pt = tp[2 * b + hh]
                        rp = r_tp[2 * b + hh]
                        for c in range(8):
                            cc = hh * 8 + c
                            S.op("pe", lambda e, pt=pt, c=c, cc=cc, b=b: e.transpose(out=pt[:, c, :], in_=xn[b][:, cc * 128:(cc + 1) * 128],
                                                                                      identity=C.ident[:]),
                                 reads=[r_xn[b], C.r], writes=[rp])
                        eng = "act" if hh == 0 else "dve"
                        if eng == "act":
                            S.op("act", lambda e, pt=pt, hh=hh, t=t: e.copy(out=xnT[:, hh * 8:hh * 8 + 8, t * 128:(t + 1) * 128], in_=pt[:]),
                                 reads=[rp], writes=[r_xnT[t]])
                        else:
                            S.op("dve", lambda e, pt=pt, hh=hh, t=t: e.tensor_copy(out=xnT[:, hh * 8:hh * 8 + 8, t * 128:(t + 1) * 128], in_=pt[:]),
                                 reads=[rp], writes=[r_xnT[t]])
                S.emit_phase()
            with ExitStack() as st:
                cos = sb(st, "cos", [128, HALF], F32)
                sin = sb(st, "sin", [128, HALF], F32)
                posi = sb(st, "posi", [128, 1024], I32)
                ang = sb(st, "ang", [128, 1024], F32)
                kf = sb(st, "kf", [128, 1024], F32)
                ki = sb(st, "ki", [128, 1024], I32)
                r_cs = Reg(const=True)
                r_tmp = Reg()
                for qd in range(2):
                    t0 = hf * HALF + qd * 1024
                    sl = slice(qd * 1024, (qd + 1) * 1024)
                    S.dma("sp", posi[:], I.pos[t0:t0 + 1024].partition_broadcast(128), writes=[r_tmp])
                    S.op("dve", lambda e: e.tensor_copy(out=ang[:], in_=posi[:]), reads=[r_tmp], writes=[r_tmp])
                    S.op("dve", lambda e: e.tensor_scalar(out=ang[:], in0=ang[:], scalar1=C.invf[:, 0:1], scalar2=None, op0=ALU.mult),
                         reads=[r_tmp, C.r], writes=[r_tmp])
                    S.op("dve", lambda e: e.tensor_scalar(out=kf[:], in0=ang[:], scalar1=1.0 / TWO_PI, scalar2=None, op0=ALU.mult),
                         reads=[r_tmp], writes=[r_tmp])
                    S.op("dve", lambda e: e.tensor_copy(out=ki[:], in_=kf[:]), reads=[r_tmp], writes=[r_tmp])
                    S.op("dve", lambda e: e.tensor_copy(out=kf[:], in_=ki[:]), reads=[r_tmp], writes=[r_tmp])
                    S.op("dve", lambda e: e.scalar_tensor_tensor(out=ang[:], in0=kf[:], scalar=-TWO_PI, in1=ang[:], op0=ALU.mult, op1=ALU.add),
                         reads=[r_tmp], writes=[r_tmp])

                    def wrap_and_sin(dst, shift):
                        S.op("dve", lambda e: e.tensor_scalar(out=kf[:], in0=ang[:], scalar1=shift, scalar2=None, op0=ALU.add),
                             reads=[r_tmp], writes=[r_tmp])
                        S.op("dve", lambda e: e.tensor_scalar(out=posi[:].bitcast(F32), in0=kf[:], scalar1=float(np.pi), scalar2=-TWO_PI,
                                                              op0=ALU.is_gt, op1=ALU.mult), reads=[r_tmp], writes=[r_tmp])
                        S.op("dve", lambda e: e.tensor_tensor(out=kf[:], in0=kf[:], in1=posi[:].bitcast(F32), op=ALU.add),
                             reads=[r_tmp], writes=[r_tmp])
                        S.op("dve", lambda e: e.tensor_scalar(out=posi[:].bitcast(F32), in0=kf[:], scalar1=-float(np.pi), scalar2=TWO_PI,
                                                              op0=ALU.is_lt, op1=ALU.mult), reads=[r_tmp], writes=[r_tmp])
                        S.op("dve", lambda e: e.tensor_tensor(out=kf[:], in0=kf[:], in1=posi[:].bitcast(F32), op=ALU.add),
                             reads=[r_tmp], writes=[r_tmp])
                        S.op("dve", lambda e: e.tensor_scalar(out=kf[:], in0=kf[:], scalar1=PI_LO, scalar2=-PI_LO, op0=ALU.min, op1=ALU.max),
                             reads=[r_tmp], writes=[r_tmp])
                        S.op("act", lambda e: e.activation(out=dst, in_=kf[:], func=AF.Sin), reads=[r_tmp], writes=[r_cs, r_tmp])
                    wrap_and_sin(sin[:, sl], 0.0)
                    wrap_and_sin(cos[:, sl], float(np.pi / 2))

                wq = [sb(st, "wq%d" % i, [128, NCH, 128], BF16) for i in range(2)]
                r_wq = [Reg() for _ in range(2)]
                q2 = [sb(st, "q2_%d" % i, [128, 512], BF16) for i in range(2)]
                qb = [sb(st, "qb_%d" % i, [128, 512], BF16) for i in range(2)]
                rs = [sb(st, "rs_%d" % i, [128, 512], F32) for i in range(2)]
                ta = [sb(st, "ta_%d" % i, [128, 512], F32) for i in range(2)]
                tb = [sb(st, "tb_%d" % i, [128, 512], F32) for i in range(2)]
                stage = [sb(st, "stg%d" % i, [128, HALF], BF16) for i in range(2)]
                r_q2 = [Reg() for _ in range(2)]
                r_qb = [Reg() for _ in range(2)]
                r_rs = [Reg() for _ in range(2)]
                r_ta = [Reg() for _ in range(2)]
                r_tb = [Reg() for _ in range(2)]
                r_stage = [Reg() for _ in range(2)]
                qp = [ps(st, "qp%d" % i, [128, 512], F32) for i in range(2)]
                sp_ = [ps(st, "ssp%d" % i, [128, 512], F32) for i in range(2)]
                rp_ = [ps(st, "rtp%d" % i, [128, 512], F32) for i in range(2)]
                r_qp = [Reg() for _ in range(2)]
                r_sp = [Reg() for _ in range(2)]
                r_rp = [Reg() for _ in range(2)]
                it = 0
                for hc in range(26):
                    wb = hc % 2
                    fam = HC_FAM[hc]
                    c0 = HC_COL[hc]
                    S.dma("pool", wq[wb][:], w_in_v[:, :, c0:c0 + 128], writes=[r_wq[wb]])
                    for blk in range(4):
                        b = it % 2
                        it += 1
                        cs = slice(blk * 512, (blk + 1) * 512)
                        for c in range(NCH):
                            S.op("pe", lambda e, b=b, wb=wb, c=c, cs=cs: e.matmul(qp[b][:], lhsT=wq[wb][:, c, :], rhs=xnT[:, c, cs],
                                                                                    start=(c == 0), stop=(c == NCH - 1)),
                                 reads=[r_wq[wb]] + r_xnT[blk * 4:blk * 4 + 4], writes=[r_qp[b]])
                        S.op("act", lambda e, b=b: e.activation(out=q2[b][:], in_=qp[b][:], func=AF.Square),
                             reads=[r_qp[b]], writes=[r_q2[b]])
                        S.op("act", lambda e, b=b: e.copy(out=qb[b][:], in_=qp[b][:]), reads=[r_qp[b]], writes=[r_qb[b]])
                        S.op("pe", lambda e, b=b: e.matmul(sp_[b][:], lhsT=C.ones[:], rhs=q2[b][:], start=True, stop=True),
                             reads=[r_q2[b], C.r], writes=[r_sp[b]])
                        S.op("pe", lambda e, b=b, fam=fam: e.matmul(rp_[b][:], lhsT=C.rotg[:, fam, :], rhs=qb[b][:], start=True, stop=True),
                             reads=[r_qb[b], C.r], writes=[r_rp[b]])
                        K.rsq(rs[b][:], sp_[b][:], 1.0 / HD, [r_sp[b]], [r_rs[b]])
                        S.op("dve", lambda e, b=b, fam=fam, cs=cs: e.scalar_tensor_tensor(out=ta[b][:], in0=qp[b][:], scalar=C.gh[:, fam:fam + 1],
                                                                                         in1=cos[:, cs], op0=ALU.mult, op1=ALU.mult),
                             reads=[r_qp[b], r_cs, C.r], writes=[r_ta[b]])
                        S.op("dve", lambda e, b=b, cs=cs: e.tensor_tensor(out=tb[b][:], in0=rp_[b][:], in1=sin[:, cs], op=ALU.mult),
                             reads=[r_rp[b], r_cs], writes=[r_tb[b]])
                        S.op("dve", lambda e, b=b: e.tensor_tensor(out=ta[b][:], in0=ta[b][:], in1=tb[b][:], op=ALU.add),
                             reads=[r_ta[b], r_tb[b]], writes=[r_ta[b]])
                        S.op("dve", lambda e, b=b, wb=wb, cs=cs: e.tensor_tensor(out=stage[wb][:, cs], in0=ta[b][:], in1=rs[b][:], op=ALU.mult),
                             reads=[r_ta[b], r_rs[b]], writes=[r_stage[wb]])
                    S.dma("sp", Sx.qkt[hc, :, hf * HALF:(hf + 1) * HALF], stage[wb][:], reads=[r_stage[wb]], writes=[R.qkt[hc][hf]])
                S.emit_phase()
            with ExitStack() as st:
                wv = sb(st, "wv", [128, NCH, 1280], BF16)
                r_wv = [Reg() for _ in range(3)]
                vst = [sb(st, "vst%d" % i, [128, 1280], BF16) for i in range(2)]
                r_vst = [Reg() for _ in range(2)]
                vp = [ps(st, "vp%d" % i, [128, 512], F32) for i in range(4)]
                r_vp = [Reg() for _ in range(4)]
                off = 0
                pieces = []
                for i, (c0, n) in enumerate(V_COLS):
                    S.dma("pool", wv[:, :, off:off + n], w_in_v[:, :, c0:c0 + n], writes=[r_wv[i]])
                    pieces.append((off, n))
                    off += n
                it = 0
                for t in range(16):
                    b = t % 2
                    tok0 = hf * HALF + t * 128
                    for i, (o, n) in enumerate(pieces):
                        pb = it % 4
                        it += 1
                        for c in range(NCH):
                            S.op("pe", lambda e, pb=pb, c=c, t=t, o=o, n=n: e.matmul(vp[pb][:, 0:n], lhsT=xnT[:, c, t * 128:(t + 1) * 128],
                                                                                     rhs=wv[:, c, o:o + n], start=(c == 0), stop=(c == NCH - 1)),
                                 reads=[r_wv[i], r_xnT[t]], writes=[r_vp[pb]])
                        if i % 2 == 0:
                            S.op("act", lambda e, pb=pb, b=b, o=o, n=n: e.copy(out=vst[b][:, o:o + n], in_=vp[pb][:, 0:n]),
                                 reads=[r_vp[pb]], writes=[r_vst[b]])
                        else:
                            S.op("dve", lambda e, pb=pb, b=b, o=o, n=n: e.tensor_copy(out=vst[b][:, o:o + n], in_=vp[pb][:, 0:n]),
                                 reads=[r_vp[pb]], writes=[r_vst[b]])
                    S.dma("sp", Sx.v[tok0:tok0 + 128, :], vst[b][:], reads=[r_vst[b]], writes=[R.v[hf * 16 + t]])
                S.emit_phase()


def phase_b(K, top):
    nc, S, I, C, Sx, R, sb, ps = K.nc, K.S, K.I, K.C, K.Sx, K.R, K.sb, K.ps
    mm, tr, act, ts, tt, stt, recip, cp = K.mm, K.tr, K.act, K.ts, K.tt, K.stt, K.recip, K.cp
    LOOK = 3
    with ExitStack() as st:
        maskA = sb(st, "maskA", [128, 20, 512], BF16)
        maskB = sb(st, "maskB", [128, 6, 512], BF16)
        r_mask = Reg(const=True)
        S.dma("sp", maskA[:], I.c_maska, writes=[r_mask])
        S.dma("sp", maskB[:], I.c_maskb, writes=[r_mask])
        QT = [sb(st, "QT%d" % i, [128, SEQ], BF16) for i in range(2)]
        KT = [sb(st, "KT%d" % i, [128, SEQ], BF16) for i in range(2)]
        V1 = [sb(st, "V1%d" % i, [128, NT, 130], BF16) for i in range(2)]
        OTs = [sb(st, "OTs%d" % i, [128, SEQ], BF16) for i in range(2)]
        r_QT = [Reg() for _ in range(2)]
        r_KT = [Reg() for _ in range(2)]
        r_V1 = [Reg() for _ in range(2)]
        r_OTs = [Reg() for _ in range(2)]
        NE = 6
        NSP = 3
        E = [sb(st, "E%d" % i, [128, 512], BF16) for i in range(NE)]
        Pm = [sb(st, "Pm%d" % i, [128, 512], BF16) for i in range(NE)]
        r_E = [Reg() for _ in range(NE)]
        r_Pm = [Reg() for _ in range(NE)]
        NO = 8
        Osb = [sb(st, "Osb%d" % i, [128, 130], F32) for i in range(NO)]
        den = [sb(st, "den%d" % i, [128, 1], F32) for i in range(NO)]
        sst = [sb(st, "sst%d" % i, [128, 1], F32) for i in range(NO)]
        obf = [sb(st, "obf%d" % i, [128, 128], BF16) for i in range(NO)]
        junk = sb(st, "junkb", [128, 128], BF16)
        r_Osb = [Reg() for _ in range(NO)]
        r_den = [Reg() for _ in range(NO)]
        r_sst = [Reg() for _ in range(NO)]
        r_obf = [Reg() for _ in range(NO)]
        r_junk = Reg()
        Sp = [ps(st, "Sp%d" % i, [128, 512], F32) for i in range(NSP)]
        Op_ = [ps(st, "Op%d" % i, [128, 512], F32) for i in range(4)]
        Tp = ps(st, "Tp", [128, 512], F32)
        tpv = Tp[:].bitcast(BF16)
        r_Sp = [Reg() for _ in range(NSP)]
        r_Op = [Reg() for _ in range(4)]
        r_Tp = [Reg() for _ in range(8)]
        for i in range(2):
            S.op("dve", lambda e, i=i: e.memset(V1[i][:, :, 128:130], 1.0), writes=[r_V1[i]])

        iters = []
        for h in range(16):
            isA = h < 8
            nj, koff = (20, -1024) if isA else (6, -128)
            for qb in range(8):
                q0 = qb * 512
                js = [j for j in range(nj) if 0 <= q0 + koff + 128 * j < SEQ]
                half = 1024 if isA else 128
                rng = {}
                for j in js:
                    rel = koff + 128 * j
                    c0 = max(0, rel - half) // 128
                    c1 = min(512, rel + 127 + half + 1 + 127) // 128
                    rng[j] = (c0, min(4, c1))
                for jn, j in enumerate(js):
                    c0, c1 = rng[j]
                    fl = {}
                    for i in range(c0, c1):
                        cov = [jj for jj in js if rng[jj][0] <= i < rng[jj][1]]
                        fl[i] = (j == cov[0], j == cov[-1])
                    iters.append(dict(h=h, qb=qb, j=j, k0=q0 + koff + 128 * j, first=(jn == 0), last=(jn == len(js) - 1),
                                      c0=c0, c1=c1, fl=fl,
                                      hstart=(qb == 0 and jn == 0), hend=(qb == 7 and jn == len(js) - 1)))
        state = {"osb": 0, "ts": 0}
        deferred = []

        def head_cfg(h):
            if h < 8:
                return h, 8 + h, 128 * h, maskA
            kvh = (h - 8) // 4
            return 16 + (h - 8), 24 + kvh, 1024 + 128 * kvh, maskB

        def load_head(h):
            hb = h % 2
            qhc, khc, vcol, _ = head_cfg(h)
            S.dma("sp", QT[hb][:], Sx.qkt[qhc], reads=R.qkt[qhc], writes=[r_QT[hb]])
            S.dma("sp", KT[hb][:], Sx.qkt[khc], reads=R.qkt[khc], writes=[r_KT[hb]])
            S.dma("sp", V1[hb][:, :, 0:128], Sx.v[:, vcol:vcol + 128].rearrange("(t p) d -> p t d", p=128),
                  reads=R.v, writes=[r_V1[hb]])

        def score(n):
            it = iters[n]
            hb = it["h"] % 2
            mask = head_cfg(it["h"])[3]
            sbi, ei = n % NSP, n % NE
            k0, q0, j = it["k0"], it["qb"] * 512, it["j"]
            a0, a1 = it["c0"] * 128, it["c1"] * 128
            mm(Sp[sbi][:, a0:a1], KT[hb][:, k0:k0 + 128], QT[hb][:, q0 + a0:q0 + a1], True, False, [r_KT[hb], r_QT[hb]], [r_Sp[sbi]])
            mm(Sp[sbi][:, a0:a1], C.ident[:], mask[:, j, a0:a1], False, True, [r_mask, C.r], [r_Sp[sbi]])
            act(Pm[ei][:, a0:a1], Sp[sbi][:, a0:a1], AF.Exp, [r_Sp[sbi]], [r_Pm[ei]], scale=SCALE)

        def post(h, qb, i, o):
            hb = h % 2
            grp = 0 if h < 8 else 1
            qt = 4 * qb + i
            if h < 8:
                recip(den[o][:], Osb[o][:, 128:129], [r_Osb[o]], [r_den[o]])
            else:
                tt(den[o][:], Osb[o][:, 128:129], C.esink[:, h - 8:h - 7], ALU.add, [r_Osb[o], C.r], [r_den[o]])
                recip(den[o][:], den[o][:], [r_den[o]], [r_den[o]])
            ts(obf[o][:], Osb[o][:, 0:128], den[o][:, 0:1], None, ALU.mult, None, [r_Osb[o], r_den[o]], [r_obf[o]])
            act(junk[:], obf[o][:], AF.Square, [r_obf[o]], [r_junk, r_sst[o]], accum_out=sst[o][:])
            tt(C.ss[:, grp, qt:qt + 1], C.ss[:, grp, qt:qt + 1], sst[o][:], ALU.add, [r_sst[o], C.r_ss], [C.r_ss])
            tsl = state["ts"] % 8
            state["ts"] += 1
            tr(tpv[:, tsl * 128:(tsl + 1) * 128], obf[o][:], [r_obf[o]], [r_Tp[tsl]])
            cp(OTs[hb][:, qt * 128:(qt + 1) * 128], tpv[:, tsl * 128:(tsl + 1) * 128], [r_Tp[tsl]], [r_OTs[hb]], eng="act")

        def pv(n):
            it = iters[n]
            h, qb = it["h"], it["qb"]
            hb = h % 2
            ei = n % NE
            k0 = it["k0"]
            for i in range(it["c0"], it["c1"]):
                mm(Op_[i][:, 0:129], Pm[ei][:, 128 * i:128 * i + 128], V1[hb][:, k0 // 128, 0:129], it["fl"][i][0], it["fl"][i][1],
                   [r_Pm[ei], r_V1[hb]], [r_Op[i]])
            if it["last"]:
                for i in range(4):
                    o = state["osb"] % NO
                    state["osb"] += 1
                    cp(Osb[o][:, 0:129], Op_[i][:, 0:129], [r_Op[i]], [r_Osb[o]])
                    deferred.append([2, (lambda h=h, qb=qb, i=i, o=o: post(h, qb, i, o))])
            if it["hend"]:
                deferred.append([3, (lambda h=h, hb=hb: S.dma("sp", Sx.ot[h], OTs[hb][:], reads=[r_OTs[hb]], writes=[R.ot[h]]))])

        def tick():
            for d in deferred:
                d[0] -= 1
            while deferred and deferred[0][0] <= 0:
                deferred.pop(0)[1]()

        N = len(iters)
        load_head(0)
        for n in range(N + LOOK):
            if n < N:
                score(n)
            if n - LOOK >= 0:
                pv(n - LOOK)
                if iters[n - LOOK]["hstart"] and iters[n - LOOK]["h"] + 1 < 16:
                    load_head(iters[n - LOOK]["h"] + 1)
            tick()
        while deferred:
            tick()
        S.emit_phase()


def phase_c(K, top):
    nc, S, I, C, Sx, R, sb, ps = K.nc, K.S, K.I, K.C, K.Sx, K.R, K.sb, K.ps
    mm, tr, act, ts, tt, stt, recip, cp = K.mm, K.tr, K.act, K.ts, K.tt, K.stt, K.recip, K.cp
    with ExitStack() as st0:
        KmT = sb(st0, "KmT", [128, 4, 256], BF16)
        Vm = sb(st0, "Vm", [128, 2, 4, 130], BF16)
        r_km = Reg(const=True)
        with ExitStack() as st:
            memx = sb(st, "memx", [128, 2, D], F32)
            memn = sb(st, "memn", [128, 2, D], BF16)
            memT = sb(st, "memT", [128, NCH, 256], BF16)
            gbc = sb(st, "gbcm", [128, D], F32)
            junk = sb(st, "junkc0", [128, D], BF16)
            wkv = sb(st, "wkv", [128, NCH, 1024], BF16)
            ssq = sb(st, "ssqm", [128, 2], F32)
            k2 = sb(st, "k2", [128, 256], BF16)
            rsk = sb(st, "rsk", [128, 256], F32)
            bank = [ps(st, "c0b%d" % i, [128, 512], F32) for i in range(4)]
            rb = [Reg() for _ in range(4)]
            r1 = Reg()
            r_w = Reg()
            S.dma("sp", memx[:], I.mem.rearrange("(t p) d -> p t d", p=128), writes=[r1])
            S.dma("sp", gbc[:], I.g_mem.partition_broadcast(128), writes=[r1])
            S.dma("pool", wkv[:], I.w_kv_mem.rearrange("(c p) n -> p c n", p=128), writes=[r_w])
            S.op("dve", lambda e: e.memset(Vm[:], 1.0), writes=[r_km])
            for mt in range(2):
                act(junk[:], memx[:, mt, :], AF.Square, [r1], [r1], accum_out=ssq[:, mt:mt + 1])
                K.rsq(ssq[:, mt:mt + 1], ssq[:, mt:mt + 1], 1.0 / D, [r1], [r1])
                stt(memn[:, mt, :], memx[:, mt, :], ssq[:, mt:mt + 1], gbc[:], ALU.mult, ALU.mult, [r1], [r1])
                for hh in range(2):
                    tpv = bank[hh][:].bitcast(BF16)
                    for c in range(8):
                        cc = hh * 8 + c
                        tr(tpv[:, c * 128:(c + 1) * 128], memn[:, mt, cc * 128:(cc + 1) * 128], [r1], [rb[hh]])
                    cp(memT[:, hh * 8:hh * 8 + 8, mt * 128:(mt + 1) * 128], tpv[:, 0:1024].rearrange("p (c n) -> p c n", c=8), [rb[hh]], [r1])
            for hm in range(4):
                for c in range(NCH):
                    mm(bank[2][:, 0:256], wkv[:, c, hm * 128:(hm + 1) * 128], memT[:, c, :], c == 0, c == NCH - 1, [r_w, r1], [rb[2]])
                act(k2[:], bank[2][:, 0:256], AF.Square, [rb[2]], [r1])
                mm(bank[3][:, 0:256], C.ones[:], k2[:], True, True, [r1, C.r], [rb[3]])
                K.rsq(rsk[:], bank[3][:, 0:256], 1.0 / HD, [rb[3]], [r1])
                stt(KmT[:, hm, :], bank[2][:, 0:256], C.gh[:, 5:6], rsk[:], ALU.mult, ALU.mult, [rb[2], r1, C.r], [r_km])
            for mt in range(2):
                for c in range(NCH):
                    mm(bank[mt][:], memT[:, c, mt * 128:(mt + 1) * 128], wkv[:, c, 512:1024], c == 0, c == NCH - 1, [r_w, r1], [rb[mt]])
                cp(Vm[:, mt, :, 0:128], bank[mt][:].rearrange("p (h d) -> p h d", h=4), [rb[mt]], [r_km])
            S.emit_phase()
        with ExitStack() as st:
            wout = K.wout
            wq = sb(st, "wqm", [128, NCH, 512], BF16)
            wo = sb(st, "wom", [128, 4, D], BF16)
            wr = sb(st, "wr", [128, NCH, N_EXP], BF16)
            gcr = sb(st, "gcr", [128, D], F32)
            gmo = sb(st, "gmo", [128, D], F32)
            rAB = sb(st, "rAB", [128, 2, NT], F32)
            r_w = Reg(const=True)
            S.dma("pool", wq[:], I.w_q_mem.rearrange("(c p) n -> p c n", p=128), writes=[r_w])
            S.dma("pool", wo[:], I.w_o_mem.rearrange("(c p) n -> p c n", p=128), writes=[r_w])
            S.dma("pool", wr[:], I.w_router.rearrange("(c p) n -> p c n", p=128), writes=[r_w])
            S.dma("sp", gcr[:], I.g_cross.partition_broadcast(128), writes=[r_w])
            S.dma("sp", gmo[:], I.g_moe.partition_broadcast(128), writes=[r_w])
            K.rsq(rAB[:], C.ss[:], 1.0 / 1024, [C.r_ss], [r_w])

            xt = [sb(st, "xt%d" % i, [128, D], F32) for i in range(2)]
            otb = [sb(st, "otb%d" % i, [128, 16, 128], BF16) for i in range(2)]
            x12 = [sb(st, "x12_%d" % i, [128, D], F32) for i in range(2)]
            hn = [sb(st, "hn%d" % i, [128, D], BF16) for i in range(2)]
            hT = [sb(st, "hT%d" % i, [128, NCH, 128], BF16) for i in range(2)]
            junk = sb(st, "junkc1", [128, D], BF16)
            sq = sb(st, "sqc", [128, 4], F32)
            q2 = sb(st, "q2c", [128, 512], BF16)
            rsq = sb(st, "rsqc", [128, 512], F32)
            qn = sb(st, "qnc", [128, 4, 128], BF16)
            E2 = [sb(st, "E2_%d" % i, [128, 4, 128], BF16) for i in range(2)]
            rden = sb(st, "rdenc", [128, 4], F32)
            o2 = sb(st, "o2c", [128, 4, 128], BF16)
            o2T = sb(st, "o2T", [128, 4, 128], BF16)
            ex = sb(st, "exr", [128, N_EXP], F32)
            sume = sb(st, "sume", [128, 1], F32)
            r_xt = [Reg() for _ in range(2)]
            r_otb = [Reg() for _ in range(2)]
            r_x12 = [Reg() for _ in range(2)]
            r_hn = [Reg() for _ in range(2)]
            r_hT = [Reg() for _ in range(2)]
            r_junk, r_sq, r_q2, r_rsq, r_qn, r_rden, r_o2, r_o2T, r_ex, r_sume = [Reg() for _ in range(10)]
            r_E2 = [Reg() for _ in range(2)]
            B = [ps(st, "c1b%d" % i, [128, 512], F32) for i in range(8)]
            rB = [Reg() for _ in range(8)]
            tpv = B[4][:].bitcast(BF16)
            ot_v = Sx.ot.rearrange("h p n -> p h n")

            def rmsnorm_tile(src, r_src, gb, dst, r_dst, col):
                act(junk[:], src, AF.Square, [r_src], [r_junk, r_sq], accum_out=sq[:, col:col + 1])
                K.rsq(sq[:, col:col + 1], sq[:, col:col + 1], 1.0 / D, [r_sq], [r_sq])
                stt(dst, src, sq[:, col:col + 1], gb[:], ALU.mult, ALU.mult, [r_src, r_sq, r_w], [r_dst])

            def transpose16(src, r_src, dst, r_dst):
                for hh in range(2):
                    for c in range(8):
                        cc = hh * 8 + c
                        tr(tpv[:, c * 128:(c + 1) * 128], src[:, cc * 128:(cc + 1) * 128], [r_src], [rB[4]])
                    cp(dst[:, hh * 8:hh * 8 + 8, :], tpv.rearrange("p (c n) -> p c n", c=8), [rB[4]], [r_dst],
                       eng=("act" if hh == 0 else "dve"))

            def load(t):
                b = t % 2
                S.dma("sp", xt[b][:], I.x[t * 128:(t + 1) * 128, :], writes=[r_xt[b]])
                S.dma("sp", otb[b][:], ot_v[:, :, t * 128:(t + 1) * 128], reads=R.ot, writes=[r_otb[b]])
            def s1_cg(t, cg):
                b = t % 2
                X = x12[b]
                pa, pb = B[(cg % 2) * 2], B[(cg % 2) * 2 + 1]
                ra, rbb = rB[(cg % 2) * 2], rB[(cg % 2) * 2 + 1]
                cs = slice(cg * 512, (cg + 1) * 512)
                for c in range(8):
                    mm(pa[:], otb[b][:, c, :], wout[:, c, cs], c == 0, c == 7, [r_otb[b], r_w], [ra])
                for c in range(8, 16):
                    mm(pb[:], otb[b][:, c, :], wout[:, c, cs], c == 8, c == 15, [r_otb[b], r_w], [rbb])
                stt(X[:, cs], pa[:], rAB[:, 0, t:t + 1], xt[b][:, cs], ALU.mult, ALU.add, [ra, r_xt[b], r_w], [r_x12[b]])
                stt(X[:, cs], pb[:], rAB[:, 1, t:t + 1], X[:, cs], ALU.mult, ALU.add, [rbb, r_x12[b], r_w], [r_x12[b]])

            def nxt(t, cg):
                if t + 1 < NT:
                    s1_cg(t + 1, cg)

            load(0)
            load(1)
            for cg in range(4):
                s1_cg(0, cg)
            for t in range(NT):
                b = t % 2
                X = x12[b]
                rmsnorm_tile(X[:], r_x12[b], gcr, hn[0][:], r_hn[0], 0)
                nxt(t, 0)
                transpose16(hn[0], r_hn[0], hT[0], r_hT[0])
                for hm in range(4):
                    for c in range(NCH):
                        mm(B[5][:, hm * 128:(hm + 1) * 128], wq[:, c, hm * 128:(hm + 1) * 128], hT[0][:, c, :], c == 0, c == NCH - 1,
                           [r_w, r_hT[0]], [rB[5]])
                act(q2[:], B[5][:], AF.Square, [rB[5]], [r_q2])
                nxt(t, 1)
                mm(B[6][:], C.ones[:], q2[:], True, True, [r_q2, C.r], [rB[6]])
                K.rsq(rsq[:], B[6][:], 1.0 / HD, [rB[6]], [r_rsq])
                stt(qn[:].rearrange("p h n -> p (h n)"), B[5][:], C.gh[:, 4:5], rsq[:], ALU.mult, ALU.mult, [rB[5], r_rsq, C.r], [r_qn])
                nxt(t, 2)
                for mt in range(2):
                    bk = 7 if mt == 0 else 4
                    for hm in range(4):
                        mm(B[bk][:, hm * 128:(hm + 1) * 128], KmT[:, hm, mt * 128:(mt + 1) * 128], qn[:, hm, :], True, True,
                           [r_km, r_qn], [rB[bk]])
                    act(E2[mt][:].rearrange("p h n -> p (h n)"), B[bk][:], AF.Exp, [rB[bk]], [r_E2[mt]], scale=SCALE)
                nxt(t, 3)
                for hm in range(4):
                    bk = 5 if hm < 2 else 6
                    o0 = (hm % 2) * 130
                    for mt in range(2):
                        mm(B[bk][:, o0:o0 + 129], E2[mt][:, hm, :], Vm[:, mt, hm, 0:129], mt == 0, mt == 1, [r_E2[mt], r_km], [rB[bk]])
                for hm in range(4):
                    bk = 5 if hm < 2 else 6
                    o0 = (hm % 2) * 130
                    recip(rden[:, hm:hm + 1], B[bk][:, o0 + 128:o0 + 129], [rB[bk]], [r_rden])
                    ts(o2[:, hm, :], B[bk][:, o0:o0 + 128], rden[:, hm:hm + 1], None, ALU.mult, None, [rB[bk], r_rden], [r_o2])
                for hm in range(4):
                    tr(tpv[:, hm * 128:(hm + 1) * 128], o2[:, hm, :], [r_o2], [rB[4]])
                cp(o2T[:].rearrange("p h n -> p (h n)"), tpv[:, 0:512], [rB[4]], [r_o2T], eng="act")
                for cg in range(4):
                    pa = B[cg % 4]
                    ra = rB[cg % 4]
                    cs = slice(cg * 512, (cg + 1) * 512)
                    for hm in range(4):
                        mm(pa[:], o2T[:, hm, :], wo[:, hm, cs], hm == 0, hm == 3, [r_o2T, r_w], [ra])
                    tt(X[:, cs], pa[:], X[:, cs], ALU.add, [ra, r_x12[b]], [r_x12[b]])
                S.dma("sp", Sx.x2[t * 128:(t + 1) * 128, :], X[:], reads=[r_x12[b]], writes=[R.x2[t]])
                rmsnorm_tile(X[:], r_x12[b], gmo, hn[1][:], r_hn[1], 1)
                S.dma("sp", Sx.h3[t * 128:(t + 1) * 128, :], hn[1][:], reads=[r_hn[1]], writes=[R.h3[t]])
                if t + 2 < NT:
                    load(t + 2)
                transpose16(hn[1], r_hn[1], hT[1], r_hT[1])
                for c in range(NCH):
                    mm(B[5][:, 0:N_EXP], hT[1][:, c, :], wr[:, c, :], c == 0, c == NCH - 1, [r_hT[1], r_w], [rB[5]])
                act(ex[:], B[5][:, 0:N_EXP], AF.Exp, [rB[5]], [r_ex, r_sume], accum_out=sume[:])
                recip(sume[:], sume[:], [r_sume], [r_sume])
                ts(C.aff[:, t, :], ex[:], sume[:, 0:1], None, ALU.mult, None, [r_ex, r_sume], [C.r_aff])
            S.emit_phase()


N_BISECT = 34


def phase_d(K, top):
    nc, S, I, C, Sx, R, sb, ps = K.nc, K.S, K.I, K.C, K.Sx, K.R, K.sb, K.ps
    mm, tr, act, ts, tt, stt, recip, cp = K.mm, K.tr, K.act, K.ts, K.tt, K.stt, K.recip, K.cp
    with ExitStack() as st:
        lo = sb(st, "lo", [128, N_EXP], F32)
        hi = sb(st, "hi", [128, N_EXP], F32)
        mid = sb(st, "mid", [128, N_EXP], F32)
        ge = sb(st, "ge", [128, N_EXP], F32)
        dl = sb(st, "dl", [128, N_EXP], F32)
        cntp = sb(st, "cntp", [128, N_EXP], F32)
        cmp_ = sb(st, "cmp", [128, NT, N_EXP], F32)
        cnt = ps(st, "cnt", [128, 512], F32)
        r = Reg()
        r_cnt = Reg()
        S.op("dve", lambda e: e.memset(lo[:], 0.0), writes=[r])
        S.op("dve", lambda e: e.memset(hi[:], 1.0), writes=[r])

        def bc(t):
            return t[:, :].unsqueeze(1).to_broadcast([128, NT, N_EXP])
        for it in range(N_BISECT):
            tt(mid[:], lo[:], hi[:], ALU.add, [r], [r])
            ts(mid[:], mid[:], 0.5, None, ALU.mult, None, [r], [r])
            tt(cmp_[:], C.aff[:], bc(mid), ALU.is_gt, [r, C.r_aff], [r])
            S.op("dve", lambda e: e.tensor_reduce(out=cntp[:], in_=cmp_[:].rearrange("p t e -> p e t"), axis=AX.X, op=ALU.add),
                 reads=[r], writes=[r])
            mm(cnt[:, 0:N_EXP], C.onesf[:], cntp[:], True, True, [r, C.r], [r_cnt])
            ts(ge[:], cnt[:, 0:N_EXP], float(CAP), None, ALU.is_ge, None, [r_cnt], [r])
            tt(dl[:], mid[:], lo[:], ALU.subtract, [r], [r])
            tt(dl[:], dl[:], ge[:], ALU.mult, [r], [r])
            tt(lo[:], lo[:], dl[:], ALU.add, [r], [r])
            tt(dl[:], hi[:], mid[:], ALU.subtract, [r], [r])
            tt(dl[:], dl[:], ge[:], ALU.mult, [r], [r])
            tt(hi[:], mid[:], dl[:], ALU.add, [r], [r])
        tt(cmp_[:], C.aff[:], bc(lo), ALU.is_gt, [r, C.r_aff], [r])
        tt(cmp_[:], cmp_[:], C.aff[:], ALU.mult, [r, C.r_aff], [r])
        S.dma("sp", Sx.gm.rearrange("(t p) e -> p t e", p=128), cmp_[:], reads=[r], writes=[R.gm])
        S.emit_phase()


def phase_e(K, top):
    nc, S, I, C, Sx, R, sb, ps = K.nc, K.S, K.I, K.C, K.Sx, K.R, K.sb, K.ps
    mm, tr, act, ts, tt, stt, recip, cp = K.mm, K.tr, K.act, K.ts, K.tt, K.stt, K.recip, K.cp
    NOT = OWN // 128
    with ExitStack() as st0:
        own = sb(st0, "own", [128, NOT], I32)
        h3T = sb(st0, "h3T", [128, NCH, OWN], BF16)
        acc = sb(st0, "acc", [128, NOT, D], F32)
        gmo = sb(st0, "gmo_", [128, NOT, N_EXP], F32)
        r_own, r_h3T, r_gmo = Reg(const=True), Reg(const=True), Reg(const=True)
        r_acc = [Reg() for _ in range(NOT)]
        with ExitStack() as st:
            h3o = sb(st, "h3o", [128, NOT, D], BF16)
            r_h3o = [Reg() for _ in range(NOT)]
            tpb = [ps(st, "tpe%d" % i, [128, 512], F32) for i in range(2)]
            r_tpb = [Reg() for _ in range(2)]
            S.dma("sp", own[:], I.own, writes=[r_own])
            for j in range(NOT):
                off = bass.IndirectOffsetOnAxis(ap=own[:, j:j + 1], axis=0)
                S.dma_fn("pool", lambda e, j=j, off=off: e.indirect_dma_start(out=h3o[:, j, :], out_offset=None, in_=Sx.h3, in_offset=off),
                         reads=[r_own] + R.h3, writes=[r_h3o[j]])
                S.dma_fn("pool", lambda e, j=j, off=off: e.indirect_dma_start(out=acc[:, j, :], out_offset=None, in_=Sx.x2, in_offset=off),
                         reads=[r_own] + R.x2, writes=[r_acc[j]])
                S.dma_fn("pool", lambda e, j=j, off=off: e.indirect_dma_start(out=gmo[:, j, :], out_offset=None, in_=Sx.gm, in_offset=off),
                         reads=[r_own, R.gm], writes=[r_gmo])
            k = 0
            for j in range(NOT):
                for hh in range(2):
                    pb = k % 2
                    k += 1
                    tpv = tpb[pb][:].bitcast(BF16)
                    for c in range(8):
                        cc = hh * 8 + c
                        tr(tpv[:, c * 128:(c + 1) * 128], h3o[:, j, cc * 128:(cc + 1) * 128], [r_h3o[j]], [r_tpb[pb]])
                    cp(h3T[:, hh * 8:hh * 8 + 8, j * 128:(j + 1) * 128], tpv.rearrange("p (c n) -> p c n", c=8), [r_tpb[pb]], [r_h3T],
                       eng=("act" if hh == 0 else "dve"))
            S.emit_phase()
        with ExitStack() as st:
            FP = 256
            NFP = D // FP
            wg = [sb(st, "wg%d" % i, [128, NCH, FP], BF16) for i in range(2)]
            wu = [sb(st, "wu%d" % i, [128, NCH, FP], BF16) for i in range(2)]
            wd = [sb(st, "wd%d" % i, [128, NCH, FP], BF16) for i in range(2)]
            r_wg = [Reg() for _ in range(2)]
            r_wu = [Reg() for _ in range(2)]
            r_wd = [Reg() for _ in range(2)]
            actT = sb(st, "actT", [128, NCH, OWN], BF16)
            r_actT = [Reg() for _ in range(NCH)]
            sg = [sb(st, "sg%d" % i, [128, 512], F32) for i in range(2)]
            r_sg = [Reg() for _ in range(2)]
            Gp = [ps(st, "Gp%d" % i, [128, 512], F32) for i in range(2)]
            Up = [ps(st, "Up%d" % i, [128, 512], F32) for i in range(2)]
            Yp = [ps(st, "Yp%d" % i, [128, 512], F32) for i in range(3)]
            r_Gp = [Reg() for _ in range(2)]
            r_Up = [Reg() for _ in range(2)]
            r_Yp = [Reg() for _ in range(3)]
            wi = 0
            di = 0
            gi = 0
            yi = 0
            for ex in range(N_EXP):
                wgv = I.w_gate[ex].rearrange("(c p) f -> p c f", p=128)
                wuv = I.w_up[ex].rearrange("(c p) f -> p c f", p=128)
                wdv = I.w_down[ex].rearrange("(c p) f -> p c f", p=128)
                for fp in range(NFP):
                    wb = wi % 2
                    wi += 1
                    S.dma("pool", wg[wb][:], wgv[:, :, fp * FP:(fp + 1) * FP], writes=[r_wg[wb]])
                    S.dma("pool", wu[wb][:], wuv[:, :, fp * FP:(fp + 1) * FP], writes=[r_wu[wb]])
                    for f2 in range(FP // 128):
                        fc = fp * (FP // 128) + f2
                        for half in range(2):
                            gb = gi % 2
                            gi += 1
                            cs = slice(half * 512, (half + 1) * 512)
                            for c in range(NCH):
                                mm(Gp[gb][:], wg[wb][:, c, f2 * 128:(f2 + 1) * 128], h3T[:, c, cs], c == 0, c == NCH - 1,
                                   [r_wg[wb], r_h3T], [r_Gp[gb]])
                            for c in range(NCH):
                                mm(Up[gb][:], wu[wb][:, c, f2 * 128:(f2 + 1) * 128], h3T[:, c, cs], c == 0, c == NCH - 1,
                                   [r_wu[wb], r_h3T], [r_Up[gb]])
                            act(sg[gb][:], Gp[gb][:], AF.Silu, [r_Gp[gb]], [r_sg[gb]])
                            tt(actT[:, fc, cs], sg[gb][:], Up[gb][:], ALU.mult, [r_sg[gb], r_Up[gb]], [r_actT[fc]])
                for dp in range(NFP):
                    db = di % 2
                    di += 1
                    S.dma("pool", wd[db][:], wdv[:, :, dp * FP:(dp + 1) * FP], writes=[r_wd[db]])
                    for j in range(NOT):
                        yb = yi % 3
                        yi += 1
                        for fc in range(NCH):
                            mm(Yp[yb][:, 0:FP], actT[:, fc, j * 128:(j + 1) * 128], wd[db][:, fc, :], fc == 0, fc == NCH - 1,
                               [r_actT[fc], r_wd[db]], [r_Yp[yb]])
                        stt(acc[:, j, dp * FP:(dp + 1) * FP], Yp[yb][:, 0:FP], gmo[:, j, ex:ex + 1], acc[:, j, dp * FP:(dp + 1) * FP],
                            ALU.mult, ALU.add, [r_Yp[yb], r_gmo, r_acc[j]], [r_acc[j]])
            outs = []
            for j in range(NOT):
                outs.append(S.dma("sp", K.out[j * 128:(j + 1) * 128, :], acc[:, j, :], reads=[r_acc[j]], writes=[]))
            S.finish(outs)
            S.emit_phase()


_NC_CACHE = {}


def _core_inputs(inp, c, consts):
    b, q = c // 4, c % 4
    m = {}
    m["x"] = np.ascontiguousarray(inp["x"][b], dtype=np.float32)
    m["mem"] = np.ascontiguousarray(inp["mem"][b], dtype=np.float32)
    m["positions"] = np.ascontiguousarray(inp["positions"][b]).astype(np.int32)
    own = (q * OWN + np.arange(OWN)).reshape(OWN // 128, 128).T.astype(np.int32)
    m["own_idx"] = np.ascontiguousarray(own)
    for n in ("g_mix", "g_cross", "g_mem", "g_moe", "g_qa", "g_ka", "g_qb", "g_kb", "g_qm", "g_km", "g_oa", "g_ob",
              "sink_b", "w_in", "w_out", "w_q_mem", "w_kv_mem", "w_o_mem", "w_router", "w_gate", "w_up", "w_down"):
        m[n] = np.ascontiguousarray(np.asarray(inp[n])[0], dtype=np.float32)
    m.update(consts)
    return m


def kernel(**inputs):
    inp = {k: np.asarray(v) for k, v in inputs.items()}
    if "nc" not in _NC_CACHE:
        _NC_CACHE["nc"] = build()
    nc = _NC_CACHE["nc"]
    consts = host_consts()
    in_maps = [_core_inputs(inp, c, consts) for c in range(8)]
    res = run_bass_kernel_spmd(nc, in_maps, core_ids=list(range(8)))
    out = np.zeros((2, SEQ, D), np.float32)
    for c in range(8):
        b, q = c // 4, c % 4
        out[b, q * OWN:(q + 1) * OWN] = np.asarray(res.results[c]["out"], dtype=np.float32)
    return out


MOE_FP = 512
MOE_NW = 6


def moe_ring_setup(K, stack):
    S, I, sb = K.S, K.I, K.sb
    FP, NW = MOE_FP, MOE_NW
    NFP = D // FP
    PPE = 3 * NFP
    K.Wb = [sb(stack, "Wb%d" % i, [128, NCH, FP], BF16) for i in range(NW)]
    K.r_W = [Reg() for _ in range(NW)]
    ring = {"next": 0}

    def piece_src(p):
        ex, k = divmod(p, PPE)
        if k < 2 * NFP:
            fp, gu = divmod(k, 2)
            w = I.w_gate if gu == 0 else I.w_up
            return w[ex].rearrange("(c p) f -> p c f", p=128)[:, :, fp * FP:(fp + 1) * FP]
        dp = k - 2 * NFP
        return I.w_down[ex].rearrange("(c p) f -> p c f", p=128)[:, :, dp * FP:(dp + 1) * FP]

    def ensure(upto):
        last = N_EXP * PPE - 1
        while ring["next"] <= min(upto, last):
            p = ring["next"]
            ring["next"] += 1
            S.dma("pool", K.Wb[p % NW][:], piece_src(p), writes=[K.r_W[p % NW]])
    K.ring_ensure = ensure
    ensure(NW - 1)


def phase_e2(K, top):
    nc, S, I, C, Sx, R, sb, ps = K.nc, K.S, K.I, K.C, K.Sx, K.R, K.sb, K.ps
    mm, tr, act, ts, tt, stt, recip, cp = K.mm, K.tr, K.act, K.ts, K.tt, K.stt, K.recip, K.cp
    NOT = OWN // 128
    NS = CAP // 128
    NTG = 3 + N_EXP
    with ExitStack() as st0:
        own = sb(st0, "own", [128, NOT], I32)
        gmo = sb(st0, "gmo_", [128, NOT, N_EXP], F32)
        mall = sb(st0, "mall", [128, NOT, N_EXP], F32)
        pos = sb(st0, "pos", [128, NOT, N_EXP], F32)
        TG = sb(st0, "TG", [128, NOT, NTG], F32)
        iota = sb(st0, "iota", [128, 512], F32)
        dummy = sb(st0, "dummy", [128, NS], F32)
        r_c = Reg(const=True)
        with ExitStack() as st:
            ut = sb(st, "ut", [128, 128], F32)
            loc = sb(st, "loc", [128, NOT], F32)
            offs = sb(st, "offs", [128, NOT, N_EXP], F32)
            x2b = [sb(st, "x2b%d" % i, [128, D], F32) for i in range(2)]
            r_x2b = [Reg() for _ in range(2)]
            pc = [ps(st, "pc%d" % i, [128, 512], F32) for i in range(2)]
            r_pc = [Reg() for _ in range(2)]
            r1 = Reg()
            S.dma("sp", own[:], I.own, writes=[r_c])
            S.dma("sp", ut[:], I.c_ut, writes=[r1])
            S.dma("sp", loc[:], I.c_loc, writes=[r1])
            S.dma("sp", iota[:], I.c_iota, writes=[r_c])
            S.dma("sp", dummy[:], I.c_dummy, writes=[r_c])
            for j in range(NOT):
                off = bass.IndirectOffsetOnAxis(ap=own[:, j:j + 1], axis=0)
                S.dma_fn("pool", lambda e, j=j, off=off: e.indirect_dma_start(out=gmo[:, j, :], out_offset=None, in_=Sx.gm, in_offset=off),
                         reads=[r_c, R.gm], writes=[r1])
                b = j % 2
                S.dma_fn("pool", lambda e, b=b, off=off: e.indirect_dma_start(out=x2b[b][:], out_offset=None, in_=Sx.x2, in_offset=off),
                         reads=[r_c] + R.x2, writes=[r_x2b[b]])
                S.dma("sp", Sx.accd[j * 128:(j + 1) * 128, :], x2b[b][:], reads=[r_x2b[b]], writes=[R.accd])
            ts(mall[:], gmo[:], 0.0, None, ALU.is_gt, None, [r1], [r1])
            flat = mall[:].rearrange("p j e -> p (j e)")
            mm(pc[0][:, 0:NOT * N_EXP], ut[:], flat, True, True, [r1], [r_pc[0]])
            mm(pc[1][:, 0:NOT * N_EXP], C.onesf[:], flat, True, True, [r1, C.r], [r_pc[1]])
            tot = pc[1][:, 0:NOT * N_EXP].rearrange("p (j e) -> p j e", e=N_EXP)
            cum = pc[0][:, 0:NOT * N_EXP].rearrange("p (j e) -> p j e", e=N_EXP)
            S.op("dve", lambda e: e.memset(offs[:, 0, :], 0.0), writes=[r1])
            for j in range(1, NOT):
                tt(offs[:, j, :], offs[:, j - 1, :], tot[:, j - 1, :], ALU.add, [r1, r_pc[1]], [r1])
            tt(pos[:], cum, offs[:], ALU.add, [r_pc[0], r1], [r1])
            ts(pos[:], pos[:], -1.0, None, ALU.add, None, [r1], [r1])
            cp(TG[:, :, 0], own[:], [r_c], [r1])
            cp(TG[:, :, 1], loc[:], [r1], [r1])
            S.op("dve", lambda e: e.memset(TG[:, :, 2], 1.0), writes=[r1])
            cp(TG[:, :, 3:NTG], gmo[:], [r1], [r1])
            S.emit_phase()
        with ExitStack() as st:
            FP, NFP, NW, PPE = MOE_FP, D // MOE_FP, MOE_NW, 3 * (D // MOE_FP)
            Wb, r_W, ensure = K.Wb, K.r_W, K.ring_ensure
            Sel = [sb(st, "Sel%d" % i, [128, NOT, 128], F32) for i in range(NS)]
            r_Sel = [Reg() for _ in range(NS)]
            sis = sb(st, "sis", [128, NS, 32], F32)
            sidf = sb(st, "sidf", [128, NS], F32)
            gi = [sb(st, "gi%d" % i, [128, NS], I32) for i in range(2)]
            si = [sb(st, "si%d" % i, [128, NS], I32) for i in range(2)]
            gs = [sb(st, "gs%d" % i, [128, NS], F32) for i in range(2)]
            r_sis = Reg()
            r_idx = [Reg() for _ in range(2)]
            xe = sb(st, "xe", [128, NS, D], BF16)
            r_xe = [Reg() for _ in range(NS)]
            xeT1 = sb(st, "xeT", [128, NCH, CAP], BF16)
            xeT = [xeT1, xeT1]
            r_xeT1 = Reg()
            r_xeT = [r_xeT1, r_xeT1]
            actT = sb(st, "actT", [128, NCH, CAP], BF16)
            r_actT = [Reg() for _ in range(NCH)]
            ygs = sb(st, "ygs", [128, NS, D], F32)
            r_ygs = [Reg() for _ in range(NS)]
            sg = [sb(st, "sg%d" % i, [128, 512], F32) for i in range(2)]
            r_sg = [Reg() for _ in range(2)]
            cnt_stg = {"n": 0}

            def wload(dst, r_dst, src):
                cnt_stg["n"] += 1
                S.dma("pool", dst, src, writes=[r_dst])
            Gp = [ps(st, "Gp%d" % i, [128, 512], F32) for i in range(2)]
            Up = [ps(st, "Up%d" % i, [128, 512], F32) for i in range(2)]
            Yp = [ps(st, "Yp%d" % i, [128, 512], F32) for i in range(2)]
            Tq = ps(st, "Tq", [128, 512], F32)
            SIp = ps(st, "SIp", [128, 512], F32)
            r_Gp = [Reg() for _ in range(2)]
            r_Up = [Reg() for _ in range(2)]
            r_Yp = [Reg() for _ in range(2)]
            r_Tq, r_SIp = Reg(), Reg()
            tqv = Tq[:].bitcast(BF16)
            cnt = {"w": 0, "d": 0, "g": 0, "y": 0}

            def prep_sel(ex):
                for s4 in range(NS):
                    for j in range(NOT):
                        ts(Sel[s4][:, j, :], iota[:, s4 * 128:(s4 + 1) * 128], pos[:, j, ex:ex + 1], mall[:, j, ex:ex + 1],
                           ALU.is_equal, ALU.mult, [r_c], [r_Sel[s4]])

            def prep_idx(ex):
                pb = ex % 2
                for s4 in range(NS):
                    sl = s4
                    for j in range(NOT):
                        mm(SIp[:, s4 * 32:s4 * 32 + NTG], Sel[sl][:, j, :], TG[:, j, :], j == 0, j == NOT - 1,
                           [r_Sel[sl], r_c], [r_SIp])
                cp(sis[:], SIp[:, 0:NS * 32].rearrange("p (s k) -> p s k", k=32), [r_SIp], [r_sis])
                cp(gi[pb][:], sis[:, :, 0], [r_sis], [r_idx[pb]])
                tt(sidf[:], sis[:, :, 2], dummy[:], ALU.mult, [r_sis, r_c], [r_sis])
                tt(sidf[:], dummy[:], sidf[:], ALU.subtract, [r_sis, r_c], [r_sis])
                tt(sidf[:], sidf[:], sis[:, :, 1], ALU.add, [r_sis], [r_sis])
                cp(si[pb][:], sidf[:], [r_sis], [r_idx[pb]])
                cp(gs[pb][:], sis[:, :, 3 + ex], [r_sis], [r_idx[pb]])
                for s4 in range(NS):
                    off = bass.IndirectOffsetOnAxis(ap=gi[pb][:, s4:s4 + 1], axis=0)
                    S.dma_fn("pool", lambda e, s4=s4, off=off: e.indirect_dma_start(out=xe[:, s4, :], out_offset=None, in_=Sx.h3, in_offset=off),
                             reads=[r_idx[pb]] + R.h3, writes=[r_xe[s4]])

            def prep_T(ex):
                pb = ex % 2
                k = 0
                for s4 in range(NS):
                    for hh in range(4):
                        for c in range(4):
                            cc = hh * 4 + c
                            tr(tqv[:, c * 128:(c + 1) * 128], xe[:, s4, cc * 128:(cc + 1) * 128], [r_xe[s4]], [r_Tq])
                        cp(xeT[pb][:, hh * 4:hh * 4 + 4, s4 * 128:(s4 + 1) * 128], tqv[:, 0:512].rearrange("p (c n) -> p c n", c=4),
                           [r_Tq], [r_xeT[pb]], eng=("act" if k % 2 == 0 else "dve"))
                        k += 1

            def compute_gu(ex, fp):
                pb = ex % 2
                pg = ex * PPE + 2 * fp
                wgt, wut = Wb[pg % NW], Wb[(pg + 1) % NW]
                rg, ru = r_W[pg % NW], r_W[(pg + 1) % NW]
                for f2 in range(FP // 128):
                    fc = fp * (FP // 128) + f2
                    gb = cnt["g"] % 2
                    cnt["g"] += 1
                    for c in range(NCH):
                        mm(Gp[gb][:], wgt[:, c, f2 * 128:(f2 + 1) * 128], xeT[pb][:, c, :], c == 0, c == NCH - 1,
                           [rg, r_xeT[pb]], [r_Gp[gb]])
                    for c in range(NCH):
                        mm(Up[gb][:], wut[:, c, f2 * 128:(f2 + 1) * 128], xeT[pb][:, c, :], c == 0, c == NCH - 1,
                           [ru, r_xeT[pb]], [r_Up[gb]])
                    act(sg[gb][:], Gp[gb][:], AF.Silu, [r_Gp[gb]], [r_sg[gb]])
                    tt(actT[:, fc, :], sg[gb][:], Up[gb][:], ALU.mult, [r_sg[gb], r_Up[gb]], [r_actT[fc]])
                ensure(pg + 1 + NW)

            def compute_d(ex, dp):
                pb = ex % 2
                pd = ex * PPE + 2 * NFP + dp
                wdt, rd_ = Wb[pd % NW], r_W[pd % NW]
                for s4 in range(NS):
                    yb = cnt["y"] % 2
                    cnt["y"] += 1
                    for fc in range(NCH):
                        mm(Yp[yb][:, 0:FP], actT[:, fc, s4 * 128:(s4 + 1) * 128], wdt[:, fc, :], fc == 0, fc == NCH - 1,
                           [r_actT[fc], rd_], [r_Yp[yb]])
                    if s4 % 2 == 0:
                        ts(ygs[:, s4, dp * FP:(dp + 1) * FP], Yp[yb][:, 0:FP], gs[pb][:, s4:s4 + 1], None, ALU.mult, None,
                           [r_Yp[yb], r_idx[pb]], [r_ygs[s4]])
                    else:
                        act(ygs[:, s4, dp * FP:(dp + 1) * FP], Yp[yb][:, 0:FP], AF.Copy, [r_Yp[yb], r_idx[pb]], [r_ygs[s4]],
                            scale=gs[pb][:, s4:s4 + 1])
                ensure(pd + NW)

            sc_prev = {"ops": []}

            def scatter(ex):
                pb = ex % 2
                extra = list(sc_prev["ops"])
                if R.accd.last_w is not None:
                    extra.append(R.accd.last_w)
                ops = []
                for s4 in range(NS):
                    off = bass.IndirectOffsetOnAxis(ap=si[pb][:, s4:s4 + 1], axis=0)
                    ops.append(S.dma_fn("pool", lambda e, s4=s4, off=off: e.indirect_dma_start(out=Sx.accd, out_offset=off, in_=ygs[:, s4, :],
                                                                                             in_offset=None, compute_op=ALU.add),
                                        reads=[r_idx[pb], r_ygs[s4]], writes=[], extra=extra))
                sc_prev["ops"] = ops

            assert NFP == 4
            prep_sel(0)
            prep_idx(0)
            prep_T(0)
            ensure(NW - 1)
            for ex in range(N_EXP):
                for fp in range(NFP):
                    compute_gu(ex, fp)
                    if fp == NFP - 2 and ex + 1 < N_EXP:
                        prep_sel(ex + 1)
                if ex + 1 < N_EXP:
                    prep_idx(ex + 1)
                for dp in range(NFP):
                    compute_d(ex, dp)
                    if dp == 1 and ex + 1 < N_EXP:
                        prep_T(ex + 1)
                scatter(ex)
            fin = S.dma("sp", K.out, Sx.accd[0:OWN, :], reads=[R.accd], writes=[], extra=sc_prev["ops"])
            S.finish([fin])
            S.emit_phase()
```
